# Optimizing a Trainium2 kernel written in Bass

```python
import math
import jax, jax.numpy as jnp
from jax import lax
import numpy as np

D_MODEL = 1024
BATCH = 2
SEQ = 8192
DEPTH = 4

N_A_LAYERS = DEPTH // 2
N_B_LAYERS = DEPTH - N_A_LAYERS
N_DENSE = (DEPTH + 1) // 2
N_MOE = DEPTH // 2

HEAD_DIM = 64
N_HEADS = D_MODEL // HEAD_DIM
LORA_DECAY = 64
LORA_ICLR = 64
LORA_VRES = 32
LORA_GATE = 160
GN_EPS = 64e-5
LN_EPS = 1e-5

MOBA_BLOCK = 256
MOBA_TOPK = 3
QUERY_CHUNK = 64

D_FF = 2816
N_EXPERTS = 8
TOP_K_EXPERTS = 2
D_FF_EXPERT = 3584

DEEPNORM_ALPHA = (2.0 * DEPTH) ** 0.25
DEEPNORM_BETA = (8.0 * DEPTH) ** -0.25

kernel_name = 'rwkv7_moba_yoco_deepnorm_moe'


def _heads(t):
    return t.reshape(t.shape[0], t.shape[1], N_HEADS, HEAD_DIM)


def layer_norm(x, g, b):
    xf = x.astype(jnp.float32)
    mu = jnp.mean(xf, axis=-1, keepdims=True)
    var = jnp.mean(jnp.square(xf - mu), axis=-1, keepdims=True)
    y = (xf - mu) * lax.rsqrt(var + LN_EPS)
    return (y * g + b).astype(x.dtype)


def deepnorm_residual(x, sub, g, b):
    return layer_norm(DEEPNORM_ALPHA * x + sub, g, b)


def wkv7_scan(r, decay, k, v, a, b):
    bsz, T, H, N = r.shape
    xs = tuple(jnp.moveaxis(t.astype(jnp.float32), 1, 0) for t in (r, decay, k, v, a, b))

    def step(S, inp):
        r_t, w_t, k_t, v_t, a_t, b_t = inp
        sa = jnp.einsum('bhvk,bhk->bhv', S, a_t)
        S = (S * w_t[:, :, None, :] + sa[..., None] * b_t[:, :, None, :]
             + v_t[..., None] * k_t[:, :, None, :])
        return S, jnp.einsum('bhvk,bhk->bhv', S, r_t)

    S0 = jnp.zeros((bsz, H, N, N), jnp.float32)
    _, y = lax.scan(step, S0, xs)
    return jnp.moveaxis(y, 0, 1)


def rwkv7_time_mix(x, v_first, mu, w_rkv, w_out, decay_w0, decay_w1, decay_w2,
                   iclr_a0, iclr_a1, iclr_a2, vres_v0, vres_v1, vres_v2,
                   gate_g1, gate_g2, k_k, k_a, r_k, gn_g, gn_b):
    bsz, T, D = x.shape
    x_prev = jnp.pad(x[:, :-1], ((0, 0), (1, 0), (0, 0)))
    xx = x_prev - x
    xm = x[None] + xx[None] * mu[:, None, None, :]
    rkv = jnp.einsum('cbtd,cde->cbte', xm[:3], w_rkv)
    r, k, v = rkv[0], rkv[1], rkv[2]
    xw, xa, xg = xm[3], xm[4], xm[5]
    w = -jax.nn.softplus(-(decay_w0 + jnp.tanh(xw @ decay_w1) @ decay_w2)) - 0.5
    if vres_v0 is None:
        v_first = v
    else:
        v = v + (v_first - v) * jax.nn.sigmoid(vres_v0 + (xm[2] @ vres_v1) @ vres_v2)
    a = jax.nn.sigmoid(iclr_a0 + (xa @ iclr_a1) @ iclr_a2)
    g = jax.nn.sigmoid(xg @ gate_g1) @ gate_g2
    kk = _heads((k * k_k).astype(jnp.float32))
    kk = kk * lax.rsqrt(jnp.maximum(jnp.sum(kk * kk, axis=-1, keepdims=True), 1e-24))
    k = k * (1 + (a - 1) * k_a)
    decay = jnp.exp(-jnp.exp(w.astype(jnp.float32)))
    a_h = _heads(a.astype(jnp.float32))
    y = wkv7_scan(_heads(r), _heads(decay), _heads(k), _heads(v), -kk, kk * a_h)
    mean = jnp.mean(y, axis=-1, keepdims=True)
    var = jnp.mean(jnp.square(y - mean), axis=-1, keepdims=True)
    y = ((y - mean) * lax.rsqrt(var + GN_EPS)).reshape(bsz, T, D) * gn_g + gn_b
    bonus = jnp.sum(_heads(r) * _heads(k) * r_k, axis=-1, keepdims=True) * _heads(v)
    y = y + bonus.reshape(bsz, T, D)
    out = (y * g) @ w_out
    return out.astype(x.dtype), v_first


def moba_shared_kv(h, w_k, w_v):
    bsz, T, D = h.shape
    nb = -(-T // MOBA_BLOCK)
    pad = nb * MOBA_BLOCK - T

    def blocks(t):
        t = _heads(t).transpose(0, 2, 1, 3)
        t = jnp.pad(t, ((0, 0), (0, 0), (0, pad), (0, 0)))
        return t.reshape(bsz, N_HEADS, nb, MOBA_BLOCK, HEAD_DIM)

    k_blocks = blocks(h @ w_k)
    v_blocks = blocks(h @ w_v)
    k_means = jnp.mean(k_blocks.astype(jnp.float32), axis=3)
    return k_blocks, v_blocks, k_means


def moba_attention(x, w_q, w_o, k_blocks, v_blocks, k_means):
    bsz, T, D = x.shape
    nb = k_blocks.shape[2]
    topk = min(MOBA_TOPK, nb)
    scale = HEAD_DIM ** -0.5
    q = _heads(x @ w_q).transpose(0, 2, 1, 3)
    n_chunks = T // QUERY_CHUNK
    q_chunks = q.reshape(bsz, N_HEADS, n_chunks, QUERY_CHUNK, HEAD_DIM).transpose(2, 0, 1, 3, 4)
    gather_blocks = jax.vmap(jax.vmap(lambda blk_arr, idx: blk_arr[idx]))

    def attend_chunk(args):
        c, q_c = args
        start = c * QUERY_CHUNK
        own = start // MOBA_BLOCK
        q_pos = start + jnp.arange(QUERY_CHUNK)
        gate = jnp.einsum('bhqd,bhnd->bhqn', q_c.astype(jnp.float32), k_means)
        gate = jnp.where(jnp.arange(nb) < own, gate, -jnp.inf)
        _, sel = lax.top_k(gate, topk)
        k_sel = gather_blocks(k_blocks, sel)
        v_sel = gather_blocks(v_blocks, sel)
        s_sel = jnp.einsum('bhqd,bhqsjd->bhqsj', q_c, k_sel).astype(jnp.float32) * scale
        slot_ok = jnp.arange(topk) < own
        s_sel = jnp.where(slot_ok[:, None], s_sel, -jnp.inf)
        s_sel = s_sel.reshape(bsz, N_HEADS, QUERY_CHUNK, topk * MOBA_BLOCK)
        k_own = lax.dynamic_index_in_dim(k_blocks, own, axis=2, keepdims=False)
        v_own = lax.dynamic_index_in_dim(v_blocks, own, axis=2, keepdims=False)
        s_own = jnp.einsum('bhqd,bhkd->bhqk', q_c, k_own).astype(jnp.float32) * scale
        k_pos = own * MOBA_BLOCK + jnp.arange(MOBA_BLOCK)
        s_own = jnp.where(k_pos[None, :] <= q_pos[:, None], s_own, -jnp.inf)
        p = jax.nn.softmax(jnp.concatenate([s_sel, s_own], axis=-1), axis=-1)
        p_sel = p[..., :topk * MOBA_BLOCK].reshape(bsz, N_HEADS, QUERY_CHUNK, topk, MOBA_BLOCK)
        p_own = p[..., topk * MOBA_BLOCK:]
        o = (jnp.einsum('bhqsj,bhqsjd->bhqd', p_sel.astype(v_sel.dtype), v_sel)
             + jnp.einsum('bhqk,bhkd->bhqd', p_own.astype(v_own.dtype), v_own))
        return o

    o = lax.map(attend_chunk, (jnp.arange(n_chunks), q_chunks))
    o = o.transpose(1, 0, 3, 2, 4).reshape(bsz, T, D)
    return (o @ w_o).astype(x.dtype)


def swiglu(x, w_gate, w_up, w_down):
    return (jax.nn.silu(x @ w_gate) * (x @ w_up)) @ w_down


def moe_swiglu(x, router, w_gate, w_up, w_down):
    logits = (x @ router).astype(jnp.float32)
    top_logits, top_idx = lax.top_k(logits, TOP_K_EXPERTS)
    top_w = jax.nn.softmax(top_logits, axis=-1)
    gates = jnp.sum(jax.nn.one_hot(top_idx, N_EXPERTS, dtype=jnp.float32) * top_w[..., None], axis=-2)
    gates = gates.astype(x.dtype)
    out = jnp.zeros_like(x)
    for e in range(N_EXPERTS):
        out = out + gates[..., e:e + 1] * swiglu(x, w_gate[e], w_up[e], w_down[e])
    return out


def setup_inputs(seed: int = 0) -> dict:
    key = jax.random.key(seed)
    ks = iter(jax.random.split(key, 48))
    D, H, N = D_MODEL, N_HEADS, HEAD_DIM
    f32 = jnp.float32

    def nrm(shape, scale):
        return jax.random.normal(next(ks), shape, f32) * scale

    def unif(shape, lo, hi):
        return jax.random.uniform(next(ks), shape, f32, lo, hi)

    beta = DEEPNORM_BETA
    nA, nV = N_A_LAYERS, max(N_A_LAYERS - 1, 0)
    rkv_scale = jnp.array([1.0, 1.0, beta], f32)[:, None, None]
    return {
        'x': nrm((BATCH, SEQ, D), 1.0),
        'rwkv_mu': unif((nA, 6, D), 0.0, 1.0),
        'rwkv_w_rkv': nrm((nA, 3, D, D), D ** -0.5) * rkv_scale,
        'rwkv_w_out': nrm((nA, D, D), D ** -0.5 * beta),
        'rwkv_decay_w0': unif((nA, D), -6.0, -1.0),
        'rwkv_decay_w1': nrm((nA, D, LORA_DECAY), D ** -0.5),
        'rwkv_decay_w2': nrm((nA, LORA_DECAY, D), 0.1 * LORA_DECAY ** -0.5),
        'rwkv_iclr_a0': nrm((nA, D), 0.1),
        'rwkv_iclr_a1': nrm((nA, D, LORA_ICLR), D ** -0.5),
        'rwkv_iclr_a2': nrm((nA, LORA_ICLR, D), 0.1 * LORA_ICLR ** -0.5),
        'rwkv_vres_v0': 1.0 + nrm((nV, D), 0.1),
        'rwkv_vres_v1': nrm((nV, D, LORA_VRES), D ** -0.5),
        'rwkv_vres_v2': nrm((nV, LORA_VRES, D), 0.1 * LORA_VRES ** -0.5),
        'rwkv_gate_g1': nrm((nA, D, LORA_GATE), D ** -0.5),
        'rwkv_gate_g2': nrm((nA, LORA_GATE, D), LORA_GATE ** -0.5),
        'rwkv_k_k': 0.85 + nrm((nA, D), 0.05),
        'rwkv_k_a': 1.0 + nrm((nA, D), 0.05),
        'rwkv_r_k': nrm((nA, H, N), 0.1),
        'rwkv_gn_g': 1.0 + nrm((nA, D), 0.05),
        'rwkv_gn_b': nrm((nA, D), 0.01),
        'moba_w_k': nrm((D, D), D ** -0.5),
        'moba_w_v': nrm((D, D), D ** -0.5 * beta),
        'moba_w_q': nrm((N_B_LAYERS, D, D), D ** -0.5),
        'moba_w_o': nrm((N_B_LAYERS, D, D), D ** -0.5 * beta),
        'ffn_w_gate': nrm((N_DENSE, D, D_FF), D ** -0.5),
        'ffn_w_up': nrm((N_DENSE, D, D_FF), D ** -0.5),
        'ffn_w_down': nrm((N_DENSE, D_FF, D), D_FF ** -0.5 * beta),
        'moe_router': nrm((N_MOE, D, N_EXPERTS), D ** -0.5),
        'moe_w_gate': nrm((N_MOE, N_EXPERTS, D, D_FF_EXPERT), D ** -0.5),
        'moe_w_up': nrm((N_MOE, N_EXPERTS, D, D_FF_EXPERT), D ** -0.5),
        'moe_w_down': nrm((N_MOE, N_EXPERTS, D_FF_EXPERT, D), D_FF_EXPERT ** -0.5 * beta),
        'ln_g': 1.0 + nrm((DEPTH, 2, D), 0.05),
        'ln_b': nrm((DEPTH, 2, D), 0.01),
    }


def reference(x, rwkv_mu, rwkv_w_rkv, rwkv_w_out, rwkv_decay_w0, rwkv_decay_w1, rwkv_decay_w2,
              rwkv_iclr_a0, rwkv_iclr_a1, rwkv_iclr_a2, rwkv_vres_v0, rwkv_vres_v1, rwkv_vres_v2,
              rwkv_gate_g1, rwkv_gate_g2, rwkv_k_k, rwkv_k_a, rwkv_r_k, rwkv_gn_g, rwkv_gn_b,
              moba_w_k, moba_w_v, moba_w_q, moba_w_o,
              ffn_w_gate, ffn_w_up, ffn_w_down,
              moe_router, moe_w_gate, moe_w_up, moe_w_down,
              ln_g, ln_b):
    h = x
    v_first = None
    kv = None
    for layer in range(DEPTH):
        if layer < N_A_LAYERS:
            i = layer
            if i == 0:
                v0, v1, v2 = None, None, None
            else:
                v0, v1, v2 = rwkv_vres_v0[i - 1], rwkv_vres_v1[i - 1], rwkv_vres_v2[i - 1]
            mix, v_first = rwkv7_time_mix(
                h, v_first, rwkv_mu[i], rwkv_w_rkv[i], rwkv_w_out[i],
                rwkv_decay_w0[i], rwkv_decay_w1[i], rwkv_decay_w2[i],
                rwkv_iclr_a0[i], rwkv_iclr_a1[i], rwkv_iclr_a2[i], v0, v1, v2,
                rwkv_gate_g1[i], rwkv_gate_g2[i], rwkv_k_k[i], rwkv_k_a[i], rwkv_r_k[i],
                rwkv_gn_g[i], rwkv_gn_b[i])
        else:
            j = layer - N_A_LAYERS
            k_blocks, v_blocks, k_means = kv
            mix = moba_attention(h, moba_w_q[j], moba_w_o[j], k_blocks, v_blocks, k_means)
        h = deepnorm_residual(h, mix, ln_g[layer, 0], ln_b[layer, 0])
        if layer % 2 == 0:
            e = layer // 2
            ffn = swiglu(h, ffn_w_gate[e], ffn_w_up[e], ffn_w_down[e])
        else:
            e = layer // 2
            ffn = moe_swiglu(h, moe_router[e], moe_w_gate[e], moe_w_up[e], moe_w_down[e])
        h = deepnorm_residual(h, ffn, ln_g[layer, 1], ln_b[layer, 1])
        if layer == N_A_LAYERS - 1:
            kv = moba_shared_kv(h, moba_w_k, moba_w_v)
    return h
```

```python
import numpy as np
import ml_dtypes
import concourse.bass as bass
import concourse.mybir as mybir
from concourse.bass_utils import run_bass_kernel_spmd

F32 = mybir.dt.float32
BF16 = mybir.dt.bfloat16
ALU = mybir.AluOpType
AF = mybir.ActivationFunctionType
AX = mybir.AxisListType

NCORES = 8
D = 1024
KC = 8
B = 2
T = 8192
NTOK = B * T
NT = NTOK // NCORES
ALPHA = (2.0 * 4) ** 0.25
LN_EPS = 1e-5
GN_EPS = 64e-5


class Sched:
    ENG = ("pe", "act", "dve", "pool", "sp")
    NDMA = 8

    def __init__(self, nc):
        self.nc = nc
        self.ops = {e: [] for e in self.ENG}
        self.lastw = {}
        self.readers = {}

    def add(self, eng, fn, reads=(), writes=(), dma=False):
        idx = len(self.ops[eng])
        deps = {}

        def dep(key, kind):
            if key is None:
                return
            if deps.get(key) != "raw":
                deps[key] = kind

        for r in reads:
            dep(self.lastw.get(r), "raw")
        for w in writes:
            dep(self.lastw.get(w), "waw")
            for k in self.readers.get(w, ()):
                dep(k, "war")
        waits = set()
        for (pe, pi), kind in deps.items():
            pdma = self.ops[pe][pi]["dma"]
            if pe == eng and not pdma and not dma:
                if eng == "pe":
                    continue
            waits.add((pe, pi))
        self.ops[eng].append(dict(fn=fn, waits=waits, dma=dma, signal=dma))
        me = (eng, idx)
        for r in reads:
            lst = self.readers.setdefault(r, [])
            if not dma:
                lst[:] = [k for k in lst if not (k[0] == eng and not self.ops[k[0]][k[1]]["dma"])]
            lst.append(me)
        for w in writes:
            self.lastw[w] = me
            self.readers[w] = []
        return me

    def op(self, eng, method, *args, r=(), w=(), dma=False, **kw):
        return self.add(eng, lambda h: getattr(h, method)(*args, **kw), reads=r, writes=w, dma=dma)

    def emit(self):
        nc = self.nc
        ops = self.ops
        for e in self.ENG:
            for op in ops[e]:
                for (pe, pi) in op["waits"]:
                    ops[pe][pi]["signal"] = True
        import contextlib
        with contextlib.ExitStack() as st:
            csem = {e: st.enter_context(nc.semaphore("c_" + e)) for e in self.ENG}
            dsem = {e: [st.enter_context(nc.semaphore("d_%s%d" % (e, i))) for i in range(self.NDMA)]
                    for e in ("act", "pool", "sp")}
            final_dma = []
            for e in self.ENG:
                cnt = 0
                dcnt = [0] * self.NDMA
                k = 0
                for op in ops[e]:
                    if op["dma"]:
                        s = k % self.NDMA
                        k += 1
                        op["sem"] = dsem[e][s]
                        op["prev"] = dcnt[s]
                        dcnt[s] += 16
                        op["val"] = dcnt[s]
                    elif op["signal"]:
                        cnt += 1
                        op["sem"] = csem[e]
                        op["val"] = cnt
                if e in dsem:
                    for s in range(self.NDMA):
                        if dcnt[s]:
                            final_dma.append((dsem[e][s], dcnt[s]))
            block = st.enter_context(nc.Block())

            def run(e, h):
                waited = {}

                def wait(sem, val):
                    key = id(sem)
                    if waited.get(key, 0) >= val:
                        return
                    waited[key] = val
                    h.wait_ge(sem, val)

                for op in ops[e]:
                    for (pe, pi) in sorted(op["waits"]):
                        p = ops[pe][pi]
                        wait(p["sem"], p["val"])
                    if op["dma"] and op["prev"]:
                        wait(op["sem"], op["prev"])
                    ins = op["fn"](h)
                    if op["dma"]:
                        ins.then_inc(op["sem"], 16)
                    elif op["signal"]:
                        ins.then_inc(op["sem"], 1)
                if e == "sp":
                    for sem, val in final_dma:
                        wait(sem, val)

            @block.tensor
            def _(h):
                run("pe", h)

            @block.scalar
            def _(h):
                run("act", h)

            @block.vector
            def _(h):
                run("dve", h)

            @block.gpsimd
            def _(h):
                run("pool", h)

            @block.sync
            def _(h):
                run("sp", h)


def _bc(ap, shape):
    return ap.broadcast_to(shape)


class Ctx:
    def __init__(self, nc, es):
        self.nc = nc
        self.es = es
        self.S = Sched(nc)

    def sb(self, name, shape, dt):
        return self.es.enter_context(self.nc.sbuf_tensor(name, shape, dt))

    def ps(self, name, shape, dt=F32):
        return self.es.enter_context(self.nc.psum_tensor(name, shape, dt))

    def dram(self, name, shape, dt, kind):
        return self.nc.dram_tensor(name, list(shape), dt, kind=kind).ap()


def emit_layernorm(cx, zt, res_z, hb, res_hb, lnp, gi, bi, onesm, psA, psB, tmp, ngroups, scale_after=None):
    S = cx.S
    mean, msq, var = tmp["mean"], tmp["msq"], tmp["var"]
    for g in range(ngroups):
        gs = slice(g * 512, (g + 1) * 512)
        zkeys = [res_z(k, g) for k in range(KC)]
        for k in range(KC):
            sq = tmp["sq%d" % (k % 2)]
            S.op("act", "activation", out=sq[:], in_=zt[:, k, gs], func=AF.Square, r=[zkeys[k]], w=[("sq", k % 2)])
            S.op("pe", "matmul", psA[:], onesm, zt[:, k, gs], start=(k == 0), stop=(k == KC - 1),
                 r=[zkeys[k], "onesm"], w=["psA"])
            S.op("pe", "matmul", psB[:], onesm, sq[:], start=(k == 0), stop=(k == KC - 1),
                 r=[("sq", k % 2), "onesm"], w=["psB"])
        S.op("act", "copy", out=mean[:], in_=psA[:], r=["psA"], w=["mean"])
        S.op("dve", "tensor_tensor", msq[:], mean[:], mean[:], ALU.mult, r=["mean"], w=["msq"])
        S.op("dve", "tensor_tensor", var[:], psB[:], msq[:], ALU.subtract, r=["psB", "msq"], w=["var"])
        S.op("act", "activation", out=var[:], in_=var[:], func=AF.Sqrt, bias=LN_EPS, scale=1.0, r=["var"], w=["var"])
        S.op("dve", "reciprocal", msq[:], var[:], r=["var"], w=["msq"])
        z3 = zt[:, :, gs]
        S.op("dve", "tensor_tensor", z3, z3, _bc(mean[:].unsqueeze(1), [128, KC, 512]), ALU.subtract,
             r=zkeys + ["mean"], w=zkeys)
        S.op("dve", "tensor_tensor", z3, z3, _bc(msq[:].unsqueeze(1), [128, KC, 512]), ALU.mult,
             r=zkeys + ["msq"], w=zkeys)
        for k in range(KC):
            S.op("act", "activation", out=zt[:, k, gs], in_=zt[:, k, gs], func=AF.Identity,
                 scale=lnp[:, gi, k:k + 1], bias=lnp[:, bi, k:k + 1], r=[zkeys[k], "lnp"], w=[zkeys[k]])
        if hb is not None:
            hkeys = [res_hb(k, g) for k in range(KC)]
            S.op("pool", "tensor_copy", out=hb[:, :, gs], in_=z3, r=zkeys, w=hkeys)
        if scale_after is not None:
            S.op("pool", "tensor_scalar", z3, z3, float(scale_after), None, ALU.mult, r=zkeys, w=zkeys)


def build_T(moe, nt=NT, dff=None, nexp=None):
    import contextlib
    G = nt // 512
    FB = 4
    if dff is None:
        dff = 3584 if moe else 2816
    if nexp is None:
        nexp = 8 if moe else 1
    nfc = dff // 128
    nblk = (nfc + FB - 1) // FB
    ntile = nt // 128
    nc = bass.Bass("TRN2", target_bir_lowering=False)
    with contextlib.ExitStack() as es:
        cx = Ctx(nc, es)
        S = cx.S
        mixT = cx.dram("mixT", [D, nt], BF16, "ExternalInput")
        hT = cx.dram("hT", [D, nt], F32, "ExternalInput")
        w_o = cx.dram("w_o", [D, D], F32, "ExternalInput")
        lnp_d = cx.dram("lnp", [128, 4, KC], F32, "ExternalInput")
        consts = cx.dram("consts", [128, 128 + 128 + 8 * 128], F32, "ExternalInput")
        if moe:
            router = cx.dram("router", [D, 8], F32, "ExternalInput")
        wg_d = cx.dram("w_gate", [nexp, D, dff], F32, "ExternalInput")
        wu_d = cx.dram("w_up", [nexp, D, dff], F32, "ExternalInput")
        wd_d = cx.dram("w_down", [nexp, dff, D], F32, "ExternalInput")
        outT = cx.dram("outT", [D, nt], F32, "ExternalOutput")
        outTb = cx.dram("outTb", [D, nt], BF16, "ExternalOutput")

        acc = cx.sb("acc", [128, KC, nt], F32)
        hb = cx.sb("hb", [128, KC, nt], BF16)
        wgu = cx.sb("wgu", [128, 4, KC, 512], BF16)
        wdn = cx.sb("wdn", [128, 2, FB, D], BF16)
        actb = cx.sb("actb", [128, FB, nt], BF16)
        cst = cx.sb("cst", [128, 128 + 128 + 8 * 128], F32)
        lnp = cx.sb("lnp_sb", [128, 4, KC], F32)
        tmp = {n: cx.sb(n, [128, 512], F32) for n in ("sq0", "sq1", "mean", "msq", "var", "sl0", "sl1", "t20", "t21")}
        onesm = cst[:, 0:128]
        ident = cst[:, 128:256]
        ps = [cx.ps("ps%d" % i, [128, 512]) for i in range(8)]
        pkey = [("ps", i) for i in range(6)] + ["psA", "psB"]
        if moe:
            rt = cx.sb("rt", [128, KC, 8], F32)
            gbs = [cx.sb("gb0", [128, nt], F32)] * 2
            lg = cx.sb("lg", [128, ntile, 8], F32)
            lg2 = cx.sb("lg2", [128, ntile, 8], F32)
            lgm = cx.sb("lgm", [128, ntile, 8], F32)
            m1 = cx.sb("m1", [128, ntile], F32)
            m2 = cx.sb("m2", [128, ntile], F32)
            GT = cx.sb("GT", [8, nt], F32)

        zk = lambda k, g: ("acc", k, g)
        hk = lambda k, g: ("hb", k, g)
        allz = lambda g: [zk(k, g) for k in range(KC)]
        allh = lambda g: [hk(k, g) for k in range(KC)]
        fm = lambda ap: ap.rearrange("(c p) t -> p c t", p=128)

        S.op("sp", "dma_start", out=cst[:], in_=consts, w=["onesm", "ident", "sel"], dma=True)
        S.op("sp", "dma_start", out=lnp[:], in_=lnp_d, w=["lnp"], dma=True)
        for g in range(G):
            gs = slice(g * 512, (g + 1) * 512)
            S.op("sp", "dma_start", out=hb[:, :, gs], in_=fm(mixT[:, gs]), w=allh(g), dma=True)
        for s in range(2):
            S.op("pool", "dma_start", out=wgu[:, s], in_=fm(w_o[:, s * 512:(s + 1) * 512]), w=[("wgu", s)], dma=True)
        for g in range(G):
            gs = slice(g * 512, (g + 1) * 512)
            S.op("sp", "dma_start", out=acc[:, :, gs], in_=fm(hT[:, gs]), w=allz(g), dma=True)
        if moe:
            S.op("sp", "dma_start", out=rt[:], in_=fm(router), w=["rt"], dma=True)

        blocks = [(e, b) for e in range(nexp) for b in range(nblk)]

        def load_block(i):
            e, b = blocks[i]
            st = i % 2
            f0 = b * FB * 128
            nf = min(FB * 128, dff - f0)
            S.op("pool", "dma_start", out=wgu[:, 2 * st, :, 0:nf], in_=fm(wg_d[e, :, f0:f0 + nf]), w=[("wgu", 2 * st)], dma=True)
            S.op("pool", "dma_start", out=wgu[:, 2 * st + 1, :, 0:nf], in_=fm(wu_d[e, :, f0:f0 + nf]), w=[("wgu", 2 * st + 1)], dma=True)
            S.op("pool", "dma_start", out=wdn[:, st, 0:nf // 128, :], in_=fm(wd_d[e, f0:f0 + nf, :]), w=[("wdn", st)], dma=True)

        pi = 0
        for g in range(G):
            gs = slice(g * 512, (g + 1) * 512)
            for ec in range(KC):
                pt, pk = ps[pi % 2], pkey[pi % 2]
                pi += 1
                s, off = divmod(ec * 128, 512)
                for k in range(KC):
                    S.op("pe", "matmul", pt[:], wgu[:, s, k, off:off + 128], hb[:, k, gs], start=(k == 0), stop=(k == KC - 1),
                         r=[("wgu", s), hk(k, g)], w=[pk])
                S.op("dve", "scalar_tensor_tensor", out=acc[:, ec, gs], in0=acc[:, ec, gs], scalar=float(ALPHA), in1=pt[:],
                     op0=ALU.mult, op1=ALU.add, r=[zk(ec, g), pk], w=[zk(ec, g)])
        load_block(0)
        emit_layernorm(cx, acc, zk, hb, hk, lnp, 0, 1, onesm, ps[6], ps[7], tmp, G)
        if len(blocks) > 1:
            load_block(1)
        if moe:
            lp = ps[6]
            for j in range(ntile):
                for k in range(KC):
                    S.op("pe", "matmul", lp[:, j * 8:(j + 1) * 8], acc[:, k, j * 128:(j + 1) * 128], rt[:, k, :],
                         start=(k == 0), stop=(k == KC - 1), r=[zk(k, j // 4), "rt"], w=["psA"])
            S.op("act", "copy", out=lg[:].rearrange("p a b -> p (a b)"), in_=lp[:, 0:ntile * 8], r=["psA"], w=["lg"])
            bc3 = lambda t: _bc(t[:].unsqueeze(2), [128, ntile, 8])
            S.op("dve", "tensor_reduce", out=m1[:], in_=lg[:], axis=AX.X, op=ALU.max, r=["lg"], w=["m1"])
            S.op("dve", "tensor_tensor", lgm[:], lg[:], bc3(m1), ALU.is_ge, r=["lg", "m1"], w=["lgm"])
            S.op("dve", "scalar_tensor_tensor", out=lg2[:], in0=lgm[:], scalar=-1e30, in1=lg[:], op0=ALU.mult, op1=ALU.add,
                 r=["lgm", "lg"], w=["lg2"])
            S.op("dve", "tensor_reduce", out=m2[:], in_=lg2[:], axis=AX.X, op=ALU.max, r=["lg2"], w=["m2"])
            S.op("dve", "tensor_tensor", lgm[:], lg[:], bc3(m2), ALU.is_ge, r=["lg", "m2"], w=["lgm"])
            S.op("dve", "tensor_tensor", lg2[:], lg[:], bc3(m1), ALU.subtract, r=["lg", "m1"], w=["lg2"])
            S.op("act", "activation", out=lg2[:], in_=lg2[:], func=AF.Exp, r=["lg2"], w=["lg2"])
            S.op("dve", "tensor_tensor", lg2[:], lg2[:], lgm[:], ALU.mult, r=["lg2", "lgm"], w=["lg2"])
            S.op("dve", "tensor_reduce", out=m1[:], in_=lg2[:], axis=AX.X, op=ALU.add, r=["lg2"], w=["m1"])
            S.op("dve", "reciprocal", m2[:], m1[:], r=["m1"], w=["m2"])
            S.op("dve", "tensor_tensor", lg[:], lg2[:], bc3(m2), ALU.mult, r=["lg2", "m2"], w=["lg"])
            for q in range(ntile // 4):
                pt = ps[7]
                for jj in range(4):
                    j = q * 4 + jj
                    S.op("pe", "transpose", pt[0:8, jj * 128:(jj + 1) * 128], lg[:, j, :], ident, r=["lg", "ident"], w=["psB"])
                S.op("act", "copy", out=GT[:, q * 512:(q + 1) * 512], in_=pt[0:8, :], r=["psB"], w=["GT"])
        for g in range(G):
            gs = slice(g * 512, (g + 1) * 512)
            S.op("pool", "tensor_scalar", acc[:, :, gs], acc[:, :, gs], float(ALPHA), None, ALU.mult, r=allz(g), w=allz(g))

        tcount = 0
        gb = gk = None
        for i, (e, b) in enumerate(blocks):
            st = i % 2
            f0 = b * FB * 128
            nf = min(FB * 128, dff - f0)
            nfb = nf // 128
            if moe and b == 0:
                gb = gbs[0]
                gk = ("gb", 0)
                for q in range(G):
                    pt, pk = ps[6 + (q % 2)], pkey[6 + (q % 2)]
                    S.op("pe", "matmul", pt[:], cst[0:8, 256 + e * 128:256 + (e + 1) * 128], GT[:, q * 512:(q + 1) * 512],
                         start=True, stop=True, r=["sel", "GT"], w=[pk])
                    S.op("act", "copy", out=gb[:, q * 512:(q + 1) * 512], in_=pt[:], r=[pk], w=[gk])
            for g in range(G):
                gs = slice(g * 512, (g + 1) * 512)
                for fc in range(nfb):
                    c2 = tcount % 2
                    tcount += 1
                    pg, pu, kg, ku = ps[c2], ps[2 + c2], pkey[c2], pkey[2 + c2]
                    sl, t2 = tmp["sl%d" % c2], tmp["t2%d" % c2]
                    ksl, kt2 = ("sl", c2), ("t2", c2)
                    for k in range(KC):
                        S.op("pe", "matmul", pg[:], wgu[:, 2 * st, k, fc * 128:(fc + 1) * 128], hb[:, k, gs],
                             start=(k == 0), stop=(k == KC - 1), r=[("wgu", 2 * st), hk(k, g)], w=[kg])
                    for k in range(KC):
                        S.op("pe", "matmul", pu[:], wgu[:, 2 * st + 1, k, fc * 128:(fc + 1) * 128], hb[:, k, gs],
                             start=(k == 0), stop=(k == KC - 1), r=[("wgu", 2 * st + 1), hk(k, g)], w=[ku])
                    S.op("act", "activation", out=sl[:], in_=pg[:], func=AF.Silu, r=[kg], w=[ksl])
                    if moe:
                        S.op("dve", "tensor_tensor", t2[:], sl[:], pu[:], ALU.mult, r=[ksl, ku], w=[kt2])
                        S.op("dve", "tensor_tensor", actb[:, fc, gs], t2[:], gb[:, gs], ALU.mult, r=[kt2, gk], w=[("act", fc, g)])
                    else:
                        S.op("dve", "tensor_tensor", actb[:, fc, gs], sl[:], pu[:], ALU.mult, r=[ksl, ku], w=[("act", fc, g)])
            for g in range(G):
                gs = slice(g * 512, (g + 1) * 512)
                for ec in range(KC):
                    pd, kd = ps[4 + (ec % 2)], pkey[4 + (ec % 2)]
                    for fc in range(nfb):
                        S.op("pe", "matmul", pd[:], wdn[:, st, fc, ec * 128:(ec + 1) * 128], actb[:, fc, gs],
                             start=(fc == 0), stop=(fc == nfb - 1), r=[("wdn", st), ("act", fc, g)], w=[kd])
                    S.op("dve", "tensor_tensor", acc[:, ec, gs], acc[:, ec, gs], pd[:], ALU.add, r=[zk(ec, g), kd], w=[zk(ec, g)])
            if i + 2 < len(blocks):
                load_block(i + 2)

        emit_layernorm(cx, acc, zk, hb, hk, lnp, 2, 3, onesm, ps[6], ps[7], tmp, G)
        for g in range(G):
            gs = slice(g * 512, (g + 1) * 512)
            S.op("sp", "dma_start", out=fm(outT[:, gs]), in_=acc[:, :, gs], r=allz(g), dma=True)
            S.op("sp", "dma_start", out=fm(outTb[:, gs]), in_=hb[:, :, gs], r=allh(g), dma=True)
        S.emit()
    return nc


def make_consts():
    c = np.zeros((128, 128 + 128 + 8 * 128), np.float32)
    c[:, 0:128] = 1.0 / D
    c[:, 128:256] = np.eye(128, dtype=np.float32)
    for e in range(8):
        c[e, 256 + e * 128:256 + (e + 1) * 128] = 1.0
    return c


def lnp_layout(ln_g, ln_b, layer):
    out = np.zeros((128, 4, KC), np.float32)
    out[:, 0] = ln_g[layer, 0].reshape(KC, 128).T
    out[:, 1] = ln_b[layer, 0].reshape(KC, 128).T
    out[:, 2] = ln_g[layer, 1].reshape(KC, 128).T
    out[:, 3] = ln_b[layer, 1].reshape(KC, 128).T
    return out


SEG = 512
NCH = SEG // 64
NBLK = 2 * NCH
NPROJ = 704
DECAY_SCALE = -float(np.exp(-0.5))


def rwkv_consts():
    c = np.zeros((128, 128 + 128 + 128 + 64 + 64), np.float32)
    c[:, 0:128] = np.eye(128, dtype=np.float32)
    c[0:64, 128:192] = 1.0
    c[64:128, 192:256] = 1.0
    j = np.arange(64)[:, None]
    i = np.arange(64)[None, :]
    c[0:64, 256:320] = (j < i)
    c[0:64, 320:384] = (j <= i)
    c[0:64, 384:448] = (i < j)
    c[0:64, 448:512] = np.eye(64, dtype=np.float32)
    return c


def build_H_rwkv(layer1, ntok_b=T, nb=B, stop_after=99):
    import contextlib
    nseg = ntok_b // SEG
    ntot = nb * ntok_b
    nc = bass.Bass("TRN2", target_bir_lowering=False)
    with contextlib.ExitStack() as es:
        cx = Ctx(nc, es)
        S = cx.S
        xT = cx.dram("xT", [D, ntot], F32, "ExternalInput")
        wproj = cx.dram("wproj", [D, NPROJ], F32, "ExternalInput")
        mu_d = cx.dram("mu", [128, KC, NPROJ], F32, "ExternalInput")
        w2_d = cx.dram("w2", [128, 3, 128], F32, "ExternalInput")
        par_d = cx.dram("par", [128, 8], F32, "ExternalInput")
        cst_d = cx.dram("cst", [128, 512], F32, "ExternalInput")
        if layer1:
            vf_d = cx.dram("vfirst", [128, ntot], F32, "ExternalInput")
        else:
            vf_o = cx.dram("vfirst_out", [128, ntot], F32, "ExternalOutput")
        mix_o = cx.dram("mix", [128, ntot], BF16, "ExternalOutput")

        f32t = lambda n, shp=(128, SEG): cx.sb(n, list(shp), F32)
        xb = [cx.sb("xb%d" % i, [128, KC, SEG + 1], BF16) for i in range(2)]
        Wc = cx.sb("Wc", [128, KC, 2, NPROJ], BF16)
        wst = cx.sb("wst", [128, NPROJ], F32)
        must = cx.sb("must", [128, NPROJ], F32)
        w2f = cx.sb("w2f", [128, 3, 128], F32)
        w2b = cx.sb("w2b", [128, 3, 128], BF16)
        par = cx.sb("par_sb", [128, 8], F32)
        cst = cx.sb("cst_sb", [128, 512], F32)
        ident = cst[:, 0:128]
        bones = cst[:, 128:256]
        m_su = cst[0:64, 256:320]
        m_iu = cst[0:64, 320:384]
        m_ak = cst[0:64, 256:384]
        m_sl = cst[0:64, 384:448]
        id64 = cst[0:64, 448:512]
        rT, kT, vT, aT, wT, gT, av, bv, P, rP, t1, t2, d0, d1 = [f32t(n) for n in
            ("rT", "kT", "vT", "aT", "wT", "gT", "av", "bv", "P", "rP", "t1", "t2", "d0", "d1")]
        lor = cx.sb("lor", [128, 3, SEG], BF16)
        AR = cx.sb("AR", [128, NCH, 2, 64], F32)
        KBf = cx.sb("KBf", [128, NCH, 2, 64], F32)
        KBh = cx.sb("KBh", [128, NCH, 2, 64], F32)
        ARblk = cx.sb("ARblk", [128, NCH, 2, 128], F32)
        Bblk = cx.sb("Bblk", [128, NCH, 2, 64], F32)
        Vtm = cx.sb("Vtm", [64, NCH, 128], F32)
        KBtm = cx.sb("KBtm", [64, NCH, 2, 128], F32)
        Ytm = cx.sb("Ytm", [64, NCH, 128], F32)
        AK = cx.sb("AK", [64, NBLK, 128], F32)
        AB = cx.sb("AB", [64, NBLK, 128], F32)
        Mp = [cx.sb("Mp%d" % i, [64, NBLK, 64], F32) for i in range(2)]
        Np = [cx.sb("Np%d" % i, [64, NBLK, 64], F32) for i in range(2)]
        Q = cx.sb("Q", [64, NBLK, 64], F32)
        H = cx.sb("Hst", [128, 128], F32)
        Xs = cx.sb("Xs", [64, 128], F32)
        Us = cx.sb("Us", [64, 128], F32)
        yT = f32t("yT")
        ob = cx.sb("ob", [128, SEG], BF16)
        ps = [cx.ps("ps%d" % i, [128, 512]) for i in range(8)]
        pk = [("ps", i) for i in range(8)]

        S.op("sp", "dma_start", out=cst[:], in_=cst_d, w=["cst"], dma=True)
        S.op("sp", "dma_start", out=par[:], in_=par_d, w=["par"], dma=True)
        S.op("sp", "dma_start", out=w2f[:], in_=w2_d, w=["w2f"], dma=True)
        S.op("dve", "tensor_copy", out=w2b[:], in_=w2f[:], r=["w2f"], w=["w2b"])
        S.op("dve", "memset", ARblk[:], 0.0, w=["ARblk"])
        S.op("dve", "memset", Bblk[:], 0.0, w=["Bblk"])
        for k in range(KC):
            S.op("sp", "dma_start", out=wst[:], in_=wproj[k * 128:(k + 1) * 128, :], w=["wst"], dma=True)
            S.op("sp", "dma_start", out=must[:], in_=mu_d[:, k, :], w=["must"], dma=True)
            S.op("dve", "tensor_tensor", must[:], wst[:], must[:], ALU.mult, r=["wst", "must"], w=["must"])
            S.op("dve", "tensor_copy", out=Wc[:, k, 1, :], in_=must[:], r=["must"], w=["Wc"])
            S.op("dve", "tensor_tensor", Wc[:, k, 0, :], wst[:], must[:], ALU.subtract, r=["wst", "must"], w=["Wc"])

        def load_x(b, s, slot):
            t0 = b * ntok_b + s * SEG
            xk = ("xb", slot)
            if s == 0:
                S.op("dve", "memset", xb[slot][:, :, 0:1], 0.0, w=[xk])
                S.op("pool", "dma_start", out=xb[slot][:, :, 1:SEG + 1], in_=xT[:, t0:t0 + SEG].rearrange("(c p) t -> p c t", p=128),
                     w=[xk], dma=True)
            else:
                S.op("pool", "dma_start", out=xb[slot][:, :, 0:SEG + 1], in_=xT[:, t0 - 1:t0 + SEG].rearrange("(c p) t -> p c t", p=128),
                     w=[xk], dma=True)

        segs = [(b, s) for b in range(nb) for s in range(nseg)]
        load_x(0, 0, 0)
        pcount = [0]

        def proj(slot, c0, ncol, key):
            i = pcount[0] % 2
            pcount[0] += 1
            n = 0
            for k in range(KC):
                for sft in range(2):
                    S.op("pe", "matmul", ps[i][0:ncol, :], Wc[:, k, sft, c0:c0 + ncol], xb[slot][:, k, (1 - sft):(1 - sft) + SEG],
                         start=(n == 0), stop=(n == 2 * KC - 1), r=["Wc", ("xb", slot)], w=[pk[i]])
                    n += 1
            return ps[i], pk[i]

        col = lambda j: par[:, j:j + 1]
        for si, (b, s) in enumerate(segs):
            slot = si % 2
            t0 = b * ntok_b + s * SEG
            if si + 1 < len(segs):
                load_x(segs[si + 1][0], segs[si + 1][1], 1 - slot)
            if s == 0:
                S.op("dve", "memset", H[:], 0.0, w=["H"])
            p, k_ = proj(slot, 0, 128, None)
            S.op("act", "copy", out=rT[:], in_=p[:], r=[k_], w=["rT"])
            p, k_ = proj(slot, 128, 128, None)
            S.op("act", "copy", out=kT[:], in_=p[:], r=[k_], w=["kT"])
            p, k_ = proj(slot, 256, 128, None)
            S.op("act", "copy", out=vT[:], in_=p[:], r=[k_], w=["vT"])
            p, k_ = proj(slot, 384, 128, None)
            S.op("act", "activation", out=lor[0:64, 0, :], in_=p[0:64, :], func=AF.Tanh, r=[k_], w=["lor0a"])
            S.op("act", "copy", out=lor[64:128, 0, :], in_=p[64:128, :], r=[k_], w=["lor0b"])
            p, k_ = proj(slot, 512, 128, None)
            S.op("act", "activation", out=lor[:, 1, :], in_=p[:], func=AF.Sigmoid, r=[k_], w=["lor1"])
            p, k_ = proj(slot, 640, 64, None)
            S.op("act", "activation", out=lor[0:32, 2, :], in_=p[0:32, :], func=AF.Sigmoid, r=[k_], w=["lor2a"])
            S.op("act", "copy", out=lor[32:64, 2, :], in_=p[32:64, :], r=[k_], w=["lor2b"])
            i = pcount[0] % 2; pcount[0] += 1
            S.op("pe", "matmul", ps[i][:], w2b[0:64, 0, :], lor[0:64, 0, :], start=True, stop=True, r=["w2b", "lor0a"], w=[pk[i]])
            S.op("act", "activation", out=wT[:], in_=ps[i][:], func=AF.Sigmoid, bias=col(0), scale=1.0, r=[pk[i], "par"], w=["wT"])
            S.op("act", "activation", out=wT[:], in_=wT[:], func=AF.Exp, scale=DECAY_SCALE, r=["wT"], w=["wT"])
            i = pcount[0] % 2; pcount[0] += 1
            S.op("pe", "matmul", ps[i][:], w2b[64:128, 0, :], lor[64:128, 0, :], start=True, stop=True, r=["w2b", "lor0b"], w=[pk[i]])
            S.op("act", "activation", out=aT[:], in_=ps[i][:], func=AF.Sigmoid, bias=col(1), scale=1.0, r=[pk[i], "par"], w=["aT"])
            i = pcount[0] % 2; pcount[0] += 1
            S.op("pe", "matmul", ps[i][:], w2b[:, 1, :], lor[:, 1, :], start=True, stop=False, r=["w2b", "lor1"], w=[pk[i]])
            S.op("pe", "matmul", ps[i][:], w2b[0:32, 2, :], lor[0:32, 2, :], start=False, stop=True, r=["w2b", "lor2a"], w=[pk[i]])
            S.op("act", "copy", out=gT[:], in_=ps[i][:], r=[pk[i]], w=["gT"])
            if layer1:
                i = pcount[0] % 2; pcount[0] += 1
                S.op("pe", "matmul", ps[i][:], w2b[32:64, 2, :], lor[32:64, 2, :], start=True, stop=True, r=["w2b", "lor2b"], w=[pk[i]])
                S.op("act", "activation", out=t1[:], in_=ps[i][:], func=AF.Sigmoid, bias=col(2), scale=1.0, r=[pk[i], "par"], w=["t1"])
                S.op("sp", "dma_start", out=t2[:], in_=vf_d[:, t0:t0 + SEG], w=["t2"], dma=True)
                S.op("dve", "tensor_tensor", t2[:], t2[:], vT[:], ALU.subtract, r=["t2", "vT"], w=["t2"])
                S.op("dve", "tensor_tensor", t2[:], t2[:], t1[:], ALU.mult, r=["t2", "t1"], w=["t2"])
                S.op("dve", "tensor_tensor", vT[:], vT[:], t2[:], ALU.add, r=["vT", "t2"], w=["vT"])
            else:
                S.op("sp", "dma_start", out=vf_o[:, t0:t0 + SEG], in_=vT[:], r=["vT"], dma=True)
            if stop_after == 0:
                break
            S.op("dve", "tensor_scalar", av[:], kT[:], col(3), None, ALU.mult, r=["kT", "par"], w=["av"])
            S.op("act", "activation", out=t1[:], in_=av[:], func=AF.Square, r=["av"], w=["t1"])
            i = pcount[0] % 2; pcount[0] += 1
            S.op("pe", "matmul", ps[i][:], bones, t1[:], start=True, stop=True, r=["cst", "t1"], w=[pk[i]])
            S.op("dve", "tensor_scalar", t2[:], ps[i][:], 1e-24, None, ALU.max, r=[pk[i]], w=["t2"])
            S.op("act", "activation", out=t2[:], in_=t2[:], func=AF.Sqrt, r=["t2"], w=["t2"])
            S.op("dve", "reciprocal", t2[:], t2[:], r=["t2"], w=["t2"])
            S.op("dve", "tensor_tensor", av[:], av[:], t2[:], ALU.mult, r=["av", "t2"], w=["av"])
            S.op("dve", "tensor_tensor", bv[:], av[:], aT[:], ALU.mult, r=["av", "aT"], w=["bv"])
            S.op("dve", "tensor_scalar", t1[:], aT[:], -1.0, col(4), ALU.add, ALU.mult, r=["aT", "par"], w=["t1"])
            S.op("dve", "scalar_tensor_tensor", out=kT[:], in0=t1[:], scalar=1.0, in1=kT[:], op0=ALU.add, op1=ALU.mult,
                 r=["t1", "kT"], w=["kT"])
            if stop_after == 1:
                break
            w3 = wT[:].rearrange("p (c j) -> p c j", j=64)
            S.op("pool", "tensor_copy", out=d0[:], in_=wT[:], r=["wT"], w=["d0"])
            S.op("pool", "memset", d0[:].rearrange("p (c j) -> p c j", j=64)[:, :, 0:1], 0.0, w=["d0"])
            S.op("pool", "memset", d1[:], 0.0, w=["d1"])
            S.op("pool", "tensor_copy", out=d1[:].rearrange("p (c j) -> p c j", j=64)[:, :, 0:1], in_=w3[:, :, 0:1], r=["wT"], w=["d1"])
            S.op("dve", "tensor_tensor_scan", P[:], d0[:], d1[:], 0.0, ALU.mult, ALU.add, r=["d0", "d1"], w=["P"])
            S.op("dve", "reciprocal", rP[:], P[:], r=["P"], w=["rP"])
            S.op("dve", "reciprocal", t1[:], wT[:], r=["wT"], w=["t1"])
            S.op("dve", "tensor_tensor", t1[:], t1[:], P[:], ALU.mult, r=["t1", "P"], w=["t1"])
            c3 = lambda t: t[:].rearrange("p (c j) -> p c j", j=64)
            S.op("dve", "scalar_tensor_tensor", out=AR[:, :, 0, :], in0=c3(av), scalar=-1.0, in1=c3(t1), op0=ALU.mult, op1=ALU.mult,
                 r=["av", "t1"], w=["AR"])
            S.op("pool", "tensor_tensor", AR[:, :, 1, :], c3(rT), c3(P), ALU.mult, r=["rT", "P"], w=["AR"])
            S.op("pool", "tensor_tensor", KBf[:, :, 0, :], c3(kT), c3(rP), ALU.mult, r=["kT", "rP"], w=["KBf"])
            S.op("dve", "tensor_tensor", KBf[:, :, 1, :], c3(bv), c3(rP), ALU.mult, r=["bv", "rP"], w=["KBf"])
            pend = c3(P)[:, :, 63:64]
            for q in range(2):
                S.op("dve" if q else "pool", "tensor_tensor", KBh[:, :, q, :], KBf[:, :, q, :], _bc(pend, [128, NCH, 64]), ALU.mult,
                     r=["KBf", "P"], w=["KBh"])
            if stop_after == 2:
                break
            for hh in range(2):
                hb = hh * 64
                S.op("pool", "tensor_copy", out=ARblk[hb:hb + 64, :, hh, :], in_=AR[hb:hb + 64, :, :, :].rearrange("p c a b -> p c (a b)"),
                     r=["AR"], w=["ARblk"])
                S.op("pool", "tensor_copy", out=Bblk[hb:hb + 64, :, hh, :], in_=KBf[hb:hb + 64, :, 1, :], r=["KBf"], w=["Bblk"])
            for half in range(2):
                for cc in range(4):
                    c = half * 4 + cc
                    S.op("pe", "transpose", ps[2][0:64, cc * 128:(cc + 1) * 128], vT[:, c * 64:(c + 1) * 64], ident,
                         r=["vT", "cst"], w=[pk[2]])
                S.op("act", "copy", out=Vtm[:, half * 4:(half + 1) * 4, :].rearrange("p c f -> p (c f)"), in_=ps[2][0:64, :], r=[pk[2]], w=["Vtm"])
            for q2 in range(4):
                for cc in range(2):
                    c = q2 * 2 + cc
                    for q in range(2):
                        S.op("pe", "transpose", ps[2][0:64, (cc * 2 + q) * 128:(cc * 2 + q + 1) * 128], KBh[:, c, q, :], ident,
                             r=["KBh", "cst"], w=[pk[2]])
                S.op("act", "copy", out=KBtm[:, q2 * 2:(q2 + 1) * 2, :, :].rearrange("p c q f -> p (c q f)"), in_=ps[2][0:64, :],
                     r=[pk[2]], w=["KBtm"])
            if stop_after == 3:
                break
            for c2 in range(NCH // 2):
                for which, dst in ((0, AK), (1, AB)):
                    for cc in range(2):
                        c = c2 * 2 + cc
                        S.op("pe", "matmul", ps[2][0:64, cc * 256:(cc + 1) * 256], KBf[:, c, which, :],
                             ARblk[:, c, :, :].rearrange("p a b -> p (a b)"), start=True, stop=True, r=["KBf", "ARblk"], w=[pk[2]])
                    S.op("dve", "tensor_tensor", dst[:, c2 * 4:(c2 + 1) * 4, :], ps[2][0:64, :].rearrange("p (a b) -> p a b", b=128),
                         _bc(m_ak.unsqueeze(1), [64, 4, 128]), ALU.mult, r=[pk[2], "cst"], w=["AK" if which == 0 else "AB"])
            for c4 in range(NCH // 4):
                for cc in range(4):
                    c = c4 * 4 + cc
                    S.op("pe", "matmul", ps[2][0:64, cc * 128:(cc + 1) * 128], AR[:, c, 0, :],
                         Bblk[:, c, :, :].rearrange("p a b -> p (a b)"), start=True, stop=True, r=["Bblk", "AR"], w=[pk[2]])
                S.op("dve", "tensor_tensor", Np[0][:, c4 * 8:(c4 + 1) * 8, :], ps[2][0:64, :].rearrange("p (a b) -> p a b", b=64),
                     _bc(m_sl.unsqueeze(1), [64, 8, 64]), ALU.mult, r=[pk[2], "cst"], w=["Np0"])
            if stop_after == 4:
                break
            S.op("pool", "tensor_copy", out=Mp[0][:], in_=AB[:, :, 0:64], r=["AB"], w=["Mp0"])
            S.op("dve", "tensor_tensor", Q[:], AB[:, :, 0:64], _bc(id64.unsqueeze(1), [64, NBLK, 64]), ALU.add, r=["AB", "cst"], w=["Q"])
            for lvl in range(5):
                cur, nxt = lvl % 2, (lvl + 1) % 2
                for g8 in range(NBLK // 8):
                    gsl = slice(g8 * 8, (g8 + 1) * 8)
                    for bi in range(8):
                        blk = g8 * 8 + bi
                        S.op("pe", "matmul", ps[3][0:64, bi * 64:(bi + 1) * 64], Np[cur][:, blk, :], Mp[cur][:, blk, :],
                             start=True, stop=True, r=["Np%d" % cur, "Mp%d" % cur], w=[pk[3]])
                    for bi in range(8):
                        blk = g8 * 8 + bi
                        S.op("pe", "matmul", ps[4][0:64, bi * 64:(bi + 1) * 64], Mp[cur][:, blk, :], Np[cur][:, blk, :],
                             start=True, stop=True, r=["Np%d" % cur, "Mp%d" % cur], w=[pk[4]])
                    S.op("act", "copy", out=Mp[nxt][:, gsl, :].rearrange("p a b -> p (a b)"), in_=ps[3][0:64, :], r=[pk[3]], w=["Mp%d" % nxt])
                    S.op("dve", "tensor_copy", out=Np[nxt][:, gsl, :].rearrange("p a b -> p (a b)"), in_=ps[4][0:64, :], r=[pk[4]], w=["Np%d" % nxt])
                    for bi in range(8):
                        blk = g8 * 8 + bi
                        S.op("pe", "matmul", ps[5][0:64, bi * 64:(bi + 1) * 64], Np[nxt][:, blk, :], Q[:, blk, :],
                             start=True, stop=True, r=["Np%d" % nxt, "Q"], w=[pk[5]])
                    S.op("dve", "tensor_tensor", Q[:, gsl, :].rearrange("p a b -> p (a b)"), Q[:, gsl, :].rearrange("p a b -> p (a b)"),
                         ps[5][0:64, :], ALU.add, r=["Q", pk[5]], w=["Q"])
            if stop_after == 5:
                break
            for c in range(NCH):
                S.op("pe", "matmul", ps[3][0:64, 0:128], AR[:, c, 0, :], H[:], start=True, stop=False, r=["AR", "H"], w=[pk[3]])
                for hh in range(2):
                    hb = hh * 64
                    blk = 2 * c + hh
                    S.op("pe", "matmul", ps[3][0:64, hb:hb + 64], AK[:, blk, 0:64], Vtm[:, c, hb:hb + 64], start=False, stop=(hh == 1),
                         r=["AK", "Vtm"], w=[pk[3]])
                S.op("act", "copy", out=Xs[:], in_=ps[3][0:64, 0:128], r=[pk[3]], w=["Xs"])
                for hh in range(2):
                    hb = hh * 64
                    blk = 2 * c + hh
                    S.op("pe", "matmul", ps[4][0:64, hb:hb + 64], Q[:, blk, :], Xs[:, hb:hb + 64], start=True, stop=True,
                         r=["Q", "Xs"], w=[pk[4]])
                S.op("act", "copy", out=Us[:], in_=ps[4][0:64, 0:128], r=[pk[4]], w=["Us"])
                S.op("pe", "matmul", ps[5][0:64, 0:128], AR[:, c, 1, :], H[:], start=True, stop=False, r=["AR", "H"], w=[pk[5]])
                for hh in range(2):
                    hb = hh * 64
                    blk = 2 * c + hh
                    S.op("pe", "matmul", ps[5][0:64, hb:hb + 64], AB[:, blk, 64:128], Us[:, hb:hb + 64], start=False, stop=False,
                         r=["AB", "Us"], w=[pk[5]])
                    S.op("pe", "matmul", ps[5][0:64, hb:hb + 64], AK[:, blk, 64:128], Vtm[:, c, hb:hb + 64], start=False, stop=(hh == 1),
                         r=["AK", "Vtm"], w=[pk[5]])
                S.op("act", "copy", out=Ytm[:, c, :], in_=ps[5][0:64, 0:128], r=[pk[5]], w=["Ytm"])
                S.op("pe", "matmul", ps[6][:, 0:128], KBtm[:, c, 0, :], Vtm[:, c, :], start=True, stop=False, r=["KBtm", "Vtm"], w=[pk[6]])
                S.op("pe", "matmul", ps[6][:, 0:128], KBtm[:, c, 1, :], Us[:], start=False, stop=True, r=["KBtm", "Us"], w=[pk[6]])
                for hh in range(2):
                    hb = hh * 64
                    S.op("dve", "scalar_tensor_tensor", out=H[hb:hb + 64, hb:hb + 64], in0=H[hb:hb + 64, hb:hb + 64],
                         scalar=c3(P)[hb:hb + 64, c, 63:64], in1=ps[6][hb:hb + 64, hb:hb + 64],
                         op0=ALU.mult, op1=ALU.add, r=["H", "P", pk[6]], w=["H"])
            if stop_after == 6:
                break
            for c in range(NCH):
                S.op("pe", "transpose", ps[7][:, c * 64:(c + 1) * 64], Ytm[:, c, :], id64, r=["Ytm", "cst"], w=[pk[7]])
            S.op("act", "copy", out=yT[:], in_=ps[7][:], r=[pk[7]], w=["yT"])
            i = pcount[0] % 2; pcount[0] += 1
            S.op("pe", "matmul", ps[i][:], bones, yT[:], start=True, stop=True, r=["cst", "yT"], w=[pk[i]])
            S.op("dve", "scalar_tensor_tensor", out=yT[:], in0=ps[i][:], scalar=-1.0 / 64, in1=yT[:], op0=ALU.mult, op1=ALU.add,
                 r=[pk[i], "yT"], w=["yT"])
            S.op("act", "activation", out=t1[:], in_=yT[:], func=AF.Square, r=["yT"], w=["t1"])
            i = pcount[0] % 2; pcount[0] += 1
            S.op("pe", "matmul", ps[i][:], bones, t1[:], start=True, stop=True, r=["cst", "t1"], w=[pk[i]])
            S.op("act", "activation", out=t2[:], in_=ps[i][:], func=AF.Sqrt, bias=GN_EPS, scale=1.0 / 64, r=[pk[i]], w=["t2"])
            S.op("dve", "reciprocal", t2[:], t2[:], r=["t2"], w=["t2"])
            S.op("dve", "tensor_tensor", yT[:], yT[:], t2[:], ALU.mult, r=["yT", "t2"], w=["yT"])
            S.op("act", "activation", out=yT[:], in_=yT[:], func=AF.Identity, scale=col(6), bias=col(7), r=["yT", "par"], w=["yT"])
            S.op("dve", "scalar_tensor_tensor", out=t1[:], in0=rT[:], scalar=col(5), in1=kT[:], op0=ALU.mult, op1=ALU.mult,
                 r=["rT", "kT", "par"], w=["t1"])
            i = pcount[0] % 2; pcount[0] += 1
            S.op("pe", "matmul", ps[i][:], bones, t1[:], start=True, stop=True, r=["cst", "t1"], w=[pk[i]])
            S.op("dve", "tensor_tensor", t2[:], ps[i][:], vT[:], ALU.mult, r=[pk[i], "vT"], w=["t2"])
            S.op("dve", "tensor_tensor", yT[:], yT[:], t2[:], ALU.add, r=["yT", "t2"], w=["yT"])
            S.op("dve", "tensor_tensor", ob[:], yT[:], gT[:], ALU.mult, r=["yT", "gT"], w=["ob"])
            S.op("sp", "dma_start", out=mix_o[:, t0:t0 + SEG], in_=ob[:], r=["ob"], dma=True)
        S.emit()
    return nc


def rwkv_host_inputs(inp, li, core):
    cs = slice(core * 128, (core + 1) * 128)
    f = np.float32
    wproj = np.zeros((D, NPROJ), f)
    mu = np.zeros((D, NPROJ), f)
    M = inp["rwkv_mu"][li]
    wproj[:, 0:128] = inp["rwkv_w_rkv"][li, 0][:, cs]; mu[:, 0:128] = M[0][:, None]
    wproj[:, 128:256] = inp["rwkv_w_rkv"][li, 1][:, cs]; mu[:, 128:256] = M[1][:, None]
    wproj[:, 256:384] = inp["rwkv_w_rkv"][li, 2][:, cs]; mu[:, 256:384] = M[2][:, None]
    wproj[:, 384:448] = inp["rwkv_decay_w1"][li]; mu[:, 384:448] = M[3][:, None]
    wproj[:, 448:512] = inp["rwkv_iclr_a1"][li]; mu[:, 448:512] = M[4][:, None]
    wproj[:, 512:672] = inp["rwkv_gate_g1"][li]; mu[:, 512:672] = M[5][:, None]
    if li > 0:
        wproj[:, 672:704] = inp["rwkv_vres_v1"][li - 1]
    mu[:, 672:704] = M[2][:, None]
    w2 = np.zeros((128, 3, 128), f)
    w2[0:64, 0] = inp["rwkv_decay_w2"][li][:, cs]
    w2[64:128, 0] = inp["rwkv_iclr_a2"][li][:, cs]
    w2[:, 1] = inp["rwkv_gate_g2"][li][0:128, cs]
    w2[0:32, 2] = inp["rwkv_gate_g2"][li][128:160, cs]
    if li > 0:
        w2[32:64, 2] = inp["rwkv_vres_v2"][li - 1][:, cs]
    par = np.zeros((128, 8), f)
    par[:, 0] = inp["rwkv_decay_w0"][li][cs]
    par[:, 1] = inp["rwkv_iclr_a0"][li][cs]
    if li > 0:
        par[:, 2] = inp["rwkv_vres_v0"][li - 1][cs]
    par[:, 3] = inp["rwkv_k_k"][li][cs]
    par[:, 4] = inp["rwkv_k_a"][li][cs]
    par[:, 5] = inp["rwkv_r_k"][li].reshape(-1)[cs]
    par[:, 6] = inp["rwkv_gn_g"][li][cs]
    par[:, 7] = inp["rwkv_gn_b"][li][cs]
    mu_l = np.ascontiguousarray(mu.reshape(KC, 128, NPROJ).transpose(1, 0, 2))
    return {"wproj": wproj, "mu": mu_l, "w2": w2, "par": par, "cst": rwkv_consts()}


NEG = -1.0e30
BLK = 256


def moba_consts():
    c = {}
    ident = np.eye(128, dtype=np.float32)
    c["identf"] = ident
    key = np.arange(128)[:, None]
    q = np.arange(128)[None, :]
    tri = np.where(key <= q, 0.0, NEG).astype(np.float32)
    oh = np.zeros((32, 32, 128), np.float32)
    for n in range(32):
        oh[n, n, :] = 1.0e30
    cb = np.zeros((128, 128 + 128 + 32 * 128), np.float32)
    cb[:, 0:128] = ident
    cb[:, 128:256] = tri
    cb[0:32, 256:] = oh.reshape(32, 32 * 128)
    return {"cstf": ident, "cstb": cb.astype(ml_dtypes.bfloat16)}


def build_H_moba(separate_kv, ntok_b=T, nb=B, dbg=()):
    import contextlib
    nseg = ntok_b // 512
    ntile = ntok_b // 128
    nblkb = ntok_b // BLK
    ntot = nb * ntok_b
    nc = bass.Bass("TRN2", target_bir_lowering=False)
    with contextlib.ExitStack() as es:
        cx = Ctx(nc, es)
        S = cx.S
        xT = cx.dram("xT", [D, ntot], F32, "ExternalInput")
        if separate_kv:
            xkvT = cx.dram("xkvT", [D, ntot], F32, "ExternalInput")
        else:
            xkvT = xT
        wqkv_d = cx.dram("wqkv", [D, 3, 128], F32, "ExternalInput")
        cstf_d = cx.dram("cstf", [128, 128], F32, "ExternalInput")
        cstb_d = cx.dram("cstb", [128, 256 + 32 * 128], BF16, "ExternalInput")
        mix_o = cx.dram("mix", [128, ntot], BF16, "ExternalOutput")

        wqkv = cx.sb("wqkv_sb", [128, KC, 3, 128], BF16)
        identf = cx.sb("identf", [128, 128], F32)
        cstb = cx.sb("cstb_sb", [128, 256 + 32 * 128], BF16)
        identb = cstb[:, 0:128]
        trib = cstb[:, 128:256]
        xq = [cx.sb("xq%d" % i, [128, KC, 512], BF16) for i in range(2)]
        xkv = [cx.sb("xkv%d" % i, [128, KC, 512], BF16) for i in range(2)] if separate_kv else xq
        KT = cx.sb("KT", [128, ntok_b], BF16)
        Va = cx.sb("Va", [128, ntile, 2, 65], BF16)
        qf = cx.sb("qf", [128, ntok_b], F32)
        qz = [cx.sb("qz%d" % i, [128, ntok_b], BF16) for i in range(2)]
        km = cx.sb("km", [128, nblkb], F32)
        kmblk = cx.sb("kmblk", [128, 2, nblkb], F32)
        gsb = cx.sb("gsb", [128, 2, 32], F32)
        top8 = cx.sb("top8", [128, 2, 8], F32)
        mm1 = cx.sb("mm1", [128, 2, 32], F32)
        mT = [cx.sb("mT%d" % i, [32, 2, 128], BF16) for i in range(2)]
        PT = [cx.sb("PT%d" % i, [128, 512], BF16) for i in range(3)]
        osb = cx.sb("osb", [128, 128], F32)
        rs = cx.sb("rs", [128, 1], F32)
        ob = cx.sb("ob", [128, 512], BF16)
        ps = [cx.ps("ps%d" % i, [128, 512]) for i in range(8)]
        pk = [("ps", i) for i in range(8)]

        S.op("sp", "dma_start", out=identf[:], in_=cstf_d, w=["identf"], dma=True)
        S.op("sp", "dma_start", out=cstb[:], in_=cstb_d, w=["cstb"], dma=True)
        S.op("pool", "dma_start", out=wqkv[:], in_=wqkv_d.rearrange("(c p) a e -> p c a e", p=128), w=["wqkv"], dma=True)
        S.op("dve", "memset", Va[:, :, :, 64:65], 1.0, w=["Va"])
        S.op("dve", "memset", qz[0][64:128, :], 0.0, w=["qz0"])
        S.op("dve", "memset", qz[1][0:64, :], 0.0, w=["qz1"])
        S.op("dve", "memset", kmblk[:], 0.0, w=["kmblk"])

        fm = lambda ap: ap.rearrange("(c p) t -> p c t", p=128)

        def load_seg(b, s, slot):
            t0 = b * ntok_b + s * 512
            S.op("pool", "dma_start", out=xq[slot][:], in_=fm(xT[:, t0:t0 + 512]), w=[("xq", slot)], dma=True)
            if separate_kv:
                S.op("pool", "dma_start", out=xkv[slot][:], in_=fm(xkvT[:, t0:t0 + 512]), w=[("xkv", slot)], dma=True)

        segs = [(b, s) for b in range(nb) for s in range(nseg)]
        kvk = (lambda slot: ("xkv", slot)) if separate_kv else (lambda slot: ("xq", slot))
        load_seg(0, 0, 0)
        scount = 0
        pcount = 0
        for b in range(nb):
            S.op("dve", "memset", gsb[:], NEG, w=["gsb"])
            S.op("dve", "memset", km[:], 0.0, w=["km"])
            for s in range(nseg if "cut0" not in dbg else 0):
                si = b * nseg + s
                slot = si % 2
                if si + 1 < len(segs):
                    load_seg(segs[si + 1][0], segs[si + 1][1], 1 - slot)
                ss = slice(s * 512, (s + 1) * 512)
                for k in range(KC):
                    S.op("pe", "matmul", ps[0][:], wqkv[:, k, 1, :], xkv[slot][:, k, :], start=(k == 0), stop=(k == KC - 1),
                         r=["wqkv", kvk(slot)], w=[pk[0]])
                for hb2 in range(2):
                    S.op("act", "activation", out=KT[:, s * 512 + hb2 * BLK:s * 512 + (hb2 + 1) * BLK], in_=ps[0][:, hb2 * BLK:(hb2 + 1) * BLK],
                         func=AF.Copy, accum_out=km[:, 2 * s + hb2:2 * s + hb2 + 1], r=[pk[0]], w=["KT", "km"])
                if "cut1" in dbg:
                    continue
                for tt in range(4):
                    for k in range(KC):
                        S.op("pe", "matmul", ps[1][:, tt * 128:(tt + 1) * 128], xkv[slot][:, k, tt * 128:(tt + 1) * 128], wqkv[:, k, 2, :],
                             start=(k == 0), stop=(k == KC - 1), r=["wqkv", kvk(slot)], w=[pk[1]])
                S.op("act", "copy", out=Va[:, 4 * s:4 * s + 4, :, 0:64], in_=ps[1][:].rearrange("p (t h d) -> p t h d", h=2, d=64),
                     r=[pk[1]], w=["Va"])
                if "cut2" in dbg:
                    continue
                for k in range(KC):
                    S.op("pe", "matmul", ps[2][:], wqkv[:, k, 0, :], xq[slot][:, k, :], start=(k == 0), stop=(k == KC - 1),
                         r=["wqkv", ("xq", slot)], w=[pk[2]])
                S.op("act", "copy", out=qf[:, ss], in_=ps[2][:], r=[pk[2]], w=["qf"])
                S.op("dve", "tensor_copy", out=qz[0][0:64, ss], in_=qf[0:64, ss], r=["qf"], w=["qz0"])
                S.op("dve", "tensor_copy", out=qz[1][64:128, ss], in_=qf[64:128, ss], r=["qf"], w=["qz1"])
            for hh in range(2):
                hb = hh * 64
                S.op("dve", "tensor_scalar", kmblk[hb:hb + 64, hh, :], km[hb:hb + 64, :], 1.0 / BLK, None, ALU.mult, r=["km"], w=["kmblk"])
            for qt in range(ntile if "proj_only" not in dbg else 0):
                own = qt // 2
                qs = slice(qt * 128, (qt + 1) * 128)
                if own > 0 and "nogate" not in dbg:
                    S.op("pe", "matmul", ps[3][:, 0:2 * nblkb], qf[:, qs], kmblk[:].rearrange("p a b -> p (a b)"), start=True, stop=True,
                         r=["qf", "kmblk"], w=[pk[3]])
                    S.op("dve", "tensor_copy", out=gsb[:, :, 0:own], in_=ps[3][:, 0:2 * nblkb].rearrange("p (a b) -> p a b", b=nblkb)[:, :, 0:own],
                         r=[pk[3]], w=["gsb"])
                    mt = mT[qt % 2]
                    mk = ("mT", qt % 2)
                    for hh in range(2):
                        S.op("dve", "max", out=top8[:, hh, :], in_=gsb[:, hh, :], r=["gsb"], w=["top8"])
                        S.op("dve", "tensor_scalar", mm1[:, hh, :], gsb[:, hh, :], top8[:, hh, 2:3], -1.0, ALU.is_ge, ALU.add,
                             r=["gsb", "top8"], w=["mm1"])
                    for hh in range(2):
                        S.op("pe", "transpose", ps[3][0:32, 256 + hh * 128:256 + (hh + 1) * 128], mm1[:, hh, :], identf[:], r=["mm1", "identf"], w=[pk[3]])
                    S.op("act", "copy", out=mt[:].rearrange("p a b -> p (a b)"), in_=ps[3][0:32, 256:512], r=[pk[3]], w=[mk])
                for hh in range(2):
                    hb = hh * 64
                    po = ps[6 + (pcount % 2)]
                    pok = pk[6 + (pcount % 2)]
                    pcount += 1
                    kts = list(range(0, qt + 1))
                    ngrp = (len(kts) + 3) // 4
                    for gi in range(ngrp):
                        grp = kts[gi * 4:(gi + 1) * 4]
                        sb_i = scount % 3
                        scount += 1
                        pS, pSk = ps[sb_i], pk[sb_i]
                        pt, ptk = PT[sb_i], ("PT", sb_i)
                        for j, kt in enumerate(grp):
                            nomask = (kt != qt and kt // 2 == own) or (kt != qt and "nogate" in dbg)
                            S.op("pe", "matmul", pS[:, j * 128:(j + 1) * 128], KT[:, kt * 128:(kt + 1) * 128], qz[hh][:, qs], start=True, stop=nomask,
                                 r=["KT", "qz%d" % hh], w=[pSk])
                            if kt == qt:
                                S.op("pe", "matmul", pS[:, j * 128:(j + 1) * 128], identb, trib, start=False, stop=True, r=["cstb"], w=[pSk])
                            elif not nomask:
                                n = kt // 2
                                S.op("pe", "matmul", pS[:, j * 128:(j + 1) * 128], cstb[0:32, 256 + n * 128:256 + (n + 1) * 128], mt[:, hh, :],
                                     start=False, stop=True, r=["cstb", mk], w=[pSk])
                        w_ = len(grp) * 128
                        S.op("act", "activation", out=pt[:, 0:w_], in_=pS[:, 0:w_], func=AF.Exp, scale=0.125, r=[pSk], w=[ptk])
                        for j, kt in enumerate(grp):
                            first = (gi == 0 and j == 0)
                            last = (gi == ngrp - 1 and j == len(grp) - 1)
                            S.op("pe", "matmul", po[:, 0:65], pt[:, j * 128:(j + 1) * 128], Va[:, kt, hh, :], start=first, stop=last,
                                 r=[ptk, "Va"], w=[pok])
                    S.op("dve", "reciprocal", rs[:], po[:, 64:65], r=[pok], w=["rs"])
                    S.op("dve", "tensor_scalar", osb[:, hb:hb + 64], po[:, 0:64], rs[:, 0:1], None, ALU.mult, r=[pok, "rs"], w=["osb"])
                S.op("pe", "transpose", ps[4][:, 0:128], osb[:], identf[:], r=["osb", "identf"], w=[pk[4]])
                S.op("act", "copy", out=ob[:, (qt % 4) * 128:(qt % 4 + 1) * 128], in_=ps[4][:, 0:128], r=[pk[4]], w=["ob"])
                if qt % 4 == 3:
                    t0 = b * ntok_b + (qt - 3) * 128
                    S.op("sp", "dma_start", out=mix_o[:, t0:t0 + 512], in_=ob[:], r=["ob"], dma=True)
        S.emit()
    return nc


def moba_host_inputs(inp, j, core):
    cs = slice(core * 128, (core + 1) * 128)
    w = np.stack([inp["moba_w_q"][j][:, cs], inp["moba_w_k"][:, cs], inp["moba_w_v"][:, cs]], axis=1)
    m = {"wqkv": np.ascontiguousarray(w.astype(np.float32))}
    m.update(moba_consts())
    return m


_PROGS = {}
_DUMP = None
_NL = 4


def _prog(name, fn):
    if name not in _PROGS:
        _PROGS[name] = fn()
    return _PROGS[name]


def _run(nc, maps):
    res = run_bass_kernel_spmd(nc, maps, core_ids=list(range(NCORES)))
    return res.results


def _t_phase(inp, layer, mixT, hT):
    moe = (layer % 2 == 1)
    e = layer // 2
    nc = _prog("T_moe" if moe else "T_dense", lambda: build_T(moe))
    if layer < 2:
        w_o = inp["rwkv_w_out"][layer]
    else:
        w_o = inp["moba_w_o"][layer - 2]
    base = {"w_o": np.ascontiguousarray(w_o, dtype=np.float32), "lnp": lnp_layout(inp["ln_g"], inp["ln_b"], layer),
            "consts": make_consts()}
    if moe:
        base["router"] = np.ascontiguousarray(inp["moe_router"][e])
        base["w_gate"] = np.ascontiguousarray(inp["moe_w_gate"][e])
        base["w_up"] = np.ascontiguousarray(inp["moe_w_up"][e])
        base["w_down"] = np.ascontiguousarray(inp["moe_w_down"][e])
    else:
        base["w_gate"] = np.ascontiguousarray(inp["ffn_w_gate"][e][None])
        base["w_up"] = np.ascontiguousarray(inp["ffn_w_up"][e][None])
        base["w_down"] = np.ascontiguousarray(inp["ffn_w_down"][e][None])
    maps = []
    for c in range(NCORES):
        m = dict(base)
        m["mixT"] = np.ascontiguousarray(mixT[:, c * NT:(c + 1) * NT])
        m["hT"] = np.ascontiguousarray(hT[:, c * NT:(c + 1) * NT])
        maps.append(m)
    res = _run(nc, maps)
    return np.concatenate([r["outT"] for r in res], axis=1)


def kernel(**inp):
    inp = {k: np.asarray(v) for k, v in inp.items()}
    x = inp["x"].astype(np.float32, copy=False)
    hT = np.ascontiguousarray(x.reshape(NTOK, D).T)
    vfirst = None
    h_kv = None
    for layer in range(_NL):
        if layer < 2:
            nc = _prog("H_rwkv%d" % layer, lambda: build_H_rwkv(layer == 1))
            maps = []
            for c in range(NCORES):
                m = rwkv_host_inputs(inp, layer, c)
                m["xT"] = hT
                if layer == 1:
                    m["vfirst"] = np.ascontiguousarray(vfirst[c * 128:(c + 1) * 128])
                maps.append(m)
            res = _run(nc, maps)
            if layer == 0:
                vfirst = np.concatenate([r["vfirst_out"] for r in res], axis=0)
        else:
            j = layer - 2
            nc = _prog("H_moba%d" % j, lambda: build_H_moba(j == 1))
            maps = []
            for c in range(NCORES):
                m = moba_host_inputs(inp, j, c)
                m["xT"] = hT
                if j == 1:
                    m["xkvT"] = h_kv
                maps.append(m)
            res = _run(nc, maps)
        mixT = np.concatenate([r["mix"] for r in res], axis=0)
        if _DUMP is not None:
            _DUMP["mix%d" % layer] = mixT
        hT = _t_phase(inp, layer, mixT, hT)
        if _DUMP is not None:
            _DUMP["h%d" % layer] = hT
        if layer == 1:
            h_kv = hT
    return np.ascontiguousarray(hT.T).reshape(B, T, D).astype(np.float32)
```

```python
import numpy as np
import ml_dtypes
import concourse.bass as bass
import concourse.mybir as mybir
from concourse.bass_utils import run_bass_kernel_spmd

F32 = mybir.dt.float32
BF16 = mybir.dt.bfloat16
ALU = mybir.AluOpType
AF = mybir.ActivationFunctionType
AX = mybir.AxisListType

NCORES = 8
D = 1024
KC = 8
B = 2
T = 8192
NTOK = B * T
NT = NTOK // NCORES
ALPHA = (2.0 * 4) ** 0.25
LN_EPS = 1e-5
GN_EPS = 64e-5


class Sched:
    ENG = ("pe", "act", "dve", "pool", "sp")
    NDMA = 8

    def __init__(self, nc):
        self.nc = nc
        self.ops = {e: [] for e in self.ENG}
        self.lastw = {}
        self.readers = {}

    def add(self, eng, fn, reads=(), writes=(), dma=False):
        idx = len(self.ops[eng])
        deps = {}

        def dep(key, kind):
            if key is None:
                return
            if deps.get(key) != "raw":
                deps[key] = kind

        for r in reads:
            dep(self.lastw.get(r), "raw")
        for w in writes:
            dep(self.lastw.get(w), "waw")
            for k in self.readers.get(w, ()):
                dep(k, "war")
        waits = set()
        for (pe, pi), kind in deps.items():
            pdma = self.ops[pe][pi]["dma"]
            if pe == eng and not pdma and not dma:
                if eng == "pe":
                    continue
            waits.add((pe, pi))
        self.ops[eng].append(dict(fn=fn, waits=waits, dma=dma, signal=dma))
        me = (eng, idx)
        for r in reads:
            lst = self.readers.setdefault(r, [])
            if not dma:
                lst[:] = [k for k in lst if not (k[0] == eng and not self.ops[k[0]][k[1]]["dma"])]
            lst.append(me)
        for w in writes:
            self.lastw[w] = me
            self.readers[w] = []
        return me

    def op(self, eng, method, *args, r=(), w=(), dma=False, **kw):
        return self.add(eng, lambda h: getattr(h, method)(*args, **kw), reads=r, writes=w, dma=dma)

    def emit(self):
        nc = self.nc
        ops = self.ops
        for e in self.ENG:
            for op in ops[e]:
                for (pe, pi) in op["waits"]:
                    ops[pe][pi]["signal"] = True
        import contextlib
        with contextlib.ExitStack() as st:
            csem = {e: st.enter_context(nc.semaphore("c_" + e)) for e in self.ENG}
            dsem = {e: [st.enter_context(nc.semaphore("d_%s%d" % (e, i))) for i in range(self.NDMA)]
                    for e in ("act", "pool", "sp")}
            final_dma = []
            for e in self.ENG:
                cnt = 0
                dcnt = [0] * self.NDMA
                k = 0
                for op in ops[e]:
                    if op["dma"]:
                        s = k % self.NDMA
                        k += 1
                        op["sem"] = dsem[e][s]
                        op["prev"] = dcnt[s]
                        dcnt[s] += 16
                        op["val"] = dcnt[s]
                    elif op["signal"]:
                        cnt += 1
                        op["sem"] = csem[e]
                        op["val"] = cnt
                if e in dsem:
                    for s in range(self.NDMA):
                        if dcnt[s]:
                            final_dma.append((dsem[e][s], dcnt[s]))
            block = st.enter_context(nc.Block())

            def run(e, h):
                waited = {}

                def wait(sem, val):
                    key = id(sem)
                    if waited.get(key, 0) >= val:
                        return
                    waited[key] = val
                    h.wait_ge(sem, val)

                for op in ops[e]:
                    for (pe, pi) in sorted(op["waits"]):
                        p = ops[pe][pi]
                        wait(p["sem"], p["val"])
                    if op["dma"] and op["prev"]:
                        wait(op["sem"], op["prev"])
                    ins = op["fn"](h)
                    if op["dma"]:
                        ins.then_inc(op["sem"], 16)
                    elif op["signal"]:
                        ins.then_inc(op["sem"], 1)
                if e == "sp":
                    for sem, val in final_dma:
                        wait(sem, val)

            @block.tensor
            def _(h):
                run("pe", h)

            @block.scalar
            def _(h):
                run("act", h)

            @block.vector
            def _(h):
                run("dve", h)

            @block.gpsimd
            def _(h):
                run("pool", h)

            @block.sync
            def _(h):
                run("sp", h)


def _bc(ap, shape):
    return ap.broadcast_to(shape)


class Ctx:
    def __init__(self, nc, es):
        self.nc = nc
        self.es = es
        self.S = Sched(nc)

    def sb(self, name, shape, dt):
        return self.es.enter_context(self.nc.sbuf_tensor(name, shape, dt))

    def ps(self, name, shape, dt=F32):
        return self.es.enter_context(self.nc.psum_tensor(name, shape, dt))

    def dram(self, name, shape, dt, kind):
        return self.nc.dram_tensor(name, list(shape), dt, kind=kind).ap()


def emit_layernorm(cx, zt, res_z, hb, res_hb, lnp, gi, bi, onesm, psA, psB, tmp, ngroups, scale_after=None):
    S = cx.S
    mean, msq, var = tmp["mean"], tmp["msq"], tmp["var"]
    for g in range(ngroups):
        gs = slice(g * 512, (g + 1) * 512)
        zkeys = [res_z(k, g) for k in range(KC)]
        for k in range(KC):
            sq = tmp["sq%d" % (k % 2)]
            S.op("act", "activation", out=sq[:], in_=zt[:, k, gs], func=AF.Square, r=[zkeys[k]], w=[("sq", k % 2)])
            S.op("pe", "matmul", psA[:], onesm, zt[:, k, gs], start=(k == 0), stop=(k == KC - 1),
                 r=[zkeys[k], "onesm"], w=["psA"])
            S.op("pe", "matmul", psB[:], onesm, sq[:], start=(k == 0), stop=(k == KC - 1),
                 r=[("sq", k % 2), "onesm"], w=["psB"])
        S.op("act", "copy", out=mean[:], in_=psA[:], r=["psA"], w=["mean"])
        S.op("dve", "tensor_tensor", msq[:], mean[:], mean[:], ALU.mult, r=["mean"], w=["msq"])
        S.op("dve", "tensor_tensor", var[:], psB[:], msq[:], ALU.subtract, r=["psB", "msq"], w=["var"])
        S.op("act", "activation", out=var[:], in_=var[:], func=AF.Sqrt, bias=LN_EPS, scale=1.0, r=["var"], w=["var"])
        S.op("dve", "reciprocal", msq[:], var[:], r=["var"], w=["msq"])
        z3 = zt[:, :, gs]
        S.op("dve", "tensor_tensor", z3, z3, _bc(mean[:].unsqueeze(1), [128, KC, 512]), ALU.subtract,
             r=zkeys + ["mean"], w=zkeys)
        S.op("dve", "tensor_tensor", z3, z3, _bc(msq[:].unsqueeze(1), [128, KC, 512]), ALU.mult,
             r=zkeys + ["msq"], w=zkeys)
        for k in range(KC):
            S.op("act", "activation", out=zt[:, k, gs], in_=zt[:, k, gs], func=AF.Identity,
                 scale=lnp[:, gi, k:k + 1], bias=lnp[:, bi, k:k + 1], r=[zkeys[k], "lnp"], w=[zkeys[k]])
        if hb is not None:
            hkeys = [res_hb(k, g) for k in range(KC)]
            S.op("pool", "tensor_copy", out=hb[:, :, gs], in_=z3, r=zkeys, w=hkeys)
        if scale_after is not None:
            S.op("pool", "tensor_scalar", z3, z3, float(scale_after), None, ALU.mult, r=zkeys, w=zkeys)


def build_T(moe, nt=NT, dff=None, nexp=None):
    import contextlib
    G = nt // 512
    FB = 4
    if dff is None:
        dff = 3584 if moe else 2816
    if nexp is None:
        nexp = 8 if moe else 1
    nfc = dff // 128
    nblk = (nfc + FB - 1) // FB
    ntile = nt // 128
    nc = bass.Bass("TRN2", target_bir_lowering=False)
    with contextlib.ExitStack() as es:
        cx = Ctx(nc, es)
        S = cx.S
        mixT = cx.dram("mixT", [D, nt], BF16, "ExternalInput")
        hT = cx.dram("hT", [D, nt], F32, "ExternalInput")
        w_o = cx.dram("w_o", [D, D], F32, "ExternalInput")
        lnp_d = cx.dram("lnp", [128, 4, KC], F32, "ExternalInput")
        consts = cx.dram("consts", [128, 128 + 128 + 8 * 128], F32, "ExternalInput")
        if moe:
            router = cx.dram("router", [D, 8], F32, "ExternalInput")
        wg_d = cx.dram("w_gate", [nexp, D, dff], F32, "ExternalInput")
        wu_d = cx.dram("w_up", [nexp, D, dff], F32, "ExternalInput")
        wd_d = cx.dram("w_down", [nexp, dff, D], F32, "ExternalInput")
        outT = cx.dram("outT", [D, nt], F32, "ExternalOutput")
        outTb = cx.dram("outTb", [D, nt], BF16, "ExternalOutput")

        acc = cx.sb("acc", [128, KC, nt], F32)
        hb = cx.sb("hb", [128, KC, nt], BF16)
        wgu = cx.sb("wgu", [128, 4, KC, 512], BF16)
        wdn = cx.sb("wdn", [128, 2, FB, D], BF16)
        actb = cx.sb("actb", [128, FB, nt], BF16)
        cst = cx.sb("cst", [128, 128 + 128 + 8 * 128], F32)
        lnp = cx.sb("lnp_sb", [128, 4, KC], F32)
        tmp = {n: cx.sb(n, [128, 512], F32) for n in ("sq0", "sq1", "mean", "msq", "var", "sl0", "sl1", "t20", "t21")}
        onesm = cst[:, 0:128]
        ident = cst[:, 128:256]
        ps = [cx.ps("ps%d" % i, [128, 512]) for i in range(8)]
        pkey = [("ps", i) for i in range(6)] + ["psA", "psB"]
        if moe:
            rt = cx.sb("rt", [128, KC, 8], F32)
            gbs = [cx.sb("gb0", [128, nt], F32)] * 2
            lg = cx.sb("lg", [128, ntile, 8], F32)
            lg2 = cx.sb("lg2", [128, ntile, 8], F32)
            lgm = cx.sb("lgm", [128, ntile, 8], F32)
            m1 = cx.sb("m1", [128, ntile], F32)
            m2 = cx.sb("m2", [128, ntile], F32)
            GT = cx.sb("GT", [8, nt], F32)

        zk = lambda k, g: ("acc", k, g)
        hk = lambda k, g: ("hb", k, g)
        allz = lambda g: [zk(k, g) for k in range(KC)]
        allh = lambda g: [hk(k, g) for k in range(KC)]
        fm = lambda ap: ap.rearrange("(c p) t -> p c t", p=128)

        S.op("sp", "dma_start", out=cst[:], in_=consts, w=["onesm", "ident", "sel"], dma=True)
        S.op("sp", "dma_start", out=lnp[:], in_=lnp_d, w=["lnp"], dma=True)
        for g in range(G):
            gs = slice(g * 512, (g + 1) * 512)
            S.op("sp", "dma_start", out=hb[:, :, gs], in_=fm(mixT[:, gs]), w=allh(g), dma=True)
        for s in range(2):
            S.op("pool", "dma_start", out=wgu[:, s], in_=fm(w_o[:, s * 512:(s + 1) * 512]), w=[("wgu", s)], dma=True)
        for g in range(G):
            gs = slice(g * 512, (g + 1) * 512)
            S.op("sp", "dma_start", out=acc[:, :, gs], in_=fm(hT[:, gs]), w=allz(g), dma=True)
        if moe:
            S.op("sp", "dma_start", out=rt[:], in_=fm(router), w=["rt"], dma=True)

        blocks = [(e, b) for e in range(nexp) for b in range(nblk)]

        def load_block(i):
            e, b = blocks[i]
            st = i % 2
            f0 = b * FB * 128
            nf = min(FB * 128, dff - f0)
            S.op("pool", "dma_start", out=wgu[:, 2 * st, :, 0:nf], in_=fm(wg_d[e, :, f0:f0 + nf]), w=[("wgu", 2 * st)], dma=True)
            S.op("pool", "dma_start", out=wgu[:, 2 * st + 1, :, 0:nf], in_=fm(wu_d[e, :, f0:f0 + nf]), w=[("wgu", 2 * st + 1)], dma=True)
            S.op("pool", "dma_start", out=wdn[:, st, 0:nf // 128, :], in_=fm(wd_d[e, f0:f0 + nf, :]), w=[("wdn", st)], dma=True)

        pi = 0
        for g in range(G):
            gs = slice(g * 512, (g + 1) * 512)
            for ec in range(KC):
                pt, pk = ps[pi % 2], pkey[pi % 2]
                pi += 1
                s, off = divmod(ec * 128, 512)
                for k in range(KC):
                    S.op("pe", "matmul", pt[:], wgu[:, s, k, off:off + 128], hb[:, k, gs], start=(k == 0), stop=(k == KC - 1),
                         r=[("wgu", s), hk(k, g)], w=[pk])
                S.op("dve", "scalar_tensor_tensor", out=acc[:, ec, gs], in0=acc[:, ec, gs], scalar=float(ALPHA), in1=pt[:],
                     op0=ALU.mult, op1=ALU.add, r=[zk(ec, g), pk], w=[zk(ec, g)])
        load_block(0)
        emit_layernorm(cx, acc, zk, hb, hk, lnp, 0, 1, onesm, ps[6], ps[7], tmp, G)
        if len(blocks) > 1:
            load_block(1)
        if moe:
            lp = ps[6]
            for j in range(ntile):
                for k in range(KC):
                    S.op("pe", "matmul", lp[:, j * 8:(j + 1) * 8], acc[:, k, j * 128:(j + 1) * 128], rt[:, k, :],
                         start=(k == 0), stop=(k == KC - 1), r=[zk(k, j // 4), "rt"], w=["psA"])
            S.op("act", "copy", out=lg[:].rearrange("p a b -> p (a b)"), in_=lp[:, 0:ntile * 8], r=["psA"], w=["lg"])
            bc3 = lambda t: _bc(t[:].unsqueeze(2), [128, ntile, 8])
            S.op("dve", "tensor_reduce", out=m1[:], in_=lg[:], axis=AX.X, op=ALU.max, r=["lg"], w=["m1"])
            S.op("dve", "tensor_tensor", lgm[:], lg[:], bc3(m1), ALU.is_ge, r=["lg", "m1"], w=["lgm"])
            S.op("dve", "scalar_tensor_tensor", out=lg2[:], in0=lgm[:], scalar=-1e30, in1=lg[:], op0=ALU.mult, op1=ALU.add,
                 r=["lgm", "lg"], w=["lg2"])
            S.op("dve", "tensor_reduce", out=m2[:], in_=lg2[:], axis=AX.X, op=ALU.max, r=["lg2"], w=["m2"])
            S.op("dve", "tensor_tensor", lgm[:], lg[:], bc3(m2), ALU.is_ge, r=["lg", "m2"], w=["lgm"])
            S.op("dve", "tensor_tensor", lg2[:], lg[:], bc3(m1), ALU.subtract, r=["lg", "m1"], w=["lg2"])
            S.op("act", "activation", out=lg2[:], in_=lg2[:], func=AF.Exp, r=["lg2"], w=["lg2"])
            S.op("dve", "tensor_tensor", lg2[:], lg2[:], lgm[:], ALU.mult, r=["lg2", "lgm"], w=["lg2"])
            S.op("dve", "tensor_reduce", out=m1[:], in_=lg2[:], axis=AX.X, op=ALU.add, r=["lg2"], w=["m1"])
            S.op("dve", "reciprocal", m2[:], m1[:], r=["m1"], w=["m2"])
            S.op("dve", "tensor_tensor", lg[:], lg2[:], bc3(m2), ALU.mult, r=["lg2", "m2"], w=["lg"])
            for q in range(ntile // 4):
                pt = ps[7]
                for jj in range(4):
                    j = q * 4 + jj
                    S.op("pe", "transpose", pt[0:8, jj * 128:(jj + 1) * 128], lg[:, j, :], ident, r=["lg", "ident"], w=["psB"])
                S.op("act", "copy", out=GT[:, q * 512:(q + 1) * 512], in_=pt[0:8, :], r=["psB"], w=["GT"])
        for g in range(G):
            gs = slice(g * 512, (g + 1) * 512)
            S.op("pool", "tensor_scalar", acc[:, :, gs], acc[:, :, gs], float(ALPHA), None, ALU.mult, r=allz(g), w=allz(g))

        tcount = 0
        gb = gk = None
        for i, (e, b) in enumerate(blocks):
            st = i % 2
            f0 = b * FB * 128
            nf = min(FB * 128, dff - f0)
            nfb = nf // 128
            if moe and b == 0:
                gb = gbs[0]
                gk = ("gb", 0)
                for q in range(G):
                    pt, pk = ps[6 + (q % 2)], pkey[6 + (q % 2)]
                    S.op("pe", "matmul", pt[:], cst[0:8, 256 + e * 128:256 + (e + 1) * 128], GT[:, q * 512:(q + 1) * 512],
                         start=True, stop=True, r=["sel", "GT"], w=[pk])
                    S.op("act", "copy", out=gb[:, q * 512:(q + 1) * 512], in_=pt[:], r=[pk], w=[gk])
            for g in range(G):
                gs = slice(g * 512, (g + 1) * 512)
                for fc in range(nfb):
                    c2 = tcount % 2
                    tcount += 1
                    pg, pu, kg, ku = ps[c2], ps[2 + c2], pkey[c2], pkey[2 + c2]
                    sl, t2 = tmp["sl%d" % c2], tmp["t2%d" % c2]
                    ksl, kt2 = ("sl", c2), ("t2", c2)
                    for k in range(KC):
                        S.op("pe", "matmul", pg[:], wgu[:, 2 * st, k, fc * 128:(fc + 1) * 128], hb[:, k, gs],
                             start=(k == 0), stop=(k == KC - 1), r=[("wgu", 2 * st), hk(k, g)], w=[kg])
                    for k in range(KC):
                        S.op("pe", "matmul", pu[:], wgu[:, 2 * st + 1, k, fc * 128:(fc + 1) * 128], hb[:, k, gs],
                             start=(k == 0), stop=(k == KC - 1), r=[("wgu", 2 * st + 1), hk(k, g)], w=[ku])
                    S.op("act", "activation", out=sl[:], in_=pg[:], func=AF.Silu, r=[kg], w=[ksl])
                    if moe:
                        S.op("dve", "tensor_tensor", t2[:], sl[:], pu[:], ALU.mult, r=[ksl, ku], w=[kt2])
                        S.op("dve", "tensor_tensor", actb[:, fc, gs], t2[:], gb[:, gs], ALU.mult, r=[kt2, gk], w=[("act", fc, g)])
                    else:
                        S.op("dve", "tensor_tensor", actb[:, fc, gs], sl[:], pu[:], ALU.mult, r=[ksl, ku], w=[("act", fc, g)])
            for g in range(G):
                gs = slice(g * 512, (g + 1) * 512)
                for ec in range(KC):
                    pd, kd = ps[4 + (ec % 2)], pkey[4 + (ec % 2)]
                    for fc in range(nfb):
                        S.op("pe", "matmul", pd[:], wdn[:, st, fc, ec * 128:(ec + 1) * 128], actb[:, fc, gs],
                             start=(fc == 0), stop=(fc == nfb - 1), r=[("wdn", st), ("act", fc, g)], w=[kd])
                    S.op("dve", "tensor_tensor", acc[:, ec, gs], acc[:, ec, gs], pd[:], ALU.add, r=[zk(ec, g), kd], w=[zk(ec, g)])
            if i + 2 < len(blocks):
                load_block(i + 2)

        emit_layernorm(cx, acc, zk, hb, hk, lnp, 2, 3, onesm, ps[6], ps[7], tmp, G)
        for g in range(G):
            gs = slice(g * 512, (g + 1) * 512)
            S.op("sp", "dma_start", out=fm(outT[:, gs]), in_=acc[:, :, gs], r=allz(g), dma=True)
            S.op("sp", "dma_start", out=fm(outTb[:, gs]), in_=hb[:, :, gs], r=allh(g), dma=True)
        S.emit()
    return nc


def make_consts():
    c = np.zeros((128, 128 + 128 + 8 * 128), np.float32)
    c[:, 0:128] = 1.0 / D
    c[:, 128:256] = np.eye(128, dtype=np.float32)
    for e in range(8):
        c[e, 256 + e * 128:256 + (e + 1) * 128] = 1.0
    return c


def lnp_layout(ln_g, ln_b, layer):
    out = np.zeros((128, 4, KC), np.float32)
    out[:, 0] = ln_g[layer, 0].reshape(KC, 128).T
    out[:, 1] = ln_b[layer, 0].reshape(KC, 128).T
    out[:, 2] = ln_g[layer, 1].reshape(KC, 128).T
    out[:, 3] = ln_b[layer, 1].reshape(KC, 128).T
    return out


SEG = 512
NCH = SEG // 64
NBLK = 2 * NCH
NPROJ = 704
DECAY_SCALE = -float(np.exp(-0.5))


def rwkv_consts():
    c = np.zeros((128, 128 + 128 + 128 + 64 + 64), np.float32)
    c[:, 0:128] = np.eye(128, dtype=np.float32)
    c[0:64, 128:192] = 1.0
    c[64:128, 192:256] = 1.0
    j = np.arange(64)[:, None]
    i = np.arange(64)[None, :]
    c[0:64, 256:320] = (j < i)
    c[0:64, 320:384] = (j <= i)
    c[0:64, 384:448] = (i < j)
    c[0:64, 448:512] = np.eye(64, dtype=np.float32)
    return c


def build_H_rwkv(layer1, ntok_b=T, nb=B, stop_after=99):
    import contextlib
    nseg = ntok_b // SEG
    ntot = nb * ntok_b
    nc = bass.Bass("TRN2", target_bir_lowering=False)
    with contextlib.ExitStack() as es:
        cx = Ctx(nc, es)
        S = cx.S
        xT = cx.dram("xT", [D, ntot], F32, "ExternalInput")
        wproj = cx.dram("wproj", [D, NPROJ], F32, "ExternalInput")
        mu_d = cx.dram("mu", [128, KC, NPROJ], F32, "ExternalInput")
        w2_d = cx.dram("w2", [128, 3, 128], F32, "ExternalInput")
        par_d = cx.dram("par", [128, 8], F32, "ExternalInput")
        cst_d = cx.dram("cst", [128, 512], F32, "ExternalInput")
        if layer1:
            vf_d = cx.dram("vfirst", [128, ntot], F32, "ExternalInput")
        else:
            vf_o = cx.dram("vfirst_out", [128, ntot], F32, "ExternalOutput")
        mix_o = cx.dram("mix", [128, ntot], BF16, "ExternalOutput")

        f32t = lambda n, shp=(128, SEG): cx.sb(n, list(shp), F32)
        xb = [cx.sb("xb%d" % i, [128, KC, SEG + 1], BF16) for i in range(2)]
        Wc = cx.sb("Wc", [128, KC, 2, NPROJ], BF16)
        wst = cx.sb("wst", [128, NPROJ], F32)
        must = cx.sb("must", [128, NPROJ], F32)
        w2f = cx.sb("w2f", [128, 3, 128], F32)
        w2b = cx.sb("w2b", [128, 3, 128], BF16)
        par = cx.sb("par_sb", [128, 8], F32)
        cst = cx.sb("cst_sb", [128, 512], F32)
        ident = cst[:, 0:128]
        bones = cst[:, 128:256]
        m_su = cst[0:64, 256:320]
        m_iu = cst[0:64, 320:384]
        m_ak = cst[0:64, 256:384]
        m_sl = cst[0:64, 384:448]
        id64 = cst[0:64, 448:512]
        rT, kT, vT, aT, wT, gT, av, bv, P, rP, t1, t2, d0, d1 = [f32t(n) for n in
            ("rT", "kT", "vT", "aT", "wT", "gT", "av", "bv", "P", "rP", "t1", "t2", "d0", "d1")]
        lor = cx.sb("lor", [128, 3, SEG], BF16)
        AR = cx.sb("AR", [128, NCH, 2, 64], F32)
        KBf = cx.sb("KBf", [128, NCH, 2, 64], F32)
        KBh = cx.sb("KBh", [128, NCH, 2, 64], F32)
        ARblk = cx.sb("ARblk", [128, NCH, 2, 128], F32)
        Bblk = cx.sb("Bblk", [128, NCH, 2, 64], F32)
        Vtm = cx.sb("Vtm", [64, NCH, 128], F32)
        KBtm = cx.sb("KBtm", [64, NCH, 2, 128], F32)
        Ytm = cx.sb("Ytm", [64, NCH, 128], F32)
        AK = cx.sb("AK", [64, NBLK, 128], F32)
        AB = cx.sb("AB", [64, NBLK, 128], F32)
        Mp = [cx.sb("Mp%d" % i, [64, NBLK, 64], F32) for i in range(2)]
        Np = [cx.sb("Np%d" % i, [64, NBLK, 64], F32) for i in range(2)]
        Q = cx.sb("Q", [64, NBLK, 64], F32)
        H = cx.sb("Hst", [128, 128], F32)
        Xs = cx.sb("Xs", [64, 128], F32)
        Us = cx.sb("Us", [64, 128], F32)
        yT = f32t("yT")
        ob = cx.sb("ob", [128, SEG], BF16)
        ps = [cx.ps("ps%d" % i, [128, 512]) for i in range(8)]
        pk = [("ps", i) for i in range(8)]

        S.op("sp", "dma_start", out=cst[:], in_=cst_d, w=["cst"], dma=True)
        S.op("sp", "dma_start", out=par[:], in_=par_d, w=["par"], dma=True)
        S.op("sp", "dma_start", out=w2f[:], in_=w2_d, w=["w2f"], dma=True)
        S.op("dve", "tensor_copy", out=w2b[:], in_=w2f[:], r=["w2f"], w=["w2b"])
        S.op("dve", "memset", ARblk[:], 0.0, w=["ARblk"])
        S.op("dve", "memset", Bblk[:], 0.0, w=["Bblk"])
        for k in range(KC):
            S.op("sp", "dma_start", out=wst[:], in_=wproj[k * 128:(k + 1) * 128, :], w=["wst"], dma=True)
            S.op("sp", "dma_start", out=must[:], in_=mu_d[:, k, :], w=["must"], dma=True)
            S.op("dve", "tensor_tensor", must[:], wst[:], must[:], ALU.mult, r=["wst", "must"], w=["must"])
            S.op("dve", "tensor_copy", out=Wc[:, k, 1, :], in_=must[:], r=["must"], w=["Wc"])
            S.op("dve", "tensor_tensor", Wc[:, k, 0, :], wst[:], must[:], ALU.subtract, r=["wst", "must"], w=["Wc"])

        def load_x(b, s, slot):
            t0 = b * ntok_b + s * SEG
            xk = ("xb", slot)
            if s == 0:
                S.op("dve", "memset", xb[slot][:, :, 0:1], 0.0, w=[xk])
                S.op("pool", "dma_start", out=xb[slot][:, :, 1:SEG + 1], in_=xT[:, t0:t0 + SEG].rearrange("(c p) t -> p c t", p=128),
                     w=[xk], dma=True)
            else:
                S.op("pool", "dma_start", out=xb[slot][:, :, 0:SEG + 1], in_=xT[:, t0 - 1:t0 + SEG].rearrange("(c p) t -> p c t", p=128),
                     w=[xk], dma=True)

        segs = [(b, s) for b in range(nb) for s in range(nseg)]
        load_x(0, 0, 0)
        pcount = [0]

        def proj(slot, c0, ncol, key):
            i = pcount[0] % 2
            pcount[0] += 1
            n = 0
            for k in range(KC):
                for sft in range(2):
                    S.op("pe", "matmul", ps[i][0:ncol, :], Wc[:, k, sft, c0:c0 + ncol], xb[slot][:, k, (1 - sft):(1 - sft) + SEG],
                         start=(n == 0), stop=(n == 2 * KC - 1), r=["Wc", ("xb", slot)], w=[pk[i]])
                    n += 1
            return ps[i], pk[i]

        col = lambda j: par[:, j:j + 1]
        for si, (b, s) in enumerate(segs):
            slot = si % 2
            t0 = b * ntok_b + s * SEG
            if si + 1 < len(segs):
                load_x(segs[si + 1][0], segs[si + 1][1], 1 - slot)
            if s == 0:
                S.op("dve", "memset", H[:], 0.0, w=["H"])
            p, k_ = proj(slot, 0, 128, None)
            S.op("act", "copy", out=rT[:], in_=p[:], r=[k_], w=["rT"])
            p, k_ = proj(slot, 128, 128, None)
            S.op("act", "copy", out=kT[:], in_=p[:], r=[k_], w=["kT"])
            p, k_ = proj(slot, 256, 128, None)
            S.op("act", "copy", out=vT[:], in_=p[:], r=[k_], w=["vT"])
            p, k_ = proj(slot, 384, 128, None)
            S.op("act", "activation", out=lor[0:64, 0, :], in_=p[0:64, :], func=AF.Tanh, r=[k_], w=["lor0a"])
            S.op("act", "copy", out=lor[64:128, 0, :], in_=p[64:128, :], r=[k_], w=["lor0b"])
            p, k_ = proj(slot, 512, 128, None)
            S.op("act", "activation", out=lor[:, 1, :], in_=p[:], func=AF.Sigmoid, r=[k_], w=["lor1"])
            p, k_ = proj(slot, 640, 64, None)
            S.op("act", "activation", out=lor[0:32, 2, :], in_=p[0:32, :], func=AF.Sigmoid, r=[k_], w=["lor2a"])
            S.op("act", "copy", out=lor[32:64, 2, :], in_=p[32:64, :], r=[k_], w=["lor2b"])
            i = pcount[0] % 2; pcount[0] += 1
            S.op("pe", "matmul", ps[i][:], w2b[0:64, 0, :], lor[0:64, 0, :], start=True, stop=True, r=["w2b", "lor0a"], w=[pk[i]])
            S.op("act", "activation", out=wT[:], in_=ps[i][:], func=AF.Sigmoid, bias=col(0), scale=1.0, r=[pk[i], "par"], w=["wT"])
            S.op("act", "activation", out=wT[:], in_=wT[:], func=AF.Exp, scale=DECAY_SCALE, r=["wT"], w=["wT"])
            i = pcount[0] % 2; pcount[0] += 1
            S.op("pe", "matmul", ps[i][:], w2b[64:128, 0, :], lor[64:128, 0, :], start=True, stop=True, r=["w2b", "lor0b"], w=[pk[i]])
            S.op("act", "activation", out=aT[:], in_=ps[i][:], func=AF.Sigmoid, bias=col(1), scale=1.0, r=[pk[i], "par"], w=["aT"])
            i = pcount[0] % 2; pcount[0] += 1
            S.op("pe", "matmul", ps[i][:], w2b[:, 1, :], lor[:, 1, :], start=True, stop=False, r=["w2b", "lor1"], w=[pk[i]])
            S.op("pe", "matmul", ps[i][:], w2b[0:32, 2, :], lor[0:32, 2, :], start=False, stop=True, r=["w2b", "lor2a"], w=[pk[i]])
            S.op("act", "copy", out=gT[:], in_=ps[i][:], r=[pk[i]], w=["gT"])
            if layer1:
                i = pcount[0] % 2; pcount[0] += 1
                S.op("pe", "matmul", ps[i][:], w2b[32:64, 2, :], lor[32:64, 2, :], start=True, stop=True, r=["w2b", "lor2b"], w=[pk[i]])
                S.op("act", "activation", out=t1[:], in_=ps[i][:], func=AF.Sigmoid, bias=col(2), scale=1.0, r=[pk[i], "par"], w=["t1"])
                S.op("sp", "dma_start", out=t2[:], in_=vf_d[:, t0:t0 + SEG], w=["t2"], dma=True)
                S.op("dve", "tensor_tensor", t2[:], t2[:], vT[:], ALU.subtract, r=["t2", "vT"], w=["t2"])
                S.op("dve", "tensor_tensor", t2[:], t2[:], t1[:], ALU.mult, r=["t2", "t1"], w=["t2"])
                S.op("dve", "tensor_tensor", vT[:], vT[:], t2[:], ALU.add, r=["vT", "t2"], w=["vT"])
            else:
                S.op("sp", "dma_start", out=vf_o[:, t0:t0 + SEG], in_=vT[:], r=["vT"], dma=True)
            if stop_after == 0:
                break
            S.op("dve", "tensor_scalar", av[:], kT[:], col(3), None, ALU.mult, r=["kT", "par"], w=["av"])
            S.op("act", "activation", out=t1[:], in_=av[:], func=AF.Square, r=["av"], w=["t1"])
            i = pcount[0] % 2; pcount[0] += 1
            S.op("pe", "matmul", ps[i][:], bones, t1[:], start=True, stop=True, r=["cst", "t1"], w=[pk[i]])
            S.op("dve", "tensor_scalar", t2[:], ps[i][:], 1e-24, None, ALU.max, r=[pk[i]], w=["t2"])
            S.op("act", "activation", out=t2[:], in_=t2[:], func=AF.Sqrt, r=["t2"], w=["t2"])
            S.op("dve", "reciprocal", t2[:], t2[:], r=["t2"], w=["t2"])
            S.op("dve", "tensor_tensor", av[:], av[:], t2[:], ALU.mult, r=["av", "t2"], w=["av"])
            S.op("dve", "tensor_tensor", bv[:], av[:], aT[:], ALU.mult, r=["av", "aT"], w=["bv"])
            S.op("dve", "tensor_scalar", t1[:], aT[:], -1.0, col(4), ALU.add, ALU.mult, r=["aT", "par"], w=["t1"])
            S.op("dve", "scalar_tensor_tensor", out=kT[:], in0=t1[:], scalar=1.0, in1=kT[:], op0=ALU.add, op1=ALU.mult,
                 r=["t1", "kT"], w=["kT"])
            if stop_after == 1:
                break
            w3 = wT[:].rearrange("p (c j) -> p c j", j=64)
            S.op("pool", "tensor_copy", out=d0[:], in_=wT[:], r=["wT"], w=["d0"])
            S.op("pool", "memset", d0[:].rearrange("p (c j) -> p c j", j=64)[:, :, 0:1], 0.0, w=["d0"])
            S.op("pool", "memset", d1[:], 0.0, w=["d1"])
            S.op("pool", "tensor_copy", out=d1[:].rearrange("p (c j) -> p c j", j=64)[:, :, 0:1], in_=w3[:, :, 0:1], r=["wT"], w=["d1"])
            S.op("dve", "tensor_tensor_scan", P[:], d0[:], d1[:], 0.0, ALU.mult, ALU.add, r=["d0", "d1"], w=["P"])
            S.op("dve", "reciprocal", rP[:], P[:], r=["P"], w=["rP"])
            S.op("dve", "reciprocal", t1[:], wT[:], r=["wT"], w=["t1"])
            S.op("dve", "tensor_tensor", t1[:], t1[:], P[:], ALU.mult, r=["t1", "P"], w=["t1"])
            c3 = lambda t: t[:].rearrange("p (c j) -> p c j", j=64)
            S.op("dve", "scalar_tensor_tensor", out=AR[:, :, 0, :], in0=c3(av), scalar=-1.0, in1=c3(t1), op0=ALU.mult, op1=ALU.mult,
                 r=["av", "t1"], w=["AR"])
            S.op("pool", "tensor_tensor", AR[:, :, 1, :], c3(rT), c3(P), ALU.mult, r=["rT", "P"], w=["AR"])
            S.op("pool", "tensor_tensor", KBf[:, :, 0, :], c3(kT), c3(rP), ALU.mult, r=["kT", "rP"], w=["KBf"])
            S.op("dve", "tensor_tensor", KBf[:, :, 1, :], c3(bv), c3(rP), ALU.mult, r=["bv", "rP"], w=["KBf"])
            pend = c3(P)[:, :, 63:64]
            for q in range(2):
                S.op("dve" if q else "pool", "tensor_tensor", KBh[:, :, q, :], KBf[:, :, q, :], _bc(pend, [128, NCH, 64]), ALU.mult,
                     r=["KBf", "P"], w=["KBh"])
            if stop_after == 2:
                break
            for hh in range(2):
                hb = hh * 64
                S.op("pool", "tensor_copy", out=ARblk[hb:hb + 64, :, hh, :], in_=AR[hb:hb + 64, :, :, :].rearrange("p c a b -> p c (a b)"),
                     r=["AR"], w=["ARblk"])
                S.op("pool", "tensor_copy", out=Bblk[hb:hb + 64, :, hh, :], in_=KBf[hb:hb + 64, :, 1, :], r=["KBf"], w=["Bblk"])
            for half in range(2):
                for cc in range(4):
                    c = half * 4 + cc
                    S.op("pe", "transpose", ps[2][0:64, cc * 128:(cc + 1) * 128], vT[:, c * 64:(c + 1) * 64], ident,
                         r=["vT", "cst"], w=[pk[2]])
                S.op("act", "copy", out=Vtm[:, half * 4:(half + 1) * 4, :].rearrange("p c f -> p (c f)"), in_=ps[2][0:64, :], r=[pk[2]], w=["Vtm"])
            for q2 in range(4):
                for cc in range(2):
                    c = q2 * 2 + cc
                    for q in range(2):
                        S.op("pe", "transpose", ps[2][0:64, (cc * 2 + q) * 128:(cc * 2 + q + 1) * 128], KBh[:, c, q, :], ident,
                             r=["KBh", "cst"], w=[pk[2]])
                S.op("act", "copy", out=KBtm[:, q2 * 2:(q2 + 1) * 2, :, :].rearrange("p c q f -> p (c q f)"), in_=ps[2][0:64, :],
                     r=[pk[2]], w=["KBtm"])
            if stop_after == 3:
                break
            for c2 in range(NCH // 2):
                for which, dst in ((0, AK), (1, AB)):
                    for cc in range(2):
                        c = c2 * 2 + cc
                        S.op("pe", "matmul", ps[2][0:64, cc * 256:(cc + 1) * 256], KBf[:, c, which, :],
                             ARblk[:, c, :, :].rearrange("p a b -> p (a b)"), start=True, stop=True, r=["KBf", "ARblk"], w=[pk[2]])
                    S.op("dve", "tensor_tensor", dst[:, c2 * 4:(c2 + 1) * 4, :], ps[2][0:64, :].rearrange("p (a b) -> p a b", b=128),
                         _bc(m_ak.unsqueeze(1), [64, 4, 128]), ALU.mult, r=[pk[2], "cst"], w=["AK" if which == 0 else "AB"])
            for c4 in range(NCH // 4):
                for cc in range(4):
                    c = c4 * 4 + cc
                    S.op("pe", "matmul", ps[2][0:64, cc * 128:(cc + 1) * 128], AR[:, c, 0, :],
                         Bblk[:, c, :, :].rearrange("p a b -> p (a b)"), start=True, stop=True, r=["Bblk", "AR"], w=[pk[2]])
                S.op("dve", "tensor_tensor", Np[0][:, c4 * 8:(c4 + 1) * 8, :], ps[2][0:64, :].rearrange("p (a b) -> p a b", b=64),
                     _bc(m_sl.unsqueeze(1), [64, 8, 64]), ALU.mult, r=[pk[2], "cst"], w=[("Np0", c4)])
            if stop_after == 4:
                break
            S.op("pool", "tensor_copy", out=Mp[0][:], in_=AB[:, :, 0:64], r=["AB"], w=[("Mp0", 0), ("Mp0", 1)])
            S.op("dve", "tensor_tensor", Q[:], AB[:, :, 0:64], _bc(id64.unsqueeze(1), [64, NBLK, 64]), ALU.add, r=["AB", "cst"], w=[("Q", 0), ("Q", 1)])
            ibank = {0: (3, 4, 5), 1: (6, 7, 2)}
            for lvl in range(5):
                cur, nxt = lvl % 2, (lvl + 1) % 2
                for g8 in range(NBLK // 8):
                    bM, bN, bQ = ibank[g8]
                    for bi in range(8):
                        blk = g8 * 8 + bi
                        S.op("pe", "matmul", ps[bM][0:64, bi * 64:(bi + 1) * 64], Np[cur][:, blk, :], Mp[cur][:, blk, :],
                             start=True, stop=True, r=[("Np%d" % cur, g8), ("Mp%d" % cur, g8)], w=[pk[bM]])
                    for bi in range(8):
                        blk = g8 * 8 + bi
                        S.op("pe", "matmul", ps[bN][0:64, bi * 64:(bi + 1) * 64], Mp[cur][:, blk, :], Np[cur][:, blk, :],
                             start=True, stop=True, r=[("Np%d" % cur, g8), ("Mp%d" % cur, g8)], w=[pk[bN]])
                for g8 in range(NBLK // 8):
                    bM, bN, bQ = ibank[g8]
                    gsl = slice(g8 * 8, (g8 + 1) * 8)
                    S.op("act", "copy", out=Mp[nxt][:, gsl, :].rearrange("p a b -> p (a b)"), in_=ps[bM][0:64, :], r=[pk[bM]], w=[("Mp%d" % nxt, g8)])
                    S.op("dve", "tensor_copy", out=Np[nxt][:, gsl, :].rearrange("p a b -> p (a b)"), in_=ps[bN][0:64, :], r=[pk[bN]], w=[("Np%d" % nxt, g8)])
                for g8 in range(NBLK // 8):
                    bM, bN, bQ = ibank[g8]
                    for bi in range(8):
                        blk = g8 * 8 + bi
                        S.op("pe", "matmul", ps[bQ][0:64, bi * 64:(bi + 1) * 64], Np[nxt][:, blk, :], Q[:, blk, :],
                             start=True, stop=True, r=[("Np%d" % nxt, g8), ("Q", g8)], w=[pk[bQ]])
                for g8 in range(NBLK // 8):
                    bM, bN, bQ = ibank[g8]
                    gsl = slice(g8 * 8, (g8 + 1) * 8)
                    S.op("dve", "tensor_tensor", Q[:, gsl, :].rearrange("p a b -> p (a b)"), Q[:, gsl, :].rearrange("p a b -> p (a b)"),
                         ps[bQ][0:64, :], ALU.add, r=[("Q", g8), pk[bQ]], w=[("Q", g8)])
            if stop_after == 5:
                break
            for c in range(NCH):
                S.op("pe", "matmul", ps[3][0:64, 0:128], AR[:, c, 0, :], H[:], start=True, stop=False, r=["AR", "H"], w=[pk[3]])
                for hh in range(2):
                    hb = hh * 64
                    blk = 2 * c + hh
                    S.op("pe", "matmul", ps[3][0:64, hb:hb + 64], AK[:, blk, 0:64], Vtm[:, c, hb:hb + 64], start=False, stop=(hh == 1),
                         r=["AK", "Vtm"], w=[pk[3]])
                S.op("act", "copy", out=Xs[:], in_=ps[3][0:64, 0:128], r=[pk[3]], w=["Xs"])
                for hh in range(2):
                    hb = hh * 64
                    blk = 2 * c + hh
                    S.op("pe", "matmul", ps[4][0:64, hb:hb + 64], Q[:, blk, :], Xs[:, hb:hb + 64], start=True, stop=True,
                         r=[("Q", blk // 8), "Xs"], w=[pk[4]])
                S.op("act", "copy", out=Us[:], in_=ps[4][0:64, 0:128], r=[pk[4]], w=["Us"])
                S.op("pe", "matmul", ps[5][0:64, 0:128], AR[:, c, 1, :], H[:], start=True, stop=False, r=["AR", "H"], w=[pk[5]])
                for hh in range(2):
                    hb = hh * 64
                    blk = 2 * c + hh
                    S.op("pe", "matmul", ps[5][0:64, hb:hb + 64], AB[:, blk, 64:128], Us[:, hb:hb + 64], start=False, stop=False,
                         r=["AB", "Us"], w=[pk[5]])
                    S.op("pe", "matmul", ps[5][0:64, hb:hb + 64], AK[:, blk, 64:128], Vtm[:, c, hb:hb + 64], start=False, stop=(hh == 1),
                         r=["AK", "Vtm"], w=[pk[5]])
                S.op("act", "copy", out=Ytm[:, c, :], in_=ps[5][0:64, 0:128], r=[pk[5]], w=["Ytm"])
                S.op("pe", "matmul", ps[6][:, 0:128], KBtm[:, c, 0, :], Vtm[:, c, :], start=True, stop=False, r=["KBtm", "Vtm"], w=[pk[6]])
                S.op("pe", "matmul", ps[6][:, 0:128], KBtm[:, c, 1, :], Us[:], start=False, stop=True, r=["KBtm", "Us"], w=[pk[6]])
                for hh in range(2):
                    hb = hh * 64
                    S.op("dve", "scalar_tensor_tensor", out=H[hb:hb + 64, hb:hb + 64], in0=H[hb:hb + 64, hb:hb + 64],
                         scalar=c3(P)[hb:hb + 64, c, 63:64], in1=ps[6][hb:hb + 64, hb:hb + 64],
                         op0=ALU.mult, op1=ALU.add, r=["H", "P", pk[6]], w=["H"])
            if stop_after == 6:
                break
            for c in range(NCH):
                S.op("pe", "transpose", ps[7][:, c * 64:(c + 1) * 64], Ytm[:, c, :], id64, r=["Ytm", "cst"], w=[pk[7]])
            S.op("act", "copy", out=yT[:], in_=ps[7][:], r=[pk[7]], w=["yT"])
            i = pcount[0] % 2; pcount[0] += 1
            S.op("pe", "matmul", ps[i][:], bones, yT[:], start=True, stop=True, r=["cst", "yT"], w=[pk[i]])
            S.op("dve", "scalar_tensor_tensor", out=yT[:], in0=ps[i][:], scalar=-1.0 / 64, in1=yT[:], op0=ALU.mult, op1=ALU.add,
                 r=[pk[i], "yT"], w=["yT"])
            S.op("act", "activation", out=t1[:], in_=yT[:], func=AF.Square, r=["yT"], w=["t1"])
            i = pcount[0] % 2; pcount[0] += 1
            S.op("pe", "matmul", ps[i][:], bones, t1[:], start=True, stop=True, r=["cst", "t1"], w=[pk[i]])
            S.op("act", "activation", out=t2[:], in_=ps[i][:], func=AF.Sqrt, bias=GN_EPS, scale=1.0 / 64, r=[pk[i]], w=["t2"])
            S.op("dve", "reciprocal", t2[:], t2[:], r=["t2"], w=["t2"])
            S.op("dve", "tensor_tensor", yT[:], yT[:], t2[:], ALU.mult, r=["yT", "t2"], w=["yT"])
            S.op("act", "activation", out=yT[:], in_=yT[:], func=AF.Identity, scale=col(6), bias=col(7), r=["yT", "par"], w=["yT"])
            S.op("dve", "scalar_tensor_tensor", out=t1[:], in0=rT[:], scalar=col(5), in1=kT[:], op0=ALU.mult, op1=ALU.mult,
                 r=["rT", "kT", "par"], w=["t1"])
            i = pcount[0] % 2; pcount[0] += 1
            S.op("pe", "matmul", ps[i][:], bones, t1[:], start=True, stop=True, r=["cst", "t1"], w=[pk[i]])
            S.op("dve", "tensor_tensor", t2[:], ps[i][:], vT[:], ALU.mult, r=[pk[i], "vT"], w=["t2"])
            S.op("dve", "tensor_tensor", yT[:], yT[:], t2[:], ALU.add, r=["yT", "t2"], w=["yT"])
            S.op("dve", "tensor_tensor", ob[:], yT[:], gT[:], ALU.mult, r=["yT", "gT"], w=["ob"])
            S.op("sp", "dma_start", out=mix_o[:, t0:t0 + SEG], in_=ob[:], r=["ob"], dma=True)
        S.emit()
    return nc


def rwkv_host_inputs(inp, li, core):
    cs = slice(core * 128, (core + 1) * 128)
    f = np.float32
    wproj = np.zeros((D, NPROJ), f)
    mu = np.zeros((D, NPROJ), f)
    M = inp["rwkv_mu"][li]
    wproj[:, 0:128] = inp["rwkv_w_rkv"][li, 0][:, cs]; mu[:, 0:128] = M[0][:, None]
    wproj[:, 128:256] = inp["rwkv_w_rkv"][li, 1][:, cs]; mu[:, 128:256] = M[1][:, None]
    wproj[:, 256:384] = inp["rwkv_w_rkv"][li, 2][:, cs]; mu[:, 256:384] = M[2][:, None]
    wproj[:, 384:448] = inp["rwkv_decay_w1"][li]; mu[:, 384:448] = M[3][:, None]
    wproj[:, 448:512] = inp["rwkv_iclr_a1"][li]; mu[:, 448:512] = M[4][:, None]
    wproj[:, 512:672] = inp["rwkv_gate_g1"][li]; mu[:, 512:672] = M[5][:, None]
    if li > 0:
        wproj[:, 672:704] = inp["rwkv_vres_v1"][li - 1]
    mu[:, 672:704] = M[2][:, None]
    w2 = np.zeros((128, 3, 128), f)
    w2[0:64, 0] = inp["rwkv_decay_w2"][li][:, cs]
    w2[64:128, 0] = inp["rwkv_iclr_a2"][li][:, cs]
    w2[:, 1] = inp["rwkv_gate_g2"][li][0:128, cs]
    w2[0:32, 2] = inp["rwkv_gate_g2"][li][128:160, cs]
    if li > 0:
        w2[32:64, 2] = inp["rwkv_vres_v2"][li - 1][:, cs]
    par = np.zeros((128, 8), f)
    par[:, 0] = inp["rwkv_decay_w0"][li][cs]
    par[:, 1] = inp["rwkv_iclr_a0"][li][cs]
    if li > 0:
        par[:, 2] = inp["rwkv_vres_v0"][li - 1][cs]
    par[:, 3] = inp["rwkv_k_k"][li][cs]
    par[:, 4] = inp["rwkv_k_a"][li][cs]
    par[:, 5] = inp["rwkv_r_k"][li].reshape(-1)[cs]
    par[:, 6] = inp["rwkv_gn_g"][li][cs]
    par[:, 7] = inp["rwkv_gn_b"][li][cs]
    mu_l = np.ascontiguousarray(mu.reshape(KC, 128, NPROJ).transpose(1, 0, 2))
    return {"wproj": wproj, "mu": mu_l, "w2": w2, "par": par, "cst": rwkv_consts()}


NEG = -1.0e30
BLK = 256


def moba_consts():
    c = {}
    ident = np.eye(128, dtype=np.float32)
    c["identf"] = ident
    key = np.arange(128)[:, None]
    q = np.arange(128)[None, :]
    tri = np.where(key <= q, 0.0, NEG).astype(np.float32)
    oh = np.zeros((32, 32, 128), np.float32)
    for n in range(32):
        oh[n, n, :] = 1.0e30
    cb = np.zeros((128, 128 + 128 + 32 * 128), np.float32)
    cb[:, 0:128] = ident
    cb[:, 128:256] = tri
    cb[0:32, 256:] = oh.reshape(32, 32 * 128)
    return {"cstf": ident, "cstb": cb.astype(ml_dtypes.bfloat16)}


def build_H_moba(separate_kv, ntok_b=T, nb=B, dbg=()):
    import contextlib
    nseg = ntok_b // 512
    ntile = ntok_b // 128
    nblkb = ntok_b // BLK
    ntot = nb * ntok_b
    nc = bass.Bass("TRN2", target_bir_lowering=False)
    with contextlib.ExitStack() as es:
        cx = Ctx(nc, es)
        S = cx.S
        xT = cx.dram("xT", [D, ntot], F32, "ExternalInput")
        if separate_kv:
            xkvT = cx.dram("xkvT", [D, ntot], F32, "ExternalInput")
        else:
            xkvT = xT
        wqkv_d = cx.dram("wqkv", [D, 3, 128], F32, "ExternalInput")
        cstf_d = cx.dram("cstf", [128, 128], F32, "ExternalInput")
        cstb_d = cx.dram("cstb", [128, 256 + 32 * 128], BF16, "ExternalInput")
        mix_o = cx.dram("mix", [128, ntot], BF16, "ExternalOutput")

        wqkv = cx.sb("wqkv_sb", [128, KC, 3, 128], BF16)
        identf = cx.sb("identf", [128, 128], F32)
        cstb = cx.sb("cstb_sb", [128, 256 + 32 * 128], BF16)
        identb = cstb[:, 0:128]
        trib = cstb[:, 128:256]
        xq = [cx.sb("xq%d" % i, [128, KC, 512], BF16) for i in range(2)]
        xkv = [cx.sb("xkv%d" % i, [128, KC, 512], BF16) for i in range(2)] if separate_kv else xq
        KT = cx.sb("KT", [128, ntok_b], BF16)
        Va = cx.sb("Va", [128, ntile, 2, 65], BF16)
        qf = cx.sb("qf", [128, ntok_b], F32)
        qz = [cx.sb("qz%d" % i, [128, ntok_b], BF16) for i in range(2)]
        km = cx.sb("km", [128, nblkb], F32)
        kmblk = cx.sb("kmblk", [128, 2, nblkb], F32)
        gsb = cx.sb("gsb", [128, 2, 32], F32)
        top8 = cx.sb("top8", [128, 2, 8], F32)
        mm1 = cx.sb("mm1", [128, 2, 32], F32)
        mT = [cx.sb("mT%d" % i, [32, 2, 128], BF16) for i in range(2)]
        PT = [cx.sb("PT%d" % i, [128, 1024], BF16) for i in range(3)]
        osb = cx.sb("osb", [128, 128], F32)
        rs = cx.sb("rs", [128, 1], F32)
        ob = cx.sb("ob", [128, 512], BF16)
        psS = [cx.ps("psS%d" % i, [128, 1024]) for i in range(2)]
        pSkeys = [("psS", i) for i in range(2)]
        ps = [psS[0][:, 0:512], psS[0][:, 512:1024], psS[1][:, 0:512], cx.ps("ps3", [128, 512]), cx.ps("ps4", [128, 512]), None,
              cx.ps("ps6", [128, 512]), cx.ps("ps7", [128, 512])]
        pk = [("psS", 0), ("psS", 0), ("psS", 1), ("ps", 3), ("ps", 4), ("ps", 5), ("ps", 6), ("ps", 7)]

        S.op("sp", "dma_start", out=identf[:], in_=cstf_d, w=["identf"], dma=True)
        S.op("sp", "dma_start", out=cstb[:], in_=cstb_d, w=["cstb"], dma=True)
        S.op("pool", "dma_start", out=wqkv[:], in_=wqkv_d.rearrange("(c p) a e -> p c a e", p=128), w=["wqkv"], dma=True)
        S.op("dve", "memset", Va[:, :, :, 64:65], 1.0, w=["Va"])
        S.op("dve", "memset", qz[0][64:128, :], 0.0, w=["qz0"])
        S.op("dve", "memset", qz[1][0:64, :], 0.0, w=["qz1"])
        S.op("dve", "memset", kmblk[:], 0.0, w=["kmblk"])

        fm = lambda ap: ap.rearrange("(c p) t -> p c t", p=128)

        def load_seg(b, s, slot):
            t0 = b * ntok_b + s * 512
            S.op("pool", "dma_start", out=xq[slot][:], in_=fm(xT[:, t0:t0 + 512]), w=[("xq", slot)], dma=True)
            if separate_kv:
                S.op("pool", "dma_start", out=xkv[slot][:], in_=fm(xkvT[:, t0:t0 + 512]), w=[("xkv", slot)], dma=True)

        segs = [(b, s) for b in range(nb) for s in range(nseg)]
        kvk = (lambda slot: ("xkv", slot)) if separate_kv else (lambda slot: ("xq", slot))
        load_seg(0, 0, 0)
        scount = 0
        pcount = 0
        for b in range(nb):
            S.op("dve", "memset", gsb[:], NEG, w=["gsb"])
            S.op("dve", "memset", km[:], 0.0, w=["km"])
            for s in range(nseg if "cut0" not in dbg else 0):
                si = b * nseg + s
                slot = si % 2
                if si + 1 < len(segs):
                    load_seg(segs[si + 1][0], segs[si + 1][1], 1 - slot)
                ss = slice(s * 512, (s + 1) * 512)
                for k in range(KC):
                    S.op("pe", "matmul", ps[0][:], wqkv[:, k, 1, :], xkv[slot][:, k, :], start=(k == 0), stop=(k == KC - 1),
                         r=["wqkv", kvk(slot)], w=[pk[0]])
                for hb2 in range(2):
                    S.op("act", "activation", out=KT[:, s * 512 + hb2 * BLK:s * 512 + (hb2 + 1) * BLK], in_=ps[0][:, hb2 * BLK:(hb2 + 1) * BLK],
                         func=AF.Copy, accum_out=km[:, 2 * s + hb2:2 * s + hb2 + 1], r=[pk[0]], w=["KT", "km"])
                if "cut1" in dbg:
                    continue
                for tt in range(4):
                    for k in range(KC):
                        S.op("pe", "matmul", ps[1][:, tt * 128:(tt + 1) * 128], xkv[slot][:, k, tt * 128:(tt + 1) * 128], wqkv[:, k, 2, :],
                             start=(k == 0), stop=(k == KC - 1), r=["wqkv", kvk(slot)], w=[pk[1]])
                S.op("act", "copy", out=Va[:, 4 * s:4 * s + 4, :, 0:64], in_=ps[1][:].rearrange("p (t h d) -> p t h d", h=2, d=64),
                     r=[pk[1]], w=["Va"])
                if "cut2" in dbg:
                    continue
                for k in range(KC):
                    S.op("pe", "matmul", ps[2][:], wqkv[:, k, 0, :], xq[slot][:, k, :], start=(k == 0), stop=(k == KC - 1),
                         r=["wqkv", ("xq", slot)], w=[pk[2]])
                S.op("act", "copy", out=qf[:, ss], in_=ps[2][:], r=[pk[2]], w=["qf"])
                S.op("dve", "tensor_copy", out=qz[0][0:64, ss], in_=qf[0:64, ss], r=["qf"], w=["qz0"])
                S.op("dve", "tensor_copy", out=qz[1][64:128, ss], in_=qf[64:128, ss], r=["qf"], w=["qz1"])
            for hh in range(2):
                hb = hh * 64
                S.op("dve", "tensor_scalar", kmblk[hb:hb + 64, hh, :], km[hb:hb + 64, :], 1.0 / BLK, None, ALU.mult, r=["km"], w=["kmblk"])
            def rec_gate1(qt):
                own = qt // 2
                qs = slice(qt * 128, (qt + 1) * 128)
                S.op("pe", "matmul", ps[3][:, 0:2 * nblkb], qf[:, qs], kmblk[:].rearrange("p a b -> p (a b)"), start=True, stop=True,
                     r=["qf", "kmblk"], w=[pk[3]])
                S.op("dve", "tensor_copy", out=gsb[:, :, 0:own], in_=ps[3][:, 0:2 * nblkb].rearrange("p (a b) -> p a b", b=nblkb)[:, :, 0:own],
                     r=[pk[3]], w=["gsb"])
                for hh in range(2):
                    S.op("dve", "max", out=top8[:, hh, :], in_=gsb[:, hh, :], r=["gsb"], w=["top8"])
                    S.op("dve", "tensor_scalar", mm1[:, hh, :], gsb[:, hh, :], top8[:, hh, 2:3], -1.0, ALU.is_ge, ALU.add,
                         r=["gsb", "top8"], w=["mm1"])

            def rec_gate2(qt):
                mt = mT[qt % 2]
                for hh in range(2):
                    S.op("pe", "transpose", ps[3][0:32, 256 + hh * 128:256 + (hh + 1) * 128], mm1[:, hh, :], identf[:], r=["mm1", "identf"], w=[pk[3]])
                S.op("act", "copy", out=mt[:].rearrange("p a b -> p (a b)"), in_=ps[3][0:32, 256:512], r=[pk[3]], w=[("mT", qt % 2)])

            items = []
            for qt in range(ntile if "proj_only" not in dbg else 0):
                for hh in range(2):
                    kts = list(range(0, qt + 1))
                    ngrp = (len(kts) + 7) // 8
                    pi_ = pcount % 2
                    pcount += 1
                    for gi in range(ngrp):
                        items.append(dict(qt=qt, hh=hh, gi=gi, ngrp=ngrp, grp=kts[gi * 8:(gi + 1) * 8], po=ps[6 + pi_], pok=pk[6 + pi_]))

            def rec_S(it):
                qt, hh, grp = it["qt"], it["hh"], it["grp"]
                own = qt // 2
                qs = slice(qt * 128, (qt + 1) * 128)
                pS, pSk = psS[it["sb"] % 2], pSkeys[it["sb"] % 2]
                pt, ptk = PT[it["sb"] % 3], ("PT", it["sb"] % 3)
                mt, mk = mT[qt % 2], ("mT", qt % 2)
                for j, kt in enumerate(grp):
                    nomask = (kt != qt and kt // 2 == own) or (kt != qt and "nogate" in dbg)
                    S.op("pe", "matmul", pS[:, j * 128:(j + 1) * 128], KT[:, kt * 128:(kt + 1) * 128], qz[hh][:, qs], start=True, stop=nomask,
                         r=["KT", "qz%d" % hh], w=[pSk])
                    if kt == qt:
                        S.op("pe", "matmul", pS[:, j * 128:(j + 1) * 128], identb, trib, start=False, stop=True, r=["cstb"], w=[pSk])
                    elif not nomask:
                        n = kt // 2
                        S.op("pe", "matmul", pS[:, j * 128:(j + 1) * 128], cstb[0:32, 256 + n * 128:256 + (n + 1) * 128], mt[:, hh, :],
                             start=False, stop=True, r=["cstb", mk], w=[pSk])
                w_ = len(grp) * 128
                S.op("act", "activation", out=pt[:, 0:w_], in_=pS[:, 0:w_], func=AF.Exp, scale=0.125, r=[pSk], w=[ptk])

            def rec_PV(it):
                qt, hh, gi, ngrp, grp, po, pok = it["qt"], it["hh"], it["gi"], it["ngrp"], it["grp"], it["po"], it["pok"]
                hb = hh * 64
                pt, ptk = PT[it["sb"] % 3], ("PT", it["sb"] % 3)
                for j, kt in enumerate(grp):
                    first = (gi == 0 and j == 0)
                    last = (gi == ngrp - 1 and j == len(grp) - 1)
                    S.op("pe", "matmul", po[:, 0:65], pt[:, j * 128:(j + 1) * 128], Va[:, kt, hh, :], start=first, stop=last,
                         r=[ptk, "Va"], w=[pok])
                if gi == ngrp - 1:
                    S.op("dve", "reciprocal", rs[:], po[:, 64:65], r=[pok], w=["rs"])
                    S.op("dve", "tensor_scalar", osb[:, hb:hb + 64], po[:, 0:64], rs[:, 0:1], None, ALU.mult, r=[pok, "rs"], w=["osb"])
                    if hh == 1:
                        S.op("pe", "transpose", ps[4][:, 0:128], osb[:], identf[:], r=["osb", "identf"], w=[pk[4]])
                        S.op("act", "copy", out=ob[:, (qt % 4) * 128:(qt % 4 + 1) * 128], in_=ps[4][:, 0:128], r=[pk[4]], w=["ob"])
                        if qt % 4 == 3:
                            t0 = b * ntok_b + (qt - 3) * 128
                            S.op("sp", "dma_start", out=mix_o[:, t0:t0 + 512], in_=ob[:], r=["ob"], dma=True)

            pending = []
            for it in items:
                it["sb"] = scount
                scount += 1
                rec_S(it)
                pending.append(it)
                if len(pending) > 1:
                    rec_PV(pending.pop(0))
                nq = it["qt"] + 1
                if "nogate" not in dbg and it["gi"] == 0 and nq < ntile and nq // 2 > 0:
                    if it["hh"] == 0:
                        rec_gate1(nq)
                    else:
                        rec_gate2(nq)
            for it in pending:
                rec_PV(it)
        S.emit()
    return nc


def moba_host_inputs(inp, j, core):
    cs = slice(core * 128, (core + 1) * 128)
    w = np.stack([inp["moba_w_q"][j][:, cs], inp["moba_w_k"][:, cs], inp["moba_w_v"][:, cs]], axis=1)
    m = {"wqkv": np.ascontiguousarray(w.astype(np.float32))}
    m.update(moba_consts())
    return m


_PROGS = {}
_DUMP = None
_NL = 4


def _prog(name, fn):
    if name not in _PROGS:
        _PROGS[name] = fn()
    return _PROGS[name]


def _run(nc, maps):
    res = run_bass_kernel_spmd(nc, maps, core_ids=list(range(NCORES)))
    return res.results


def _t_phase(inp, layer, mixT, hT):
    moe = (layer % 2 == 1)
    e = layer // 2
    nc = _prog("T_moe" if moe else "T_dense", lambda: build_T(moe))
    if layer < 2:
        w_o = inp["rwkv_w_out"][layer]
    else:
        w_o = inp["moba_w_o"][layer - 2]
    base = {"w_o": np.ascontiguousarray(w_o, dtype=np.float32), "lnp": lnp_layout(inp["ln_g"], inp["ln_b"], layer),
            "consts": make_consts()}
    if moe:
        base["router"] = np.ascontiguousarray(inp["moe_router"][e])
        base["w_gate"] = np.ascontiguousarray(inp["moe_w_gate"][e])
        base["w_up"] = np.ascontiguousarray(inp["moe_w_up"][e])
        base["w_down"] = np.ascontiguousarray(inp["moe_w_down"][e])
    else:
        base["w_gate"] = np.ascontiguousarray(inp["ffn_w_gate"][e][None])
        base["w_up"] = np.ascontiguousarray(inp["ffn_w_up"][e][None])
        base["w_down"] = np.ascontiguousarray(inp["ffn_w_down"][e][None])
    maps = []
    for c in range(NCORES):
        m = dict(base)
        m["mixT"] = np.ascontiguousarray(mixT[:, c * NT:(c + 1) * NT])
        m["hT"] = np.ascontiguousarray(hT[:, c * NT:(c + 1) * NT])
        maps.append(m)
    res = _run(nc, maps)
    return np.concatenate([r["outT"] for r in res], axis=1)


def kernel(**inp):
    inp = {k: np.asarray(v) for k, v in inp.items()}
    x = inp["x"].astype(np.float32, copy=False)
    hT = np.ascontiguousarray(x.reshape(NTOK, D).T)
    vfirst = None
    h_kv = None
    for layer in range(_NL):
        if layer < 2:
            nc = _prog("H_rwkv%d" % layer, lambda: build_H_rwkv(layer == 1))
            maps = []
            for c in range(NCORES):
                m = rwkv_host_inputs(inp, layer, c)
                m["xT"] = hT
                if layer == 1:
                    m["vfirst"] = np.ascontiguousarray(vfirst[c * 128:(c + 1) * 128])
                maps.append(m)
            res = _run(nc, maps)
            if layer == 0:
                vfirst = np.concatenate([r["vfirst_out"] for r in res], axis=0)
        else:
            j = layer - 2
            nc = _prog("H_moba%d" % j, lambda: build_H_moba(j == 1))
            maps = []
            for c in range(NCORES):
                m = moba_host_inputs(inp, j, c)
                m["xT"] = hT
                if j == 1:
                    m["xkvT"] = h_kv
                maps.append(m)
            res = _run(nc, maps)
        mixT = np.concatenate([r["mix"] for r in res], axis=0)
        if _DUMP is not None:
            _DUMP["mix%d" % layer] = mixT
        hT = _t_phase(inp, layer, mixT, hT)
        if _DUMP is not None:
            _DUMP["h%d" % layer] = hT
        if layer == 1:
            h_kv = hT
    return np.ascontiguousarray(hT.T).reshape(B, T, D).astype(np.float32)
```

```python
import numpy as np
import ml_dtypes
import concourse.bass as bass
import concourse.mybir as mybir
from concourse.bass_utils import run_bass_kernel_spmd

F32 = mybir.dt.float32
BF16 = mybir.dt.bfloat16
ALU = mybir.AluOpType
AF = mybir.ActivationFunctionType
AX = mybir.AxisListType

NCORES = 8
D = 1024
KC = 8
B = 2
T = 8192
NTOK = B * T
NT = NTOK // NCORES
ALPHA = (2.0 * 4) ** 0.25
LN_EPS = 1e-5
GN_EPS = 64e-5


class Sched:
    ENG = ("pe", "act", "dve", "pool", "sp")
    NDMA = 8

    def __init__(self, nc):
        self.nc = nc
        self.ops = {e: [] for e in self.ENG}
        self.lastw = {}
        self.readers = {}
        self.alias = {}

    def add(self, eng, fn, reads=(), writes=(), dma=False):
        idx = len(self.ops[eng])
        deps = {}

        def dep(key, kind):
            if key is None:
                return
            if deps.get(key) != "raw":
                deps[key] = kind

        for r in reads:
            dep(self.lastw.get(r), "raw")
        for w in writes:
            dep(self.lastw.get(w), "waw")
            for k in self.readers.get(w, ()):
                dep(k, "war")
        waits = set()
        for (pe, pi), kind in deps.items():
            pdma = self.ops[pe][pi]["dma"]
            if pe == eng and not pdma and not dma:
                if eng == "pe":
                    continue
            waits.add((pe, pi))
        self.ops[eng].append(dict(fn=fn, waits=waits, dma=dma, signal=dma))
        me = (eng, idx)
        for r in reads:
            lst = self.readers.setdefault(r, [])
            if not dma:
                lst[:] = [k for k in lst if not (k[0] == eng and not self.ops[k[0]][k[1]]["dma"])]
            lst.append(me)
        for w in writes:
            self.lastw[w] = me
            self.readers[w] = []
        return me

    def op(self, eng, method, *args, r=(), w=(), dma=False, **kw):
        al = self.alias
        if al:
            r = [k if isinstance(k, tuple) else al.get(k, k) for k in r]
            w = [k if isinstance(k, tuple) else al.get(k, k) for k in w]
        return self.add(eng, lambda h: getattr(h, method)(*args, **kw), reads=r, writes=w, dma=dma)

    def emit(self):
        nc = self.nc
        ops = self.ops
        for e in self.ENG:
            for op in ops[e]:
                for (pe, pi) in op["waits"]:
                    ops[pe][pi]["signal"] = True
        import contextlib
        with contextlib.ExitStack() as st:
            csem = {e: st.enter_context(nc.semaphore("c_" + e)) for e in self.ENG}
            dsem = {e: [st.enter_context(nc.semaphore("d_%s%d" % (e, i))) for i in range(self.NDMA)]
                    for e in ("act", "pool", "sp")}
            final_dma = []
            for e in self.ENG:
                cnt = 0
                dcnt = [0] * self.NDMA
                k = 0
                for op in ops[e]:
                    if op["dma"]:
                        s = k % self.NDMA
                        k += 1
                        op["sem"] = dsem[e][s]
                        op["prev"] = dcnt[s]
                        dcnt[s] += 16
                        op["val"] = dcnt[s]
                    elif op["signal"]:
                        cnt += 1
                        op["sem"] = csem[e]
                        op["val"] = cnt
                if e in dsem:
                    for s in range(self.NDMA):
                        if dcnt[s]:
                            final_dma.append((dsem[e][s], dcnt[s]))
            block = st.enter_context(nc.Block())

            def run(e, h):
                waited = {}

                def wait(sem, val):
                    key = id(sem)
                    if waited.get(key, 0) >= val:
                        return
                    waited[key] = val
                    h.wait_ge(sem, val)

                for op in ops[e]:
                    for (pe, pi) in sorted(op["waits"]):
                        p = ops[pe][pi]
                        wait(p["sem"], p["val"])
                    if op["dma"] and op["prev"]:
                        wait(op["sem"], op["prev"])
                    ins = op["fn"](h)
                    if op["dma"]:
                        ins.then_inc(op["sem"], 16)
                    elif op["signal"]:
                        ins.then_inc(op["sem"], 1)
                if e == "sp":
                    for sem, val in final_dma:
                        wait(sem, val)

            @block.tensor
            def _(h):
                run("pe", h)

            @block.scalar
            def _(h):
                run("act", h)

            @block.vector
            def _(h):
                run("dve", h)

            @block.gpsimd
            def _(h):
                run("pool", h)

            @block.sync
            def _(h):
                run("sp", h)


def _bc(ap, shape):
    return ap.broadcast_to(shape)


class Ctx:
    def __init__(self, nc, es):
        self.nc = nc
        self.es = es
        self.S = Sched(nc)

    def sb(self, name, shape, dt):
        return self.es.enter_context(self.nc.sbuf_tensor(name, shape, dt))

    def ps(self, name, shape, dt=F32):
        return self.es.enter_context(self.nc.psum_tensor(name, shape, dt))

    def dram(self, name, shape, dt, kind):
        return self.nc.dram_tensor(name, list(shape), dt, kind=kind).ap()


def emit_layernorm(cx, zt, res_z, hb, res_hb, lnp, gi, bi, onesm, psA, psB, tmp, ngroups, scale_after=None):
    S = cx.S
    mean, msq, var = tmp["mean"], tmp["msq"], tmp["var"]
    for g in range(ngroups):
        gs = slice(g * 512, (g + 1) * 512)
        zkeys = [res_z(k, g) for k in range(KC)]
        for k in range(KC):
            sq = tmp["sq%d" % (k % 2)]
            S.op("act", "activation", out=sq[:], in_=zt[:, k, gs], func=AF.Square, r=[zkeys[k]], w=[("sq", k % 2)])
            S.op("pe", "matmul", psA[:], onesm, zt[:, k, gs], start=(k == 0), stop=(k == KC - 1),
                 r=[zkeys[k], "onesm"], w=["psA"])
            S.op("pe", "matmul", psB[:], onesm, sq[:], start=(k == 0), stop=(k == KC - 1),
                 r=[("sq", k % 2), "onesm"], w=["psB"])
        S.op("act", "copy", out=mean[:], in_=psA[:], r=["psA"], w=["mean"])
        S.op("dve", "tensor_tensor", msq[:], mean[:], mean[:], ALU.mult, r=["mean"], w=["msq"])
        S.op("dve", "tensor_tensor", var[:], psB[:], msq[:], ALU.subtract, r=["psB", "msq"], w=["var"])
        S.op("act", "activation", out=var[:], in_=var[:], func=AF.Sqrt, bias=LN_EPS, scale=1.0, r=["var"], w=["var"])
        S.op("dve", "reciprocal", msq[:], var[:], r=["var"], w=["msq"])
        z3 = zt[:, :, gs]
        S.op("dve", "tensor_tensor", z3, z3, _bc(mean[:].unsqueeze(1), [128, KC, 512]), ALU.subtract,
             r=zkeys + ["mean"], w=zkeys)
        S.op("dve", "tensor_tensor", z3, z3, _bc(msq[:].unsqueeze(1), [128, KC, 512]), ALU.mult,
             r=zkeys + ["msq"], w=zkeys)
        for k in range(KC):
            S.op("act", "activation", out=zt[:, k, gs], in_=zt[:, k, gs], func=AF.Identity,
                 scale=lnp[:, gi, k:k + 1], bias=lnp[:, bi, k:k + 1], r=[zkeys[k], "lnp"], w=[zkeys[k]])
        if hb is not None:
            hkeys = [res_hb(k, g) for k in range(KC)]
            S.op("pool", "tensor_copy", out=hb[:, :, gs], in_=z3, r=zkeys, w=hkeys)
        if scale_after is not None:
            S.op("pool", "tensor_scalar", z3, z3, float(scale_after), None, ALU.mult, r=zkeys, w=zkeys)


def build_T(moe, nt=NT, dff=None, nexp=None):
    import contextlib
    G = nt // 512
    FB = 4
    if dff is None:
        dff = 3584 if moe else 2816
    if nexp is None:
        nexp = 8 if moe else 1
    nfc = dff // 128
    nblk = (nfc + FB - 1) // FB
    ntile = nt // 128
    nc = bass.Bass("TRN2", target_bir_lowering=False)
    with contextlib.ExitStack() as es:
        cx = Ctx(nc, es)
        S = cx.S
        mixT = cx.dram("mixT", [D, nt], BF16, "ExternalInput")
        hT = cx.dram("hT", [D, nt], F32, "ExternalInput")
        w_o = cx.dram("w_o", [D, D], F32, "ExternalInput")
        lnp_d = cx.dram("lnp", [128, 4, KC], F32, "ExternalInput")
        consts = cx.dram("consts", [128, 128 + 128 + 8 * 128], F32, "ExternalInput")
        if moe:
            router = cx.dram("router", [D, 8], F32, "ExternalInput")
        wg_d = cx.dram("w_gate", [nexp, D, dff], F32, "ExternalInput")
        wu_d = cx.dram("w_up", [nexp, D, dff], F32, "ExternalInput")
        wd_d = cx.dram("w_down", [nexp, dff, D], F32, "ExternalInput")
        outT = cx.dram("outT", [D, nt], F32, "ExternalOutput")
        outTb = cx.dram("outTb", [D, nt], BF16, "ExternalOutput")

        acc = cx.sb("acc", [128, KC, nt], F32)
        hb = cx.sb("hb", [128, KC, nt], BF16)
        wgu = cx.sb("wgu", [128, 4, KC, 512], BF16)
        wdn = cx.sb("wdn", [128, 2, FB, D], BF16)
        actb = cx.sb("actb", [128, FB, nt], BF16)
        cst = cx.sb("cst", [128, 128 + 128 + 8 * 128], F32)
        lnp = cx.sb("lnp_sb", [128, 4, KC], F32)
        tmp = {n: cx.sb(n, [128, 512], F32) for n in ("sq0", "sq1", "mean", "msq", "var", "sl0", "sl1", "t20", "t21")}
        onesm = cst[:, 0:128]
        ident = cst[:, 128:256]
        ps = [cx.ps("ps%d" % i, [128, 512]) for i in range(8)]
        pkey = [("ps", i) for i in range(6)] + ["psA", "psB"]
        if moe:
            rt = cx.sb("rt", [128, KC, 8], F32)
            gbs = [cx.sb("gb0", [128, nt], F32)] * 2
            lg = cx.sb("lg", [128, ntile, 8], F32)
            lg2 = cx.sb("lg2", [128, ntile, 8], F32)
            lgm = cx.sb("lgm", [128, ntile, 8], F32)
            m1 = cx.sb("m1", [128, ntile], F32)
            m2 = cx.sb("m2", [128, ntile], F32)
            GT = cx.sb("GT", [8, nt], F32)

        zk = lambda k, g: ("acc", k, g)
        hk = lambda k, g: ("hb", k, g)
        allz = lambda g: [zk(k, g) for k in range(KC)]
        allh = lambda g: [hk(k, g) for k in range(KC)]
        fm = lambda ap: ap.rearrange("(c p) t -> p c t", p=128)

        S.op("sp", "dma_start", out=cst[:], in_=consts, w=["onesm", "ident", "sel"], dma=True)
        S.op("sp", "dma_start", out=lnp[:], in_=lnp_d, w=["lnp"], dma=True)
        for g in range(G):
            gs = slice(g * 512, (g + 1) * 512)
            S.op("sp", "dma_start", out=hb[:, :, gs], in_=fm(mixT[:, gs]), w=allh(g), dma=True)
        for s in range(2):
            S.op("pool", "dma_start", out=wgu[:, s], in_=fm(w_o[:, s * 512:(s + 1) * 512]), w=[("wgu", s)], dma=True)
        for g in range(G):
            gs = slice(g * 512, (g + 1) * 512)
            S.op("sp", "dma_start", out=acc[:, :, gs], in_=fm(hT[:, gs]), w=allz(g), dma=True)
        if moe:
            S.op("sp", "dma_start", out=rt[:], in_=fm(router), w=["rt"], dma=True)

        blocks = [(e, b) for e in range(nexp) for b in range(nblk)]

        def load_block(i):
            e, b = blocks[i]
            st = i % 2
            f0 = b * FB * 128
            nf = min(FB * 128, dff - f0)
            S.op("pool", "dma_start", out=wgu[:, 2 * st, :, 0:nf], in_=fm(wg_d[e, :, f0:f0 + nf]), w=[("wgu", 2 * st)], dma=True)
            S.op("pool", "dma_start", out=wgu[:, 2 * st + 1, :, 0:nf], in_=fm(wu_d[e, :, f0:f0 + nf]), w=[("wgu", 2 * st + 1)], dma=True)
            S.op("pool", "dma_start", out=wdn[:, st, 0:nf // 128, :], in_=fm(wd_d[e, f0:f0 + nf, :]), w=[("wdn", st)], dma=True)

        pi = 0
        for g in range(G):
            gs = slice(g * 512, (g + 1) * 512)
            for ec in range(KC):
                pt, pk = ps[pi % 2], pkey[pi % 2]
                pi += 1
                s, off = divmod(ec * 128, 512)
                for k in range(KC):
                    S.op("pe", "matmul", pt[:], wgu[:, s, k, off:off + 128], hb[:, k, gs], start=(k == 0), stop=(k == KC - 1),
                         r=[("wgu", s), hk(k, g)], w=[pk])
                S.op("dve", "scalar_tensor_tensor", out=acc[:, ec, gs], in0=acc[:, ec, gs], scalar=float(ALPHA), in1=pt[:],
                     op0=ALU.mult, op1=ALU.add, r=[zk(ec, g), pk], w=[zk(ec, g)])
        load_block(0)
        emit_layernorm(cx, acc, zk, hb, hk, lnp, 0, 1, onesm, ps[6], ps[7], tmp, G)
        if len(blocks) > 1:
            load_block(1)
        if moe:
            lp = ps[6]
            for j in range(ntile):
                for k in range(KC):
                    S.op("pe", "matmul", lp[:, j * 8:(j + 1) * 8], acc[:, k, j * 128:(j + 1) * 128], rt[:, k, :],
                         start=(k == 0), stop=(k == KC - 1), r=[zk(k, j // 4), "rt"], w=["psA"])
            S.op("act", "copy", out=lg[:].rearrange("p a b -> p (a b)"), in_=lp[:, 0:ntile * 8], r=["psA"], w=["lg"])
            bc3 = lambda t: _bc(t[:].unsqueeze(2), [128, ntile, 8])
            S.op("dve", "tensor_reduce", out=m1[:], in_=lg[:], axis=AX.X, op=ALU.max, r=["lg"], w=["m1"])
            S.op("dve", "tensor_tensor", lgm[:], lg[:], bc3(m1), ALU.is_ge, r=["lg", "m1"], w=["lgm"])
            S.op("dve", "scalar_tensor_tensor", out=lg2[:], in0=lgm[:], scalar=-1e30, in1=lg[:], op0=ALU.mult, op1=ALU.add,
                 r=["lgm", "lg"], w=["lg2"])
            S.op("dve", "tensor_reduce", out=m2[:], in_=lg2[:], axis=AX.X, op=ALU.max, r=["lg2"], w=["m2"])
            S.op("dve", "tensor_tensor", lgm[:], lg[:], bc3(m2), ALU.is_ge, r=["lg", "m2"], w=["lgm"])
            S.op("dve", "tensor_tensor", lg2[:], lg[:], bc3(m1), ALU.subtract, r=["lg", "m1"], w=["lg2"])
            S.op("act", "activation", out=lg2[:], in_=lg2[:], func=AF.Exp, r=["lg2"], w=["lg2"])
            S.op("dve", "tensor_tensor", lg2[:], lg2[:], lgm[:], ALU.mult, r=["lg2", "lgm"], w=["lg2"])
            S.op("dve", "tensor_reduce", out=m1[:], in_=lg2[:], axis=AX.X, op=ALU.add, r=["lg2"], w=["m1"])
            S.op("dve", "reciprocal", m2[:], m1[:], r=["m1"], w=["m2"])
            S.op("dve", "tensor_tensor", lg[:], lg2[:], bc3(m2), ALU.mult, r=["lg2", "m2"], w=["lg"])
            for q in range(ntile // 4):
                pt = ps[7]
                for jj in range(4):
                    j = q * 4 + jj
                    S.op("pe", "transpose", pt[0:8, jj * 128:(jj + 1) * 128], lg[:, j, :], ident, r=["lg", "ident"], w=["psB"])
                S.op("act", "copy", out=GT[:, q * 512:(q + 1) * 512], in_=pt[0:8, :], r=["psB"], w=["GT"])
        for g in range(G):
            gs = slice(g * 512, (g + 1) * 512)
            S.op("pool", "tensor_scalar", acc[:, :, gs], acc[:, :, gs], float(ALPHA), None, ALU.mult, r=allz(g), w=allz(g))

        tcount = 0
        gb = gk = None
        for i, (e, b) in enumerate(blocks):
            st = i % 2
            f0 = b * FB * 128
            nf = min(FB * 128, dff - f0)
            nfb = nf // 128
            if moe and b == 0:
                gb = gbs[0]
                gk = ("gb", 0)
                for q in range(G):
                    pt, pk = ps[6 + (q % 2)], pkey[6 + (q % 2)]
                    S.op("pe", "matmul", pt[:], cst[0:8, 256 + e * 128:256 + (e + 1) * 128], GT[:, q * 512:(q + 1) * 512],
                         start=True, stop=True, r=["sel", "GT"], w=[pk])
                    S.op("act", "copy", out=gb[:, q * 512:(q + 1) * 512], in_=pt[:], r=[pk], w=[gk])
            for g in range(G):
                gs = slice(g * 512, (g + 1) * 512)
                for fc in range(nfb):
                    c2 = tcount % 2
                    tcount += 1
                    pg, pu, kg, ku = ps[c2], ps[2 + c2], pkey[c2], pkey[2 + c2]
                    sl, t2 = tmp["sl%d" % c2], tmp["t2%d" % c2]
                    ksl, kt2 = ("sl", c2), ("t2", c2)
                    for k in range(KC):
                        S.op("pe", "matmul", pg[:], wgu[:, 2 * st, k, fc * 128:(fc + 1) * 128], hb[:, k, gs],
                             start=(k == 0), stop=(k == KC - 1), r=[("wgu", 2 * st), hk(k, g)], w=[kg])
                    for k in range(KC):
                        S.op("pe", "matmul", pu[:], wgu[:, 2 * st + 1, k, fc * 128:(fc + 1) * 128], hb[:, k, gs],
                             start=(k == 0), stop=(k == KC - 1), r=[("wgu", 2 * st + 1), hk(k, g)], w=[ku])
                    S.op("act", "activation", out=sl[:], in_=pg[:], func=AF.Silu, r=[kg], w=[ksl])
                    if moe:
                        S.op("dve", "tensor_tensor", t2[:], sl[:], pu[:], ALU.mult, r=[ksl, ku], w=[kt2])
                        S.op("dve", "tensor_tensor", actb[:, fc, gs], t2[:], gb[:, gs], ALU.mult, r=[kt2, gk], w=[("act", fc, g)])
                    else:
                        S.op("dve", "tensor_tensor", actb[:, fc, gs], sl[:], pu[:], ALU.mult, r=[ksl, ku], w=[("act", fc, g)])
            for g in range(G):
                gs = slice(g * 512, (g + 1) * 512)
                for ec in range(KC):
                    pd, kd = ps[4 + (ec % 2)], pkey[4 + (ec % 2)]
                    for fc in range(nfb):
                        S.op("pe", "matmul", pd[:], wdn[:, st, fc, ec * 128:(ec + 1) * 128], actb[:, fc, gs],
                             start=(fc == 0), stop=(fc == nfb - 1), r=[("wdn", st), ("act", fc, g)], w=[kd])
                    S.op("dve", "tensor_tensor", acc[:, ec, gs], acc[:, ec, gs], pd[:], ALU.add, r=[zk(ec, g), kd], w=[zk(ec, g)])
            if i + 2 < len(blocks):
                load_block(i + 2)

        emit_layernorm(cx, acc, zk, hb, hk, lnp, 2, 3, onesm, ps[6], ps[7], tmp, G)
        for g in range(G):
            gs = slice(g * 512, (g + 1) * 512)
            S.op("sp", "dma_start", out=fm(outT[:, gs]), in_=acc[:, :, gs], r=allz(g), dma=True)
            S.op("sp", "dma_start", out=fm(outTb[:, gs]), in_=hb[:, :, gs], r=allh(g), dma=True)
        S.emit()
    return nc


def make_consts():
    c = np.zeros((128, 128 + 128 + 8 * 128), np.float32)
    c[:, 0:128] = 1.0 / D
    c[:, 128:256] = np.eye(128, dtype=np.float32)
    for e in range(8):
        c[e, 256 + e * 128:256 + (e + 1) * 128] = 1.0
    return c


def lnp_layout(ln_g, ln_b, layer):
    out = np.zeros((128, 4, KC), np.float32)
    out[:, 0] = ln_g[layer, 0].reshape(KC, 128).T
    out[:, 1] = ln_b[layer, 0].reshape(KC, 128).T
    out[:, 2] = ln_g[layer, 1].reshape(KC, 128).T
    out[:, 3] = ln_b[layer, 1].reshape(KC, 128).T
    return out


SEG = 512
NCH = SEG // 64
NBLK = 2 * NCH
NPROJ = 704
DECAY_SCALE = -float(np.exp(-0.5))


def rwkv_consts():
    c = np.zeros((128, 128 + 128 + 128 + 64 + 64), np.float32)
    c[:, 0:128] = np.eye(128, dtype=np.float32)
    c[0:64, 128:192] = 1.0
    c[64:128, 192:256] = 1.0
    j = np.arange(64)[:, None]
    i = np.arange(64)[None, :]
    c[0:64, 256:320] = (j < i)
    c[0:64, 320:384] = (j <= i)
    c[0:64, 384:448] = (i < j)
    c[0:64, 448:512] = np.eye(64, dtype=np.float32)
    return c


def build_H_rwkv(layer1, ntok_b=T, nb=B, stop_after=99):
    import contextlib
    nseg = ntok_b // SEG
    ntot = nb * ntok_b
    nc = bass.Bass("TRN2", target_bir_lowering=False)
    with contextlib.ExitStack() as es:
        cx = Ctx(nc, es)
        S = cx.S
        xT = cx.dram("xT", [D, ntot], F32, "ExternalInput")
        wproj = cx.dram("wproj", [D, NPROJ], F32, "ExternalInput")
        mu_d = cx.dram("mu", [128, KC, NPROJ], F32, "ExternalInput")
        w2_d = cx.dram("w2", [128, 3, 128], F32, "ExternalInput")
        par_d = cx.dram("par", [128, 8], F32, "ExternalInput")
        cst_d = cx.dram("cst", [128, 512], F32, "ExternalInput")
        if layer1:
            vf_d = cx.dram("vfirst", [128, ntot], F32, "ExternalInput")
        else:
            vf_o = cx.dram("vfirst_out", [128, ntot], F32, "ExternalOutput")
        mix_o = cx.dram("mix", [128, ntot], BF16, "ExternalOutput")

        f32t = lambda n, shp=(128, SEG): cx.sb(n, list(shp), F32)
        xb = [cx.sb("xb%d" % i, [128, KC, SEG + 1], BF16) for i in range(2)]
        Wc = cx.sb("Wc", [128, KC, 2, NPROJ], BF16)
        wst = cx.sb("wst", [128, NPROJ], F32)
        must = cx.sb("must", [128, NPROJ], F32)
        w2f = cx.sb("w2f", [128, 3, 128], F32)
        w2b = cx.sb("w2b", [128, 3, 128], BF16)
        par = cx.sb("par_sb", [128, 8], F32)
        cst = cx.sb("cst_sb", [128, 512], F32)
        ident = cst[:, 0:128]
        bones = cst[:, 128:256]
        m_su = cst[0:64, 256:320]
        m_iu = cst[0:64, 320:384]
        m_ak = cst[0:64, 256:384]
        m_sl = cst[0:64, 384:448]
        id64 = cst[0:64, 448:512]
        rT, kT, vT, aT, wT, gT, av, bv, P, rP, t1, t2, d0, d1 = [f32t(n) for n in
            ("rT", "kT", "vT", "aT", "wT", "gT", "av", "bv", "P", "rP", "t1", "t2", "d0", "d1")]
        lor = cx.sb("lor", [128, 3, SEG], BF16)
        AR = cx.sb("AR", [128, NCH, 2, 64], F32)
        KBf = cx.sb("KBf", [128, NCH, 2, 64], F32)
        KBh = cx.sb("KBh", [128, NCH, 2, 64], F32)
        ARblk = cx.sb("ARblk", [128, NCH, 2, 128], F32)
        Bblk = cx.sb("Bblk", [128, NCH, 2, 64], F32)
        Vtm = cx.sb("Vtm", [64, NCH, 128], F32)
        KBtm = cx.sb("KBtm", [64, NCH, 2, 128], F32)
        Ytm = cx.sb("Ytm", [64, NCH, 128], F32)
        AK = cx.sb("AK", [64, NBLK, 128], F32)
        AB = cx.sb("AB", [64, NBLK, 128], F32)
        Mp = [cx.sb("Mp%d" % i, [64, NBLK, 64], F32) for i in range(2)]
        Np = [cx.sb("Np%d" % i, [64, NBLK, 64], F32) for i in range(2)]
        Q = cx.sb("Q", [64, NBLK, 64], F32)
        H = cx.sb("Hst", [128, 128], F32)
        Xs = cx.sb("Xs", [64, 128], F32)
        Us = cx.sb("Us", [64, 128], F32)
        yT = f32t("yT")
        ob = cx.sb("ob", [128, SEG], BF16)
        ps = [cx.ps("ps%d" % i, [128, 512]) for i in range(8)]
        pk = [("ps", i) for i in range(8)]

        S.op("sp", "dma_start", out=cst[:], in_=cst_d, w=["cst"], dma=True)
        S.op("sp", "dma_start", out=par[:], in_=par_d, w=["par"], dma=True)
        S.op("sp", "dma_start", out=w2f[:], in_=w2_d, w=["w2f"], dma=True)
        S.op("dve", "tensor_copy", out=w2b[:], in_=w2f[:], r=["w2f"], w=["w2b"])
        S.op("dve", "memset", ARblk[:], 0.0, w=["ARblk"])
        S.op("dve", "memset", Bblk[:], 0.0, w=["Bblk"])
        for k in range(KC):
            S.op("sp", "dma_start", out=wst[:], in_=wproj[k * 128:(k + 1) * 128, :], w=["wst"], dma=True)
            S.op("sp", "dma_start", out=must[:], in_=mu_d[:, k, :], w=["must"], dma=True)
            S.op("dve", "tensor_tensor", must[:], wst[:], must[:], ALU.mult, r=["wst", "must"], w=["must"])
            S.op("dve", "tensor_copy", out=Wc[:, k, 1, :], in_=must[:], r=["must"], w=["Wc"])
            S.op("dve", "tensor_tensor", Wc[:, k, 0, :], wst[:], must[:], ALU.subtract, r=["wst", "must"], w=["Wc"])

        def load_x(b, s, slot):
            t0 = b * ntok_b + s * SEG
            xk = ("xb", slot)
            if s == 0:
                S.op("dve", "memset", xb[slot][:, :, 0:1], 0.0, w=[xk])
                S.op("pool", "dma_start", out=xb[slot][:, :, 1:SEG + 1], in_=xT[:, t0:t0 + SEG].rearrange("(c p) t -> p c t", p=128),
                     w=[xk], dma=True)
            else:
                S.op("pool", "dma_start", out=xb[slot][:, :, 0:SEG + 1], in_=xT[:, t0 - 1:t0 + SEG].rearrange("(c p) t -> p c t", p=128),
                     w=[xk], dma=True)

        segs = [(b, s) for b in range(nb) for s in range(nseg)]
        load_x(0, 0, 0)
        pcount = [0]

        def proj(slot, c0, ncol, key):
            i = pcount[0] % 2
            pcount[0] += 1
            n = 0
            for k in range(KC):
                for sft in range(2):
                    S.op("pe", "matmul", ps[i][0:ncol, :], Wc[:, k, sft, c0:c0 + ncol], xb[slot][:, k, (1 - sft):(1 - sft) + SEG],
                         start=(n == 0), stop=(n == 2 * KC - 1), r=["Wc", ("xb", slot)], w=[pk[i]])
                    n += 1
            return ps[i], pk[i]

        col = lambda j: par[:, j:j + 1]
        for si, (b, s) in enumerate(segs):
            slot = si % 2
            t0 = b * ntok_b + s * SEG
            if si + 1 < len(segs):
                load_x(segs[si + 1][0], segs[si + 1][1], 1 - slot)
            if s == 0:
                S.op("dve", "memset", H[:], 0.0, w=["H"])
            p, k_ = proj(slot, 0, 128, None)
            S.op("act", "copy", out=rT[:], in_=p[:], r=[k_], w=["rT"])
            p, k_ = proj(slot, 128, 128, None)
            S.op("act", "copy", out=kT[:], in_=p[:], r=[k_], w=["kT"])
            p, k_ = proj(slot, 256, 128, None)
            S.op("act", "copy", out=vT[:], in_=p[:], r=[k_], w=["vT"])
            p, k_ = proj(slot, 384, 128, None)
            S.op("act", "activation", out=lor[0:64, 0, :], in_=p[0:64, :], func=AF.Tanh, r=[k_], w=["lor0a"])
            S.op("act", "copy", out=lor[64:128, 0, :], in_=p[64:128, :], r=[k_], w=["lor0b"])
            p, k_ = proj(slot, 512, 128, None)
            S.op("act", "activation", out=lor[:, 1, :], in_=p[:], func=AF.Sigmoid, r=[k_], w=["lor1"])
            p, k_ = proj(slot, 640, 64, None)
            S.op("act", "activation", out=lor[0:32, 2, :], in_=p[0:32, :], func=AF.Sigmoid, r=[k_], w=["lor2a"])
            S.op("act", "copy", out=lor[32:64, 2, :], in_=p[32:64, :], r=[k_], w=["lor2b"])
            i = pcount[0] % 2; pcount[0] += 1
            S.op("pe", "matmul", ps[i][:], w2b[0:64, 0, :], lor[0:64, 0, :], start=True, stop=True, r=["w2b", "lor0a"], w=[pk[i]])
            S.op("act", "activation", out=wT[:], in_=ps[i][:], func=AF.Sigmoid, bias=col(0), scale=1.0, r=[pk[i], "par"], w=["wT"])
            S.op("act", "activation", out=wT[:], in_=wT[:], func=AF.Exp, scale=DECAY_SCALE, r=["wT"], w=["wT"])
            i = pcount[0] % 2; pcount[0] += 1
            S.op("pe", "matmul", ps[i][:], w2b[64:128, 0, :], lor[64:128, 0, :], start=True, stop=True, r=["w2b", "lor0b"], w=[pk[i]])
            S.op("act", "activation", out=aT[:], in_=ps[i][:], func=AF.Sigmoid, bias=col(1), scale=1.0, r=[pk[i], "par"], w=["aT"])
            i = pcount[0] % 2; pcount[0] += 1
            S.op("pe", "matmul", ps[i][:], w2b[:, 1, :], lor[:, 1, :], start=True, stop=False, r=["w2b", "lor1"], w=[pk[i]])
            S.op("pe", "matmul", ps[i][:], w2b[0:32, 2, :], lor[0:32, 2, :], start=False, stop=True, r=["w2b", "lor2a"], w=[pk[i]])
            S.op("act", "copy", out=gT[:], in_=ps[i][:], r=[pk[i]], w=["gT"])
            if layer1:
                i = pcount[0] % 2; pcount[0] += 1
                S.op("pe", "matmul", ps[i][:], w2b[32:64, 2, :], lor[32:64, 2, :], start=True, stop=True, r=["w2b", "lor2b"], w=[pk[i]])
                S.op("act", "activation", out=t1[:], in_=ps[i][:], func=AF.Sigmoid, bias=col(2), scale=1.0, r=[pk[i], "par"], w=["t1"])
                S.op("sp", "dma_start", out=t2[:], in_=vf_d[:, t0:t0 + SEG], w=["t2"], dma=True)
                S.op("dve", "tensor_tensor", t2[:], t2[:], vT[:], ALU.subtract, r=["t2", "vT"], w=["t2"])
                S.op("dve", "tensor_tensor", t2[:], t2[:], t1[:], ALU.mult, r=["t2", "t1"], w=["t2"])
                S.op("dve", "tensor_tensor", vT[:], vT[:], t2[:], ALU.add, r=["vT", "t2"], w=["vT"])
            else:
                S.op("sp", "dma_start", out=vf_o[:, t0:t0 + SEG], in_=vT[:], r=["vT"], dma=True)
            if stop_after == 0:
                break
            S.op("dve", "tensor_scalar", av[:], kT[:], col(3), None, ALU.mult, r=["kT", "par"], w=["av"])
            S.op("act", "activation", out=t1[:], in_=av[:], func=AF.Square, r=["av"], w=["t1"])
            i = pcount[0] % 2; pcount[0] += 1
            S.op("pe", "matmul", ps[i][:], bones, t1[:], start=True, stop=True, r=["cst", "t1"], w=[pk[i]])
            S.op("dve", "tensor_scalar", t2[:], ps[i][:], 1e-24, None, ALU.max, r=[pk[i]], w=["t2"])
            S.op("act", "activation", out=t2[:], in_=t2[:], func=AF.Sqrt, r=["t2"], w=["t2"])
            S.op("dve", "reciprocal", t2[:], t2[:], r=["t2"], w=["t2"])
            S.op("dve", "tensor_tensor", av[:], av[:], t2[:], ALU.mult, r=["av", "t2"], w=["av"])
            S.op("dve", "tensor_tensor", bv[:], av[:], aT[:], ALU.mult, r=["av", "aT"], w=["bv"])
            S.op("dve", "tensor_scalar", t1[:], aT[:], -1.0, col(4), ALU.add, ALU.mult, r=["aT", "par"], w=["t1"])
            S.op("dve", "scalar_tensor_tensor", out=kT[:], in0=t1[:], scalar=1.0, in1=kT[:], op0=ALU.add, op1=ALU.mult,
                 r=["t1", "kT"], w=["kT"])
            if stop_after == 1:
                break
            w3 = wT[:].rearrange("p (c j) -> p c j", j=64)
            S.op("pool", "tensor_copy", out=d0[:], in_=wT[:], r=["wT"], w=["d0"])
            S.op("pool", "memset", d0[:].rearrange("p (c j) -> p c j", j=64)[:, :, 0:1], 0.0, w=["d0"])
            S.op("pool", "memset", d1[:], 0.0, w=["d1"])
            S.op("pool", "tensor_copy", out=d1[:].rearrange("p (c j) -> p c j", j=64)[:, :, 0:1], in_=w3[:, :, 0:1], r=["wT"], w=["d1"])
            S.op("dve", "tensor_tensor_scan", P[:], d0[:], d1[:], 0.0, ALU.mult, ALU.add, r=["d0", "d1"], w=["P"])
            S.op("dve", "reciprocal", rP[:], P[:], r=["P"], w=["rP"])
            S.op("dve", "reciprocal", t1[:], wT[:], r=["wT"], w=["t1"])
            S.op("dve", "tensor_tensor", t1[:], t1[:], P[:], ALU.mult, r=["t1", "P"], w=["t1"])
            c3 = lambda t: t[:].rearrange("p (c j) -> p c j", j=64)
            S.op("dve", "scalar_tensor_tensor", out=AR[:, :, 0, :], in0=c3(av), scalar=-1.0, in1=c3(t1), op0=ALU.mult, op1=ALU.mult,
                 r=["av", "t1"], w=["AR"])
            S.op("pool", "tensor_tensor", AR[:, :, 1, :], c3(rT), c3(P), ALU.mult, r=["rT", "P"], w=["AR"])
            S.op("pool", "tensor_tensor", KBf[:, :, 0, :], c3(kT), c3(rP), ALU.mult, r=["kT", "rP"], w=["KBf"])
            S.op("dve", "tensor_tensor", KBf[:, :, 1, :], c3(bv), c3(rP), ALU.mult, r=["bv", "rP"], w=["KBf"])
            pend = c3(P)[:, :, 63:64]
            for q in range(2):
                S.op("dve" if q else "pool", "tensor_tensor", KBh[:, :, q, :], KBf[:, :, q, :], _bc(pend, [128, NCH, 64]), ALU.mult,
                     r=["KBf", "P"], w=["KBh"])
            if stop_after == 2:
                break
            for hh in range(2):
                hb = hh * 64
                S.op("pool", "tensor_copy", out=ARblk[hb:hb + 64, :, hh, :], in_=AR[hb:hb + 64, :, :, :].rearrange("p c a b -> p c (a b)"),
                     r=["AR"], w=["ARblk"])
                S.op("pool", "tensor_copy", out=Bblk[hb:hb + 64, :, hh, :], in_=KBf[hb:hb + 64, :, 1, :], r=["KBf"], w=["Bblk"])
            for half in range(2):
                for cc in range(4):
                    c = half * 4 + cc
                    S.op("pe", "transpose", ps[2][0:64, cc * 128:(cc + 1) * 128], vT[:, c * 64:(c + 1) * 64], ident,
                         r=["vT", "cst"], w=[pk[2]])
                S.op("act", "copy", out=Vtm[:, half * 4:(half + 1) * 4, :].rearrange("p c f -> p (c f)"), in_=ps[2][0:64, :], r=[pk[2]], w=["Vtm"])
            for q2 in range(4):
                for cc in range(2):
                    c = q2 * 2 + cc
                    for q in range(2):
                        S.op("pe", "transpose", ps[2][0:64, (cc * 2 + q) * 128:(cc * 2 + q + 1) * 128], KBh[:, c, q, :], ident,
                             r=["KBh", "cst"], w=[pk[2]])
                S.op("act", "copy", out=KBtm[:, q2 * 2:(q2 + 1) * 2, :, :].rearrange("p c q f -> p (c q f)"), in_=ps[2][0:64, :],
                     r=[pk[2]], w=["KBtm"])
            if stop_after == 3:
                break
            for c2 in range(NCH // 2):
                for which, dst in ((0, AK), (1, AB)):
                    for cc in range(2):
                        c = c2 * 2 + cc
                        S.op("pe", "matmul", ps[2][0:64, cc * 256:(cc + 1) * 256], KBf[:, c, which, :],
                             ARblk[:, c, :, :].rearrange("p a b -> p (a b)"), start=True, stop=True, r=["KBf", "ARblk"], w=[pk[2]])
                    S.op("dve", "tensor_tensor", dst[:, c2 * 4:(c2 + 1) * 4, :], ps[2][0:64, :].rearrange("p (a b) -> p a b", b=128),
                         _bc(m_ak.unsqueeze(1), [64, 4, 128]), ALU.mult, r=[pk[2], "cst"], w=["AK" if which == 0 else "AB"])
            for c4 in range(NCH // 4):
                for cc in range(4):
                    c = c4 * 4 + cc
                    S.op("pe", "matmul", ps[2][0:64, cc * 128:(cc + 1) * 128], AR[:, c, 0, :],
                         Bblk[:, c, :, :].rearrange("p a b -> p (a b)"), start=True, stop=True, r=["Bblk", "AR"], w=[pk[2]])
                S.op("dve", "tensor_tensor", Np[0][:, c4 * 8:(c4 + 1) * 8, :], ps[2][0:64, :].rearrange("p (a b) -> p a b", b=64),
                     _bc(m_sl.unsqueeze(1), [64, 8, 64]), ALU.mult, r=[pk[2], "cst"], w=[("Np0", c4)])
            if stop_after == 4:
                break
            S.op("pool", "tensor_copy", out=Mp[0][:], in_=AB[:, :, 0:64], r=["AB"], w=[("Mp0", 0), ("Mp0", 1)])
            S.op("dve", "tensor_tensor", Q[:], AB[:, :, 0:64], _bc(id64.unsqueeze(1), [64, NBLK, 64]), ALU.add, r=["AB", "cst"], w=[("Q", 0), ("Q", 1)])
            ibank = {0: (3, 4, 5), 1: (6, 7, 2)}
            for lvl in range(5):
                cur, nxt = lvl % 2, (lvl + 1) % 2
                for g8 in range(NBLK // 8):
                    bM, bN, bQ = ibank[g8]
                    for bi in range(8):
                        blk = g8 * 8 + bi
                        S.op("pe", "matmul", ps[bM][0:64, bi * 64:(bi + 1) * 64], Np[cur][:, blk, :], Mp[cur][:, blk, :],
                             start=True, stop=True, r=[("Np%d" % cur, g8), ("Mp%d" % cur, g8)], w=[pk[bM]])
                    for bi in range(8):
                        blk = g8 * 8 + bi
                        S.op("pe", "matmul", ps[bN][0:64, bi * 64:(bi + 1) * 64], Mp[cur][:, blk, :], Np[cur][:, blk, :],
                             start=True, stop=True, r=[("Np%d" % cur, g8), ("Mp%d" % cur, g8)], w=[pk[bN]])
                for g8 in range(NBLK // 8):
                    bM, bN, bQ = ibank[g8]
                    gsl = slice(g8 * 8, (g8 + 1) * 8)
                    S.op("act", "copy", out=Mp[nxt][:, gsl, :].rearrange("p a b -> p (a b)"), in_=ps[bM][0:64, :], r=[pk[bM]], w=[("Mp%d" % nxt, g8)])
                    S.op("dve", "tensor_copy", out=Np[nxt][:, gsl, :].rearrange("p a b -> p (a b)"), in_=ps[bN][0:64, :], r=[pk[bN]], w=[("Np%d" % nxt, g8)])
                for g8 in range(NBLK // 8):
                    bM, bN, bQ = ibank[g8]
                    for bi in range(8):
                        blk = g8 * 8 + bi
                        S.op("pe", "matmul", ps[bQ][0:64, bi * 64:(bi + 1) * 64], Np[nxt][:, blk, :], Q[:, blk, :],
                             start=True, stop=True, r=[("Np%d" % nxt, g8), ("Q", g8)], w=[pk[bQ]])
                for g8 in range(NBLK // 8):
                    bM, bN, bQ = ibank[g8]
                    gsl = slice(g8 * 8, (g8 + 1) * 8)
                    S.op("dve", "tensor_tensor", Q[:, gsl, :].rearrange("p a b -> p (a b)"), Q[:, gsl, :].rearrange("p a b -> p (a b)"),
                         ps[bQ][0:64, :], ALU.add, r=[("Q", g8), pk[bQ]], w=[("Q", g8)])
            if stop_after == 5:
                break
            for c in range(NCH):
                S.op("pe", "matmul", ps[3][0:64, 0:128], AR[:, c, 0, :], H[:], start=True, stop=False, r=["AR", "H"], w=[pk[3]])
                for hh in range(2):
                    hb = hh * 64
                    blk = 2 * c + hh
                    S.op("pe", "matmul", ps[3][0:64, hb:hb + 64], AK[:, blk, 0:64], Vtm[:, c, hb:hb + 64], start=False, stop=(hh == 1),
                         r=["AK", "Vtm"], w=[pk[3]])
                S.op("act", "copy", out=Xs[:], in_=ps[3][0:64, 0:128], r=[pk[3]], w=["Xs"])
                for hh in range(2):
                    hb = hh * 64
                    blk = 2 * c + hh
                    S.op("pe", "matmul", ps[4][0:64, hb:hb + 64], Q[:, blk, :], Xs[:, hb:hb + 64], start=True, stop=True,
                         r=[("Q", blk // 8), "Xs"], w=[pk[4]])
                S.op("act", "copy", out=Us[:], in_=ps[4][0:64, 0:128], r=[pk[4]], w=["Us"])
                S.op("pe", "matmul", ps[5][0:64, 0:128], AR[:, c, 1, :], H[:], start=True, stop=False, r=["AR", "H"], w=[pk[5]])
                for hh in range(2):
                    hb = hh * 64
                    blk = 2 * c + hh
                    S.op("pe", "matmul", ps[5][0:64, hb:hb + 64], AB[:, blk, 64:128], Us[:, hb:hb + 64], start=False, stop=False,
                         r=["AB", "Us"], w=[pk[5]])
                    S.op("pe", "matmul", ps[5][0:64, hb:hb + 64], AK[:, blk, 64:128], Vtm[:, c, hb:hb + 64], start=False, stop=(hh == 1),
                         r=["AK", "Vtm"], w=[pk[5]])
                S.op("act", "copy", out=Ytm[:, c, :], in_=ps[5][0:64, 0:128], r=[pk[5]], w=["Ytm"])
                S.op("pe", "matmul", ps[6][:, 0:128], KBtm[:, c, 0, :], Vtm[:, c, :], start=True, stop=False, r=["KBtm", "Vtm"], w=[pk[6]])
                S.op("pe", "matmul", ps[6][:, 0:128], KBtm[:, c, 1, :], Us[:], start=False, stop=True, r=["KBtm", "Us"], w=[pk[6]])
                for hh in range(2):
                    hb = hh * 64
                    S.op("dve", "scalar_tensor_tensor", out=H[hb:hb + 64, hb:hb + 64], in0=H[hb:hb + 64, hb:hb + 64],
                         scalar=c3(P)[hb:hb + 64, c, 63:64], in1=ps[6][hb:hb + 64, hb:hb + 64],
                         op0=ALU.mult, op1=ALU.add, r=["H", "P", pk[6]], w=["H"])
            if stop_after == 6:
                break
            for c in range(NCH):
                S.op("pe", "transpose", ps[7][:, c * 64:(c + 1) * 64], Ytm[:, c, :], id64, r=["Ytm", "cst"], w=[pk[7]])
            S.op("act", "copy", out=yT[:], in_=ps[7][:], r=[pk[7]], w=["yT"])
            i = pcount[0] % 2; pcount[0] += 1
            S.op("pe", "matmul", ps[i][:], bones, yT[:], start=True, stop=True, r=["cst", "yT"], w=[pk[i]])
            S.op("dve", "scalar_tensor_tensor", out=yT[:], in0=ps[i][:], scalar=-1.0 / 64, in1=yT[:], op0=ALU.mult, op1=ALU.add,
                 r=[pk[i], "yT"], w=["yT"])
            S.op("act", "activation", out=t1[:], in_=yT[:], func=AF.Square, r=["yT"], w=["t1"])
            i = pcount[0] % 2; pcount[0] += 1
            S.op("pe", "matmul", ps[i][:], bones, t1[:], start=True, stop=True, r=["cst", "t1"], w=[pk[i]])
            S.op("act", "activation", out=t2[:], in_=ps[i][:], func=AF.Sqrt, bias=GN_EPS, scale=1.0 / 64, r=[pk[i]], w=["t2"])
            S.op("dve", "reciprocal", t2[:], t2[:], r=["t2"], w=["t2"])
            S.op("dve", "tensor_tensor", yT[:], yT[:], t2[:], ALU.mult, r=["yT", "t2"], w=["yT"])
            S.op("act", "activation", out=yT[:], in_=yT[:], func=AF.Identity, scale=col(6), bias=col(7), r=["yT", "par"], w=["yT"])
            S.op("dve", "scalar_tensor_tensor", out=t1[:], in0=rT[:], scalar=col(5), in1=kT[:], op0=ALU.mult, op1=ALU.mult,
                 r=["rT", "kT", "par"], w=["t1"])
            i = pcount[0] % 2; pcount[0] += 1
            S.op("pe", "matmul", ps[i][:], bones, t1[:], start=True, stop=True, r=["cst", "t1"], w=[pk[i]])
            S.op("dve", "tensor_tensor", t2[:], ps[i][:], vT[:], ALU.mult, r=[pk[i], "vT"], w=["t2"])
            S.op("dve", "tensor_tensor", yT[:], yT[:], t2[:], ALU.add, r=["yT", "t2"], w=["yT"])
            S.op("dve", "tensor_tensor", ob[:], yT[:], gT[:], ALU.mult, r=["yT", "gT"], w=["ob"])
            S.op("sp", "dma_start", out=mix_o[:, t0:t0 + SEG], in_=ob[:], r=["ob"], dma=True)
        S.emit()
    return nc


def build_H_rwkv2(layer1, ntok_b=T, nb=B, stop_after=99):
    import contextlib
    nseg = ntok_b // SEG
    ntot = nb * ntok_b
    nc = bass.Bass("TRN2", target_bir_lowering=False)
    with contextlib.ExitStack() as es:
        cx = Ctx(nc, es)
        S = cx.S
        xT = cx.dram("xT", [D, ntot], F32, "ExternalInput")
        wproj = cx.dram("wproj", [D, NPROJ], F32, "ExternalInput")
        mu_d = cx.dram("mu", [128, KC, NPROJ], F32, "ExternalInput")
        w2_d = cx.dram("w2", [128, 3, 128], F32, "ExternalInput")
        par_d = cx.dram("par", [128, 8], F32, "ExternalInput")
        cst_d = cx.dram("cst", [128, 512], F32, "ExternalInput")
        if layer1:
            vf_d = cx.dram("vfirst", [128, ntot], F32, "ExternalInput")
        else:
            vf_o = cx.dram("vfirst_out", [128, ntot], F32, "ExternalOutput")
        mix_o = cx.dram("mix", [128, ntot], BF16, "ExternalOutput")

        f32t = lambda n, shp=(128, SEG): cx.sb(n, list(shp), F32)
        xb = [cx.sb("xb%d" % i, [128, KC, SEG + 1], BF16) for i in range(2)]
        Wc = cx.sb("Wc", [128, KC, 2, NPROJ], BF16)
        wst = cx.sb("wst", [128, NPROJ], F32)
        must = cx.sb("must", [128, NPROJ], F32)
        w2f = cx.sb("w2f", [128, 3, 128], F32)
        w2b = cx.sb("w2b", [128, 3, 128], BF16)
        par = cx.sb("par_sb", [128, 8], F32)
        cst = cx.sb("cst_sb", [128, 512], F32)
        ident = cst[:, 0:128]
        bones = cst[:, 128:256]
        m_su = cst[0:64, 256:320]
        m_iu = cst[0:64, 320:384]
        m_ak = cst[0:64, 256:384]
        m_sl = cst[0:64, 384:448]
        id64 = cst[0:64, 448:512]
        aT, wT, av, bv, rP, t1, t2, d0, d1 = [f32t(n) for n in
            ("aT", "wT", "av", "bv", "rP", "t1", "t2", "d0", "d1")]
        u1, u2 = d0, d1
        rTs, kTs, vTs, gTs, Ps = [[f32t("%s%d" % (n, i)) for i in range(2)] for n in ("rT", "kT", "vT", "gT", "P")]
        lor = cx.sb("lor", [128, 3, SEG], BF16)
        ARs = [cx.sb("AR%d" % i, [128, NCH, 2, 64], F32) for i in range(2)]
        KBf = cx.sb("KBf", [128, NCH, 2, 64], F32)
        KBh = cx.sb("KBh", [128, NCH, 2, 64], F32)
        ARblk = cx.sb("ARblk", [128, NCH, 2, 128], F32)
        Bblk = cx.sb("Bblk", [128, NCH, 2, 64], F32)
        Vtms = [cx.sb("Vtm%d" % i, [64, NCH, 128], F32) for i in range(2)]
        KBtms = [cx.sb("KBtm%d" % i, [64, NCH, 2, 128], F32) for i in range(2)]
        Ytm = cx.sb("Ytm", [64, NCH, 128], F32)
        AKs = [cx.sb("AK%d" % i, [64, NBLK, 128], F32) for i in range(2)]
        ABs = [cx.sb("AB%d" % i, [64, NBLK, 128], F32) for i in range(2)]
        Mp = [cx.sb("Mp%d" % i, [64, NBLK, 64], F32) for i in range(2)]
        Np = [cx.sb("Np%d" % i, [64, NBLK, 64], F32) for i in range(2)]
        Qs = [cx.sb("Q%d" % i, [64, NBLK, 64], F32) for i in range(2)]
        H = cx.sb("Hst", [128, 128], F32)
        Xs = cx.sb("Xs", [64, 128], F32)
        Us = cx.sb("Us", [64, 128], F32)
        yT = f32t("yT")
        ob = cx.sb("ob", [128, SEG], BF16)
        ps = [cx.ps("ps%d" % i, [128, 512]) for i in range(8)]
        pk = [("ps", i) for i in range(8)]

        S.op("sp", "dma_start", out=cst[:], in_=cst_d, w=["cst"], dma=True)
        S.op("sp", "dma_start", out=par[:], in_=par_d, w=["par"], dma=True)
        S.op("sp", "dma_start", out=w2f[:], in_=w2_d, w=["w2f"], dma=True)
        S.op("dve", "tensor_copy", out=w2b[:], in_=w2f[:], r=["w2f"], w=["w2b"])
        S.op("dve", "memset", ARblk[:], 0.0, w=["ARblk"])
        S.op("dve", "memset", Bblk[:], 0.0, w=["Bblk"])
        for k in range(KC):
            S.op("sp", "dma_start", out=wst[:], in_=wproj[k * 128:(k + 1) * 128, :], w=["wst"], dma=True)
            S.op("sp", "dma_start", out=must[:], in_=mu_d[:, k, :], w=["must"], dma=True)
            S.op("dve", "tensor_tensor", must[:], wst[:], must[:], ALU.mult, r=["wst", "must"], w=["must"])
            S.op("dve", "tensor_copy", out=Wc[:, k, 1, :], in_=must[:], r=["must"], w=["Wc"])
            S.op("dve", "tensor_tensor", Wc[:, k, 0, :], wst[:], must[:], ALU.subtract, r=["wst", "must"], w=["Wc"])

        def load_x(b, s, slot):
            t0 = b * ntok_b + s * SEG
            xk = ("xb", slot)
            if s == 0:
                S.op("dve", "memset", xb[slot][:, :, 0:1], 0.0, w=[xk])
                S.op("pool", "dma_start", out=xb[slot][:, :, 1:SEG + 1], in_=xT[:, t0:t0 + SEG].rearrange("(c p) t -> p c t", p=128),
                     w=[xk], dma=True)
            else:
                S.op("pool", "dma_start", out=xb[slot][:, :, 0:SEG + 1], in_=xT[:, t0 - 1:t0 + SEG].rearrange("(c p) t -> p c t", p=128),
                     w=[xk], dma=True)

        segs = [(b, s) for b in range(nb) for s in range(nseg)]
        load_x(0, 0, 0)
        pcount = [0]

        def proj(slot, c0, ncol, key):
            i = pcount[0] % 2
            pcount[0] += 1
            n = 0
            for k in range(KC):
                for sft in range(2):
                    S.op("pe", "matmul", ps[i][0:ncol, :], Wc[:, k, sft, c0:c0 + ncol], xb[slot][:, k, (1 - sft):(1 - sft) + SEG],
                         start=(n == 0), stop=(n == 2 * KC - 1), r=["Wc", ("xb", slot)], w=[pk[i]])
                    n += 1
            return ps[i], pk[i]

        col = lambda j: par[:, j:j + 1]
        def prep_gen(si):
            b, s = segs[si]
            p_ = si % 2
            rT, kT, vT, gT, P, AR, Vtm, KBtm, AK, AB, Q = (rTs[p_], kTs[p_], vTs[p_], gTs[p_], Ps[p_], ARs[p_], Vtms[p_], KBtms[p_],
                                                          AKs[p_], ABs[p_], Qs[p_])
            c3 = lambda t: t[:].rearrange("p (c j) -> p c j", j=64)
            yield
            slot = si % 2
            t0 = b * ntok_b + s * SEG
            if si + 1 < len(segs):
                load_x(segs[si + 1][0], segs[si + 1][1], 1 - slot)
            p, k_ = proj(slot, 0, 128, None)
            S.op("act", "copy", out=rT[:], in_=p[:], r=[k_], w=["rT"])
            yield
            p, k_ = proj(slot, 128, 128, None)
            S.op("act", "copy", out=kT[:], in_=p[:], r=[k_], w=["kT"])
            yield
            p, k_ = proj(slot, 256, 128, None)
            S.op("act", "copy", out=vT[:], in_=p[:], r=[k_], w=["vT"])
            yield
            p, k_ = proj(slot, 384, 128, None)
            S.op("act", "activation", out=lor[0:64, 0, :], in_=p[0:64, :], func=AF.Tanh, r=[k_], w=["lor0a"])
            S.op("act", "copy", out=lor[64:128, 0, :], in_=p[64:128, :], r=[k_], w=["lor0b"])
            yield
            p, k_ = proj(slot, 512, 128, None)
            S.op("act", "activation", out=lor[:, 1, :], in_=p[:], func=AF.Sigmoid, r=[k_], w=["lor1"])
            yield
            p, k_ = proj(slot, 640, 64, None)
            S.op("act", "activation", out=lor[0:32, 2, :], in_=p[0:32, :], func=AF.Sigmoid, r=[k_], w=["lor2a"])
            S.op("act", "copy", out=lor[32:64, 2, :], in_=p[32:64, :], r=[k_], w=["lor2b"])
            yield
            i = pcount[0] % 2; pcount[0] += 1
            S.op("pe", "matmul", ps[i][:], w2b[0:64, 0, :], lor[0:64, 0, :], start=True, stop=True, r=["w2b", "lor0a"], w=[pk[i]])
            S.op("act", "activation", out=wT[:], in_=ps[i][:], func=AF.Sigmoid, bias=col(0), scale=1.0, r=[pk[i], "par"], w=["wT"])
            S.op("act", "activation", out=wT[:], in_=wT[:], func=AF.Exp, scale=DECAY_SCALE, r=["wT"], w=["wT"])
            i = pcount[0] % 2; pcount[0] += 1
            S.op("pe", "matmul", ps[i][:], w2b[64:128, 0, :], lor[64:128, 0, :], start=True, stop=True, r=["w2b", "lor0b"], w=[pk[i]])
            S.op("act", "activation", out=aT[:], in_=ps[i][:], func=AF.Sigmoid, bias=col(1), scale=1.0, r=[pk[i], "par"], w=["aT"])
            i = pcount[0] % 2; pcount[0] += 1
            S.op("pe", "matmul", ps[i][:], w2b[:, 1, :], lor[:, 1, :], start=True, stop=False, r=["w2b", "lor1"], w=[pk[i]])
            S.op("pe", "matmul", ps[i][:], w2b[0:32, 2, :], lor[0:32, 2, :], start=False, stop=True, r=["w2b", "lor2a"], w=[pk[i]])
            S.op("act", "copy", out=gT[:], in_=ps[i][:], r=[pk[i]], w=["gT"])
            if layer1:
                i = pcount[0] % 2; pcount[0] += 1
                S.op("pe", "matmul", ps[i][:], w2b[32:64, 2, :], lor[32:64, 2, :], start=True, stop=True, r=["w2b", "lor2b"], w=[pk[i]])
                S.op("act", "activation", out=t1[:], in_=ps[i][:], func=AF.Sigmoid, bias=col(2), scale=1.0, r=[pk[i], "par"], w=["t1"])
                S.op("sp", "dma_start", out=t2[:], in_=vf_d[:, t0:t0 + SEG], w=["t2"], dma=True)
                S.op("dve", "tensor_tensor", t2[:], t2[:], vT[:], ALU.subtract, r=["t2", "vT"], w=["t2"])
                S.op("dve", "tensor_tensor", t2[:], t2[:], t1[:], ALU.mult, r=["t2", "t1"], w=["t2"])
                S.op("dve", "tensor_tensor", vT[:], vT[:], t2[:], ALU.add, r=["vT", "t2"], w=["vT"])
            else:
                S.op("sp", "dma_start", out=vf_o[:, t0:t0 + SEG], in_=vT[:], r=["vT"], dma=True)
            yield
            S.op("dve", "tensor_scalar", av[:], kT[:], col(3), None, ALU.mult, r=["kT", "par"], w=["av"])
            S.op("act", "activation", out=t1[:], in_=av[:], func=AF.Square, r=["av"], w=["t1"])
            i = pcount[0] % 2; pcount[0] += 1
            S.op("pe", "matmul", ps[i][:], bones, t1[:], start=True, stop=True, r=["cst", "t1"], w=[pk[i]])
            S.op("dve", "tensor_scalar", t2[:], ps[i][:], 1e-24, None, ALU.max, r=[pk[i]], w=["t2"])
            S.op("act", "activation", out=t2[:], in_=t2[:], func=AF.Sqrt, r=["t2"], w=["t2"])
            S.op("dve", "reciprocal", t2[:], t2[:], r=["t2"], w=["t2"])
            S.op("dve", "tensor_tensor", av[:], av[:], t2[:], ALU.mult, r=["av", "t2"], w=["av"])
            S.op("dve", "tensor_tensor", bv[:], av[:], aT[:], ALU.mult, r=["av", "aT"], w=["bv"])
            yield
            S.op("dve", "tensor_scalar", t1[:], aT[:], -1.0, col(4), ALU.add, ALU.mult, r=["aT", "par"], w=["t1"])
            S.op("dve", "scalar_tensor_tensor", out=kT[:], in0=t1[:], scalar=1.0, in1=kT[:], op0=ALU.add, op1=ALU.mult,
                 r=["t1", "kT"], w=["kT"])
            yield
            w3 = wT[:].rearrange("p (c j) -> p c j", j=64)
            S.op("pool", "tensor_copy", out=d0[:], in_=wT[:], r=["wT"], w=["d0"])
            S.op("pool", "memset", d0[:].rearrange("p (c j) -> p c j", j=64)[:, :, 0:1], 0.0, w=["d0"])
            S.op("pool", "memset", d1[:], 0.0, w=["d1"])
            S.op("pool", "tensor_copy", out=d1[:].rearrange("p (c j) -> p c j", j=64)[:, :, 0:1], in_=w3[:, :, 0:1], r=["wT"], w=["d1"])
            S.op("dve", "tensor_tensor_scan", P[:], d0[:], d1[:], 0.0, ALU.mult, ALU.add, r=["d0", "d1"], w=["P"])
            S.op("dve", "reciprocal", rP[:], P[:], r=["P"], w=["rP"])
            S.op("dve", "reciprocal", t1[:], wT[:], r=["wT"], w=["t1"])
            S.op("dve", "tensor_tensor", t1[:], t1[:], P[:], ALU.mult, r=["t1", "P"], w=["t1"])
            c3 = lambda t: t[:].rearrange("p (c j) -> p c j", j=64)
            yield
            S.op("dve", "scalar_tensor_tensor", out=AR[:, :, 0, :], in0=c3(av), scalar=-1.0, in1=c3(t1), op0=ALU.mult, op1=ALU.mult,
                 r=["av", "t1"], w=["AR"])
            S.op("pool", "tensor_tensor", AR[:, :, 1, :], c3(rT), c3(P), ALU.mult, r=["rT", "P"], w=["AR"])
            S.op("pool", "tensor_tensor", KBf[:, :, 0, :], c3(kT), c3(rP), ALU.mult, r=["kT", "rP"], w=["KBf"])
            S.op("dve", "tensor_tensor", KBf[:, :, 1, :], c3(bv), c3(rP), ALU.mult, r=["bv", "rP"], w=["KBf"])
            pend = c3(P)[:, :, 63:64]
            for q in range(2):
                S.op("dve" if q else "pool", "tensor_tensor", KBh[:, :, q, :], KBf[:, :, q, :], _bc(pend, [128, NCH, 64]), ALU.mult,
                     r=["KBf", "P"], w=["KBh"])
            for hh in range(2):
                hb = hh * 64
                S.op("pool", "tensor_copy", out=ARblk[hb:hb + 64, :, hh, :], in_=AR[hb:hb + 64, :, :, :].rearrange("p c a b -> p c (a b)"),
                     r=["AR"], w=["ARblk"])
                S.op("pool", "tensor_copy", out=Bblk[hb:hb + 64, :, hh, :], in_=KBf[hb:hb + 64, :, 1, :], r=["KBf"], w=["Bblk"])
            yield
            for half in range(2):
                for cc in range(4):
                    c = half * 4 + cc
                    S.op("pe", "transpose", ps[2][0:64, cc * 128:(cc + 1) * 128], vT[:, c * 64:(c + 1) * 64], ident,
                         r=["vT", "cst"], w=[pk[2]])
                S.op("act", "copy", out=Vtm[:, half * 4:(half + 1) * 4, :].rearrange("p c f -> p (c f)"), in_=ps[2][0:64, :], r=[pk[2]], w=["Vtm"])
            for q2 in range(4):
                for cc in range(2):
                    c = q2 * 2 + cc
                    for q in range(2):
                        S.op("pe", "transpose", ps[2][0:64, (cc * 2 + q) * 128:(cc * 2 + q + 1) * 128], KBh[:, c, q, :], ident,
                             r=["KBh", "cst"], w=[pk[2]])
                S.op("act", "copy", out=KBtm[:, q2 * 2:(q2 + 1) * 2, :, :].rearrange("p c q f -> p (c q f)"), in_=ps[2][0:64, :],
                     r=[pk[2]], w=["KBtm"])
            yield
            for c2 in range(NCH // 2):
                for which, dst in ((0, AK), (1, AB)):
                    for cc in range(2):
                        c = c2 * 2 + cc
                        S.op("pe", "matmul", ps[2][0:64, cc * 256:(cc + 1) * 256], KBf[:, c, which, :],
                             ARblk[:, c, :, :].rearrange("p a b -> p (a b)"), start=True, stop=True, r=["KBf", "ARblk"], w=[pk[2]])
                    S.op("dve", "tensor_tensor", dst[:, c2 * 4:(c2 + 1) * 4, :], ps[2][0:64, :].rearrange("p (a b) -> p a b", b=128),
                         _bc(m_ak.unsqueeze(1), [64, 4, 128]), ALU.mult, r=[pk[2], "cst"], w=["AK" if which == 0 else "AB"])
            for c4 in range(NCH // 4):
                for cc in range(4):
                    c = c4 * 4 + cc
                    S.op("pe", "matmul", ps[2][0:64, cc * 128:(cc + 1) * 128], AR[:, c, 0, :],
                         Bblk[:, c, :, :].rearrange("p a b -> p (a b)"), start=True, stop=True, r=["Bblk", "AR"], w=[pk[2]])
                S.op("dve", "tensor_tensor", Np[0][:, c4 * 8:(c4 + 1) * 8, :], ps[2][0:64, :].rearrange("p (a b) -> p a b", b=64),
                     _bc(m_sl.unsqueeze(1), [64, 8, 64]), ALU.mult, r=[pk[2], "cst"], w=[("Np0", c4)])
            yield
            S.op("pool", "tensor_copy", out=Mp[0][:], in_=AB[:, :, 0:64], r=["AB"], w=[("Mp0", 0), ("Mp0", 1)])
            S.op("dve", "tensor_tensor", Q[:], AB[:, :, 0:64], _bc(id64.unsqueeze(1), [64, NBLK, 64]), ALU.add, r=["AB", "cst"], w=[("Q", 0, p_), ("Q", 1, p_)])
            ibank = {0: (3, 4, 5), 1: (0, 1, 2)}
            for lvl in range(5):
                yield
                cur, nxt = lvl % 2, (lvl + 1) % 2
                for g8 in range(NBLK // 8):
                    bM, bN, bQ = ibank[g8]
                    for bi in range(8):
                        blk = g8 * 8 + bi
                        S.op("pe", "matmul", ps[bM][0:64, bi * 64:(bi + 1) * 64], Np[cur][:, blk, :], Mp[cur][:, blk, :],
                             start=True, stop=True, r=[("Np%d" % cur, g8), ("Mp%d" % cur, g8)], w=[pk[bM]])
                    for bi in range(8):
                        blk = g8 * 8 + bi
                        S.op("pe", "matmul", ps[bN][0:64, bi * 64:(bi + 1) * 64], Mp[cur][:, blk, :], Np[cur][:, blk, :],
                             start=True, stop=True, r=[("Np%d" % cur, g8), ("Mp%d" % cur, g8)], w=[pk[bN]])
                for g8 in range(NBLK // 8):
                    bM, bN, bQ = ibank[g8]
                    gsl = slice(g8 * 8, (g8 + 1) * 8)
                    S.op("act", "copy", out=Mp[nxt][:, gsl, :].rearrange("p a b -> p (a b)"), in_=ps[bM][0:64, :], r=[pk[bM]], w=[("Mp%d" % nxt, g8)])
                    S.op("dve", "tensor_copy", out=Np[nxt][:, gsl, :].rearrange("p a b -> p (a b)"), in_=ps[bN][0:64, :], r=[pk[bN]], w=[("Np%d" % nxt, g8)])
                for g8 in range(NBLK // 8):
                    bM, bN, bQ = ibank[g8]
                    for bi in range(8):
                        blk = g8 * 8 + bi
                        S.op("pe", "matmul", ps[bQ][0:64, bi * 64:(bi + 1) * 64], Np[nxt][:, blk, :], Q[:, blk, :],
                             start=True, stop=True, r=[("Np%d" % nxt, g8), ("Q", g8, p_)], w=[pk[bQ]])
                for g8 in range(NBLK // 8):
                    bM, bN, bQ = ibank[g8]
                    gsl = slice(g8 * 8, (g8 + 1) * 8)
                    S.op("dve", "tensor_tensor", Q[:, gsl, :].rearrange("p a b -> p (a b)"), Q[:, gsl, :].rearrange("p a b -> p (a b)"),
                         ps[bQ][0:64, :], ALU.add, r=[("Q", g8, p_), pk[bQ]], w=[("Q", g8, p_)])

        def chunk_gen(si):
            b, s = segs[si]
            t0 = b * ntok_b + s * SEG
            p_ = si % 2
            rT, kT, vT, gT, P, AR, Vtm, KBtm, AK, AB, Q = (rTs[p_], kTs[p_], vTs[p_], gTs[p_], Ps[p_], ARs[p_], Vtms[p_], KBtms[p_],
                                                          AKs[p_], ABs[p_], Qs[p_])
            c3 = lambda t: t[:].rearrange("p (c j) -> p c j", j=64)
            if s == 0:
                S.op("dve", "memset", H[:], 0.0, w=["H"])
            for c in range(NCH):
                yield
                S.op("pe", "matmul", ps[6][0:64, 0:128], AR[:, c, 0, :], H[:], start=True, stop=False, r=["AR", "H"], w=[pk[6]])
                for hh in range(2):
                    hb = hh * 64
                    blk = 2 * c + hh
                    S.op("pe", "matmul", ps[6][0:64, hb:hb + 64], AK[:, blk, 0:64], Vtm[:, c, hb:hb + 64], start=False, stop=(hh == 1),
                         r=["AK", "Vtm"], w=[pk[6]])
                S.op("act", "copy", out=Xs[:], in_=ps[6][0:64, 0:128], r=[pk[6]], w=["Xs"])
                for hh in range(2):
                    hb = hh * 64
                    blk = 2 * c + hh
                    S.op("pe", "matmul", ps[6][0:64, 128 + hb:128 + hb + 64], Q[:, blk, :], Xs[:, hb:hb + 64], start=True, stop=True,
                         r=[("Q", blk // 8, p_), "Xs"], w=[pk[6]])
                S.op("act", "copy", out=Us[:], in_=ps[6][0:64, 128:256], r=[pk[6]], w=["Us"])
                S.op("pe", "matmul", ps[6][0:64, 256:384], AR[:, c, 1, :], H[:], start=True, stop=False, r=["AR", "H"], w=[pk[6]])
                for hh in range(2):
                    hb = hh * 64
                    blk = 2 * c + hh
                    S.op("pe", "matmul", ps[6][0:64, 256 + hb:256 + hb + 64], AB[:, blk, 64:128], Us[:, hb:hb + 64], start=False, stop=False,
                         r=["AB", "Us"], w=[pk[6]])
                    S.op("pe", "matmul", ps[6][0:64, 256 + hb:256 + hb + 64], AK[:, blk, 64:128], Vtm[:, c, hb:hb + 64], start=False, stop=(hh == 1),
                         r=["AK", "Vtm"], w=[pk[6]])
                S.op("act", "copy", out=Ytm[:, c, :], in_=ps[6][0:64, 256:384], r=[pk[6]], w=["Ytm"])
                S.op("pe", "matmul", ps[7][:, 0:128], KBtm[:, c, 0, :], Vtm[:, c, :], start=True, stop=False, r=["KBtm", "Vtm"], w=[pk[7]])
                S.op("pe", "matmul", ps[7][:, 0:128], KBtm[:, c, 1, :], Us[:], start=False, stop=True, r=["KBtm", "Us"], w=[pk[7]])
                for hh in range(2):
                    hb = hh * 64
                    S.op("dve", "scalar_tensor_tensor", out=H[hb:hb + 64, hb:hb + 64], in0=H[hb:hb + 64, hb:hb + 64],
                         scalar=c3(P)[hb:hb + 64, c, 63:64], in1=ps[7][hb:hb + 64, hb:hb + 64],
                         op0=ALU.mult, op1=ALU.add, r=["H", "P", pk[7]], w=["H"])
            yield
            for c in range(NCH):
                S.op("pe", "transpose", ps[6][:, c * 64:(c + 1) * 64], Ytm[:, c, :], id64, r=["Ytm", "cst"], w=[pk[6]])
            S.op("act", "copy", out=yT[:], in_=ps[6][:], r=[pk[6]], w=["yT"])
            S.op("pe", "matmul", ps[7][:], bones, yT[:], start=True, stop=True, r=["cst", "yT"], w=[pk[7]])
            S.op("dve", "scalar_tensor_tensor", out=yT[:], in0=ps[7][:], scalar=-1.0 / 64, in1=yT[:], op0=ALU.mult, op1=ALU.add,
                 r=[pk[7], "yT"], w=["yT"])
            S.op("act", "activation", out=u1[:], in_=yT[:], func=AF.Square, r=["yT"], w=["d0"])
            S.op("pe", "matmul", ps[7][:], bones, u1[:], start=True, stop=True, r=["cst", "d0"], w=[pk[7]])
            S.op("act", "activation", out=u2[:], in_=ps[7][:], func=AF.Sqrt, bias=GN_EPS, scale=1.0 / 64, r=[pk[7]], w=["d1"])
            S.op("dve", "reciprocal", u2[:], u2[:], r=["d1"], w=["d1"])
            S.op("dve", "tensor_tensor", yT[:], yT[:], u2[:], ALU.mult, r=["yT", "d1"], w=["yT"])
            S.op("act", "activation", out=yT[:], in_=yT[:], func=AF.Identity, scale=col(6), bias=col(7), r=["yT", "par"], w=["yT"])
            S.op("dve", "scalar_tensor_tensor", out=u1[:], in0=rT[:], scalar=col(5), in1=kT[:], op0=ALU.mult, op1=ALU.mult,
                 r=["rT", "kT", "par"], w=["d0"])
            S.op("pe", "matmul", ps[7][:], bones, u1[:], start=True, stop=True, r=["cst", "d0"], w=[pk[7]])
            S.op("dve", "tensor_tensor", u2[:], ps[7][:], vT[:], ALU.mult, r=[pk[7], "vT"], w=["d1"])
            S.op("dve", "tensor_tensor", yT[:], yT[:], u2[:], ALU.add, r=["yT", "d1"], w=["yT"])
            S.op("dve", "tensor_tensor", ob[:], yT[:], gT[:], ALU.mult, r=["yT", "gT"], w=["ob"])
            S.op("sp", "dma_start", out=mix_o[:, t0:t0 + SEG], in_=ob[:], r=["ob"], dma=True)

        def step(gen, al):
            S.alias = al
            try:
                next(gen)
                return True
            except StopIteration:
                return False

        aliases = [{n: (n, q) for n in ['rT', 'kT', 'vT', 'gT', 'P', 'AR', 'Vtm', 'KBtm', 'AK', 'AB']} for q in range(2)]
        g = prep_gen(0)
        while step(g, aliases[0]):
            pass
        for si in range(len(segs)):
            nxt = prep_gen(si + 1) if si + 1 < len(segs) else None
            cg = chunk_gen(si)
            alive = nxt is not None
            while step(cg, aliases[si % 2]):
                for _ in range(2):
                    if alive:
                        alive = step(nxt, aliases[(si + 1) % 2])
            while alive:
                alive = step(nxt, aliases[(si + 1) % 2])
        S.alias = {}
        S.emit()
    return nc


def rwkv_host_inputs(inp, li, core):
    cs = slice(core * 128, (core + 1) * 128)
    f = np.float32
    wproj = np.zeros((D, NPROJ), f)
    mu = np.zeros((D, NPROJ), f)
    M = inp["rwkv_mu"][li]
    wproj[:, 0:128] = inp["rwkv_w_rkv"][li, 0][:, cs]; mu[:, 0:128] = M[0][:, None]
    wproj[:, 128:256] = inp["rwkv_w_rkv"][li, 1][:, cs]; mu[:, 128:256] = M[1][:, None]
    wproj[:, 256:384] = inp["rwkv_w_rkv"][li, 2][:, cs]; mu[:, 256:384] = M[2][:, None]
    wproj[:, 384:448] = inp["rwkv_decay_w1"][li]; mu[:, 384:448] = M[3][:, None]
    wproj[:, 448:512] = inp["rwkv_iclr_a1"][li]; mu[:, 448:512] = M[4][:, None]
    wproj[:, 512:672] = inp["rwkv_gate_g1"][li]; mu[:, 512:672] = M[5][:, None]
    if li > 0:
        wproj[:, 672:704] = inp["rwkv_vres_v1"][li - 1]
    mu[:, 672:704] = M[2][:, None]
    w2 = np.zeros((128, 3, 128), f)
    w2[0:64, 0] = inp["rwkv_decay_w2"][li][:, cs]
    w2[64:128, 0] = inp["rwkv_iclr_a2"][li][:, cs]
    w2[:, 1] = inp["rwkv_gate_g2"][li][0:128, cs]
    w2[0:32, 2] = inp["rwkv_gate_g2"][li][128:160, cs]
    if li > 0:
        w2[32:64, 2] = inp["rwkv_vres_v2"][li - 1][:, cs]
    par = np.zeros((128, 8), f)
    par[:, 0] = inp["rwkv_decay_w0"][li][cs]
    par[:, 1] = inp["rwkv_iclr_a0"][li][cs]
    if li > 0:
        par[:, 2] = inp["rwkv_vres_v0"][li - 1][cs]
    par[:, 3] = inp["rwkv_k_k"][li][cs]
    par[:, 4] = inp["rwkv_k_a"][li][cs]
    par[:, 5] = inp["rwkv_r_k"][li].reshape(-1)[cs]
    par[:, 6] = inp["rwkv_gn_g"][li][cs]
    par[:, 7] = inp["rwkv_gn_b"][li][cs]
    mu_l = np.ascontiguousarray(mu.reshape(KC, 128, NPROJ).transpose(1, 0, 2))
    return {"wproj": wproj, "mu": mu_l, "w2": w2, "par": par, "cst": rwkv_consts()}


NEG = -1.0e30
BLK = 256


def moba_consts():
    c = {}
    ident = np.eye(128, dtype=np.float32)
    c["identf"] = ident
    key = np.arange(128)[:, None]
    q = np.arange(128)[None, :]
    tri = np.where(key <= q, 0.0, NEG).astype(np.float32)
    oh = np.zeros((32, 32, 128), np.float32)
    for n in range(32):
        oh[n, n, :] = 1.0e30
    cb = np.zeros((128, 128 + 128 + 32 * 128), np.float32)
    cb[:, 0:128] = ident
    cb[:, 128:256] = tri
    cb[0:32, 256:] = oh.reshape(32, 32 * 128)
    return {"cstf": ident, "cstb": cb.astype(ml_dtypes.bfloat16)}


def build_H_moba(separate_kv, ntok_b=T, nb=B, dbg=()):
    import contextlib
    nseg = ntok_b // 512
    ntile = ntok_b // 128
    nblkb = ntok_b // BLK
    ntot = nb * ntok_b
    nc = bass.Bass("TRN2", target_bir_lowering=False)
    with contextlib.ExitStack() as es:
        cx = Ctx(nc, es)
        S = cx.S
        xT = cx.dram("xT", [D, ntot], F32, "ExternalInput")
        if separate_kv:
            xkvT = cx.dram("xkvT", [D, ntot], F32, "ExternalInput")
        else:
            xkvT = xT
        wqkv_d = cx.dram("wqkv", [D, 3, 128], F32, "ExternalInput")
        cstf_d = cx.dram("cstf", [128, 128], F32, "ExternalInput")
        cstb_d = cx.dram("cstb", [128, 256 + 32 * 128], BF16, "ExternalInput")
        mix_o = cx.dram("mix", [128, ntot], BF16, "ExternalOutput")

        wqkv = cx.sb("wqkv_sb", [128, KC, 3, 128], BF16)
        identf = cx.sb("identf", [128, 128], F32)
        cstb = cx.sb("cstb_sb", [128, 256 + 32 * 128], BF16)
        identb = cstb[:, 0:128]
        trib = cstb[:, 128:256]
        xq = [cx.sb("xq%d" % i, [128, KC, 512], BF16) for i in range(2)]
        xkv = [cx.sb("xkv%d" % i, [128, KC, 512], BF16) for i in range(2)] if separate_kv else xq
        KT = cx.sb("KT", [128, ntok_b], BF16)
        Va = cx.sb("Va", [128, ntile, 2, 65], BF16)
        qf = cx.sb("qf", [128, ntok_b], F32)
        qz = [cx.sb("qz%d" % i, [128, ntok_b], BF16) for i in range(2)]
        km = cx.sb("km", [128, nblkb], F32)
        kmblk = cx.sb("kmblk", [128, 2, nblkb], F32)
        gsb = cx.sb("gsb", [128, 2, 32], F32)
        top8 = cx.sb("top8", [128, 2, 8], F32)
        mm1 = cx.sb("mm1", [128, 2, 32], F32)
        mT = [cx.sb("mT%d" % i, [32, 2, 128], BF16) for i in range(2)]
        PT = [cx.sb("PT%d" % i, [128, 1024], BF16) for i in range(3)]
        osb = cx.sb("osb", [128, 128], F32)
        rs = cx.sb("rs", [128, 1], F32)
        ob = cx.sb("ob", [128, 512], BF16)
        psS = [cx.ps("psS%d" % i, [128, 1024]) for i in range(2)]
        pSkeys = [("psS", i) for i in range(2)]
        ps = [psS[0][:, 0:512], psS[0][:, 512:1024], psS[1][:, 0:512], cx.ps("ps3", [128, 512]), cx.ps("ps4", [128, 512]), None,
              cx.ps("ps6", [128, 512]), cx.ps("ps7", [128, 512])]
        pk = [("psS", 0), ("psS", 0), ("psS", 1), ("ps", 3), ("ps", 4), ("ps", 5), ("ps", 6), ("ps", 7)]

        S.op("sp", "dma_start", out=identf[:], in_=cstf_d, w=["identf"], dma=True)
        S.op("sp", "dma_start", out=cstb[:], in_=cstb_d, w=["cstb"], dma=True)
        S.op("pool", "dma_start", out=wqkv[:], in_=wqkv_d.rearrange("(c p) a e -> p c a e", p=128), w=["wqkv"], dma=True)
        S.op("dve", "memset", Va[:, :, :, 64:65], 1.0, w=["Va"])
        S.op("dve", "memset", qz[0][64:128, :], 0.0, w=["qz0"])
        S.op("dve", "memset", qz[1][0:64, :], 0.0, w=["qz1"])
        S.op("dve", "memset", kmblk[:], 0.0, w=["kmblk"])

        fm = lambda ap: ap.rearrange("(c p) t -> p c t", p=128)

        def load_seg(b, s, slot):
            t0 = b * ntok_b + s * 512
            S.op("pool", "dma_start", out=xq[slot][:], in_=fm(xT[:, t0:t0 + 512]), w=[("xq", slot)], dma=True)
            if separate_kv:
                S.op("pool", "dma_start", out=xkv[slot][:], in_=fm(xkvT[:, t0:t0 + 512]), w=[("xkv", slot)], dma=True)

        segs = [(b, s) for b in range(nb) for s in range(nseg)]
        kvk = (lambda slot: ("xkv", slot)) if separate_kv else (lambda slot: ("xq", slot))
        load_seg(0, 0, 0)
        scount = 0
        pcount = 0
        for b in range(nb):
            S.op("dve", "memset", gsb[:], NEG, w=["gsb"])
            S.op("dve", "memset", km[:], 0.0, w=["km"])
            for s in range(nseg if "cut0" not in dbg else 0):
                si = b * nseg + s
                slot = si % 2
                if si + 1 < len(segs):
                    load_seg(segs[si + 1][0], segs[si + 1][1], 1 - slot)
                ss = slice(s * 512, (s + 1) * 512)
                for k in range(KC):
                    S.op("pe", "matmul", ps[0][:], wqkv[:, k, 1, :], xkv[slot][:, k, :], start=(k == 0), stop=(k == KC - 1),
                         r=["wqkv", kvk(slot)], w=[pk[0]])
                for hb2 in range(2):
                    S.op("act", "activation", out=KT[:, s * 512 + hb2 * BLK:s * 512 + (hb2 + 1) * BLK], in_=ps[0][:, hb2 * BLK:(hb2 + 1) * BLK],
                         func=AF.Copy, accum_out=km[:, 2 * s + hb2:2 * s + hb2 + 1], r=[pk[0]], w=["KT", "km"])
                if "cut1" in dbg:
                    continue
                for tt in range(4):
                    for k in range(KC):
                        S.op("pe", "matmul", ps[1][:, tt * 128:(tt + 1) * 128], xkv[slot][:, k, tt * 128:(tt + 1) * 128], wqkv[:, k, 2, :],
                             start=(k == 0), stop=(k == KC - 1), r=["wqkv", kvk(slot)], w=[pk[1]])
                S.op("act", "copy", out=Va[:, 4 * s:4 * s + 4, :, 0:64], in_=ps[1][:].rearrange("p (t h d) -> p t h d", h=2, d=64),
                     r=[pk[1]], w=["Va"])
                if "cut2" in dbg:
                    continue
                for k in range(KC):
                    S.op("pe", "matmul", ps[2][:], wqkv[:, k, 0, :], xq[slot][:, k, :], start=(k == 0), stop=(k == KC - 1),
                         r=["wqkv", ("xq", slot)], w=[pk[2]])
                S.op("act", "copy", out=qf[:, ss], in_=ps[2][:], r=[pk[2]], w=["qf"])
                S.op("dve", "tensor_copy", out=qz[0][0:64, ss], in_=qf[0:64, ss], r=["qf"], w=["qz0"])
                S.op("dve", "tensor_copy", out=qz[1][64:128, ss], in_=qf[64:128, ss], r=["qf"], w=["qz1"])
            for hh in range(2):
                hb = hh * 64
                S.op("dve", "tensor_scalar", kmblk[hb:hb + 64, hh, :], km[hb:hb + 64, :], 1.0 / BLK, None, ALU.mult, r=["km"], w=["kmblk"])
            def rec_gate1(qt):
                own = qt // 2
                qs = slice(qt * 128, (qt + 1) * 128)
                S.op("pe", "matmul", ps[3][:, 0:2 * nblkb], qf[:, qs], kmblk[:].rearrange("p a b -> p (a b)"), start=True, stop=True,
                     r=["qf", "kmblk"], w=[pk[3]])
                S.op("dve", "tensor_copy", out=gsb[:, :, 0:own], in_=ps[3][:, 0:2 * nblkb].rearrange("p (a b) -> p a b", b=nblkb)[:, :, 0:own],
                     r=[pk[3]], w=["gsb"])
                for hh in range(2):
                    S.op("dve", "max", out=top8[:, hh, :], in_=gsb[:, hh, :], r=["gsb"], w=["top8"])
                    S.op("dve", "tensor_scalar", mm1[:, hh, :], gsb[:, hh, :], top8[:, hh, 2:3], -1.0, ALU.is_ge, ALU.add,
                         r=["gsb", "top8"], w=["mm1"])

            def rec_gate2(qt):
                mt = mT[qt % 2]
                for hh in range(2):
                    S.op("pe", "transpose", ps[3][0:32, 256 + hh * 128:256 + (hh + 1) * 128], mm1[:, hh, :], identf[:], r=["mm1", "identf"], w=[pk[3]])
                S.op("act", "copy", out=mt[:].rearrange("p a b -> p (a b)"), in_=ps[3][0:32, 256:512], r=[pk[3]], w=[("mT", qt % 2)])

            items = []
            for qt in range(ntile if "proj_only" not in dbg else 0):
                for hh in range(2):
                    kts = list(range(0, qt + 1))
                    ngrp = (len(kts) + 7) // 8
                    pi_ = pcount % 2
                    pcount += 1
                    for gi in range(ngrp):
                        items.append(dict(qt=qt, hh=hh, gi=gi, ngrp=ngrp, grp=kts[gi * 8:(gi + 1) * 8], po=ps[6 + pi_], pok=pk[6 + pi_]))

            def rec_S(it):
                qt, hh, grp = it["qt"], it["hh"], it["grp"]
                own = qt // 2
                qs = slice(qt * 128, (qt + 1) * 128)
                pS, pSk = psS[it["sb"] % 2], pSkeys[it["sb"] % 2]
                pt, ptk = PT[it["sb"] % 3], ("PT", it["sb"] % 3)
                mt, mk = mT[qt % 2], ("mT", qt % 2)
                for j, kt in enumerate(grp):
                    nomask = (kt != qt and kt // 2 == own) or (kt != qt and "nogate" in dbg)
                    S.op("pe", "matmul", pS[:, j * 128:(j + 1) * 128], KT[:, kt * 128:(kt + 1) * 128], qz[hh][:, qs], start=True, stop=nomask,
                         r=["KT", "qz%d" % hh], w=[pSk])
                    if kt == qt:
                        S.op("pe", "matmul", pS[:, j * 128:(j + 1) * 128], identb, trib, start=False, stop=True, r=["cstb"], w=[pSk])
                    elif not nomask:
                        n = kt // 2
                        S.op("pe", "matmul", pS[:, j * 128:(j + 1) * 128], cstb[0:32, 256 + n * 128:256 + (n + 1) * 128], mt[:, hh, :],
                             start=False, stop=True, r=["cstb", mk], w=[pSk])
                w_ = len(grp) * 128
                S.op("act", "activation", out=pt[:, 0:w_], in_=pS[:, 0:w_], func=AF.Exp, scale=0.125, r=[pSk], w=[ptk])

            def rec_PV(it):
                qt, hh, gi, ngrp, grp, po, pok = it["qt"], it["hh"], it["gi"], it["ngrp"], it["grp"], it["po"], it["pok"]
                hb = hh * 64
                pt, ptk = PT[it["sb"] % 3], ("PT", it["sb"] % 3)
                for j, kt in enumerate(grp):
                    first = (gi == 0 and j == 0)
                    last = (gi == ngrp - 1 and j == len(grp) - 1)
                    S.op("pe", "matmul", po[:, 0:65], pt[:, j * 128:(j + 1) * 128], Va[:, kt, hh, :], start=first, stop=last,
                         r=[ptk, "Va"], w=[pok])
                if gi == ngrp - 1:
                    S.op("dve", "reciprocal", rs[:], po[:, 64:65], r=[pok], w=["rs"])
                    S.op("dve", "tensor_scalar", osb[:, hb:hb + 64], po[:, 0:64], rs[:, 0:1], None, ALU.mult, r=[pok, "rs"], w=["osb"])
                    if hh == 1:
                        S.op("pe", "transpose", ps[4][:, 0:128], osb[:], identf[:], r=["osb", "identf"], w=[pk[4]])
                        S.op("act", "copy", out=ob[:, (qt % 4) * 128:(qt % 4 + 1) * 128], in_=ps[4][:, 0:128], r=[pk[4]], w=["ob"])
                        if qt % 4 == 3:
                            t0 = b * ntok_b + (qt - 3) * 128
                            S.op("sp", "dma_start", out=mix_o[:, t0:t0 + 512], in_=ob[:], r=["ob"], dma=True)

            pending = []
            for it in items:
                it["sb"] = scount
                scount += 1
                rec_S(it)
                pending.append(it)
                if len(pending) > 1:
                    rec_PV(pending.pop(0))
                nq = it["qt"] + 1
                if "nogate" not in dbg and it["gi"] == 0 and nq < ntile and nq // 2 > 0:
                    if it["hh"] == 0:
                        rec_gate1(nq)
                    else:
                        rec_gate2(nq)
            for it in pending:
                rec_PV(it)
        S.emit()
    return nc


def moba_host_inputs(inp, j, core):
    cs = slice(core * 128, (core + 1) * 128)
    w = np.stack([inp["moba_w_q"][j][:, cs], inp["moba_w_k"][:, cs], inp["moba_w_v"][:, cs]], axis=1)
    m = {"wqkv": np.ascontiguousarray(w.astype(np.float32))}
    m.update(moba_consts())
    return m


_PROGS = {}
_DUMP = None
_NL = 4


def _prog(name, fn):
    if name not in _PROGS:
        _PROGS[name] = fn()
    return _PROGS[name]


def _run(nc, maps):
    res = run_bass_kernel_spmd(nc, maps, core_ids=list(range(NCORES)))
    return res.results


def _t_phase(inp, layer, mixT, hT):
    moe = (layer % 2 == 1)
    e = layer // 2
    nc = _prog("T_moe" if moe else "T_dense", lambda: build_T(moe))
    if layer < 2:
        w_o = inp["rwkv_w_out"][layer]
    else:
        w_o = inp["moba_w_o"][layer - 2]
    base = {"w_o": np.ascontiguousarray(w_o, dtype=np.float32), "lnp": lnp_layout(inp["ln_g"], inp["ln_b"], layer),
            "consts": make_consts()}
    if moe:
        base["router"] = np.ascontiguousarray(inp["moe_router"][e])
        base["w_gate"] = np.ascontiguousarray(inp["moe_w_gate"][e])
        base["w_up"] = np.ascontiguousarray(inp["moe_w_up"][e])
        base["w_down"] = np.ascontiguousarray(inp["moe_w_down"][e])
    else:
        base["w_gate"] = np.ascontiguousarray(inp["ffn_w_gate"][e][None])
        base["w_up"] = np.ascontiguousarray(inp["ffn_w_up"][e][None])
        base["w_down"] = np.ascontiguousarray(inp["ffn_w_down"][e][None])
    maps = []
    for c in range(NCORES):
        m = dict(base)
        m["mixT"] = np.ascontiguousarray(mixT[:, c * NT:(c + 1) * NT])
        m["hT"] = np.ascontiguousarray(hT[:, c * NT:(c + 1) * NT])
        maps.append(m)
    res = _run(nc, maps)
    return np.concatenate([r["outT"] for r in res], axis=1)


def kernel(**inp):
    inp = {k: np.asarray(v) for k, v in inp.items()}
    x = inp["x"].astype(np.float32, copy=False)
    hT = np.ascontiguousarray(x.reshape(NTOK, D).T)
    vfirst = None
    h_kv = None
    for layer in range(_NL):
        if layer < 2:
            nc = _prog("H_rwkv%d" % layer, lambda: build_H_rwkv2(layer == 1))
            maps = []
            for c in range(NCORES):
                m = rwkv_host_inputs(inp, layer, c)
                m["xT"] = hT
                if layer == 1:
                    m["vfirst"] = np.ascontiguousarray(vfirst[c * 128:(c + 1) * 128])
                maps.append(m)
            res = _run(nc, maps)
            if layer == 0:
                vfirst = np.concatenate([r["vfirst_out"] for r in res], axis=0)
        else:
            j = layer - 2
            nc = _prog("H_moba%d" % j, lambda: build_H_moba(j == 1))
            maps = []
            for c in range(NCORES):
                m = moba_host_inputs(inp, j, c)
                m["xT"] = hT
                if j == 1:
                    m["xkvT"] = h_kv
                maps.append(m)
            res = _run(nc, maps)
        mixT = np.concatenate([r["mix"] for r in res], axis=0)
        if _DUMP is not None:
            _DUMP["mix%d" % layer] = mixT
        hT = _t_phase(inp, layer, mixT, hT)
        if _DUMP is not None:
            _DUMP["h%d" % layer] = hT
        if layer == 1:
            h_kv = hT
    return np.ascontiguousarray(hT.T).reshape(B, T, D).astype(np.float32)
```

```python
import numpy as np
import ml_dtypes
import concourse.bass as bass
import concourse.mybir as mybir
from concourse.bass_utils import run_bass_kernel_spmd

F32 = mybir.dt.float32
BF16 = mybir.dt.bfloat16
ALU = mybir.AluOpType
AF = mybir.ActivationFunctionType
AX = mybir.AxisListType

NCORES = 8
D = 1024
KC = 8
B = 2
T = 8192
NTOK = B * T
NT = NTOK // NCORES
ALPHA = (2.0 * 4) ** 0.25
LN_EPS = 1e-5
GN_EPS = 64e-5


class Sched:
    ENG = ("pe", "act", "dve", "pool", "sp")
    NDMA = 8

    def __init__(self, nc):
        self.nc = nc
        self.ops = {e: [] for e in self.ENG}
        self.lastw = {}
        self.readers = {}
        self.alias = {}

    def add(self, eng, fn, reads=(), writes=(), dma=False):
        idx = len(self.ops[eng])
        deps = {}

        def dep(key, kind):
            if key is None:
                return
            if deps.get(key) != "raw":
                deps[key] = kind

        for r in reads:
            dep(self.lastw.get(r), "raw")
        for w in writes:
            dep(self.lastw.get(w), "waw")
            for k in self.readers.get(w, ()):
                dep(k, "war")
        waits = set()
        for (pe, pi), kind in deps.items():
            pdma = self.ops[pe][pi]["dma"]
            if pe == eng and not pdma and not dma:
                if eng == "pe":
                    continue
            waits.add((pe, pi))
        self.ops[eng].append(dict(fn=fn, waits=waits, dma=dma, signal=dma))
        me = (eng, idx)
        for r in reads:
            lst = self.readers.setdefault(r, [])
            if not dma:
                lst[:] = [k for k in lst if not (k[0] == eng and not self.ops[k[0]][k[1]]["dma"])]
            lst.append(me)
        for w in writes:
            self.lastw[w] = me
            self.readers[w] = []
        return me

    def op(self, eng, method, *args, r=(), w=(), dma=False, **kw):
        al = self.alias
        if al:
            r = [k if isinstance(k, tuple) else al.get(k, k) for k in r]
            w = [k if isinstance(k, tuple) else al.get(k, k) for k in w]
        return self.add(eng, lambda h: getattr(h, method)(*args, **kw), reads=r, writes=w, dma=dma)

    def emit(self):
        nc = self.nc
        ops = self.ops
        for e in self.ENG:
            for op in ops[e]:
                for (pe, pi) in op["waits"]:
                    ops[pe][pi]["signal"] = True
        import contextlib
        with contextlib.ExitStack() as st:
            csem = {e: st.enter_context(nc.semaphore("c_" + e)) for e in self.ENG}
            dsem = {e: [st.enter_context(nc.semaphore("d_%s%d" % (e, i))) for i in range(self.NDMA)]
                    for e in ("act", "pool", "sp")}
            final_dma = []
            for e in self.ENG:
                cnt = 0
                dcnt = [0] * self.NDMA
                k = 0
                for op in ops[e]:
                    if op["dma"]:
                        s = k % self.NDMA
                        k += 1
                        op["sem"] = dsem[e][s]
                        op["prev"] = dcnt[s]
                        dcnt[s] += 16
                        op["val"] = dcnt[s]
                    elif op["signal"]:
                        cnt += 1
                        op["sem"] = csem[e]
                        op["val"] = cnt
                if e in dsem:
                    for s in range(self.NDMA):
                        if dcnt[s]:
                            final_dma.append((dsem[e][s], dcnt[s]))
            block = st.enter_context(nc.Block())

            def run(e, h):
                waited = {}

                def wait(sem, val):
                    key = id(sem)
                    if waited.get(key, 0) >= val:
                        return
                    waited[key] = val
                    h.wait_ge(sem, val)

                for op in ops[e]:
                    for (pe, pi) in sorted(op["waits"]):
                        p = ops[pe][pi]
                        wait(p["sem"], p["val"])
                    if op["dma"] and op["prev"]:
                        wait(op["sem"], op["prev"])
                    ins = op["fn"](h)
                    if op["dma"]:
                        ins.then_inc(op["sem"], 16)
                    elif op["signal"]:
                        ins.then_inc(op["sem"], 1)
                if e == "sp":
                    for sem, val in final_dma:
                        wait(sem, val)

            @block.tensor
            def _(h):
                run("pe", h)

            @block.scalar
            def _(h):
                run("act", h)

            @block.vector
            def _(h):
                run("dve", h)

            @block.gpsimd
            def _(h):
                run("pool", h)

            @block.sync
            def _(h):
                run("sp", h)


def _bc(ap, shape):
    return ap.broadcast_to(shape)


class Ctx:
    def __init__(self, nc, es):
        self.nc = nc
        self.es = es
        self.S = Sched(nc)

    def sb(self, name, shape, dt):
        return self.es.enter_context(self.nc.sbuf_tensor(name, shape, dt))

    def ps(self, name, shape, dt=F32):
        return self.es.enter_context(self.nc.psum_tensor(name, shape, dt))

    def dram(self, name, shape, dt, kind):
        return self.nc.dram_tensor(name, list(shape), dt, kind=kind).ap()


def emit_layernorm(cx, zt, res_z, hb, res_hb, lnp, gi, bi, onesm, psA, psB, tmp, ngroups, scale_after=None):
    S = cx.S
    mean, msq, var = tmp["mean"], tmp["msq"], tmp["var"]
    for g in range(ngroups):
        gs = slice(g * 512, (g + 1) * 512)
        zkeys = [res_z(k, g) for k in range(KC)]
        for k in range(KC):
            sq = tmp["sq%d" % (k % 2)]
            S.op("act", "activation", out=sq[:], in_=zt[:, k, gs], func=AF.Square, r=[zkeys[k]], w=[("sq", k % 2)])
            S.op("pe", "matmul", psA[:], onesm, zt[:, k, gs], start=(k == 0), stop=(k == KC - 1),
                 r=[zkeys[k], "onesm"], w=["psA"])
            S.op("pe", "matmul", psB[:], onesm, sq[:], start=(k == 0), stop=(k == KC - 1),
                 r=[("sq", k % 2), "onesm"], w=["psB"])
        S.op("act", "copy", out=mean[:], in_=psA[:], r=["psA"], w=["mean"])
        S.op("dve", "tensor_tensor", msq[:], mean[:], mean[:], ALU.mult, r=["mean"], w=["msq"])
        S.op("dve", "tensor_tensor", var[:], psB[:], msq[:], ALU.subtract, r=["psB", "msq"], w=["var"])
        S.op("act", "activation", out=var[:], in_=var[:], func=AF.Sqrt, bias=LN_EPS, scale=1.0, r=["var"], w=["var"])
        S.op("dve", "reciprocal", msq[:], var[:], r=["var"], w=["msq"])
        z3 = zt[:, :, gs]
        S.op("dve", "tensor_tensor", z3, z3, _bc(mean[:].unsqueeze(1), [128, KC, 512]), ALU.subtract,
             r=zkeys + ["mean"], w=zkeys)
        S.op("dve", "tensor_tensor", z3, z3, _bc(msq[:].unsqueeze(1), [128, KC, 512]), ALU.mult,
             r=zkeys + ["msq"], w=zkeys)
        for k in range(KC):
            S.op("act", "activation", out=zt[:, k, gs], in_=zt[:, k, gs], func=AF.Identity,
                 scale=lnp[:, gi, k:k + 1], bias=lnp[:, bi, k:k + 1], r=[zkeys[k], "lnp"], w=[zkeys[k]])
        if hb is not None:
            hkeys = [res_hb(k, g) for k in range(KC)]
            S.op("pool", "tensor_copy", out=hb[:, :, gs], in_=z3, r=zkeys, w=hkeys)
        if scale_after is not None:
            S.op("pool", "tensor_scalar", z3, z3, float(scale_after), None, ALU.mult, r=zkeys, w=zkeys)


def build_T(moe, nt=NT, dff=None, nexp=None):
    import contextlib
    G = nt // 512
    FB = 4
    if dff is None:
        dff = 3584 if moe else 2816
    if nexp is None:
        nexp = 8 if moe else 1
    nfc = dff // 128
    nblk = (nfc + FB - 1) // FB
    ntile = nt // 128
    nc = bass.Bass("TRN2", target_bir_lowering=False)
    with contextlib.ExitStack() as es:
        cx = Ctx(nc, es)
        S = cx.S
        mixT = cx.dram("mixT", [D, nt], BF16, "ExternalInput")
        hT = cx.dram("hT", [D, nt], F32, "ExternalInput")
        w_o = cx.dram("w_o", [D, D], F32, "ExternalInput")
        lnp_d = cx.dram("lnp", [128, 4, KC], F32, "ExternalInput")
        consts = cx.dram("consts", [128, 128 + 128 + 8 * 128], F32, "ExternalInput")
        if moe:
            router = cx.dram("router", [D, 8], F32, "ExternalInput")
        wg_d = cx.dram("w_gate", [nexp, D, dff], F32, "ExternalInput")
        wu_d = cx.dram("w_up", [nexp, D, dff], F32, "ExternalInput")
        wd_d = cx.dram("w_down", [nexp, dff, D], F32, "ExternalInput")
        outT = cx.dram("outT", [D, nt], F32, "ExternalOutput")
        outTb = cx.dram("outTb", [D, nt], BF16, "ExternalOutput")

        acc = cx.sb("acc", [128, KC, nt], F32)
        hb = cx.sb("hb", [128, KC, nt], BF16)
        wgu = cx.sb("wgu", [128, 4, KC, 512], BF16)
        wdn = cx.sb("wdn", [128, 2, FB, D], BF16)
        actb = cx.sb("actb", [128, FB, nt], BF16)
        cst = cx.sb("cst", [128, 128 + 128 + 8 * 128], F32)
        lnp = cx.sb("lnp_sb", [128, 4, KC], F32)
        tmp = {n: cx.sb(n, [128, 512], F32) for n in ("sq0", "sq1", "mean", "msq", "var", "sl0", "sl1", "t20", "t21")}
        onesm = cst[:, 0:128]
        ident = cst[:, 128:256]
        ps = [cx.ps("ps%d" % i, [128, 512]) for i in range(8)]
        pkey = [("ps", i) for i in range(6)] + ["psA", "psB"]
        if moe:
            rt = cx.sb("rt", [128, KC, 8], F32)
            gbs = [cx.sb("gb0", [128, nt], F32)] * 2
            lg = cx.sb("lg", [128, ntile, 8], F32)
            lg2 = cx.sb("lg2", [128, ntile, 8], F32)
            lgm = cx.sb("lgm", [128, ntile, 8], F32)
            m1 = cx.sb("m1", [128, ntile], F32)
            m2 = cx.sb("m2", [128, ntile], F32)
            GT = cx.sb("GT", [8, nt], F32)

        zk = lambda k, g: ("acc", k, g)
        hk = lambda k, g: ("hb", k, g)
        allz = lambda g: [zk(k, g) for k in range(KC)]
        allh = lambda g: [hk(k, g) for k in range(KC)]
        fm = lambda ap: ap.rearrange("(c p) t -> p c t", p=128)

        S.op("sp", "dma_start", out=cst[:], in_=consts, w=["onesm", "ident", "sel"], dma=True)
        S.op("sp", "dma_start", out=lnp[:], in_=lnp_d, w=["lnp"], dma=True)
        for g in range(G):
            gs = slice(g * 512, (g + 1) * 512)
            S.op("sp", "dma_start", out=hb[:, :, gs], in_=fm(mixT[:, gs]), w=allh(g), dma=True)
        for s in range(2):
            S.op("pool", "dma_start", out=wgu[:, s], in_=fm(w_o[:, s * 512:(s + 1) * 512]), w=[("wgu", s)], dma=True)
        for g in range(G):
            gs = slice(g * 512, (g + 1) * 512)
            S.op("sp", "dma_start", out=acc[:, :, gs], in_=fm(hT[:, gs]), w=allz(g), dma=True)
        if moe:
            S.op("sp", "dma_start", out=rt[:], in_=fm(router), w=["rt"], dma=True)

        blocks = [(e, b) for e in range(nexp) for b in range(nblk)]

        def load_block(i):
            e, b = blocks[i]
            st = i % 2
            f0 = b * FB * 128
            nf = min(FB * 128, dff - f0)
            S.op("pool", "dma_start", out=wgu[:, 2 * st, :, 0:nf], in_=fm(wg_d[e, :, f0:f0 + nf]), w=[("wgu", 2 * st)], dma=True)
            S.op("pool", "dma_start", out=wgu[:, 2 * st + 1, :, 0:nf], in_=fm(wu_d[e, :, f0:f0 + nf]), w=[("wgu", 2 * st + 1)], dma=True)
            S.op("pool", "dma_start", out=wdn[:, st, 0:nf // 128, :], in_=fm(wd_d[e, f0:f0 + nf, :]), w=[("wdn", st)], dma=True)

        pi = 0
        for g in range(G):
            gs = slice(g * 512, (g + 1) * 512)
            for ec in range(KC):
                pt, pk = ps[pi % 2], pkey[pi % 2]
                pi += 1
                s, off = divmod(ec * 128, 512)
                for k in range(KC):
                    S.op("pe", "matmul", pt[:], wgu[:, s, k, off:off + 128], hb[:, k, gs], start=(k == 0), stop=(k == KC - 1),
                         r=[("wgu", s), hk(k, g)], w=[pk])
                S.op("dve", "scalar_tensor_tensor", out=acc[:, ec, gs], in0=acc[:, ec, gs], scalar=float(ALPHA), in1=pt[:],
                     op0=ALU.mult, op1=ALU.add, r=[zk(ec, g), pk], w=[zk(ec, g)])
        load_block(0)
        emit_layernorm(cx, acc, zk, hb, hk, lnp, 0, 1, onesm, ps[6], ps[7], tmp, G)
        if len(blocks) > 1:
            load_block(1)
        if moe:
            lp = ps[6]
            for j in range(ntile):
                for k in range(KC):
                    S.op("pe", "matmul", lp[:, j * 8:(j + 1) * 8], acc[:, k, j * 128:(j + 1) * 128], rt[:, k, :],
                         start=(k == 0), stop=(k == KC - 1), r=[zk(k, j // 4), "rt"], w=["psA"])
            S.op("act", "copy", out=lg[:].rearrange("p a b -> p (a b)"), in_=lp[:, 0:ntile * 8], r=["psA"], w=["lg"])
            bc3 = lambda t: _bc(t[:].unsqueeze(2), [128, ntile, 8])
            S.op("dve", "tensor_reduce", out=m1[:], in_=lg[:], axis=AX.X, op=ALU.max, r=["lg"], w=["m1"])
            S.op("dve", "tensor_tensor", lgm[:], lg[:], bc3(m1), ALU.is_ge, r=["lg", "m1"], w=["lgm"])
            S.op("dve", "scalar_tensor_tensor", out=lg2[:], in0=lgm[:], scalar=-1e30, in1=lg[:], op0=ALU.mult, op1=ALU.add,
                 r=["lgm", "lg"], w=["lg2"])
            S.op("dve", "tensor_reduce", out=m2[:], in_=lg2[:], axis=AX.X, op=ALU.max, r=["lg2"], w=["m2"])
            S.op("dve", "tensor_tensor", lgm[:], lg[:], bc3(m2), ALU.is_ge, r=["lg", "m2"], w=["lgm"])
            S.op("dve", "tensor_tensor", lg2[:], lg[:], bc3(m1), ALU.subtract, r=["lg", "m1"], w=["lg2"])
            S.op("act", "activation", out=lg2[:], in_=lg2[:], func=AF.Exp, r=["lg2"], w=["lg2"])
            S.op("dve", "tensor_tensor", lg2[:], lg2[:], lgm[:], ALU.mult, r=["lg2", "lgm"], w=["lg2"])
            S.op("dve", "tensor_reduce", out=m1[:], in_=lg2[:], axis=AX.X, op=ALU.add, r=["lg2"], w=["m1"])
            S.op("dve", "reciprocal", m2[:], m1[:], r=["m1"], w=["m2"])
            S.op("dve", "tensor_tensor", lg[:], lg2[:], bc3(m2), ALU.mult, r=["lg2", "m2"], w=["lg"])
            for q in range(ntile // 4):
                pt = ps[7]
                for jj in range(4):
                    j = q * 4 + jj
                    S.op("pe", "transpose", pt[0:8, jj * 128:(jj + 1) * 128], lg[:, j, :], ident, r=["lg", "ident"], w=["psB"])
                S.op("act", "copy", out=GT[:, q * 512:(q + 1) * 512], in_=pt[0:8, :], r=["psB"], w=["GT"])
        for g in range(G):
            gs = slice(g * 512, (g + 1) * 512)
            S.op("pool", "tensor_scalar", acc[:, :, gs], acc[:, :, gs], float(ALPHA), None, ALU.mult, r=allz(g), w=allz(g))

        tcount = 0
        gb = gk = None
        for i, (e, b) in enumerate(blocks):
            st = i % 2
            f0 = b * FB * 128
            nf = min(FB * 128, dff - f0)
            nfb = nf // 128
            if moe and b == 0:
                gb = gbs[0]
                gk = ("gb", 0)
                for q in range(G):
                    pt, pk = ps[6 + (q % 2)], pkey[6 + (q % 2)]
                    S.op("pe", "matmul", pt[:], cst[0:8, 256 + e * 128:256 + (e + 1) * 128], GT[:, q * 512:(q + 1) * 512],
                         start=True, stop=True, r=["sel", "GT"], w=[pk])
                    S.op("act", "copy", out=gb[:, q * 512:(q + 1) * 512], in_=pt[:], r=[pk], w=[gk])
            for g in range(G):
                gs = slice(g * 512, (g + 1) * 512)
                for fc in range(nfb):
                    c2 = tcount % 2
                    tcount += 1
                    pg, pu, kg, ku = ps[c2], ps[2 + c2], pkey[c2], pkey[2 + c2]
                    sl, t2 = tmp["sl%d" % c2], tmp["t2%d" % c2]
                    ksl, kt2 = ("sl", c2), ("t2", c2)
                    for k in range(KC):
                        S.op("pe", "matmul", pg[:], wgu[:, 2 * st, k, fc * 128:(fc + 1) * 128], hb[:, k, gs],
                             start=(k == 0), stop=(k == KC - 1), r=[("wgu", 2 * st), hk(k, g)], w=[kg])
                    for k in range(KC):
                        S.op("pe", "matmul", pu[:], wgu[:, 2 * st + 1, k, fc * 128:(fc + 1) * 128], hb[:, k, gs],
                             start=(k == 0), stop=(k == KC - 1), r=[("wgu", 2 * st + 1), hk(k, g)], w=[ku])
                    S.op("act", "activation", out=sl[:], in_=pg[:], func=AF.Silu, r=[kg], w=[ksl])
                    if moe:
                        S.op("dve", "tensor_tensor", t2[:], sl[:], pu[:], ALU.mult, r=[ksl, ku], w=[kt2])
                        S.op("dve", "tensor_tensor", actb[:, fc, gs], t2[:], gb[:, gs], ALU.mult, r=[kt2, gk], w=[("act", fc, g)])
                    else:
                        S.op("dve", "tensor_tensor", actb[:, fc, gs], sl[:], pu[:], ALU.mult, r=[ksl, ku], w=[("act", fc, g)])
            for g in range(G):
                gs = slice(g * 512, (g + 1) * 512)
                for ec in range(KC):
                    pd, kd = ps[4 + (ec % 2)], pkey[4 + (ec % 2)]
                    for fc in range(nfb):
                        S.op("pe", "matmul", pd[:], wdn[:, st, fc, ec * 128:(ec + 1) * 128], actb[:, fc, gs],
                             start=(fc == 0), stop=(fc == nfb - 1), r=[("wdn", st), ("act", fc, g)], w=[kd])
                    S.op("dve", "tensor_tensor", acc[:, ec, gs], acc[:, ec, gs], pd[:], ALU.add, r=[zk(ec, g), kd], w=[zk(ec, g)])
            if i + 2 < len(blocks):
                load_block(i + 2)

        emit_layernorm(cx, acc, zk, hb, hk, lnp, 2, 3, onesm, ps[6], ps[7], tmp, G)
        for g in range(G):
            gs = slice(g * 512, (g + 1) * 512)
            S.op("sp", "dma_start", out=fm(outT[:, gs]), in_=acc[:, :, gs], r=allz(g), dma=True)
            S.op("sp", "dma_start", out=fm(outTb[:, gs]), in_=hb[:, :, gs], r=allh(g), dma=True)
        S.emit()
    return nc


def make_consts():
    c = np.zeros((128, 128 + 128 + 8 * 128), np.float32)
    c[:, 0:128] = 1.0 / D
    c[:, 128:256] = np.eye(128, dtype=np.float32)
    for e in range(8):
        c[e, 256 + e * 128:256 + (e + 1) * 128] = 1.0
    return c


def lnp_layout(ln_g, ln_b, layer):
    out = np.zeros((128, 4, KC), np.float32)
    out[:, 0] = ln_g[layer, 0].reshape(KC, 128).T
    out[:, 1] = ln_b[layer, 0].reshape(KC, 128).T
    out[:, 2] = ln_g[layer, 1].reshape(KC, 128).T
    out[:, 3] = ln_b[layer, 1].reshape(KC, 128).T
    return out


SEG = 512
NCH = SEG // 64
NBLK = 2 * NCH
NPROJ = 704
DECAY_SCALE = -float(np.exp(-0.5))


def rwkv_consts():
    c = np.zeros((128, 128 + 128 + 128 + 64 + 64), np.float32)
    c[:, 0:128] = np.eye(128, dtype=np.float32)
    c[0:64, 128:192] = 1.0
    c[64:128, 192:256] = 1.0
    j = np.arange(64)[:, None]
    i = np.arange(64)[None, :]
    c[0:64, 256:320] = (j < i)
    c[0:64, 320:384] = (j <= i)
    c[0:64, 384:448] = (i < j)
    c[0:64, 448:512] = np.eye(64, dtype=np.float32)
    return c


def build_H_rwkv(layer1, ntok_b=T, nb=B, stop_after=99):
    import contextlib
    nseg = ntok_b // SEG
    ntot = nb * ntok_b
    nc = bass.Bass("TRN2", target_bir_lowering=False)
    with contextlib.ExitStack() as es:
        cx = Ctx(nc, es)
        S = cx.S
        xT = cx.dram("xT", [D, ntot], F32, "ExternalInput")
        wproj = cx.dram("wproj", [D, NPROJ], F32, "ExternalInput")
        mu_d = cx.dram("mu", [128, KC, NPROJ], F32, "ExternalInput")
        w2_d = cx.dram("w2", [128, 3, 128], F32, "ExternalInput")
        par_d = cx.dram("par", [128, 8], F32, "ExternalInput")
        cst_d = cx.dram("cst", [128, 512], F32, "ExternalInput")
        if layer1:
            vf_d = cx.dram("vfirst", [128, ntot], F32, "ExternalInput")
        else:
            vf_o = cx.dram("vfirst_out", [128, ntot], F32, "ExternalOutput")
        mix_o = cx.dram("mix", [128, ntot], BF16, "ExternalOutput")

        f32t = lambda n, shp=(128, SEG): cx.sb(n, list(shp), F32)
        xb = [cx.sb("xb%d" % i, [128, KC, SEG + 1], BF16) for i in range(2)]
        Wc = cx.sb("Wc", [128, KC, 2, NPROJ], BF16)
        wst = cx.sb("wst", [128, NPROJ], F32)
        must = cx.sb("must", [128, NPROJ], F32)
        w2f = cx.sb("w2f", [128, 3, 128], F32)
        w2b = cx.sb("w2b", [128, 3, 128], BF16)
        par = cx.sb("par_sb", [128, 8], F32)
        cst = cx.sb("cst_sb", [128, 512], F32)
        ident = cst[:, 0:128]
        bones = cst[:, 128:256]
        m_su = cst[0:64, 256:320]
        m_iu = cst[0:64, 320:384]
        m_ak = cst[0:64, 256:384]
        m_sl = cst[0:64, 384:448]
        id64 = cst[0:64, 448:512]
        rT, kT, vT, aT, wT, gT, av, bv, P, rP, t1, t2, d0, d1 = [f32t(n) for n in
            ("rT", "kT", "vT", "aT", "wT", "gT", "av", "bv", "P", "rP", "t1", "t2", "d0", "d1")]
        lor = cx.sb("lor", [128, 3, SEG], BF16)
        AR = cx.sb("AR", [128, NCH, 2, 64], F32)
        KBf = cx.sb("KBf", [128, NCH, 2, 64], F32)
        KBh = cx.sb("KBh", [128, NCH, 2, 64], F32)
        ARblk = cx.sb("ARblk", [128, NCH, 2, 128], F32)
        Bblk = cx.sb("Bblk", [128, NCH, 2, 64], F32)
        Vtm = cx.sb("Vtm", [64, NCH, 128], F32)
        KBtm = cx.sb("KBtm", [64, NCH, 2, 128], F32)
        Ytm = cx.sb("Ytm", [64, NCH, 128], F32)
        AK = cx.sb("AK", [64, NBLK, 128], F32)
        AB = cx.sb("AB", [64, NBLK, 128], F32)
        Mp = [cx.sb("Mp%d" % i, [64, NBLK, 64], F32) for i in range(2)]
        Np = [cx.sb("Np%d" % i, [64, NBLK, 64], F32) for i in range(2)]
        Q = cx.sb("Q", [64, NBLK, 64], F32)
        H = cx.sb("Hst", [128, 128], F32)
        Xs = cx.sb("Xs", [64, 128], F32)
        Us = cx.sb("Us", [64, 128], F32)
        yT = f32t("yT")
        ob = cx.sb("ob", [128, SEG], BF16)
        ps = [cx.ps("ps%d" % i, [128, 512]) for i in range(8)]
        pk = [("ps", i) for i in range(8)]

        S.op("sp", "dma_start", out=cst[:], in_=cst_d, w=["cst"], dma=True)
        S.op("sp", "dma_start", out=par[:], in_=par_d, w=["par"], dma=True)
        S.op("sp", "dma_start", out=w2f[:], in_=w2_d, w=["w2f"], dma=True)
        S.op("dve", "tensor_copy", out=w2b[:], in_=w2f[:], r=["w2f"], w=["w2b"])
        S.op("dve", "memset", ARblk[:], 0.0, w=["ARblk"])
        S.op("dve", "memset", Bblk[:], 0.0, w=["Bblk"])
        for k in range(KC):
            S.op("sp", "dma_start", out=wst[:], in_=wproj[k * 128:(k + 1) * 128, :], w=["wst"], dma=True)
            S.op("sp", "dma_start", out=must[:], in_=mu_d[:, k, :], w=["must"], dma=True)
            S.op("dve", "tensor_tensor", must[:], wst[:], must[:], ALU.mult, r=["wst", "must"], w=["must"])
            S.op("dve", "tensor_copy", out=Wc[:, k, 1, :], in_=must[:], r=["must"], w=["Wc"])
            S.op("dve", "tensor_tensor", Wc[:, k, 0, :], wst[:], must[:], ALU.subtract, r=["wst", "must"], w=["Wc"])

        def load_x(b, s, slot):
            t0 = b * ntok_b + s * SEG
            xk = ("xb", slot)
            if s == 0:
                S.op("dve", "memset", xb[slot][:, :, 0:1], 0.0, w=[xk])
                S.op("pool", "dma_start", out=xb[slot][:, :, 1:SEG + 1], in_=xT[:, t0:t0 + SEG].rearrange("(c p) t -> p c t", p=128),
                     w=[xk], dma=True)
            else:
                S.op("pool", "dma_start", out=xb[slot][:, :, 0:SEG + 1], in_=xT[:, t0 - 1:t0 + SEG].rearrange("(c p) t -> p c t", p=128),
                     w=[xk], dma=True)

        segs = [(b, s) for b in range(nb) for s in range(nseg)]
        load_x(0, 0, 0)
        pcount = [0]

        def proj(slot, c0, ncol, key):
            i = pcount[0] % 2
            pcount[0] += 1
            n = 0
            for k in range(KC):
                for sft in range(2):
                    S.op("pe", "matmul", ps[i][0:ncol, :], Wc[:, k, sft, c0:c0 + ncol], xb[slot][:, k, (1 - sft):(1 - sft) + SEG],
                         start=(n == 0), stop=(n == 2 * KC - 1), r=["Wc", ("xb", slot)], w=[pk[i]])
                    n += 1
            return ps[i], pk[i]

        col = lambda j: par[:, j:j + 1]
        for si, (b, s) in enumerate(segs):
            slot = si % 2
            t0 = b * ntok_b + s * SEG
            if si + 1 < len(segs):
                load_x(segs[si + 1][0], segs[si + 1][1], 1 - slot)
            if s == 0:
                S.op("dve", "memset", H[:], 0.0, w=["H"])
            p, k_ = proj(slot, 0, 128, None)
            S.op("act", "copy", out=rT[:], in_=p[:], r=[k_], w=["rT"])
            p, k_ = proj(slot, 128, 128, None)
            S.op("act", "copy", out=kT[:], in_=p[:], r=[k_], w=["kT"])
            p, k_ = proj(slot, 256, 128, None)
            S.op("act", "copy", out=vT[:], in_=p[:], r=[k_], w=["vT"])
            p, k_ = proj(slot, 384, 128, None)
            S.op("act", "activation", out=lor[0:64, 0, :], in_=p[0:64, :], func=AF.Tanh, r=[k_], w=["lor0a"])
            S.op("act", "copy", out=lor[64:128, 0, :], in_=p[64:128, :], r=[k_], w=["lor0b"])
            p, k_ = proj(slot, 512, 128, None)
            S.op("act", "activation", out=lor[:, 1, :], in_=p[:], func=AF.Sigmoid, r=[k_], w=["lor1"])
            p, k_ = proj(slot, 640, 64, None)
            S.op("act", "activation", out=lor[0:32, 2, :], in_=p[0:32, :], func=AF.Sigmoid, r=[k_], w=["lor2a"])
            S.op("act", "copy", out=lor[32:64, 2, :], in_=p[32:64, :], r=[k_], w=["lor2b"])
            i = pcount[0] % 2; pcount[0] += 1
            S.op("pe", "matmul", ps[i][:], w2b[0:64, 0, :], lor[0:64, 0, :], start=True, stop=True, r=["w2b", "lor0a"], w=[pk[i]])
            S.op("act", "activation", out=wT[:], in_=ps[i][:], func=AF.Sigmoid, bias=col(0), scale=1.0, r=[pk[i], "par"], w=["wT"])
            S.op("act", "activation", out=wT[:], in_=wT[:], func=AF.Exp, scale=DECAY_SCALE, r=["wT"], w=["wT"])
            i = pcount[0] % 2; pcount[0] += 1
            S.op("pe", "matmul", ps[i][:], w2b[64:128, 0, :], lor[64:128, 0, :], start=True, stop=True, r=["w2b", "lor0b"], w=[pk[i]])
            S.op("act", "activation", out=aT[:], in_=ps[i][:], func=AF.Sigmoid, bias=col(1), scale=1.0, r=[pk[i], "par"], w=["aT"])
            i = pcount[0] % 2; pcount[0] += 1
            S.op("pe", "matmul", ps[i][:], w2b[:, 1, :], lor[:, 1, :], start=True, stop=False, r=["w2b", "lor1"], w=[pk[i]])
            S.op("pe", "matmul", ps[i][:], w2b[0:32, 2, :], lor[0:32, 2, :], start=False, stop=True, r=["w2b", "lor2a"], w=[pk[i]])
            S.op("act", "copy", out=gT[:], in_=ps[i][:], r=[pk[i]], w=["gT"])
            if layer1:
                i = pcount[0] % 2; pcount[0] += 1
                S.op("pe", "matmul", ps[i][:], w2b[32:64, 2, :], lor[32:64, 2, :], start=True, stop=True, r=["w2b", "lor2b"], w=[pk[i]])
                S.op("act", "activation", out=t1[:], in_=ps[i][:], func=AF.Sigmoid, bias=col(2), scale=1.0, r=[pk[i], "par"], w=["t1"])
                S.op("sp", "dma_start", out=t2[:], in_=vf_d[:, t0:t0 + SEG], w=["t2"], dma=True)
                S.op("dve", "tensor_tensor", t2[:], t2[:], vT[:], ALU.subtract, r=["t2", "vT"], w=["t2"])
                S.op("dve", "tensor_tensor", t2[:], t2[:], t1[:], ALU.mult, r=["t2", "t1"], w=["t2"])
                S.op("dve", "tensor_tensor", vT[:], vT[:], t2[:], ALU.add, r=["vT", "t2"], w=["vT"])
            else:
                S.op("sp", "dma_start", out=vf_o[:, t0:t0 + SEG], in_=vT[:], r=["vT"], dma=True)
            if stop_after == 0:
                break
            S.op("dve", "tensor_scalar", av[:], kT[:], col(3), None, ALU.mult, r=["kT", "par"], w=["av"])
            S.op("act", "activation", out=t1[:], in_=av[:], func=AF.Square, r=["av"], w=["t1"])
            i = pcount[0] % 2; pcount[0] += 1
            S.op("pe", "matmul", ps[i][:], bones, t1[:], start=True, stop=True, r=["cst", "t1"], w=[pk[i]])
            S.op("dve", "tensor_scalar", t2[:], ps[i][:], 1e-24, None, ALU.max, r=[pk[i]], w=["t2"])
            S.op("act", "activation", out=t2[:], in_=t2[:], func=AF.Sqrt, r=["t2"], w=["t2"])
            S.op("dve", "reciprocal", t2[:], t2[:], r=["t2"], w=["t2"])
            S.op("dve", "tensor_tensor", av[:], av[:], t2[:], ALU.mult, r=["av", "t2"], w=["av"])
            S.op("dve", "tensor_tensor", bv[:], av[:], aT[:], ALU.mult, r=["av", "aT"], w=["bv"])
            S.op("dve", "tensor_scalar", t1[:], aT[:], -1.0, col(4), ALU.add, ALU.mult, r=["aT", "par"], w=["t1"])
            S.op("dve", "scalar_tensor_tensor", out=kT[:], in0=t1[:], scalar=1.0, in1=kT[:], op0=ALU.add, op1=ALU.mult,
                 r=["t1", "kT"], w=["kT"])
            if stop_after == 1:
                break
            w3 = wT[:].rearrange("p (c j) -> p c j", j=64)
            S.op("pool", "tensor_copy", out=d0[:], in_=wT[:], r=["wT"], w=["d0"])
            S.op("pool", "memset", d0[:].rearrange("p (c j) -> p c j", j=64)[:, :, 0:1], 0.0, w=["d0"])
            S.op("pool", "memset", d1[:], 0.0, w=["d1"])
            S.op("pool", "tensor_copy", out=d1[:].rearrange("p (c j) -> p c j", j=64)[:, :, 0:1], in_=w3[:, :, 0:1], r=["wT"], w=["d1"])
            S.op("dve", "tensor_tensor_scan", P[:], d0[:], d1[:], 0.0, ALU.mult, ALU.add, r=["d0", "d1"], w=["P"])
            S.op("dve", "reciprocal", rP[:], P[:], r=["P"], w=["rP"])
            S.op("dve", "reciprocal", t1[:], wT[:], r=["wT"], w=["t1"])
            S.op("dve", "tensor_tensor", t1[:], t1[:], P[:], ALU.mult, r=["t1", "P"], w=["t1"])
            c3 = lambda t: t[:].rearrange("p (c j) -> p c j", j=64)
            S.op("dve", "scalar_tensor_tensor", out=AR[:, :, 0, :], in0=c3(av), scalar=-1.0, in1=c3(t1), op0=ALU.mult, op1=ALU.mult,
                 r=["av", "t1"], w=["AR"])
            S.op("pool", "tensor_tensor", AR[:, :, 1, :], c3(rT), c3(P), ALU.mult, r=["rT", "P"], w=["AR"])
            S.op("pool", "tensor_tensor", KBf[:, :, 0, :], c3(kT), c3(rP), ALU.mult, r=["kT", "rP"], w=["KBf"])
            S.op("dve", "tensor_tensor", KBf[:, :, 1, :], c3(bv), c3(rP), ALU.mult, r=["bv", "rP"], w=["KBf"])
            pend = c3(P)[:, :, 63:64]
            for q in range(2):
                S.op("dve" if q else "pool", "tensor_tensor", KBh[:, :, q, :], KBf[:, :, q, :], _bc(pend, [128, NCH, 64]), ALU.mult,
                     r=["KBf", "P"], w=["KBh"])
            if stop_after == 2:
                break
            for hh in range(2):
                hb = hh * 64
                S.op("pool", "tensor_copy", out=ARblk[hb:hb + 64, :, hh, :], in_=AR[hb:hb + 64, :, :, :].rearrange("p c a b -> p c (a b)"),
                     r=["AR"], w=["ARblk"])
                S.op("pool", "tensor_copy", out=Bblk[hb:hb + 64, :, hh, :], in_=KBf[hb:hb + 64, :, 1, :], r=["KBf"], w=["Bblk"])
            for half in range(2):
                for cc in range(4):
                    c = half * 4 + cc
                    S.op("pe", "transpose", ps[2][0:64, cc * 128:(cc + 1) * 128], vT[:, c * 64:(c + 1) * 64], ident,
                         r=["vT", "cst"], w=[pk[2]])
                S.op("act", "copy", out=Vtm[:, half * 4:(half + 1) * 4, :].rearrange("p c f -> p (c f)"), in_=ps[2][0:64, :], r=[pk[2]], w=["Vtm"])
            for q2 in range(4):
                for cc in range(2):
                    c = q2 * 2 + cc
                    for q in range(2):
                        S.op("pe", "transpose", ps[2][0:64, (cc * 2 + q) * 128:(cc * 2 + q + 1) * 128], KBh[:, c, q, :], ident,
                             r=["KBh", "cst"], w=[pk[2]])
                S.op("act", "copy", out=KBtm[:, q2 * 2:(q2 + 1) * 2, :, :].rearrange("p c q f -> p (c q f)"), in_=ps[2][0:64, :],
                     r=[pk[2]], w=["KBtm"])
            if stop_after == 3:
                break
            for c2 in range(NCH // 2):
                for which, dst in ((0, AK), (1, AB)):
                    for cc in range(2):
                        c = c2 * 2 + cc
                        S.op("pe", "matmul", ps[2][0:64, cc * 256:(cc + 1) * 256], KBf[:, c, which, :],
                             ARblk[:, c, :, :].rearrange("p a b -> p (a b)"), start=True, stop=True, r=["KBf", "ARblk"], w=[pk[2]])
                    S.op("dve", "tensor_tensor", dst[:, c2 * 4:(c2 + 1) * 4, :], ps[2][0:64, :].rearrange("p (a b) -> p a b", b=128),
                         _bc(m_ak.unsqueeze(1), [64, 4, 128]), ALU.mult, r=[pk[2], "cst"], w=["AK" if which == 0 else "AB"])
            for c4 in range(NCH // 4):
                for cc in range(4):
                    c = c4 * 4 + cc
                    S.op("pe", "matmul", ps[2][0:64, cc * 128:(cc + 1) * 128], AR[:, c, 0, :],
                         Bblk[:, c, :, :].rearrange("p a b -> p (a b)"), start=True, stop=True, r=["Bblk", "AR"], w=[pk[2]])
                S.op("dve", "tensor_tensor", Np[0][:, c4 * 8:(c4 + 1) * 8, :], ps[2][0:64, :].rearrange("p (a b) -> p a b", b=64),
                     _bc(m_sl.unsqueeze(1), [64, 8, 64]), ALU.mult, r=[pk[2], "cst"], w=[("Np0", c4)])
            if stop_after == 4:
                break
            S.op("pool", "tensor_copy", out=Mp[0][:], in_=AB[:, :, 0:64], r=["AB"], w=[("Mp0", 0), ("Mp0", 1)])
            S.op("dve", "tensor_tensor", Q[:], AB[:, :, 0:64], _bc(id64.unsqueeze(1), [64, NBLK, 64]), ALU.add, r=["AB", "cst"], w=[("Q", 0), ("Q", 1)])
            ibank = {0: (3, 4, 5), 1: (6, 7, 2)}
            for lvl in range(5):
                cur, nxt = lvl % 2, (lvl + 1) % 2
                for g8 in range(NBLK // 8):
                    bM, bN, bQ = ibank[g8]
                    for bi in range(8):
                        blk = g8 * 8 + bi
                        S.op("pe", "matmul", ps[bM][0:64, bi * 64:(bi + 1) * 64], Np[cur][:, blk, :], Mp[cur][:, blk, :],
                             start=True, stop=True, r=[("Np%d" % cur, g8), ("Mp%d" % cur, g8)], w=[pk[bM]])
                    for bi in range(8):
                        blk = g8 * 8 + bi
                        S.op("pe", "matmul", ps[bN][0:64, bi * 64:(bi + 1) * 64], Mp[cur][:, blk, :], Np[cur][:, blk, :],
                             start=True, stop=True, r=[("Np%d" % cur, g8), ("Mp%d" % cur, g8)], w=[pk[bN]])
                for g8 in range(NBLK // 8):
                    bM, bN, bQ = ibank[g8]
                    gsl = slice(g8 * 8, (g8 + 1) * 8)
                    S.op("act", "copy", out=Mp[nxt][:, gsl, :].rearrange("p a b -> p (a b)"), in_=ps[bM][0:64, :], r=[pk[bM]], w=[("Mp%d" % nxt, g8)])
                    S.op("dve", "tensor_copy", out=Np[nxt][:, gsl, :].rearrange("p a b -> p (a b)"), in_=ps[bN][0:64, :], r=[pk[bN]], w=[("Np%d" % nxt, g8)])
                for g8 in range(NBLK // 8):
                    bM, bN, bQ = ibank[g8]
                    for bi in range(8):
                        blk = g8 * 8 + bi
                        S.op("pe", "matmul", ps[bQ][0:64, bi * 64:(bi + 1) * 64], Np[nxt][:, blk, :], Q[:, blk, :],
                             start=True, stop=True, r=[("Np%d" % nxt, g8), ("Q", g8)], w=[pk[bQ]])
                for g8 in range(NBLK // 8):
                    bM, bN, bQ = ibank[g8]
                    gsl = slice(g8 * 8, (g8 + 1) * 8)
                    S.op("dve", "tensor_tensor", Q[:, gsl, :].rearrange("p a b -> p (a b)"), Q[:, gsl, :].rearrange("p a b -> p (a b)"),
                         ps[bQ][0:64, :], ALU.add, r=[("Q", g8), pk[bQ]], w=[("Q", g8)])
            if stop_after == 5:
                break
            for c in range(NCH):
                S.op("pe", "matmul", ps[3][0:64, 0:128], AR[:, c, 0, :], H[:], start=True, stop=False, r=["AR", "H"], w=[pk[3]])
                for hh in range(2):
                    hb = hh * 64
                    blk = 2 * c + hh
                    S.op("pe", "matmul", ps[3][0:64, hb:hb + 64], AK[:, blk, 0:64], Vtm[:, c, hb:hb + 64], start=False, stop=(hh == 1),
                         r=["AK", "Vtm"], w=[pk[3]])
                S.op("act", "copy", out=Xs[:], in_=ps[3][0:64, 0:128], r=[pk[3]], w=["Xs"])
                for hh in range(2):
                    hb = hh * 64
                    blk = 2 * c + hh
                    S.op("pe", "matmul", ps[4][0:64, hb:hb + 64], Q[:, blk, :], Xs[:, hb:hb + 64], start=True, stop=True,
                         r=[("Q", blk // 8), "Xs"], w=[pk[4]])
                S.op("act", "copy", out=Us[:], in_=ps[4][0:64, 0:128], r=[pk[4]], w=["Us"])
                S.op("pe", "matmul", ps[5][0:64, 0:128], AR[:, c, 1, :], H[:], start=True, stop=False, r=["AR", "H"], w=[pk[5]])
                for hh in range(2):
                    hb = hh * 64
                    blk = 2 * c + hh
                    S.op("pe", "matmul", ps[5][0:64, hb:hb + 64], AB[:, blk, 64:128], Us[:, hb:hb + 64], start=False, stop=False,
                         r=["AB", "Us"], w=[pk[5]])
                    S.op("pe", "matmul", ps[5][0:64, hb:hb + 64], AK[:, blk, 64:128], Vtm[:, c, hb:hb + 64], start=False, stop=(hh == 1),
                         r=["AK", "Vtm"], w=[pk[5]])
                S.op("act", "copy", out=Ytm[:, c, :], in_=ps[5][0:64, 0:128], r=[pk[5]], w=["Ytm"])
                S.op("pe", "matmul", ps[6][:, 0:128], KBtm[:, c, 0, :], Vtm[:, c, :], start=True, stop=False, r=["KBtm", "Vtm"], w=[pk[6]])
                S.op("pe", "matmul", ps[6][:, 0:128], KBtm[:, c, 1, :], Us[:], start=False, stop=True, r=["KBtm", "Us"], w=[pk[6]])
                for hh in range(2):
                    hb = hh * 64
                    S.op("dve", "scalar_tensor_tensor", out=H[hb:hb + 64, hb:hb + 64], in0=H[hb:hb + 64, hb:hb + 64],
                         scalar=c3(P)[hb:hb + 64, c, 63:64], in1=ps[6][hb:hb + 64, hb:hb + 64],
                         op0=ALU.mult, op1=ALU.add, r=["H", "P", pk[6]], w=["H"])
            if stop_after == 6:
                break
            for c in range(NCH):
                S.op("pe", "transpose", ps[7][:, c * 64:(c + 1) * 64], Ytm[:, c, :], id64, r=["Ytm", "cst"], w=[pk[7]])
            S.op("act", "copy", out=yT[:], in_=ps[7][:], r=[pk[7]], w=["yT"])
            i = pcount[0] % 2; pcount[0] += 1
            S.op("pe", "matmul", ps[i][:], bones, yT[:], start=True, stop=True, r=["cst", "yT"], w=[pk[i]])
            S.op("dve", "scalar_tensor_tensor", out=yT[:], in0=ps[i][:], scalar=-1.0 / 64, in1=yT[:], op0=ALU.mult, op1=ALU.add,
                 r=[pk[i], "yT"], w=["yT"])
            S.op("act", "activation", out=t1[:], in_=yT[:], func=AF.Square, r=["yT"], w=["t1"])
            i = pcount[0] % 2; pcount[0] += 1
            S.op("pe", "matmul", ps[i][:], bones, t1[:], start=True, stop=True, r=["cst", "t1"], w=[pk[i]])
            S.op("act", "activation", out=t2[:], in_=ps[i][:], func=AF.Sqrt, bias=GN_EPS, scale=1.0 / 64, r=[pk[i]], w=["t2"])
            S.op("dve", "reciprocal", t2[:], t2[:], r=["t2"], w=["t2"])
            S.op("dve", "tensor_tensor", yT[:], yT[:], t2[:], ALU.mult, r=["yT", "t2"], w=["yT"])
            S.op("act", "activation", out=yT[:], in_=yT[:], func=AF.Identity, scale=col(6), bias=col(7), r=["yT", "par"], w=["yT"])
            S.op("dve", "scalar_tensor_tensor", out=t1[:], in0=rT[:], scalar=col(5), in1=kT[:], op0=ALU.mult, op1=ALU.mult,
                 r=["rT", "kT", "par"], w=["t1"])
            i = pcount[0] % 2; pcount[0] += 1
            S.op("pe", "matmul", ps[i][:], bones, t1[:], start=True, stop=True, r=["cst", "t1"], w=[pk[i]])
            S.op("dve", "tensor_tensor", t2[:], ps[i][:], vT[:], ALU.mult, r=[pk[i], "vT"], w=["t2"])
            S.op("dve", "tensor_tensor", yT[:], yT[:], t2[:], ALU.add, r=["yT", "t2"], w=["yT"])
            S.op("dve", "tensor_tensor", ob[:], yT[:], gT[:], ALU.mult, r=["yT", "gT"], w=["ob"])
            S.op("sp", "dma_start", out=mix_o[:, t0:t0 + SEG], in_=ob[:], r=["ob"], dma=True)
        S.emit()
    return nc


def build_H_rwkv2(layer1, ntok_b=T, nb=B, stop_after=99):
    import contextlib
    nseg = ntok_b // SEG
    ntot = nb * ntok_b
    nc = bass.Bass("TRN2", target_bir_lowering=False)
    with contextlib.ExitStack() as es:
        cx = Ctx(nc, es)
        S = cx.S
        xT = cx.dram("xT", [D, ntot], F32, "ExternalInput")
        wproj = cx.dram("wproj", [D, NPROJ], F32, "ExternalInput")
        mu_d = cx.dram("mu", [128, KC, NPROJ], F32, "ExternalInput")
        w2_d = cx.dram("w2", [128, 3, 128], F32, "ExternalInput")
        par_d = cx.dram("par", [128, 8], F32, "ExternalInput")
        cst_d = cx.dram("cst", [128, 512], F32, "ExternalInput")
        if layer1:
            vf_d = cx.dram("vfirst", [128, ntot], F32, "ExternalInput")
        else:
            vf_o = cx.dram("vfirst_out", [128, ntot], F32, "ExternalOutput")
        mix_o = cx.dram("mix", [128, ntot], BF16, "ExternalOutput")

        f32t = lambda n, shp=(128, SEG): cx.sb(n, list(shp), F32)
        xb = [cx.sb("xb%d" % i, [128, KC, SEG + 1], BF16) for i in range(2)]
        Wc = cx.sb("Wc", [128, KC, 2, NPROJ], BF16)
        wst = cx.sb("wst", [128, NPROJ], F32)
        must = cx.sb("must", [128, NPROJ], F32)
        w2f = cx.sb("w2f", [128, 3, 128], F32)
        w2b = cx.sb("w2b", [128, 3, 128], BF16)
        par = cx.sb("par_sb", [128, 8], F32)
        cst = cx.sb("cst_sb", [128, 512], F32)
        ident = cst[:, 0:128]
        bones = cst[:, 128:256]
        m_su = cst[0:64, 256:320]
        m_iu = cst[0:64, 320:384]
        m_ak = cst[0:64, 256:384]
        m_sl = cst[0:64, 384:448]
        id64 = cst[0:64, 448:512]
        aT, wT, av, bv, rP, t1, t2, d0, d1 = [f32t(n) for n in
            ("aT", "wT", "av", "bv", "rP", "t1", "t2", "d0", "d1")]
        u1, u2 = d0, d1
        rTs, kTs, vTs, gTs, Ps = [[f32t("%s%d" % (n, i)) for i in range(2)] for n in ("rT", "kT", "vT", "gT", "P")]
        lor = cx.sb("lor", [128, 3, SEG], BF16)
        ARs = [cx.sb("AR%d" % i, [128, NCH, 2, 64], F32) for i in range(2)]
        KBf = cx.sb("KBf", [128, NCH, 2, 64], F32)
        KBh = cx.sb("KBh", [128, NCH, 2, 64], F32)
        ARblk = cx.sb("ARblk", [128, NCH, 2, 128], F32)
        Bblk = cx.sb("Bblk", [128, NCH, 2, 64], F32)
        Vtms = [cx.sb("Vtm%d" % i, [64, NCH, 128], F32) for i in range(2)]
        KBtms = [cx.sb("KBtm%d" % i, [64, NCH, 2, 128], F32) for i in range(2)]
        Ytm = cx.sb("Ytm", [64, NCH, 128], F32)
        AKs = [cx.sb("AK%d" % i, [64, NBLK, 128], F32) for i in range(2)]
        ABs = [cx.sb("AB%d" % i, [64, NBLK, 128], F32) for i in range(2)]
        Mp = [cx.sb("Mp%d" % i, [64, NBLK, 64], F32) for i in range(2)]
        Np = [cx.sb("Np%d" % i, [64, NBLK, 64], F32) for i in range(2)]
        Qs = [cx.sb("Q%d" % i, [64, NBLK, 64], F32) for i in range(2)]
        H = cx.sb("Hst", [128, 128], F32)
        Xs = cx.sb("Xs", [64, 128], F32)
        Us = cx.sb("Us", [64, 128], F32)
        yT = f32t("yT")
        ob = cx.sb("ob", [128, SEG], BF16)
        ps = [cx.ps("ps%d" % i, [128, 512]) for i in range(8)]
        pk = [("ps", i) for i in range(8)]

        S.op("sp", "dma_start", out=cst[:], in_=cst_d, w=["cst"], dma=True)
        S.op("sp", "dma_start", out=par[:], in_=par_d, w=["par"], dma=True)
        S.op("sp", "dma_start", out=w2f[:], in_=w2_d, w=["w2f"], dma=True)
        S.op("dve", "tensor_copy", out=w2b[:], in_=w2f[:], r=["w2f"], w=["w2b"])
        S.op("dve", "memset", ARblk[:], 0.0, w=["ARblk"])
        S.op("dve", "memset", Bblk[:], 0.0, w=["Bblk"])
        for k in range(KC):
            S.op("sp", "dma_start", out=wst[:], in_=wproj[k * 128:(k + 1) * 128, :], w=["wst"], dma=True)
            S.op("sp", "dma_start", out=must[:], in_=mu_d[:, k, :], w=["must"], dma=True)
            S.op("dve", "tensor_tensor", must[:], wst[:], must[:], ALU.mult, r=["wst", "must"], w=["must"])
            S.op("dve", "tensor_copy", out=Wc[:, k, 1, :], in_=must[:], r=["must"], w=["Wc"])
            S.op("dve", "tensor_tensor", Wc[:, k, 0, :], wst[:], must[:], ALU.subtract, r=["wst", "must"], w=["Wc"])

        def load_x(b, s, slot):
            t0 = b * ntok_b + s * SEG
            xk = ("xb", slot)
            if s == 0:
                S.op("dve", "memset", xb[slot][:, :, 0:1], 0.0, w=[xk])
                S.op("pool", "dma_start", out=xb[slot][:, :, 1:SEG + 1], in_=xT[:, t0:t0 + SEG].rearrange("(c p) t -> p c t", p=128),
                     w=[xk], dma=True)
            else:
                S.op("pool", "dma_start", out=xb[slot][:, :, 0:SEG + 1], in_=xT[:, t0 - 1:t0 + SEG].rearrange("(c p) t -> p c t", p=128),
                     w=[xk], dma=True)

        segs = [(b, s) for b in range(nb) for s in range(nseg)]
        load_x(0, 0, 0)
        pcount = [0]

        def proj(slot, c0, ncol, key):
            i = pcount[0] % 2
            pcount[0] += 1
            n = 0
            for k in range(KC):
                for sft in range(2):
                    S.op("pe", "matmul", ps[i][0:ncol, :], Wc[:, k, sft, c0:c0 + ncol], xb[slot][:, k, (1 - sft):(1 - sft) + SEG],
                         start=(n == 0), stop=(n == 2 * KC - 1), r=["Wc", ("xb", slot)], w=[pk[i]])
                    n += 1
            return ps[i], pk[i]

        col = lambda j: par[:, j:j + 1]
        def prep_gen(si):
            b, s = segs[si]
            p_ = si % 2
            rT, kT, vT, gT, P, AR, Vtm, KBtm, AK, AB, Q = (rTs[p_], kTs[p_], vTs[p_], gTs[p_], Ps[p_], ARs[p_], Vtms[p_], KBtms[p_],
                                                          AKs[p_], ABs[p_], Qs[p_])
            c3 = lambda t: t[:].rearrange("p (c j) -> p c j", j=64)
            yield
            slot = si % 2
            t0 = b * ntok_b + s * SEG
            if si + 1 < len(segs):
                load_x(segs[si + 1][0], segs[si + 1][1], 1 - slot)
            p, k_ = proj(slot, 0, 128, None)
            S.op("act", "copy", out=rT[:], in_=p[:], r=[k_], w=["rT"])
            yield
            p, k_ = proj(slot, 128, 128, None)
            S.op("act", "copy", out=kT[:], in_=p[:], r=[k_], w=["kT"])
            yield
            p, k_ = proj(slot, 256, 128, None)
            S.op("act", "copy", out=vT[:], in_=p[:], r=[k_], w=["vT"])
            yield
            p, k_ = proj(slot, 384, 128, None)
            S.op("act", "activation", out=lor[0:64, 0, :], in_=p[0:64, :], func=AF.Tanh, r=[k_], w=["lor0a"])
            S.op("act", "copy", out=lor[64:128, 0, :], in_=p[64:128, :], r=[k_], w=["lor0b"])
            yield
            p, k_ = proj(slot, 512, 128, None)
            S.op("act", "activation", out=lor[:, 1, :], in_=p[:], func=AF.Sigmoid, r=[k_], w=["lor1"])
            yield
            p, k_ = proj(slot, 640, 64, None)
            S.op("act", "activation", out=lor[0:32, 2, :], in_=p[0:32, :], func=AF.Sigmoid, r=[k_], w=["lor2a"])
            S.op("act", "copy", out=lor[32:64, 2, :], in_=p[32:64, :], r=[k_], w=["lor2b"])
            yield
            i = pcount[0] % 2; pcount[0] += 1
            S.op("pe", "matmul", ps[i][:], w2b[0:64, 0, :], lor[0:64, 0, :], start=True, stop=True, r=["w2b", "lor0a"], w=[pk[i]])
            S.op("act", "activation", out=wT[:], in_=ps[i][:], func=AF.Sigmoid, bias=col(0), scale=1.0, r=[pk[i], "par"], w=["wT"])
            S.op("act", "activation", out=wT[:], in_=wT[:], func=AF.Exp, scale=DECAY_SCALE, r=["wT"], w=["wT"])
            i = pcount[0] % 2; pcount[0] += 1
            S.op("pe", "matmul", ps[i][:], w2b[64:128, 0, :], lor[64:128, 0, :], start=True, stop=True, r=["w2b", "lor0b"], w=[pk[i]])
            S.op("act", "activation", out=aT[:], in_=ps[i][:], func=AF.Sigmoid, bias=col(1), scale=1.0, r=[pk[i], "par"], w=["aT"])
            i = pcount[0] % 2; pcount[0] += 1
            S.op("pe", "matmul", ps[i][:], w2b[:, 1, :], lor[:, 1, :], start=True, stop=False, r=["w2b", "lor1"], w=[pk[i]])
            S.op("pe", "matmul", ps[i][:], w2b[0:32, 2, :], lor[0:32, 2, :], start=False, stop=True, r=["w2b", "lor2a"], w=[pk[i]])
            S.op("act", "copy", out=gT[:], in_=ps[i][:], r=[pk[i]], w=["gT"])
            if layer1:
                i = pcount[0] % 2; pcount[0] += 1
                S.op("pe", "matmul", ps[i][:], w2b[32:64, 2, :], lor[32:64, 2, :], start=True, stop=True, r=["w2b", "lor2b"], w=[pk[i]])
                S.op("act", "activation", out=t1[:], in_=ps[i][:], func=AF.Sigmoid, bias=col(2), scale=1.0, r=[pk[i], "par"], w=["t1"])
                S.op("sp", "dma_start", out=t2[:], in_=vf_d[:, t0:t0 + SEG], w=["t2"], dma=True)
                S.op("dve", "tensor_tensor", t2[:], t2[:], vT[:], ALU.subtract, r=["t2", "vT"], w=["t2"])
                S.op("dve", "tensor_tensor", t2[:], t2[:], t1[:], ALU.mult, r=["t2", "t1"], w=["t2"])
                S.op("dve", "tensor_tensor", vT[:], vT[:], t2[:], ALU.add, r=["vT", "t2"], w=["vT"])
            else:
                S.op("sp", "dma_start", out=vf_o[:, t0:t0 + SEG], in_=vT[:], r=["vT"], dma=True)
            yield
            S.op("dve", "tensor_scalar", av[:], kT[:], col(3), None, ALU.mult, r=["kT", "par"], w=["av"])
            S.op("act", "activation", out=t1[:], in_=av[:], func=AF.Square, r=["av"], w=["t1"])
            i = pcount[0] % 2; pcount[0] += 1
            S.op("pe", "matmul", ps[i][:], bones, t1[:], start=True, stop=True, r=["cst", "t1"], w=[pk[i]])
            S.op("dve", "tensor_scalar", t2[:], ps[i][:], 1e-24, None, ALU.max, r=[pk[i]], w=["t2"])
            S.op("act", "activation", out=t2[:], in_=t2[:], func=AF.Sqrt, r=["t2"], w=["t2"])
            S.op("dve", "reciprocal", t2[:], t2[:], r=["t2"], w=["t2"])
            S.op("dve", "tensor_tensor", av[:], av[:], t2[:], ALU.mult, r=["av", "t2"], w=["av"])
            S.op("dve", "tensor_tensor", bv[:], av[:], aT[:], ALU.mult, r=["av", "aT"], w=["bv"])
            yield
            S.op("dve", "tensor_scalar", t1[:], aT[:], -1.0, col(4), ALU.add, ALU.mult, r=["aT", "par"], w=["t1"])
            S.op("dve", "scalar_tensor_tensor", out=kT[:], in0=t1[:], scalar=1.0, in1=kT[:], op0=ALU.add, op1=ALU.mult,
                 r=["t1", "kT"], w=["kT"])
            yield
            w3 = wT[:].rearrange("p (c j) -> p c j", j=64)
            S.op("pool", "tensor_copy", out=d0[:], in_=wT[:], r=["wT"], w=["d0"])
            S.op("pool", "memset", d0[:].rearrange("p (c j) -> p c j", j=64)[:, :, 0:1], 0.0, w=["d0"])
            S.op("pool", "memset", d1[:], 0.0, w=["d1"])
            S.op("pool", "tensor_copy", out=d1[:].rearrange("p (c j) -> p c j", j=64)[:, :, 0:1], in_=w3[:, :, 0:1], r=["wT"], w=["d1"])
            S.op("dve", "tensor_tensor_scan", P[:], d0[:], d1[:], 0.0, ALU.mult, ALU.add, r=["d0", "d1"], w=["P"])
            S.op("dve", "reciprocal", rP[:], P[:], r=["P"], w=["rP"])
            S.op("dve", "reciprocal", t1[:], wT[:], r=["wT"], w=["t1"])
            S.op("dve", "tensor_tensor", t1[:], t1[:], P[:], ALU.mult, r=["t1", "P"], w=["t1"])
            c3 = lambda t: t[:].rearrange("p (c j) -> p c j", j=64)
            yield
            S.op("dve", "scalar_tensor_tensor", out=AR[:, :, 0, :], in0=c3(av), scalar=-1.0, in1=c3(t1), op0=ALU.mult, op1=ALU.mult,
                 r=["av", "t1"], w=["AR"])
            S.op("pool", "tensor_tensor", AR[:, :, 1, :], c3(rT), c3(P), ALU.mult, r=["rT", "P"], w=["AR"])
            S.op("pool", "tensor_tensor", KBf[:, :, 0, :], c3(kT), c3(rP), ALU.mult, r=["kT", "rP"], w=["KBf"])
            S.op("dve", "tensor_tensor", KBf[:, :, 1, :], c3(bv), c3(rP), ALU.mult, r=["bv", "rP"], w=["KBf"])
            pend = c3(P)[:, :, 63:64]
            for q in range(2):
                S.op("dve" if q else "pool", "tensor_tensor", KBh[:, :, q, :], KBf[:, :, q, :], _bc(pend, [128, NCH, 64]), ALU.mult,
                     r=["KBf", "P"], w=["KBh"])
            for hh in range(2):
                hb = hh * 64
                S.op("pool", "tensor_copy", out=ARblk[hb:hb + 64, :, hh, :], in_=AR[hb:hb + 64, :, :, :].rearrange("p c a b -> p c (a b)"),
                     r=["AR"], w=["ARblk"])
                S.op("pool", "tensor_copy", out=Bblk[hb:hb + 64, :, hh, :], in_=KBf[hb:hb + 64, :, 1, :], r=["KBf"], w=["Bblk"])
            yield
            for half in range(2):
                for cc in range(4):
                    c = half * 4 + cc
                    S.op("pe", "transpose", ps[2][0:64, cc * 128:(cc + 1) * 128], vT[:, c * 64:(c + 1) * 64], ident,
                         r=["vT", "cst"], w=[pk[2]])
                S.op("act", "copy", out=Vtm[:, half * 4:(half + 1) * 4, :].rearrange("p c f -> p (c f)"), in_=ps[2][0:64, :], r=[pk[2]], w=["Vtm"])
            for q2 in range(4):
                for cc in range(2):
                    c = q2 * 2 + cc
                    for q in range(2):
                        S.op("pe", "transpose", ps[2][0:64, (cc * 2 + q) * 128:(cc * 2 + q + 1) * 128], KBh[:, c, q, :], ident,
                             r=["KBh", "cst"], w=[pk[2]])
                S.op("act", "copy", out=KBtm[:, q2 * 2:(q2 + 1) * 2, :, :].rearrange("p c q f -> p (c q f)"), in_=ps[2][0:64, :],
                     r=[pk[2]], w=["KBtm"])
            yield
            for c2 in range(NCH // 2):
                for which, dst in ((0, AK), (1, AB)):
                    for cc in range(2):
                        c = c2 * 2 + cc
                        S.op("pe", "matmul", ps[2][0:64, cc * 256:(cc + 1) * 256], KBf[:, c, which, :],
                             ARblk[:, c, :, :].rearrange("p a b -> p (a b)"), start=True, stop=True, r=["KBf", "ARblk"], w=[pk[2]])
                    S.op("dve", "tensor_tensor", dst[:, c2 * 4:(c2 + 1) * 4, :], ps[2][0:64, :].rearrange("p (a b) -> p a b", b=128),
                         _bc(m_ak.unsqueeze(1), [64, 4, 128]), ALU.mult, r=[pk[2], "cst"], w=["AK" if which == 0 else "AB"])
            for c4 in range(NCH // 4):
                for cc in range(4):
                    c = c4 * 4 + cc
                    S.op("pe", "matmul", ps[2][0:64, cc * 128:(cc + 1) * 128], AR[:, c, 0, :],
                         Bblk[:, c, :, :].rearrange("p a b -> p (a b)"), start=True, stop=True, r=["Bblk", "AR"], w=[pk[2]])
                S.op("dve", "tensor_tensor", Np[0][:, c4 * 8:(c4 + 1) * 8, :], ps[2][0:64, :].rearrange("p (a b) -> p a b", b=64),
                     _bc(m_sl.unsqueeze(1), [64, 8, 64]), ALU.mult, r=[pk[2], "cst"], w=[("Np0", c4)])
            yield
            S.op("pool", "tensor_copy", out=Mp[0][:], in_=AB[:, :, 0:64], r=["AB"], w=[("Mp0", 0), ("Mp0", 1)])
            S.op("dve", "tensor_tensor", Q[:], AB[:, :, 0:64], _bc(id64.unsqueeze(1), [64, NBLK, 64]), ALU.add, r=["AB", "cst"], w=[("Q", 0, p_), ("Q", 1, p_)])
            ibank = {0: (3, 4, 5), 1: (0, 1, 2)}
            for lvl in range(5):
                yield
                cur, nxt = lvl % 2, (lvl + 1) % 2
                for g8 in range(NBLK // 8):
                    bM, bN, bQ = ibank[g8]
                    for bi in range(8):
                        blk = g8 * 8 + bi
                        S.op("pe", "matmul", ps[bM][0:64, bi * 64:(bi + 1) * 64], Np[cur][:, blk, :], Mp[cur][:, blk, :],
                             start=True, stop=True, r=[("Np%d" % cur, g8), ("Mp%d" % cur, g8)], w=[pk[bM]])
                    for bi in range(8):
                        blk = g8 * 8 + bi
                        S.op("pe", "matmul", ps[bN][0:64, bi * 64:(bi + 1) * 64], Mp[cur][:, blk, :], Np[cur][:, blk, :],
                             start=True, stop=True, r=[("Np%d" % cur, g8), ("Mp%d" % cur, g8)], w=[pk[bN]])
                for g8 in range(NBLK // 8):
                    bM, bN, bQ = ibank[g8]
                    gsl = slice(g8 * 8, (g8 + 1) * 8)
                    S.op("act", "copy", out=Mp[nxt][:, gsl, :].rearrange("p a b -> p (a b)"), in_=ps[bM][0:64, :], r=[pk[bM]], w=[("Mp%d" % nxt, g8)])
                    S.op("dve", "tensor_copy", out=Np[nxt][:, gsl, :].rearrange("p a b -> p (a b)"), in_=ps[bN][0:64, :], r=[pk[bN]], w=[("Np%d" % nxt, g8)])
                for g8 in range(NBLK // 8):
                    bM, bN, bQ = ibank[g8]
                    for bi in range(8):
                        blk = g8 * 8 + bi
                        S.op("pe", "matmul", ps[bQ][0:64, bi * 64:(bi + 1) * 64], Np[nxt][:, blk, :], Q[:, blk, :],
                             start=True, stop=True, r=[("Np%d" % nxt, g8), ("Q", g8, p_)], w=[pk[bQ]])
                for g8 in range(NBLK // 8):
                    bM, bN, bQ = ibank[g8]
                    gsl = slice(g8 * 8, (g8 + 1) * 8)
                    S.op("dve", "tensor_tensor", Q[:, gsl, :].rearrange("p a b -> p (a b)"), Q[:, gsl, :].rearrange("p a b -> p (a b)"),
                         ps[bQ][0:64, :], ALU.add, r=[("Q", g8, p_), pk[bQ]], w=[("Q", g8, p_)])

        def chunk_gen(si):
            b, s = segs[si]
            t0 = b * ntok_b + s * SEG
            p_ = si % 2
            rT, kT, vT, gT, P, AR, Vtm, KBtm, AK, AB, Q = (rTs[p_], kTs[p_], vTs[p_], gTs[p_], Ps[p_], ARs[p_], Vtms[p_], KBtms[p_],
                                                          AKs[p_], ABs[p_], Qs[p_])
            c3 = lambda t: t[:].rearrange("p (c j) -> p c j", j=64)
            if s == 0:
                S.op("dve", "memset", H[:], 0.0, w=["H"])
            for c in range(NCH):
                yield
                S.op("pe", "matmul", ps[6][0:64, 0:128], AR[:, c, 0, :], H[:], start=True, stop=False, r=["AR", "H"], w=[pk[6]])
                for hh in range(2):
                    hb = hh * 64
                    blk = 2 * c + hh
                    S.op("pe", "matmul", ps[6][0:64, hb:hb + 64], AK[:, blk, 0:64], Vtm[:, c, hb:hb + 64], start=False, stop=(hh == 1),
                         r=["AK", "Vtm"], w=[pk[6]])
                S.op("act", "copy", out=Xs[:], in_=ps[6][0:64, 0:128], r=[pk[6]], w=["Xs"])
                for hh in range(2):
                    hb = hh * 64
                    blk = 2 * c + hh
                    S.op("pe", "matmul", ps[6][0:64, 128 + hb:128 + hb + 64], Q[:, blk, :], Xs[:, hb:hb + 64], start=True, stop=True,
                         r=[("Q", blk // 8, p_), "Xs"], w=[pk[6]])
                S.op("act", "copy", out=Us[:], in_=ps[6][0:64, 128:256], r=[pk[6]], w=["Us"])
                S.op("pe", "matmul", ps[6][0:64, 256:384], AR[:, c, 1, :], H[:], start=True, stop=False, r=["AR", "H"], w=[pk[6]])
                for hh in range(2):
                    hb = hh * 64
                    blk = 2 * c + hh
                    S.op("pe", "matmul", ps[6][0:64, 256 + hb:256 + hb + 64], AB[:, blk, 64:128], Us[:, hb:hb + 64], start=False, stop=False,
                         r=["AB", "Us"], w=[pk[6]])
                    S.op("pe", "matmul", ps[6][0:64, 256 + hb:256 + hb + 64], AK[:, blk, 64:128], Vtm[:, c, hb:hb + 64], start=False, stop=(hh == 1),
                         r=["AK", "Vtm"], w=[pk[6]])
                S.op("act", "copy", out=Ytm[:, c, :], in_=ps[6][0:64, 256:384], r=[pk[6]], w=["Ytm"])
                S.op("pe", "matmul", ps[7][:, 0:128], KBtm[:, c, 0, :], Vtm[:, c, :], start=True, stop=False, r=["KBtm", "Vtm"], w=[pk[7]])
                S.op("pe", "matmul", ps[7][:, 0:128], KBtm[:, c, 1, :], Us[:], start=False, stop=True, r=["KBtm", "Us"], w=[pk[7]])
                for hh in range(2):
                    hb = hh * 64
                    S.op("dve", "scalar_tensor_tensor", out=H[hb:hb + 64, hb:hb + 64], in0=H[hb:hb + 64, hb:hb + 64],
                         scalar=c3(P)[hb:hb + 64, c, 63:64], in1=ps[7][hb:hb + 64, hb:hb + 64],
                         op0=ALU.mult, op1=ALU.add, r=["H", "P", pk[7]], w=["H"])
            yield
            for c in range(NCH):
                S.op("pe", "transpose", ps[6][:, c * 64:(c + 1) * 64], Ytm[:, c, :], id64, r=["Ytm", "cst"], w=[pk[6]])
            S.op("act", "copy", out=yT[:], in_=ps[6][:], r=[pk[6]], w=["yT"])
            S.op("pe", "matmul", ps[7][:], bones, yT[:], start=True, stop=True, r=["cst", "yT"], w=[pk[7]])
            S.op("dve", "scalar_tensor_tensor", out=yT[:], in0=ps[7][:], scalar=-1.0 / 64, in1=yT[:], op0=ALU.mult, op1=ALU.add,
                 r=[pk[7], "yT"], w=["yT"])
            S.op("act", "activation", out=u1[:], in_=yT[:], func=AF.Square, r=["yT"], w=["d0"])
            S.op("pe", "matmul", ps[7][:], bones, u1[:], start=True, stop=True, r=["cst", "d0"], w=[pk[7]])
            S.op("act", "activation", out=u2[:], in_=ps[7][:], func=AF.Sqrt, bias=GN_EPS, scale=1.0 / 64, r=[pk[7]], w=["d1"])
            S.op("dve", "reciprocal", u2[:], u2[:], r=["d1"], w=["d1"])
            S.op("dve", "tensor_tensor", yT[:], yT[:], u2[:], ALU.mult, r=["yT", "d1"], w=["yT"])
            S.op("act", "activation", out=yT[:], in_=yT[:], func=AF.Identity, scale=col(6), bias=col(7), r=["yT", "par"], w=["yT"])
            S.op("dve", "scalar_tensor_tensor", out=u1[:], in0=rT[:], scalar=col(5), in1=kT[:], op0=ALU.mult, op1=ALU.mult,
                 r=["rT", "kT", "par"], w=["d0"])
            S.op("pe", "matmul", ps[7][:], bones, u1[:], start=True, stop=True, r=["cst", "d0"], w=[pk[7]])
            S.op("dve", "tensor_tensor", u2[:], ps[7][:], vT[:], ALU.mult, r=[pk[7], "vT"], w=["d1"])
            S.op("dve", "tensor_tensor", yT[:], yT[:], u2[:], ALU.add, r=["yT", "d1"], w=["yT"])
            S.op("dve", "tensor_tensor", ob[:], yT[:], gT[:], ALU.mult, r=["yT", "gT"], w=["ob"])
            S.op("sp", "dma_start", out=mix_o[:, t0:t0 + SEG], in_=ob[:], r=["ob"], dma=True)

        def step(gen, al):
            S.alias = al
            try:
                next(gen)
                return True
            except StopIteration:
                return False

        aliases = [{n: (n, q) for n in ['rT', 'kT', 'vT', 'gT', 'P', 'AR', 'Vtm', 'KBtm', 'AK', 'AB']} for q in range(2)]
        g = prep_gen(0)
        while step(g, aliases[0]):
            pass
        for si in range(len(segs)):
            nxt = prep_gen(si + 1) if si + 1 < len(segs) else None
            cg = chunk_gen(si)
            alive = nxt is not None
            while step(cg, aliases[si % 2]):
                for _ in range(2):
                    if alive:
                        alive = step(nxt, aliases[(si + 1) % 2])
            while alive:
                alive = step(nxt, aliases[(si + 1) % 2])
        S.alias = {}
        S.emit()
    return nc


def rwkv_host_inputs(inp, li, core):
    cs = slice(core * 128, (core + 1) * 128)
    f = np.float32
    wproj = np.zeros((D, NPROJ), f)
    mu = np.zeros((D, NPROJ), f)
    M = inp["rwkv_mu"][li]
    wproj[:, 0:128] = inp["rwkv_w_rkv"][li, 0][:, cs]; mu[:, 0:128] = M[0][:, None]
    wproj[:, 128:256] = inp["rwkv_w_rkv"][li, 1][:, cs]; mu[:, 128:256] = M[1][:, None]
    wproj[:, 256:384] = inp["rwkv_w_rkv"][li, 2][:, cs]; mu[:, 256:384] = M[2][:, None]
    wproj[:, 384:448] = inp["rwkv_decay_w1"][li]; mu[:, 384:448] = M[3][:, None]
    wproj[:, 448:512] = inp["rwkv_iclr_a1"][li]; mu[:, 448:512] = M[4][:, None]
    wproj[:, 512:672] = inp["rwkv_gate_g1"][li]; mu[:, 512:672] = M[5][:, None]
    if li > 0:
        wproj[:, 672:704] = inp["rwkv_vres_v1"][li - 1]
    mu[:, 672:704] = M[2][:, None]
    w2 = np.zeros((128, 3, 128), f)
    w2[0:64, 0] = inp["rwkv_decay_w2"][li][:, cs]
    w2[64:128, 0] = inp["rwkv_iclr_a2"][li][:, cs]
    w2[:, 1] = inp["rwkv_gate_g2"][li][0:128, cs]
    w2[0:32, 2] = inp["rwkv_gate_g2"][li][128:160, cs]
    if li > 0:
        w2[32:64, 2] = inp["rwkv_vres_v2"][li - 1][:, cs]
    par = np.zeros((128, 8), f)
    par[:, 0] = inp["rwkv_decay_w0"][li][cs]
    par[:, 1] = inp["rwkv_iclr_a0"][li][cs]
    if li > 0:
        par[:, 2] = inp["rwkv_vres_v0"][li - 1][cs]
    par[:, 3] = inp["rwkv_k_k"][li][cs]
    par[:, 4] = inp["rwkv_k_a"][li][cs]
    par[:, 5] = inp["rwkv_r_k"][li].reshape(-1)[cs]
    par[:, 6] = inp["rwkv_gn_g"][li][cs]
    par[:, 7] = inp["rwkv_gn_b"][li][cs]
    mu_l = np.ascontiguousarray(mu.reshape(KC, 128, NPROJ).transpose(1, 0, 2))
    return {"wproj": wproj, "mu": mu_l, "w2": w2, "par": par, "cst": rwkv_consts()}


NEG = -1.0e30
BLK = 256


def moba_consts():
    c = {}
    ident = np.eye(128, dtype=np.float32)
    c["identf"] = ident
    key = np.arange(128)[:, None]
    q = np.arange(128)[None, :]
    tri = np.where(key <= q, 0.0, NEG).astype(np.float32)
    oh = np.zeros((32, 32, 128), np.float32)
    for n in range(32):
        oh[n, n, :] = 1.0e30
    cb = np.zeros((128, 128 + 128 + 32 * 128), np.float32)
    cb[:, 0:128] = ident
    cb[:, 128:256] = tri
    cb[0:32, 256:] = oh.reshape(32, 32 * 128)
    return {"cstf": ident, "cstb": cb.astype(ml_dtypes.bfloat16)}


def build_H_moba(separate_kv, ntok_b=T, nb=B, dbg=()):
    import contextlib
    nseg = ntok_b // 512
    ntile = ntok_b // 128
    nblkb = ntok_b // BLK
    ntot = nb * ntok_b
    nc = bass.Bass("TRN2", target_bir_lowering=False)
    with contextlib.ExitStack() as es:
        cx = Ctx(nc, es)
        S = cx.S
        xT = cx.dram("xT", [D, ntot], F32, "ExternalInput")
        if separate_kv:
            xkvT = cx.dram("xkvT", [D, ntot], F32, "ExternalInput")
        else:
            xkvT = xT
        wqkv_d = cx.dram("wqkv", [D, 3, 128], F32, "ExternalInput")
        cstf_d = cx.dram("cstf", [128, 128], F32, "ExternalInput")
        cstb_d = cx.dram("cstb", [128, 256 + 32 * 128], BF16, "ExternalInput")
        ind_d = cx.dram("ind", [32, ntok_b], BF16, "ExternalInput")
        mix_o = cx.dram("mix", [128, ntot], BF16, "ExternalOutput")

        wqkv = cx.sb("wqkv_sb", [128, KC, 3, 128], BF16)
        identf = cx.sb("identf", [128, 128], F32)
        cstb = cx.sb("cstb_sb", [128, 256 + 32 * 128], BF16)
        identb = cstb[:, 0:128]
        trib = cstb[:, 128:256]
        xq = [cx.sb("xq%d" % i, [128, KC, 512], BF16) for i in range(2)]
        xkv = [cx.sb("xkv%d" % i, [128, KC, 512], BF16) for i in range(2)] if separate_kv else xq
        KTa = [cx.sb("KTa%d" % i, [128, ntok_b], BF16) for i in range(2)]
        Va = cx.sb("Va", [128, ntile, 2, 65], BF16)
        qf = cx.sb("qf", [128, ntok_b], F32)
        qz = [cx.sb("qz%d" % i, [128, ntok_b], BF16) for i in range(2)]
        km = cx.sb("km", [128, nblkb], F32)
        kmblk = cx.sb("kmblk", [128, 2, nblkb], F32)
        gsb = cx.sb("gsb", [128, 2, 32], F32)
        top8 = cx.sb("top8", [128, 2, 8], F32)
        mm1p = [cx.sb("mm1p%d" % i, [128, 128], F32) for i in range(2)]
        mT = [cx.sb("mT%d" % i, [32, 2, 128], BF16) for i in range(2)]
        PT = [cx.sb("PT%d" % i, [128, 1024], BF16) for i in range(3)]
        osb = cx.sb("osb", [128, 128], F32)
        rs = cx.sb("rs", [128, 1], F32)
        ob = cx.sb("ob", [128, 512], BF16)
        psS = [cx.ps("psS%d" % i, [128, 1024]) for i in range(2)]
        pSkeys = [("psS", i) for i in range(2)]
        ps = [psS[0][:, 0:512], psS[0][:, 512:1024], psS[1][:, 0:512], cx.ps("ps3", [128, 512]), cx.ps("ps4", [128, 512]), None,
              cx.ps("ps6", [128, 512]), cx.ps("ps7", [128, 512])]
        pk = [("psS", 0), ("psS", 0), ("psS", 1), ("ps", 3), ("ps", 4), ("ps", 5), ("ps", 6), ("ps", 7)]

        S.op("sp", "dma_start", out=identf[:], in_=cstf_d, w=["identf"], dma=True)
        S.op("sp", "dma_start", out=cstb[:], in_=cstb_d, w=["cstb"], dma=True)
        S.op("pool", "dma_start", out=wqkv[:], in_=wqkv_d.rearrange("(c p) a e -> p c a e", p=128), w=["wqkv"], dma=True)
        S.op("dve", "memset", Va[:, :, :, 64:65], 1.0, w=["Va"])
        S.op("dve", "memset", qz[0][64:128, :], 0.0, w=[("qa", 0, t_) for t_ in range(ntile)])
        S.op("dve", "memset", qz[1][0:64, :], 0.0, w=[("qa", 1, t_) for t_ in range(ntile)])
        S.op("dve", "memset", kmblk[:], 0.0, w=["kmblk"])
        S.op("dve", "memset", KTa[0][64:128, :], 0.0, w=["KTa0"])
        S.op("dve", "memset", KTa[1][0:64, :], 0.0, w=["KTa1"])
        S.op("sp", "dma_start", out=KTa[0][64:96, :], in_=ind_d, w=["KTa0"], dma=True)
        S.op("sp", "dma_start", out=KTa[1][0:32, :], in_=ind_d, w=["KTa1"], dma=True)
        for hh in range(2):
            S.op("dve", "memset", mm1p[hh][:], 0.0, w=["mm1p%d" % hh])

        fm = lambda ap: ap.rearrange("(c p) t -> p c t", p=128)

        def load_seg(b, s, slot):
            t0 = b * ntok_b + s * 512
            S.op("pool", "dma_start", out=xq[slot][:], in_=fm(xT[:, t0:t0 + 512]), w=[("xq", slot)], dma=True)
            if separate_kv:
                S.op("pool", "dma_start", out=xkv[slot][:], in_=fm(xkvT[:, t0:t0 + 512]), w=[("xkv", slot)], dma=True)

        segs = [(b, s) for b in range(nb) for s in range(nseg)]
        kvk = (lambda slot: ("xkv", slot)) if separate_kv else (lambda slot: ("xq", slot))
        load_seg(0, 0, 0)
        scount = 0
        pcount = 0
        for b in range(nb):
            S.op("dve", "memset", gsb[:], NEG, w=["gsb"])
            S.op("dve", "memset", km[:], 0.0, w=["km"])
            S.op("dve", "memset", qz[0][64:96, 0:256], 0.0, w=[("qa", 0, 0), ("qa", 0, 1)])
            S.op("dve", "memset", qz[1][0:32, 0:256], 0.0, w=[("qa", 1, 0), ("qa", 1, 1)])
            for s in range(nseg if "cut0" not in dbg else 0):
                si = b * nseg + s
                slot = si % 2
                if si + 1 < len(segs):
                    load_seg(segs[si + 1][0], segs[si + 1][1], 1 - slot)
                ss = slice(s * 512, (s + 1) * 512)
                for k in range(KC):
                    S.op("pe", "matmul", ps[0][:], wqkv[:, k, 1, :], xkv[slot][:, k, :], start=(k == 0), stop=(k == KC - 1),
                         r=["wqkv", kvk(slot)], w=[pk[0]])
                for hb2 in range(2):
                    for hh in range(2):
                        hb = hh * 64
                        S.op("act", "activation", out=KTa[hh][hb:hb + 64, s * 512 + hb2 * BLK:s * 512 + (hb2 + 1) * BLK],
                             in_=ps[0][hb:hb + 64, hb2 * BLK:(hb2 + 1) * BLK], func=AF.Copy,
                             accum_out=km[hb:hb + 64, 2 * s + hb2:2 * s + hb2 + 1], r=[pk[0]], w=["KTa%d" % hh, "km"])
                if "cut1" in dbg:
                    continue
                for tt in range(4):
                    for k in range(KC):
                        S.op("pe", "matmul", ps[1][:, tt * 128:(tt + 1) * 128], xkv[slot][:, k, tt * 128:(tt + 1) * 128], wqkv[:, k, 2, :],
                             start=(k == 0), stop=(k == KC - 1), r=["wqkv", kvk(slot)], w=[pk[1]])
                S.op("act", "copy", out=Va[:, 4 * s:4 * s + 4, :, 0:64], in_=ps[1][:].rearrange("p (t h d) -> p t h d", h=2, d=64),
                     r=[pk[1]], w=["Va"])
                if "cut2" in dbg:
                    continue
                for k in range(KC):
                    S.op("pe", "matmul", ps[2][:], wqkv[:, k, 0, :], xq[slot][:, k, :], start=(k == 0), stop=(k == KC - 1),
                         r=["wqkv", ("xq", slot)], w=[pk[2]])
                S.op("act", "copy", out=qf[:, ss], in_=ps[2][:], r=[pk[2]], w=["qf"])
                S.op("dve", "tensor_copy", out=qz[0][0:64, ss], in_=qf[0:64, ss], r=["qf"], w=[("qd", 0)])
                S.op("dve", "tensor_copy", out=qz[1][64:128, ss], in_=qf[64:128, ss], r=["qf"], w=[("qd", 1)])
            for hh in range(2):
                hb = hh * 64
                S.op("dve", "tensor_scalar", kmblk[hb:hb + 64, hh, :], km[hb:hb + 64, :], 1.0 / BLK, None, ALU.mult, r=["km"], w=["kmblk"])
            def rec_gate1(qt):
                own = qt // 2
                qs = slice(qt * 128, (qt + 1) * 128)
                S.op("pe", "matmul", ps[3][:, 0:2 * nblkb], qf[:, qs], kmblk[:].rearrange("p a b -> p (a b)"), start=True, stop=True,
                     r=["qf", "kmblk"], w=[pk[3]])
                S.op("dve", "tensor_copy", out=gsb[:, :, 0:own], in_=ps[3][:, 0:2 * nblkb].rearrange("p (a b) -> p a b", b=nblkb)[:, :, 0:own],
                     r=[pk[3]], w=["gsb"])
                for hh in range(2):
                    base = 64 if hh == 0 else 0
                    S.op("dve", "max", out=top8[:, hh, :], in_=gsb[:, hh, :], r=["gsb"], w=["top8"])
                    S.op("dve", "tensor_scalar", mm1p[hh][:, base:base + 32], gsb[:, hh, :], top8[:, hh, 2:3], -1.0, ALU.is_ge, ALU.add,
                         r=["gsb", "top8"], w=["mm1p%d" % hh])
                    S.op("dve", "memset", mm1p[hh][:, base + own:base + own + 1], 0.0, w=["mm1p%d" % hh])

            def rec_gate2(qt):
                qs = slice(qt * 128, (qt + 1) * 128)
                for hh in range(2):
                    S.op("pe", "matmul", ps[3][:, 256 + hh * 128:256 + (hh + 1) * 128], mm1p[hh][:], identf[:], start=True, stop=True,
                         r=["mm1p%d" % hh, "identf"], w=[pk[3]])
                S.op("act", "copy", out=qz[0][64:96, qs], in_=ps[3][64:96, 256:384], r=[pk[3]], w=[("qa", 0, qt)])
                S.op("act", "copy", out=qz[1][0:32, qs], in_=ps[3][0:32, 384:512], r=[pk[3]], w=[("qa", 1, qt)])

            items = []
            for qt in range(ntile if "proj_only" not in dbg else 0):
                for hh in range(2):
                    kts = list(range(0, qt + 1))
                    ngrp = (len(kts) + 7) // 8
                    pi_ = pcount % 2
                    pcount += 1
                    for gi in range(ngrp):
                        items.append(dict(qt=qt, hh=hh, gi=gi, ngrp=ngrp, grp=kts[gi * 8:(gi + 1) * 8], po=ps[6 + pi_], pok=pk[6 + pi_]))

            def rec_S(it):
                qt, hh, grp = it["qt"], it["hh"], it["grp"]
                own = qt // 2
                qs = slice(qt * 128, (qt + 1) * 128)
                pS, pSk = psS[it["sb"] % 2], pSkeys[it["sb"] % 2]
                pt, ptk = PT[it["sb"] % 3], ("PT", it["sb"] % 3)
                for j, kt in enumerate(grp):
                    S.op("pe", "matmul", pS[:, j * 128:(j + 1) * 128], KTa[hh][:, kt * 128:(kt + 1) * 128], qz[hh][:, qs], start=True, stop=(kt != qt),
                         r=["KTa%d" % hh, ("qd", hh), ("qa", hh, qt)], w=[pSk])
                    if kt == qt:
                        S.op("pe", "matmul", pS[:, j * 128:(j + 1) * 128], identb, trib, start=False, stop=True, r=["cstb"], w=[pSk])
                w_ = len(grp) * 128
                S.op("act", "activation", out=pt[:, 0:w_], in_=pS[:, 0:w_], func=AF.Exp, scale=0.125, r=[pSk], w=[ptk])

            def rec_PV(it):
                qt, hh, gi, ngrp, grp, po, pok = it["qt"], it["hh"], it["gi"], it["ngrp"], it["grp"], it["po"], it["pok"]
                hb = hh * 64
                pt, ptk = PT[it["sb"] % 3], ("PT", it["sb"] % 3)
                for j, kt in enumerate(grp):
                    first = (gi == 0 and j == 0)
                    last = (gi == ngrp - 1 and j == len(grp) - 1)
                    S.op("pe", "matmul", po[:, 0:65], pt[:, j * 128:(j + 1) * 128], Va[:, kt, hh, :], start=first, stop=last,
                         r=[ptk, "Va"], w=[pok])
                if gi == ngrp - 1:
                    S.op("dve", "reciprocal", rs[:], po[:, 64:65], r=[pok], w=["rs"])
                    S.op("dve", "tensor_scalar", osb[:, hb:hb + 64], po[:, 0:64], rs[:, 0:1], None, ALU.mult, r=[pok, "rs"], w=["osb"])
                    if hh == 1:
                        S.op("pe", "transpose", ps[4][:, 0:128], osb[:], identf[:], r=["osb", "identf"], w=[pk[4]])
                        S.op("act", "copy", out=ob[:, (qt % 4) * 128:(qt % 4 + 1) * 128], in_=ps[4][:, 0:128], r=[pk[4]], w=["ob"])
                        if qt % 4 == 3:
                            t0 = b * ntok_b + (qt - 3) * 128
                            S.op("sp", "dma_start", out=mix_o[:, t0:t0 + 512], in_=ob[:], r=["ob"], dma=True)

            pending = []
            for it in items:
                it["sb"] = scount
                scount += 1
                rec_S(it)
                pending.append(it)
                if len(pending) > 1:
                    rec_PV(pending.pop(0))
                nq = it["qt"] + 1
                if "nogate" not in dbg and it["gi"] == 0 and nq < ntile and nq // 2 > 0:
                    if it["hh"] == 0:
                        rec_gate1(nq)
                    else:
                        rec_gate2(nq)
            for it in pending:
                rec_PV(it)
        S.emit()
    return nc


def moba_host_inputs(inp, j, core, ntok_b=T):
    cs = slice(core * 128, (core + 1) * 128)
    w = np.stack([inp["moba_w_q"][j][:, cs], inp["moba_w_k"][:, cs], inp["moba_w_v"][:, cs]], axis=1)
    m = {"wqkv": np.ascontiguousarray(w.astype(np.float32))}
    m.update(moba_consts())
    ind = np.zeros((32, ntok_b), np.float32)
    for n in range(32):
        ind[n, n * BLK:(n + 1) * BLK] = 1.0e30
    m["ind"] = ind[:, :ntok_b].astype(ml_dtypes.bfloat16)
    return m


_PROGS = {}
_DUMP = None
_NL = 4


def _prog(name, fn):
    if name not in _PROGS:
        _PROGS[name] = fn()
    return _PROGS[name]


def _run(nc, maps):
    res = run_bass_kernel_spmd(nc, maps, core_ids=list(range(NCORES)))
    return res.results


def _t_phase(inp, layer, mixT, hT):
    moe = (layer % 2 == 1)
    e = layer // 2
    nc = _prog("T_moe" if moe else "T_dense", lambda: build_T(moe))
    if layer < 2:
        w_o = inp["rwkv_w_out"][layer]
    else:
        w_o = inp["moba_w_o"][layer - 2]
    base = {"w_o": np.ascontiguousarray(w_o, dtype=np.float32), "lnp": lnp_layout(inp["ln_g"], inp["ln_b"], layer),
            "consts": make_consts()}
    if moe:
        base["router"] = np.ascontiguousarray(inp["moe_router"][e])
        base["w_gate"] = np.ascontiguousarray(inp["moe_w_gate"][e])
        base["w_up"] = np.ascontiguousarray(inp["moe_w_up"][e])
        base["w_down"] = np.ascontiguousarray(inp["moe_w_down"][e])
    else:
        base["w_gate"] = np.ascontiguousarray(inp["ffn_w_gate"][e][None])
        base["w_up"] = np.ascontiguousarray(inp["ffn_w_up"][e][None])
        base["w_down"] = np.ascontiguousarray(inp["ffn_w_down"][e][None])
    maps = []
    for c in range(NCORES):
        m = dict(base)
        m["mixT"] = np.ascontiguousarray(mixT[:, c * NT:(c + 1) * NT])
        m["hT"] = np.ascontiguousarray(hT[:, c * NT:(c + 1) * NT])
        maps.append(m)
    res = _run(nc, maps)
    return np.concatenate([r["outT"] for r in res], axis=1)


def kernel(**inp):
    inp = {k: np.asarray(v) for k, v in inp.items()}
    x = inp["x"].astype(np.float32, copy=False)
    hT = np.ascontiguousarray(x.reshape(NTOK, D).T)
    vfirst = None
    h_kv = None
    for layer in range(_NL):
        if layer < 2:
            nc = _prog("H_rwkv%d" % layer, lambda: build_H_rwkv2(layer == 1))
            maps = []
            for c in range(NCORES):
                m = rwkv_host_inputs(inp, layer, c)
                m["xT"] = hT
                if layer == 1:
                    m["vfirst"] = np.ascontiguousarray(vfirst[c * 128:(c + 1) * 128])
                maps.append(m)
            res = _run(nc, maps)
            if layer == 0:
                vfirst = np.concatenate([r["vfirst_out"] for r in res], axis=0)
        else:
            j = layer - 2
            nc = _prog("H_moba%d" % j, lambda: build_H_moba(j == 1))
            maps = []
            for c in range(NCORES):
                m = moba_host_inputs(inp, j, c)
                m["xT"] = hT
                if j == 1:
                    m["xkvT"] = h_kv
                maps.append(m)
            res = _run(nc, maps)
        mixT = np.concatenate([r["mix"] for r in res], axis=0)
        if _DUMP is not None:
            _DUMP["mix%d" % layer] = mixT
        hT = _t_phase(inp, layer, mixT, hT)
        if _DUMP is not None:
            _DUMP["h%d" % layer] = hT
        if layer == 1:
            h_kv = hT
    return np.ascontiguousarray(hT.T).reshape(B, T, D).astype(np.float32)
```

```python
import numpy as np
import ml_dtypes
import concourse.bass as bass
import concourse.mybir as mybir
from concourse.bass_utils import run_bass_kernel_spmd

F32 = mybir.dt.float32
BF16 = mybir.dt.bfloat16
ALU = mybir.AluOpType
AF = mybir.ActivationFunctionType
AX = mybir.AxisListType

NCORES = 8
D = 1024
KC = 8
B = 2
T = 8192
NTOK = B * T
NT = NTOK // NCORES
ALPHA = (2.0 * 4) ** 0.25
LN_EPS = 1e-5
GN_EPS = 64e-5


class Sched:
    ENG = ("pe", "act", "dve", "pool", "sp")
    NDMA = 8

    def __init__(self, nc):
        self.nc = nc
        self.ops = {e: [] for e in self.ENG}
        self.lastw = {}
        self.readers = {}
        self.alias = {}
        self.strict = True

    def add(self, eng, fn, reads=(), writes=(), dma=False):
        idx = len(self.ops[eng])
        deps = {}

        def dep(key, kind):
            if key is None:
                return
            if deps.get(key) != "raw":
                deps[key] = kind

        for r in reads:
            dep(self.lastw.get(r), "raw")
        for w in writes:
            dep(self.lastw.get(w), "waw")
            for k in self.readers.get(w, ()):
                dep(k, "war")
        waits = set()
        for (pe, pi), kind in deps.items():
            pdma = self.ops[pe][pi]["dma"]
            if pe == eng and not pdma and not dma:
                if eng == "pe":
                    continue
                if kind != "raw" and eng != "pool" and not self.strict:
                    continue
            waits.add((pe, pi))
        self.ops[eng].append(dict(fn=fn, waits=waits, dma=dma, signal=dma))
        me = (eng, idx)
        for r in reads:
            lst = self.readers.setdefault(r, [])
            if not dma:
                lst[:] = [k for k in lst if not (k[0] == eng and not self.ops[k[0]][k[1]]["dma"])]
            lst.append(me)
        for w in writes:
            self.lastw[w] = me
            self.readers[w] = []
        return me

    def op(self, eng, method, *args, r=(), w=(), dma=False, **kw):
        al = self.alias
        if al:
            r = [k if isinstance(k, tuple) else al.get(k, k) for k in r]
            w = [k if isinstance(k, tuple) else al.get(k, k) for k in w]
        return self.add(eng, lambda h: getattr(h, method)(*args, **kw), reads=r, writes=w, dma=dma)

    def emit(self):
        nc = self.nc
        ops = self.ops
        for e in self.ENG:
            for op in ops[e]:
                for (pe, pi) in op["waits"]:
                    ops[pe][pi]["signal"] = True
        import contextlib
        with contextlib.ExitStack() as st:
            csem = {e: st.enter_context(nc.semaphore("c_" + e)) for e in self.ENG}
            dsem = {e: [st.enter_context(nc.semaphore("d_%s%d" % (e, i))) for i in range(self.NDMA)]
                    for e in ("act", "pool", "sp")}
            final_dma = []
            for e in self.ENG:
                cnt = 0
                dcnt = [0] * self.NDMA
                k = 0
                for op in ops[e]:
                    if op["dma"]:
                        s = k % self.NDMA
                        k += 1
                        op["sem"] = dsem[e][s]
                        op["prev"] = dcnt[s]
                        dcnt[s] += 16
                        op["val"] = dcnt[s]
                    elif op["signal"]:
                        cnt += 1
                        op["sem"] = csem[e]
                        op["val"] = cnt
                if e in dsem:
                    for s in range(self.NDMA):
                        if dcnt[s]:
                            final_dma.append((dsem[e][s], dcnt[s]))
            block = st.enter_context(nc.Block())

            def run(e, h):
                waited = {}

                def wait(sem, val):
                    key = id(sem)
                    if waited.get(key, 0) >= val:
                        return
                    waited[key] = val
                    h.wait_ge(sem, val)

                for op in ops[e]:
                    for (pe, pi) in sorted(op["waits"]):
                        p = ops[pe][pi]
                        wait(p["sem"], p["val"])
                    if op["dma"] and op["prev"]:
                        wait(op["sem"], op["prev"])
                    ins = op["fn"](h)
                    if op["dma"]:
                        ins.then_inc(op["sem"], 16)
                    elif op["signal"]:
                        ins.then_inc(op["sem"], 1)
                if e == "sp":
                    for sem, val in final_dma:
                        wait(sem, val)

            @block.tensor
            def _(h):
                run("pe", h)

            @block.scalar
            def _(h):
                run("act", h)

            @block.vector
            def _(h):
                run("dve", h)

            @block.gpsimd
            def _(h):
                run("pool", h)

            @block.sync
            def _(h):
                run("sp", h)


def _bc(ap, shape):
    return ap.broadcast_to(shape)


class Ctx:
    def __init__(self, nc, es):
        self.nc = nc
        self.es = es
        self.S = Sched(nc)

    def sb(self, name, shape, dt):
        return self.es.enter_context(self.nc.sbuf_tensor(name, shape, dt))

    def ps(self, name, shape, dt=F32):
        return self.es.enter_context(self.nc.psum_tensor(name, shape, dt))

    def dram(self, name, shape, dt, kind):
        return self.nc.dram_tensor(name, list(shape), dt, kind=kind).ap()


def emit_layernorm(cx, zt, res_z, hb, res_hb, lnp, gi, bi, onesm, psA, psB, tmp, ngroups, scale_after=None):
    S = cx.S
    mean, msq, var = tmp["mean"], tmp["msq"], tmp["var"]
    for g in range(ngroups):
        gs = slice(g * 512, (g + 1) * 512)
        zkeys = [res_z(k, g) for k in range(KC)]
        for k in range(KC):
            sq = tmp["sq%d" % (k % 2)]
            S.op("act", "activation", out=sq[:], in_=zt[:, k, gs], func=AF.Square, r=[zkeys[k]], w=[("sq", k % 2)])
            S.op("pe", "matmul", psA[:], onesm, zt[:, k, gs], start=(k == 0), stop=(k == KC - 1),
                 r=[zkeys[k], "onesm"], w=["psA"])
            S.op("pe", "matmul", psB[:], onesm, sq[:], start=(k == 0), stop=(k == KC - 1),
                 r=[("sq", k % 2), "onesm"], w=["psB"])
        S.op("act", "copy", out=mean[:], in_=psA[:], r=["psA"], w=["mean"])
        S.op("dve", "tensor_tensor", msq[:], mean[:], mean[:], ALU.mult, r=["mean"], w=["msq"])
        S.op("dve", "tensor_tensor", var[:], psB[:], msq[:], ALU.subtract, r=["psB", "msq"], w=["var"])
        S.op("act", "activation", out=var[:], in_=var[:], func=AF.Sqrt, bias=LN_EPS, scale=1.0, r=["var"], w=["var"])
        S.op("dve", "reciprocal", msq[:], var[:], r=["var"], w=["msq"])
        z3 = zt[:, :, gs]
        S.op("dve", "tensor_tensor", z3, z3, _bc(mean[:].unsqueeze(1), [128, KC, 512]), ALU.subtract,
             r=zkeys + ["mean"], w=zkeys)
        S.op("dve", "tensor_tensor", z3, z3, _bc(msq[:].unsqueeze(1), [128, KC, 512]), ALU.mult,
             r=zkeys + ["msq"], w=zkeys)
        for k in range(KC):
            S.op("act", "activation", out=zt[:, k, gs], in_=zt[:, k, gs], func=AF.Identity,
                 scale=lnp[:, gi, k:k + 1], bias=lnp[:, bi, k:k + 1], r=[zkeys[k], "lnp"], w=[zkeys[k]])
        if hb is not None:
            hkeys = [res_hb(k, g) for k in range(KC)]
            S.op("pool", "tensor_copy", out=hb[:, :, gs], in_=z3, r=zkeys, w=hkeys)
        if scale_after is not None:
            S.op("pool", "tensor_scalar", z3, z3, float(scale_after), None, ALU.mult, r=zkeys, w=zkeys)


def build_T(moe, nt=NT, dff=None, nexp=None):
    import contextlib
    G = nt // 512
    FB = 4
    if dff is None:
        dff = 3584 if moe else 2816
    if nexp is None:
        nexp = 8 if moe else 1
    nfc = dff // 128
    nblk = (nfc + FB - 1) // FB
    ntile = nt // 128
    nc = bass.Bass("TRN2", target_bir_lowering=False)
    with contextlib.ExitStack() as es:
        cx = Ctx(nc, es)
        S = cx.S
        mixT = cx.dram("mixT", [D, nt], BF16, "ExternalInput")
        hT = cx.dram("hT", [D, nt], F32, "ExternalInput")
        w_o = cx.dram("w_o", [D, D], F32, "ExternalInput")
        lnp_d = cx.dram("lnp", [128, 4, KC], F32, "ExternalInput")
        consts = cx.dram("consts", [128, 128 + 128 + 8 * 128], F32, "ExternalInput")
        if moe:
            router = cx.dram("router", [D, 8], F32, "ExternalInput")
        wg_d = cx.dram("w_gate", [nexp, D, dff], F32, "ExternalInput")
        wu_d = cx.dram("w_up", [nexp, D, dff], F32, "ExternalInput")
        wd_d = cx.dram("w_down", [nexp, dff, D], F32, "ExternalInput")
        outT = cx.dram("outT", [D, nt], F32, "ExternalOutput")
        outTb = cx.dram("outTb", [D, nt], BF16, "ExternalOutput")

        acc = cx.sb("acc", [128, KC, nt], F32)
        hb = cx.sb("hb", [128, KC, nt], BF16)
        wgu = cx.sb("wgu", [128, 4, KC, 512], BF16)
        wdn = cx.sb("wdn", [128, 2, FB, D], BF16)
        actb = cx.sb("actb", [128, FB, nt], BF16)
        cst = cx.sb("cst", [128, 128 + 128 + 8 * 128], F32)
        lnp = cx.sb("lnp_sb", [128, 4, KC], F32)
        tmp = {n: cx.sb(n, [128, 512], F32) for n in ("sq0", "sq1", "mean", "msq", "var", "sl0", "sl1", "t20", "t21")}
        onesm = cst[:, 0:128]
        ident = cst[:, 128:256]
        ps = [cx.ps("ps%d" % i, [128, 512]) for i in range(8)]
        pkey = [("ps", i) for i in range(6)] + ["psA", "psB"]
        if moe:
            rt = cx.sb("rt", [128, KC, 8], F32)
            gbs = [cx.sb("gb0", [128, nt], F32)] * 2
            lg = cx.sb("lg", [128, ntile, 8], F32)
            lg2 = cx.sb("lg2", [128, ntile, 8], F32)
            lgm = cx.sb("lgm", [128, ntile, 8], F32)
            m1 = cx.sb("m1", [128, ntile], F32)
            m2 = cx.sb("m2", [128, ntile], F32)
            GT = cx.sb("GT", [8, nt], F32)

        zk = lambda k, g: ("acc", k, g)
        hk = lambda k, g: ("hb", k, g)
        allz = lambda g: [zk(k, g) for k in range(KC)]
        allh = lambda g: [hk(k, g) for k in range(KC)]
        fm = lambda ap: ap.rearrange("(c p) t -> p c t", p=128)

        S.op("sp", "dma_start", out=cst[:], in_=consts, w=["onesm", "ident", "sel"], dma=True)
        S.op("sp", "dma_start", out=lnp[:], in_=lnp_d, w=["lnp"], dma=True)
        for g in range(G):
            gs = slice(g * 512, (g + 1) * 512)
            S.op("sp", "dma_start", out=hb[:, :, gs], in_=fm(mixT[:, gs]), w=allh(g), dma=True)
        for s in range(2):
            S.op("pool", "dma_start", out=wgu[:, s], in_=fm(w_o[:, s * 512:(s + 1) * 512]), w=[("wgu", s)], dma=True)
        for g in range(G):
            gs = slice(g * 512, (g + 1) * 512)
            S.op("sp", "dma_start", out=acc[:, :, gs], in_=fm(hT[:, gs]), w=allz(g), dma=True)
        if moe:
            S.op("sp", "dma_start", out=rt[:], in_=fm(router), w=["rt"], dma=True)

        blocks = [(e, b) for e in range(nexp) for b in range(nblk)]

        def load_block(i):
            e, b = blocks[i]
            st = i % 2
            f0 = b * FB * 128
            nf = min(FB * 128, dff - f0)
            S.op("pool", "dma_start", out=wgu[:, 2 * st, :, 0:nf], in_=fm(wg_d[e, :, f0:f0 + nf]), w=[("wgu", 2 * st)], dma=True)
            S.op("pool", "dma_start", out=wgu[:, 2 * st + 1, :, 0:nf], in_=fm(wu_d[e, :, f0:f0 + nf]), w=[("wgu", 2 * st + 1)], dma=True)
            S.op("pool", "dma_start", out=wdn[:, st, 0:nf // 128, :], in_=fm(wd_d[e, f0:f0 + nf, :]), w=[("wdn", st)], dma=True)

        pi = 0
        for g in range(G):
            gs = slice(g * 512, (g + 1) * 512)
            for ec in range(KC):
                pt, pk = ps[pi % 2], pkey[pi % 2]
                pi += 1
                s, off = divmod(ec * 128, 512)
                for k in range(KC):
                    S.op("pe", "matmul", pt[:], wgu[:, s, k, off:off + 128], hb[:, k, gs], start=(k == 0), stop=(k == KC - 1),
                         r=[("wgu", s), hk(k, g)], w=[pk])
                S.op("dve", "scalar_tensor_tensor", out=acc[:, ec, gs], in0=acc[:, ec, gs], scalar=float(ALPHA), in1=pt[:],
                     op0=ALU.mult, op1=ALU.add, r=[zk(ec, g), pk], w=[zk(ec, g)])
        load_block(0)
        emit_layernorm(cx, acc, zk, hb, hk, lnp, 0, 1, onesm, ps[6], ps[7], tmp, G)
        if len(blocks) > 1:
            load_block(1)
        if moe:
            lp = ps[6]
            for j in range(ntile):
                for k in range(KC):
                    S.op("pe", "matmul", lp[:, j * 8:(j + 1) * 8], acc[:, k, j * 128:(j + 1) * 128], rt[:, k, :],
                         start=(k == 0), stop=(k == KC - 1), r=[zk(k, j // 4), "rt"], w=["psA"])
            S.op("act", "copy", out=lg[:].rearrange("p a b -> p (a b)"), in_=lp[:, 0:ntile * 8], r=["psA"], w=["lg"])
            bc3 = lambda t: _bc(t[:].unsqueeze(2), [128, ntile, 8])
            S.op("dve", "tensor_reduce", out=m1[:], in_=lg[:], axis=AX.X, op=ALU.max, r=["lg"], w=["m1"])
            S.op("dve", "tensor_tensor", lgm[:], lg[:], bc3(m1), ALU.is_ge, r=["lg", "m1"], w=["lgm"])
            S.op("dve", "scalar_tensor_tensor", out=lg2[:], in0=lgm[:], scalar=-1e30, in1=lg[:], op0=ALU.mult, op1=ALU.add,
                 r=["lgm", "lg"], w=["lg2"])
            S.op("dve", "tensor_reduce", out=m2[:], in_=lg2[:], axis=AX.X, op=ALU.max, r=["lg2"], w=["m2"])
            S.op("dve", "tensor_tensor", lgm[:], lg[:], bc3(m2), ALU.is_ge, r=["lg", "m2"], w=["lgm"])
            S.op("dve", "tensor_tensor", lg2[:], lg[:], bc3(m1), ALU.subtract, r=["lg", "m1"], w=["lg2"])
            S.op("act", "activation", out=lg2[:], in_=lg2[:], func=AF.Exp, r=["lg2"], w=["lg2"])
            S.op("dve", "tensor_tensor", lg2[:], lg2[:], lgm[:], ALU.mult, r=["lg2", "lgm"], w=["lg2"])
            S.op("dve", "tensor_reduce", out=m1[:], in_=lg2[:], axis=AX.X, op=ALU.add, r=["lg2"], w=["m1"])
            S.op("dve", "reciprocal", m2[:], m1[:], r=["m1"], w=["m2"])
            S.op("dve", "tensor_tensor", lg[:], lg2[:], bc3(m2), ALU.mult, r=["lg2", "m2"], w=["lg"])
            for q in range(ntile // 4):
                pt = ps[7]
                for jj in range(4):
                    j = q * 4 + jj
                    S.op("pe", "transpose", pt[0:8, jj * 128:(jj + 1) * 128], lg[:, j, :], ident, r=["lg", "ident"], w=["psB"])
                S.op("act", "copy", out=GT[:, q * 512:(q + 1) * 512], in_=pt[0:8, :], r=["psB"], w=["GT"])
        for g in range(G):
            gs = slice(g * 512, (g + 1) * 512)
            S.op("pool", "tensor_scalar", acc[:, :, gs], acc[:, :, gs], float(ALPHA), None, ALU.mult, r=allz(g), w=allz(g))

        tcount = 0
        gb = gk = None
        for i, (e, b) in enumerate(blocks):
            st = i % 2
            f0 = b * FB * 128
            nf = min(FB * 128, dff - f0)
            nfb = nf // 128
            if moe and b == 0:
                gb = gbs[0]
                gk = ("gb", 0)
                for q in range(G):
                    pt, pk = ps[6 + (q % 2)], pkey[6 + (q % 2)]
                    S.op("pe", "matmul", pt[:], cst[0:8, 256 + e * 128:256 + (e + 1) * 128], GT[:, q * 512:(q + 1) * 512],
                         start=True, stop=True, r=["sel", "GT"], w=[pk])
                    S.op("act", "copy", out=gb[:, q * 512:(q + 1) * 512], in_=pt[:], r=[pk], w=[gk])
            for g in range(G):
                gs = slice(g * 512, (g + 1) * 512)
                for fc in range(nfb):
                    c2 = tcount % 2
                    tcount += 1
                    pg, pu, kg, ku = ps[c2], ps[2 + c2], pkey[c2], pkey[2 + c2]
                    sl, t2 = tmp["sl%d" % c2], tmp["t2%d" % c2]
                    ksl, kt2 = ("sl", c2), ("t2", c2)
                    for k in range(KC):
                        S.op("pe", "matmul", pg[:], wgu[:, 2 * st, k, fc * 128:(fc + 1) * 128], hb[:, k, gs],
                             start=(k == 0), stop=(k == KC - 1), r=[("wgu", 2 * st), hk(k, g)], w=[kg])
                    for k in range(KC):
                        S.op("pe", "matmul", pu[:], wgu[:, 2 * st + 1, k, fc * 128:(fc + 1) * 128], hb[:, k, gs],
                             start=(k == 0), stop=(k == KC - 1), r=[("wgu", 2 * st + 1), hk(k, g)], w=[ku])
                    S.op("act", "activation", out=sl[:], in_=pg[:], func=AF.Silu, r=[kg], w=[ksl])
                    if moe:
                        S.op("dve", "tensor_tensor", t2[:], sl[:], pu[:], ALU.mult, r=[ksl, ku], w=[kt2])
                        S.op("dve", "tensor_tensor", actb[:, fc, gs], t2[:], gb[:, gs], ALU.mult, r=[kt2, gk], w=[("act", fc, g)])
                    else:
                        S.op("dve", "tensor_tensor", actb[:, fc, gs], sl[:], pu[:], ALU.mult, r=[ksl, ku], w=[("act", fc, g)])
            for g in range(G):
                gs = slice(g * 512, (g + 1) * 512)
                for ec in range(KC):
                    pd, kd = ps[4 + (ec % 2)], pkey[4 + (ec % 2)]
                    for fc in range(nfb):
                        S.op("pe", "matmul", pd[:], wdn[:, st, fc, ec * 128:(ec + 1) * 128], actb[:, fc, gs],
                             start=(fc == 0), stop=(fc == nfb - 1), r=[("wdn", st), ("act", fc, g)], w=[kd])
                    S.op("dve", "tensor_tensor", acc[:, ec, gs], acc[:, ec, gs], pd[:], ALU.add, r=[zk(ec, g), kd], w=[zk(ec, g)])
            if i + 2 < len(blocks):
                load_block(i + 2)

        emit_layernorm(cx, acc, zk, hb, hk, lnp, 2, 3, onesm, ps[6], ps[7], tmp, G)
        for g in range(G):
            gs = slice(g * 512, (g + 1) * 512)
            S.op("sp", "dma_start", out=fm(outT[:, gs]), in_=acc[:, :, gs], r=allz(g), dma=True)
            S.op("sp", "dma_start", out=fm(outTb[:, gs]), in_=hb[:, :, gs], r=allh(g), dma=True)
        S.emit()
    return nc


def make_consts():
    c = np.zeros((128, 128 + 128 + 8 * 128), np.float32)
    c[:, 0:128] = 1.0 / D
    c[:, 128:256] = np.eye(128, dtype=np.float32)
    for e in range(8):
        c[e, 256 + e * 128:256 + (e + 1) * 128] = 1.0
    return c


def lnp_layout(ln_g, ln_b, layer):
    out = np.zeros((128, 4, KC), np.float32)
    out[:, 0] = ln_g[layer, 0].reshape(KC, 128).T
    out[:, 1] = ln_b[layer, 0].reshape(KC, 128).T
    out[:, 2] = ln_g[layer, 1].reshape(KC, 128).T
    out[:, 3] = ln_b[layer, 1].reshape(KC, 128).T
    return out


SEG = 512
NCH = SEG // 64
NBLK = 2 * NCH
NPROJ = 704
DECAY_SCALE = -float(np.exp(-0.5))


def rwkv_consts():
    c = np.zeros((128, 128 + 128 + 128 + 64 + 64), np.float32)
    c[:, 0:128] = np.eye(128, dtype=np.float32)
    c[0:64, 128:192] = 1.0
    c[64:128, 192:256] = 1.0
    j = np.arange(64)[:, None]
    i = np.arange(64)[None, :]
    c[0:64, 256:320] = (j < i)
    c[0:64, 320:384] = (j <= i)
    c[0:64, 384:448] = (i < j)
    c[0:64, 448:512] = np.eye(64, dtype=np.float32)
    return c


def build_H_rwkv(layer1, ntok_b=T, nb=B, stop_after=99):
    import contextlib
    nseg = ntok_b // SEG
    ntot = nb * ntok_b
    nc = bass.Bass("TRN2", target_bir_lowering=False)
    with contextlib.ExitStack() as es:
        cx = Ctx(nc, es)
        S = cx.S
        xT = cx.dram("xT", [D, ntot], F32, "ExternalInput")
        wproj = cx.dram("wproj", [D, NPROJ], F32, "ExternalInput")
        mu_d = cx.dram("mu", [128, KC, NPROJ], F32, "ExternalInput")
        w2_d = cx.dram("w2", [128, 3, 128], F32, "ExternalInput")
        par_d = cx.dram("par", [128, 8], F32, "ExternalInput")
        cst_d = cx.dram("cst", [128, 512], F32, "ExternalInput")
        if layer1:
            vf_d = cx.dram("vfirst", [128, ntot], F32, "ExternalInput")
        else:
            vf_o = cx.dram("vfirst_out", [128, ntot], F32, "ExternalOutput")
        mix_o = cx.dram("mix", [128, ntot], BF16, "ExternalOutput")

        f32t = lambda n, shp=(128, SEG): cx.sb(n, list(shp), F32)
        xb = [cx.sb("xb%d" % i, [128, KC, SEG + 1], BF16) for i in range(2)]
        Wc = cx.sb("Wc", [128, KC, 2, NPROJ], BF16)
        wst = cx.sb("wst", [128, NPROJ], F32)
        must = cx.sb("must", [128, NPROJ], F32)
        w2f = cx.sb("w2f", [128, 3, 128], F32)
        w2b = cx.sb("w2b", [128, 3, 128], BF16)
        par = cx.sb("par_sb", [128, 8], F32)
        cst = cx.sb("cst_sb", [128, 512], F32)
        ident = cst[:, 0:128]
        bones = cst[:, 128:256]
        m_su = cst[0:64, 256:320]
        m_iu = cst[0:64, 320:384]
        m_ak = cst[0:64, 256:384]
        m_sl = cst[0:64, 384:448]
        id64 = cst[0:64, 448:512]
        rT, kT, vT, aT, wT, gT, av, bv, P, rP, t1, t2, d0, d1 = [f32t(n) for n in
            ("rT", "kT", "vT", "aT", "wT", "gT", "av", "bv", "P", "rP", "t1", "t2", "d0", "d1")]
        lor = cx.sb("lor", [128, 3, SEG], BF16)
        AR = cx.sb("AR", [128, NCH, 2, 64], F32)
        KBf = cx.sb("KBf", [128, NCH, 2, 64], F32)
        KBh = cx.sb("KBh", [128, NCH, 2, 64], F32)
        ARblk = cx.sb("ARblk", [128, NCH, 2, 128], F32)
        Bblk = cx.sb("Bblk", [128, NCH, 2, 64], F32)
        Vtm = cx.sb("Vtm", [64, NCH, 128], F32)
        KBtm = cx.sb("KBtm", [64, NCH, 2, 128], F32)
        Ytm = cx.sb("Ytm", [64, NCH, 128], F32)
        AK = cx.sb("AK", [64, NBLK, 128], F32)
        AB = cx.sb("AB", [64, NBLK, 128], F32)
        Mp = [cx.sb("Mp%d" % i, [64, NBLK, 64], F32) for i in range(2)]
        Np = [cx.sb("Np%d" % i, [64, NBLK, 64], F32) for i in range(2)]
        Q = cx.sb("Q", [64, NBLK, 64], F32)
        H = cx.sb("Hst", [128, 128], F32)
        Xs = cx.sb("Xs", [64, 128], F32)
        Us = cx.sb("Us", [64, 128], F32)
        yT = f32t("yT")
        ob = cx.sb("ob", [128, SEG], BF16)
        ps = [cx.ps("ps%d" % i, [128, 512]) for i in range(8)]
        pk = [("ps", i) for i in range(8)]

        S.op("sp", "dma_start", out=cst[:], in_=cst_d, w=["cst"], dma=True)
        S.op("sp", "dma_start", out=par[:], in_=par_d, w=["par"], dma=True)
        S.op("sp", "dma_start", out=w2f[:], in_=w2_d, w=["w2f"], dma=True)
        S.op("dve", "tensor_copy", out=w2b[:], in_=w2f[:], r=["w2f"], w=["w2b"])
        S.op("dve", "memset", ARblk[:], 0.0, w=["ARblk"])
        S.op("dve", "memset", Bblk[:], 0.0, w=["Bblk"])
        for k in range(KC):
            S.op("sp", "dma_start", out=wst[:], in_=wproj[k * 128:(k + 1) * 128, :], w=["wst"], dma=True)
            S.op("sp", "dma_start", out=must[:], in_=mu_d[:, k, :], w=["must"], dma=True)
            S.op("dve", "tensor_tensor", must[:], wst[:], must[:], ALU.mult, r=["wst", "must"], w=["must"])
            S.op("dve", "tensor_copy", out=Wc[:, k, 1, :], in_=must[:], r=["must"], w=["Wc"])
            S.op("dve", "tensor_tensor", Wc[:, k, 0, :], wst[:], must[:], ALU.subtract, r=["wst", "must"], w=["Wc"])

        def load_x(b, s, slot):
            t0 = b * ntok_b + s * SEG
            xk = ("xb", slot)
            if s == 0:
                S.op("dve", "memset", xb[slot][:, :, 0:1], 0.0, w=[xk])
                S.op("pool", "dma_start", out=xb[slot][:, :, 1:SEG + 1], in_=xT[:, t0:t0 + SEG].rearrange("(c p) t -> p c t", p=128),
                     w=[xk], dma=True)
            else:
                S.op("pool", "dma_start", out=xb[slot][:, :, 0:SEG + 1], in_=xT[:, t0 - 1:t0 + SEG].rearrange("(c p) t -> p c t", p=128),
                     w=[xk], dma=True)

        segs = [(b, s) for b in range(nb) for s in range(nseg)]
        load_x(0, 0, 0)
        pcount = [0]

        def proj(slot, c0, ncol, key):
            i = pcount[0] % 2
            pcount[0] += 1
            n = 0
            for k in range(KC):
                for sft in range(2):
                    S.op("pe", "matmul", ps[i][0:ncol, :], Wc[:, k, sft, c0:c0 + ncol], xb[slot][:, k, (1 - sft):(1 - sft) + SEG],
                         start=(n == 0), stop=(n == 2 * KC - 1), r=["Wc", ("xb", slot)], w=[pk[i]])
                    n += 1
            return ps[i], pk[i]

        col = lambda j: par[:, j:j + 1]
        for si, (b, s) in enumerate(segs):
            slot = si % 2
            t0 = b * ntok_b + s * SEG
            if si + 1 < len(segs):
                load_x(segs[si + 1][0], segs[si + 1][1], 1 - slot)
            if s == 0:
                S.op("dve", "memset", H[:], 0.0, w=["H"])
            p, k_ = proj(slot, 0, 128, None)
            S.op("act", "copy", out=rT[:], in_=p[:], r=[k_], w=["rT"])
            p, k_ = proj(slot, 128, 128, None)
            S.op("act", "copy", out=kT[:], in_=p[:], r=[k_], w=["kT"])
            p, k_ = proj(slot, 256, 128, None)
            S.op("act", "copy", out=vT[:], in_=p[:], r=[k_], w=["vT"])
            p, k_ = proj(slot, 384, 128, None)
            S.op("act", "activation", out=lor[0:64, 0, :], in_=p[0:64, :], func=AF.Tanh, r=[k_], w=["lor0a"])
            S.op("act", "copy", out=lor[64:128, 0, :], in_=p[64:128, :], r=[k_], w=["lor0b"])
            p, k_ = proj(slot, 512, 128, None)
            S.op("act", "activation", out=lor[:, 1, :], in_=p[:], func=AF.Sigmoid, r=[k_], w=["lor1"])
            p, k_ = proj(slot, 640, 64, None)
            S.op("act", "activation", out=lor[0:32, 2, :], in_=p[0:32, :], func=AF.Sigmoid, r=[k_], w=["lor2a"])
            S.op("act", "copy", out=lor[32:64, 2, :], in_=p[32:64, :], r=[k_], w=["lor2b"])
            i = pcount[0] % 2; pcount[0] += 1
            S.op("pe", "matmul", ps[i][:], w2b[0:64, 0, :], lor[0:64, 0, :], start=True, stop=True, r=["w2b", "lor0a"], w=[pk[i]])
            S.op("act", "activation", out=wT[:], in_=ps[i][:], func=AF.Sigmoid, bias=col(0), scale=1.0, r=[pk[i], "par"], w=["wT"])
            S.op("act", "activation", out=wT[:], in_=wT[:], func=AF.Exp, scale=DECAY_SCALE, r=["wT"], w=["wT"])
            i = pcount[0] % 2; pcount[0] += 1
            S.op("pe", "matmul", ps[i][:], w2b[64:128, 0, :], lor[64:128, 0, :], start=True, stop=True, r=["w2b", "lor0b"], w=[pk[i]])
            S.op("act", "activation", out=aT[:], in_=ps[i][:], func=AF.Sigmoid, bias=col(1), scale=1.0, r=[pk[i], "par"], w=["aT"])
            i = pcount[0] % 2; pcount[0] += 1
            S.op("pe", "matmul", ps[i][:], w2b[:, 1, :], lor[:, 1, :], start=True, stop=False, r=["w2b", "lor1"], w=[pk[i]])
            S.op("pe", "matmul", ps[i][:], w2b[0:32, 2, :], lor[0:32, 2, :], start=False, stop=True, r=["w2b", "lor2a"], w=[pk[i]])
            S.op("act", "copy", out=gT[:], in_=ps[i][:], r=[pk[i]], w=["gT"])
            if layer1:
                i = pcount[0] % 2; pcount[0] += 1
                S.op("pe", "matmul", ps[i][:], w2b[32:64, 2, :], lor[32:64, 2, :], start=True, stop=True, r=["w2b", "lor2b"], w=[pk[i]])
                S.op("act", "activation", out=t1[:], in_=ps[i][:], func=AF.Sigmoid, bias=col(2), scale=1.0, r=[pk[i], "par"], w=["t1"])
                S.op("sp", "dma_start", out=t2[:], in_=vf_d[:, t0:t0 + SEG], w=["t2"], dma=True)
                S.op("dve", "tensor_tensor", t2[:], t2[:], vT[:], ALU.subtract, r=["t2", "vT"], w=["t2"])
                S.op("dve", "tensor_tensor", t2[:], t2[:], t1[:], ALU.mult, r=["t2", "t1"], w=["t2"])
                S.op("dve", "tensor_tensor", vT[:], vT[:], t2[:], ALU.add, r=["vT", "t2"], w=["vT"])
            else:
                S.op("sp", "dma_start", out=vf_o[:, t0:t0 + SEG], in_=vT[:], r=["vT"], dma=True)
            if stop_after == 0:
                break
            S.op("dve", "tensor_scalar", av[:], kT[:], col(3), None, ALU.mult, r=["kT", "par"], w=["av"])
            S.op("act", "activation", out=t1[:], in_=av[:], func=AF.Square, r=["av"], w=["t1"])
            i = pcount[0] % 2; pcount[0] += 1
            S.op("pe", "matmul", ps[i][:], bones, t1[:], start=True, stop=True, r=["cst", "t1"], w=[pk[i]])
            S.op("dve", "tensor_scalar", t2[:], ps[i][:], 1e-24, None, ALU.max, r=[pk[i]], w=["t2"])
            S.op("act", "activation", out=t2[:], in_=t2[:], func=AF.Sqrt, r=["t2"], w=["t2"])
            S.op("dve", "reciprocal", t2[:], t2[:], r=["t2"], w=["t2"])
            S.op("dve", "tensor_tensor", av[:], av[:], t2[:], ALU.mult, r=["av", "t2"], w=["av"])
            S.op("dve", "tensor_tensor", bv[:], av[:], aT[:], ALU.mult, r=["av", "aT"], w=["bv"])
            S.op("dve", "tensor_scalar", t1[:], aT[:], -1.0, col(4), ALU.add, ALU.mult, r=["aT", "par"], w=["t1"])
            S.op("dve", "scalar_tensor_tensor", out=kT[:], in0=t1[:], scalar=1.0, in1=kT[:], op0=ALU.add, op1=ALU.mult,
                 r=["t1", "kT"], w=["kT"])
            if stop_after == 1:
                break
            w3 = wT[:].rearrange("p (c j) -> p c j", j=64)
            S.op("pool", "tensor_copy", out=d0[:], in_=wT[:], r=["wT"], w=["d0"])
            S.op("pool", "memset", d0[:].rearrange("p (c j) -> p c j", j=64)[:, :, 0:1], 0.0, w=["d0"])
            S.op("pool", "memset", d1[:], 0.0, w=["d1"])
            S.op("pool", "tensor_copy", out=d1[:].rearrange("p (c j) -> p c j", j=64)[:, :, 0:1], in_=w3[:, :, 0:1], r=["wT"], w=["d1"])
            S.op("dve", "tensor_tensor_scan", P[:], d0[:], d1[:], 0.0, ALU.mult, ALU.add, r=["d0", "d1"], w=["P"])
            S.op("dve", "reciprocal", rP[:], P[:], r=["P"], w=["rP"])
            S.op("dve", "reciprocal", t1[:], wT[:], r=["wT"], w=["t1"])
            S.op("dve", "tensor_tensor", t1[:], t1[:], P[:], ALU.mult, r=["t1", "P"], w=["t1"])
            c3 = lambda t: t[:].rearrange("p (c j) -> p c j", j=64)
            S.op("dve", "scalar_tensor_tensor", out=AR[:, :, 0, :], in0=c3(av), scalar=-1.0, in1=c3(t1), op0=ALU.mult, op1=ALU.mult,
                 r=["av", "t1"], w=["AR"])
            S.op("pool", "tensor_tensor", AR[:, :, 1, :], c3(rT), c3(P), ALU.mult, r=["rT", "P"], w=["AR"])
            S.op("pool", "tensor_tensor", KBf[:, :, 0, :], c3(kT), c3(rP), ALU.mult, r=["kT", "rP"], w=["KBf"])
            S.op("dve", "tensor_tensor", KBf[:, :, 1, :], c3(bv), c3(rP), ALU.mult, r=["bv", "rP"], w=["KBf"])
            pend = c3(P)[:, :, 63:64]
            for q in range(2):
                S.op("dve" if q else "pool", "tensor_tensor", KBh[:, :, q, :], KBf[:, :, q, :], _bc(pend, [128, NCH, 64]), ALU.mult,
                     r=["KBf", "P"], w=["KBh"])
            if stop_after == 2:
                break
            for hh in range(2):
                hb = hh * 64
                S.op("pool", "tensor_copy", out=ARblk[hb:hb + 64, :, hh, :], in_=AR[hb:hb + 64, :, :, :].rearrange("p c a b -> p c (a b)"),
                     r=["AR"], w=["ARblk"])
                S.op("pool", "tensor_copy", out=Bblk[hb:hb + 64, :, hh, :], in_=KBf[hb:hb + 64, :, 1, :], r=["KBf"], w=["Bblk"])
            for half in range(2):
                for cc in range(4):
                    c = half * 4 + cc
                    S.op("pe", "transpose", ps[2][0:64, cc * 128:(cc + 1) * 128], vT[:, c * 64:(c + 1) * 64], ident,
                         r=["vT", "cst"], w=[pk[2]])
                S.op("act", "copy", out=Vtm[:, half * 4:(half + 1) * 4, :].rearrange("p c f -> p (c f)"), in_=ps[2][0:64, :], r=[pk[2]], w=["Vtm"])
            for q2 in range(4):
                for cc in range(2):
                    c = q2 * 2 + cc
                    for q in range(2):
                        S.op("pe", "transpose", ps[2][0:64, (cc * 2 + q) * 128:(cc * 2 + q + 1) * 128], KBh[:, c, q, :], ident,
                             r=["KBh", "cst"], w=[pk[2]])
                S.op("act", "copy", out=KBtm[:, q2 * 2:(q2 + 1) * 2, :, :].rearrange("p c q f -> p (c q f)"), in_=ps[2][0:64, :],
                     r=[pk[2]], w=["KBtm"])
            if stop_after == 3:
                break
            for c2 in range(NCH // 2):
                for which, dst in ((0, AK), (1, AB)):
                    for cc in range(2):
                        c = c2 * 2 + cc
                        S.op("pe", "matmul", ps[2][0:64, cc * 256:(cc + 1) * 256], KBf[:, c, which, :],
                             ARblk[:, c, :, :].rearrange("p a b -> p (a b)"), start=True, stop=True, r=["KBf", "ARblk"], w=[pk[2]])
                    S.op("dve", "tensor_tensor", dst[:, c2 * 4:(c2 + 1) * 4, :], ps[2][0:64, :].rearrange("p (a b) -> p a b", b=128),
                         _bc(m_ak.unsqueeze(1), [64, 4, 128]), ALU.mult, r=[pk[2], "cst"], w=["AK" if which == 0 else "AB"])
            for c4 in range(NCH // 4):
                for cc in range(4):
                    c = c4 * 4 + cc
                    S.op("pe", "matmul", ps[2][0:64, cc * 128:(cc + 1) * 128], AR[:, c, 0, :],
                         Bblk[:, c, :, :].rearrange("p a b -> p (a b)"), start=True, stop=True, r=["Bblk", "AR"], w=[pk[2]])
                S.op("dve", "tensor_tensor", Np[0][:, c4 * 8:(c4 + 1) * 8, :], ps[2][0:64, :].rearrange("p (a b) -> p a b", b=64),
                     _bc(m_sl.unsqueeze(1), [64, 8, 64]), ALU.mult, r=[pk[2], "cst"], w=[("Np0", c4)])
            if stop_after == 4:
                break
            S.op("pool", "tensor_copy", out=Mp[0][:], in_=AB[:, :, 0:64], r=["AB"], w=[("Mp0", 0), ("Mp0", 1)])
            S.op("dve", "tensor_tensor", Q[:], AB[:, :, 0:64], _bc(id64.unsqueeze(1), [64, NBLK, 64]), ALU.add, r=["AB", "cst"], w=[("Q", 0), ("Q", 1)])
            ibank = {0: (3, 4, 5), 1: (6, 7, 2)}
            for lvl in range(5):
                cur, nxt = lvl % 2, (lvl + 1) % 2
                for g8 in range(NBLK // 8):
                    bM, bN, bQ = ibank[g8]
                    for bi in range(8):
                        blk = g8 * 8 + bi
                        S.op("pe", "matmul", ps[bM][0:64, bi * 64:(bi + 1) * 64], Np[cur][:, blk, :], Mp[cur][:, blk, :],
                             start=True, stop=True, r=[("Np%d" % cur, g8), ("Mp%d" % cur, g8)], w=[pk[bM]])
                    for bi in range(8):
                        blk = g8 * 8 + bi
                        S.op("pe", "matmul", ps[bN][0:64, bi * 64:(bi + 1) * 64], Mp[cur][:, blk, :], Np[cur][:, blk, :],
                             start=True, stop=True, r=[("Np%d" % cur, g8), ("Mp%d" % cur, g8)], w=[pk[bN]])
                for g8 in range(NBLK // 8):
                    bM, bN, bQ = ibank[g8]
                    gsl = slice(g8 * 8, (g8 + 1) * 8)
                    S.op("act", "copy", out=Mp[nxt][:, gsl, :].rearrange("p a b -> p (a b)"), in_=ps[bM][0:64, :], r=[pk[bM]], w=[("Mp%d" % nxt, g8)])
                    S.op("dve", "tensor_copy", out=Np[nxt][:, gsl, :].rearrange("p a b -> p (a b)"), in_=ps[bN][0:64, :], r=[pk[bN]], w=[("Np%d" % nxt, g8)])
                for g8 in range(NBLK // 8):
                    bM, bN, bQ = ibank[g8]
                    for bi in range(8):
                        blk = g8 * 8 + bi
                        S.op("pe", "matmul", ps[bQ][0:64, bi * 64:(bi + 1) * 64], Np[nxt][:, blk, :], Q[:, blk, :],
                             start=True, stop=True, r=[("Np%d" % nxt, g8), ("Q", g8)], w=[pk[bQ]])
                for g8 in range(NBLK // 8):
                    bM, bN, bQ = ibank[g8]
                    gsl = slice(g8 * 8, (g8 + 1) * 8)
                    S.op("dve", "tensor_tensor", Q[:, gsl, :].rearrange("p a b -> p (a b)"), Q[:, gsl, :].rearrange("p a b -> p (a b)"),
                         ps[bQ][0:64, :], ALU.add, r=[("Q", g8), pk[bQ]], w=[("Q", g8)])
            if stop_after == 5:
                break
            for c in range(NCH):
                S.op("pe", "matmul", ps[3][0:64, 0:128], AR[:, c, 0, :], H[:], start=True, stop=False, r=["AR", "H"], w=[pk[3]])
                for hh in range(2):
                    hb = hh * 64
                    blk = 2 * c + hh
                    S.op("pe", "matmul", ps[3][0:64, hb:hb + 64], AK[:, blk, 0:64], Vtm[:, c, hb:hb + 64], start=False, stop=(hh == 1),
                         r=["AK", "Vtm"], w=[pk[3]])
                S.op("act", "copy", out=Xs[:], in_=ps[3][0:64, 0:128], r=[pk[3]], w=["Xs"])
                for hh in range(2):
                    hb = hh * 64
                    blk = 2 * c + hh
                    S.op("pe", "matmul", ps[4][0:64, hb:hb + 64], Q[:, blk, :], Xs[:, hb:hb + 64], start=True, stop=True,
                         r=[("Q", blk // 8), "Xs"], w=[pk[4]])
                S.op("act", "copy", out=Us[:], in_=ps[4][0:64, 0:128], r=[pk[4]], w=["Us"])
                S.op("pe", "matmul", ps[5][0:64, 0:128], AR[:, c, 1, :], H[:], start=True, stop=False, r=["AR", "H"], w=[pk[5]])
                for hh in range(2):
                    hb = hh * 64
                    blk = 2 * c + hh
                    S.op("pe", "matmul", ps[5][0:64, hb:hb + 64], AB[:, blk, 64:128], Us[:, hb:hb + 64], start=False, stop=False,
                         r=["AB", "Us"], w=[pk[5]])
                    S.op("pe", "matmul", ps[5][0:64, hb:hb + 64], AK[:, blk, 64:128], Vtm[:, c, hb:hb + 64], start=False, stop=(hh == 1),
                         r=["AK", "Vtm"], w=[pk[5]])
                S.op("act", "copy", out=Ytm[:, c, :], in_=ps[5][0:64, 0:128], r=[pk[5]], w=["Ytm"])
                S.op("pe", "matmul", ps[6][:, 0:128], KBtm[:, c, 0, :], Vtm[:, c, :], start=True, stop=False, r=["KBtm", "Vtm"], w=[pk[6]])
                S.op("pe", "matmul", ps[6][:, 0:128], KBtm[:, c, 1, :], Us[:], start=False, stop=True, r=["KBtm", "Us"], w=[pk[6]])
                for hh in range(2):
                    hb = hh * 64
                    S.op("dve", "scalar_tensor_tensor", out=H[hb:hb + 64, hb:hb + 64], in0=H[hb:hb + 64, hb:hb + 64],
                         scalar=c3(P)[hb:hb + 64, c, 63:64], in1=ps[6][hb:hb + 64, hb:hb + 64],
                         op0=ALU.mult, op1=ALU.add, r=["H", "P", pk[6]], w=["H"])
            if stop_after == 6:
                break
            for c in range(NCH):
                S.op("pe", "transpose", ps[7][:, c * 64:(c + 1) * 64], Ytm[:, c, :], id64, r=["Ytm", "cst"], w=[pk[7]])
            S.op("act", "copy", out=yT[:], in_=ps[7][:], r=[pk[7]], w=["yT"])
            i = pcount[0] % 2; pcount[0] += 1
            S.op("pe", "matmul", ps[i][:], bones, yT[:], start=True, stop=True, r=["cst", "yT"], w=[pk[i]])
            S.op("dve", "scalar_tensor_tensor", out=yT[:], in0=ps[i][:], scalar=-1.0 / 64, in1=yT[:], op0=ALU.mult, op1=ALU.add,
                 r=[pk[i], "yT"], w=["yT"])
            S.op("act", "activation", out=t1[:], in_=yT[:], func=AF.Square, r=["yT"], w=["t1"])
            i = pcount[0] % 2; pcount[0] += 1
            S.op("pe", "matmul", ps[i][:], bones, t1[:], start=True, stop=True, r=["cst", "t1"], w=[pk[i]])
            S.op("act", "activation", out=t2[:], in_=ps[i][:], func=AF.Sqrt, bias=GN_EPS, scale=1.0 / 64, r=[pk[i]], w=["t2"])
            S.op("dve", "reciprocal", t2[:], t2[:], r=["t2"], w=["t2"])
            S.op("dve", "tensor_tensor", yT[:], yT[:], t2[:], ALU.mult, r=["yT", "t2"], w=["yT"])
            S.op("act", "activation", out=yT[:], in_=yT[:], func=AF.Identity, scale=col(6), bias=col(7), r=["yT", "par"], w=["yT"])
            S.op("dve", "scalar_tensor_tensor", out=t1[:], in0=rT[:], scalar=col(5), in1=kT[:], op0=ALU.mult, op1=ALU.mult,
                 r=["rT", "kT", "par"], w=["t1"])
            i = pcount[0] % 2; pcount[0] += 1
            S.op("pe", "matmul", ps[i][:], bones, t1[:], start=True, stop=True, r=["cst", "t1"], w=[pk[i]])
            S.op("dve", "tensor_tensor", t2[:], ps[i][:], vT[:], ALU.mult, r=[pk[i], "vT"], w=["t2"])
            S.op("dve", "tensor_tensor", yT[:], yT[:], t2[:], ALU.add, r=["yT", "t2"], w=["yT"])
            S.op("dve", "tensor_tensor", ob[:], yT[:], gT[:], ALU.mult, r=["yT", "gT"], w=["ob"])
            S.op("sp", "dma_start", out=mix_o[:, t0:t0 + SEG], in_=ob[:], r=["ob"], dma=True)
        S.emit()
    return nc


def build_H_rwkv2(layer1, ntok_b=T, nb=B, stop_after=99):
    import contextlib
    nseg = ntok_b // SEG
    ntot = nb * ntok_b
    nc = bass.Bass("TRN2", target_bir_lowering=False)
    with contextlib.ExitStack() as es:
        cx = Ctx(nc, es)
        S = cx.S
        S.strict = _STRICT
        xT = cx.dram("xT", [D, ntot], F32, "ExternalInput")
        wproj = cx.dram("wproj", [D, NPROJ], F32, "ExternalInput")
        mu_d = cx.dram("mu", [128, KC, NPROJ], F32, "ExternalInput")
        w2_d = cx.dram("w2", [128, 3, 128], F32, "ExternalInput")
        par_d = cx.dram("par", [128, 8], F32, "ExternalInput")
        cst_d = cx.dram("cst", [128, 512], F32, "ExternalInput")
        if layer1:
            vf_d = cx.dram("vfirst", [128, ntot], F32, "ExternalInput")
        else:
            vf_o = cx.dram("vfirst_out", [128, ntot], F32, "ExternalOutput")
        mix_o = cx.dram("mix", [128, ntot], BF16, "ExternalOutput")

        f32t = lambda n, shp=(128, SEG): cx.sb(n, list(shp), F32)
        xb = [cx.sb("xb%d" % i, [128, KC, SEG + 1], BF16) for i in range(2)]
        Wc = cx.sb("Wc", [128, KC, 2, NPROJ], BF16)
        wst = cx.sb("wst", [128, NPROJ], F32)
        must = cx.sb("must", [128, NPROJ], F32)
        w2f = cx.sb("w2f", [128, 3, 128], F32)
        w2b = cx.sb("w2b", [128, 3, 128], BF16)
        par = cx.sb("par_sb", [128, 8], F32)
        cst = cx.sb("cst_sb", [128, 512], F32)
        ident = cst[:, 0:128]
        bones = cst[:, 128:256]
        m_su = cst[0:64, 256:320]
        m_iu = cst[0:64, 320:384]
        m_ak = cst[0:64, 256:384]
        m_sl = cst[0:64, 384:448]
        id64 = cst[0:64, 448:512]
        aT, wT, av, bv, rP, t1, t2, d0, d1 = [f32t(n) for n in
            ("aT", "wT", "av", "bv", "rP", "t1", "t2", "d0", "d1")]
        u1, u2 = d0, d1
        rTs, kTs, vTs, gTs, Ps = [[f32t("%s%d" % (n, i)) for i in range(2)] for n in ("rT", "kT", "vT", "gT", "P")]
        lor = cx.sb("lor", [128, 3, SEG], BF16)
        ARs = [cx.sb("AR%d" % i, [128, NCH, 2, 64], F32) for i in range(2)]
        KBf = cx.sb("KBf", [128, NCH, 2, 64], F32)
        KBh = cx.sb("KBh", [128, NCH, 2, 64], F32)
        ARblk = cx.sb("ARblk", [128, NCH, 2, 128], F32)
        Bblk = cx.sb("Bblk", [128, NCH, 2, 64], F32)
        Vtms = [cx.sb("Vtm%d" % i, [64, NCH, 128], F32) for i in range(2)]
        KBtms = [cx.sb("KBtm%d" % i, [64, NCH, 2, 128], F32) for i in range(2)]
        Ytm = cx.sb("Ytm", [64, NCH, 128], F32)
        AKs = [cx.sb("AK%d" % i, [64, NBLK, 128], F32) for i in range(2)]
        ABs = [cx.sb("AB%d" % i, [64, NBLK, 128], F32) for i in range(2)]
        Mp = [cx.sb("Mp%d" % i, [64, NBLK, 64], BF16) for i in range(2)]
        Np = [cx.sb("Np%d" % i, [64, NBLK, 64], BF16) for i in range(2)]
        Qb = cx.sb("Qb", [64, NBLK, 64], BF16)
        Qs = [cx.sb("Q%d" % i, [64, NBLK, 64], F32) for i in range(2)]
        H = cx.sb("Hst", [128, 128], F32)
        Xs = cx.sb("Xs", [64, 128], F32)
        Us = cx.sb("Us", [64, 128], F32)
        yT = f32t("yT")
        ob = cx.sb("ob", [128, SEG], BF16)
        ps = [cx.ps("ps%d" % i, [128, 512]) for i in range(8)]
        pk = [("ps", i) for i in range(8)]

        S.op("sp", "dma_start", out=cst[:], in_=cst_d, w=["cst"], dma=True)
        S.op("sp", "dma_start", out=par[:], in_=par_d, w=["par"], dma=True)
        S.op("sp", "dma_start", out=w2f[:], in_=w2_d, w=["w2f"], dma=True)
        S.op("dve", "tensor_copy", out=w2b[:], in_=w2f[:], r=["w2f"], w=["w2b"])
        S.op("dve", "memset", ARblk[:], 0.0, w=["ARblk"])
        S.op("dve", "memset", Bblk[:], 0.0, w=["Bblk"])
        for k in range(KC):
            S.op("sp", "dma_start", out=wst[:], in_=wproj[k * 128:(k + 1) * 128, :], w=["wst"], dma=True)
            S.op("sp", "dma_start", out=must[:], in_=mu_d[:, k, :], w=["must"], dma=True)
            S.op("dve", "tensor_tensor", must[:], wst[:], must[:], ALU.mult, r=["wst", "must"], w=["must"])
            S.op("dve", "tensor_copy", out=Wc[:, k, 1, :], in_=must[:], r=["must"], w=["Wc"])
            S.op("dve", "tensor_tensor", Wc[:, k, 0, :], wst[:], must[:], ALU.subtract, r=["wst", "must"], w=["Wc"])

        def load_x(b, s, slot):
            t0 = b * ntok_b + s * SEG
            xk = ("xb", slot)
            if s == 0:
                S.op("dve", "memset", xb[slot][:, :, 0:1], 0.0, w=[xk])
                S.op("pool", "dma_start", out=xb[slot][:, :, 1:SEG + 1], in_=xT[:, t0:t0 + SEG].rearrange("(c p) t -> p c t", p=128),
                     w=[xk], dma=True)
            else:
                S.op("pool", "dma_start", out=xb[slot][:, :, 0:SEG + 1], in_=xT[:, t0 - 1:t0 + SEG].rearrange("(c p) t -> p c t", p=128),
                     w=[xk], dma=True)

        segs = [(b, s) for b in range(nb) for s in range(nseg)]
        load_x(0, 0, 0)
        pcount = [0]

        def proj(slot, c0, ncol, key):
            i = pcount[0] % 2
            pcount[0] += 1
            n = 0
            for k in range(KC):
                for sft in range(2):
                    S.op("pe", "matmul", ps[i][0:ncol, :], Wc[:, k, sft, c0:c0 + ncol], xb[slot][:, k, (1 - sft):(1 - sft) + SEG],
                         start=(n == 0), stop=(n == 2 * KC - 1), r=["Wc", ("xb", slot)], w=[pk[i]])
                    n += 1
            return ps[i], pk[i]

        col = lambda j: par[:, j:j + 1]
        def prep_gen(si):
            b, s = segs[si]
            p_ = si % 2
            rT, kT, vT, gT, P, AR, Vtm, KBtm, AK, AB, Q = (rTs[p_], kTs[p_], vTs[p_], gTs[p_], Ps[p_], ARs[p_], Vtms[p_], KBtms[p_],
                                                          AKs[p_], ABs[p_], Qs[p_])
            c3 = lambda t: t[:].rearrange("p (c j) -> p c j", j=64)
            yield
            slot = si % 2
            t0 = b * ntok_b + s * SEG
            if si + 1 < len(segs):
                load_x(segs[si + 1][0], segs[si + 1][1], 1 - slot)
            p, k_ = proj(slot, 0, 128, None)
            S.op("act", "copy", out=rT[:], in_=p[:], r=[k_], w=["rT"])
            yield
            p, k_ = proj(slot, 128, 128, None)
            S.op("act", "copy", out=kT[:], in_=p[:], r=[k_], w=["kT"])
            yield
            p, k_ = proj(slot, 256, 128, None)
            S.op("act", "copy", out=vT[:], in_=p[:], r=[k_], w=["vT"])
            yield
            p, k_ = proj(slot, 384, 128, None)
            S.op("act", "activation", out=lor[0:64, 0, :], in_=p[0:64, :], func=AF.Tanh, r=[k_], w=["lor0a"])
            S.op("act", "copy", out=lor[64:128, 0, :], in_=p[64:128, :], r=[k_], w=["lor0b"])
            yield
            p, k_ = proj(slot, 512, 128, None)
            S.op("act", "activation", out=lor[:, 1, :], in_=p[:], func=AF.Sigmoid, r=[k_], w=["lor1"])
            yield
            p, k_ = proj(slot, 640, 64, None)
            S.op("act", "activation", out=lor[0:32, 2, :], in_=p[0:32, :], func=AF.Sigmoid, r=[k_], w=["lor2a"])
            S.op("act", "copy", out=lor[32:64, 2, :], in_=p[32:64, :], r=[k_], w=["lor2b"])
            yield
            i = pcount[0] % 2; pcount[0] += 1
            S.op("pe", "matmul", ps[i][:], w2b[0:64, 0, :], lor[0:64, 0, :], start=True, stop=True, r=["w2b", "lor0a"], w=[pk[i]])
            S.op("act", "activation", out=wT[:], in_=ps[i][:], func=AF.Sigmoid, bias=col(0), scale=1.0, r=[pk[i], "par"], w=["wT"])
            S.op("act", "activation", out=wT[:], in_=wT[:], func=AF.Exp, scale=DECAY_SCALE, r=["wT"], w=["wT"])
            i = pcount[0] % 2; pcount[0] += 1
            S.op("pe", "matmul", ps[i][:], w2b[64:128, 0, :], lor[64:128, 0, :], start=True, stop=True, r=["w2b", "lor0b"], w=[pk[i]])
            S.op("act", "activation", out=aT[:], in_=ps[i][:], func=AF.Sigmoid, bias=col(1), scale=1.0, r=[pk[i], "par"], w=["aT"])
            i = pcount[0] % 2; pcount[0] += 1
            S.op("pe", "matmul", ps[i][:], w2b[:, 1, :], lor[:, 1, :], start=True, stop=False, r=["w2b", "lor1"], w=[pk[i]])
            S.op("pe", "matmul", ps[i][:], w2b[0:32, 2, :], lor[0:32, 2, :], start=False, stop=True, r=["w2b", "lor2a"], w=[pk[i]])
            S.op("act", "copy", out=gT[:], in_=ps[i][:], r=[pk[i]], w=["gT"])
            if layer1:
                i = pcount[0] % 2; pcount[0] += 1
                S.op("pe", "matmul", ps[i][:], w2b[32:64, 2, :], lor[32:64, 2, :], start=True, stop=True, r=["w2b", "lor2b"], w=[pk[i]])
                S.op("act", "activation", out=t1[:], in_=ps[i][:], func=AF.Sigmoid, bias=col(2), scale=1.0, r=[pk[i], "par"], w=["t1"])
                S.op("sp", "dma_start", out=t2[:], in_=vf_d[:, t0:t0 + SEG], w=["t2"], dma=True)
                S.op("dve", "tensor_tensor", t2[:], t2[:], vT[:], ALU.subtract, r=["t2", "vT"], w=["t2"])
                S.op("dve", "tensor_tensor", t2[:], t2[:], t1[:], ALU.mult, r=["t2", "t1"], w=["t2"])
                S.op("dve", "tensor_tensor", vT[:], vT[:], t2[:], ALU.add, r=["vT", "t2"], w=["vT"])
            else:
                S.op("sp", "dma_start", out=vf_o[:, t0:t0 + SEG], in_=vT[:], r=["vT"], dma=True)
            yield
            S.op("dve", "tensor_scalar", av[:], kT[:], col(3), None, ALU.mult, r=["kT", "par"], w=["av"])
            S.op("act", "activation", out=t1[:], in_=av[:], func=AF.Square, r=["av"], w=["t1"])
            i = pcount[0] % 2; pcount[0] += 1
            S.op("pe", "matmul", ps[i][:], bones, t1[:], start=True, stop=True, r=["cst", "t1"], w=[pk[i]])
            S.op("dve", "tensor_scalar", t2[:], ps[i][:], 1e-24, None, ALU.max, r=[pk[i]], w=["t2"])
            S.op("act", "activation", out=t2[:], in_=t2[:], func=AF.Sqrt, r=["t2"], w=["t2"])
            S.op("dve", "reciprocal", t2[:], t2[:], r=["t2"], w=["t2"])
            S.op("dve", "tensor_tensor", av[:], av[:], t2[:], ALU.mult, r=["av", "t2"], w=["av"])
            S.op("dve", "tensor_tensor", bv[:], av[:], aT[:], ALU.mult, r=["av", "aT"], w=["bv"])
            yield
            S.op("dve", "tensor_scalar", t1[:], aT[:], -1.0, col(4), ALU.add, ALU.mult, r=["aT", "par"], w=["t1"])
            S.op("dve", "scalar_tensor_tensor", out=kT[:], in0=t1[:], scalar=1.0, in1=kT[:], op0=ALU.add, op1=ALU.mult,
                 r=["t1", "kT"], w=["kT"])
            yield
            w3 = wT[:].rearrange("p (c j) -> p c j", j=64)
            S.op("pool", "tensor_copy", out=d0[:], in_=wT[:], r=["wT"], w=["d0"])
            S.op("pool", "memset", d0[:].rearrange("p (c j) -> p c j", j=64)[:, :, 0:1], 0.0, w=["d0"])
            S.op("pool", "memset", d1[:], 0.0, w=["d1"])
            S.op("pool", "tensor_copy", out=d1[:].rearrange("p (c j) -> p c j", j=64)[:, :, 0:1], in_=w3[:, :, 0:1], r=["wT"], w=["d1"])
            S.op("dve", "tensor_tensor_scan", P[:], d0[:], d1[:], 0.0, ALU.mult, ALU.add, r=["d0", "d1"], w=["P"])
            S.op("dve", "reciprocal", rP[:], P[:], r=["P"], w=["rP"])
            S.op("dve", "reciprocal", t1[:], wT[:], r=["wT"], w=["t1"])
            S.op("dve", "tensor_tensor", t1[:], t1[:], P[:], ALU.mult, r=["t1", "P"], w=["t1"])
            c3 = lambda t: t[:].rearrange("p (c j) -> p c j", j=64)
            yield
            S.op("dve", "scalar_tensor_tensor", out=AR[:, :, 0, :], in0=c3(av), scalar=-1.0, in1=c3(t1), op0=ALU.mult, op1=ALU.mult,
                 r=["av", "t1"], w=["AR"])
            S.op("pool", "tensor_tensor", AR[:, :, 1, :], c3(rT), c3(P), ALU.mult, r=["rT", "P"], w=["AR"])
            S.op("pool", "tensor_tensor", KBf[:, :, 0, :], c3(kT), c3(rP), ALU.mult, r=["kT", "rP"], w=["KBf"])
            S.op("dve", "tensor_tensor", KBf[:, :, 1, :], c3(bv), c3(rP), ALU.mult, r=["bv", "rP"], w=["KBf"])
            pend = c3(P)[:, :, 63:64]
            for q in range(2):
                S.op("dve" if q else "pool", "tensor_tensor", KBh[:, :, q, :], KBf[:, :, q, :], _bc(pend, [128, NCH, 64]), ALU.mult,
                     r=["KBf", "P"], w=["KBh"])
            for hh in range(2):
                hb = hh * 64
                S.op("pool", "tensor_copy", out=ARblk[hb:hb + 64, :, hh, :], in_=AR[hb:hb + 64, :, :, :].rearrange("p c a b -> p c (a b)"),
                     r=["AR"], w=["ARblk"])
                S.op("pool", "tensor_copy", out=Bblk[hb:hb + 64, :, hh, :], in_=KBf[hb:hb + 64, :, 1, :], r=["KBf"], w=["Bblk"])
            yield
            for half in range(2):
                for cc in range(4):
                    c = half * 4 + cc
                    S.op("pe", "transpose", ps[2][0:64, cc * 128:(cc + 1) * 128], vT[:, c * 64:(c + 1) * 64], ident,
                         r=["vT", "cst"], w=[pk[2]])
                S.op("act", "copy", out=Vtm[:, half * 4:(half + 1) * 4, :].rearrange("p c f -> p (c f)"), in_=ps[2][0:64, :], r=[pk[2]], w=["Vtm"])
            for q2 in range(4):
                for cc in range(2):
                    c = q2 * 2 + cc
                    for q in range(2):
                        S.op("pe", "transpose", ps[2][0:64, (cc * 2 + q) * 128:(cc * 2 + q + 1) * 128], KBh[:, c, q, :], ident,
                             r=["KBh", "cst"], w=[pk[2]])
                S.op("act", "copy", out=KBtm[:, q2 * 2:(q2 + 1) * 2, :, :].rearrange("p c q f -> p (c q f)"), in_=ps[2][0:64, :],
                     r=[pk[2]], w=["KBtm"])
            yield
            for c2 in range(NCH // 2):
                for which, dst in ((0, AK), (1, AB)):
                    for cc in range(2):
                        c = c2 * 2 + cc
                        S.op("pe", "matmul", ps[2][0:64, cc * 256:(cc + 1) * 256], KBf[:, c, which, :],
                             ARblk[:, c, :, :].rearrange("p a b -> p (a b)"), start=True, stop=True, r=["KBf", "ARblk"], w=[pk[2]])
                    S.op("dve", "tensor_tensor", dst[:, c2 * 4:(c2 + 1) * 4, :], ps[2][0:64, :].rearrange("p (a b) -> p a b", b=128),
                         _bc(m_ak.unsqueeze(1), [64, 4, 128]), ALU.mult, r=[pk[2], "cst"], w=["AK" if which == 0 else "AB"])
            for c4 in range(NCH // 4):
                for cc in range(4):
                    c = c4 * 4 + cc
                    S.op("pe", "matmul", ps[2][0:64, cc * 128:(cc + 1) * 128], AR[:, c, 0, :],
                         Bblk[:, c, :, :].rearrange("p a b -> p (a b)"), start=True, stop=True, r=["Bblk", "AR"], w=[pk[2]])
                S.op("dve", "tensor_tensor", Np[0][:, c4 * 8:(c4 + 1) * 8, :], ps[2][0:64, :].rearrange("p (a b) -> p a b", b=64),
                     _bc(m_sl.unsqueeze(1), [64, 8, 64]), ALU.mult, r=[pk[2], "cst"], w=[("Np0", c4)])
            yield
            S.op("pool", "tensor_copy", out=Mp[0][:], in_=AB[:, :, 0:64], r=["AB"], w=[("Mp0", 0), ("Mp0", 1)])
            S.op("dve", "tensor_tensor", Q[:], AB[:, :, 0:64], _bc(id64.unsqueeze(1), [64, NBLK, 64]), ALU.add, r=["AB", "cst"], w=[("Q", 0, p_), ("Q", 1, p_)])
            S.op("pool", "tensor_copy", out=Qb[:], in_=Q[:], r=[("Q", 0, p_), ("Q", 1, p_)], w=[("Qb", 0), ("Qb", 1)])
            ibank = {0: (3, 4, 5), 1: (0, 1, 2)}
            for lvl in range(5):
                yield
                cur, nxt = lvl % 2, (lvl + 1) % 2
                for g8 in range(NBLK // 8):
                    bM, bN, bQ = ibank[g8]
                    for bi in range(8 if lvl < 4 else 0):
                        blk = g8 * 8 + bi
                        S.op("pe", "matmul", ps[bM][0:64, bi * 64:(bi + 1) * 64], Np[cur][:, blk, :], Mp[cur][:, blk, :],
                             start=True, stop=True, r=[("Np%d" % cur, g8), ("Mp%d" % cur, g8)], w=[pk[bM]])
                    for bi in range(8):
                        blk = g8 * 8 + bi
                        S.op("pe", "matmul", ps[bN][0:64, bi * 64:(bi + 1) * 64], Mp[cur][:, blk, :], Np[cur][:, blk, :],
                             start=True, stop=True, r=[("Np%d" % cur, g8), ("Mp%d" % cur, g8)], w=[pk[bN]])
                for g8 in range(NBLK // 8):
                    bM, bN, bQ = ibank[g8]
                    gsl = slice(g8 * 8, (g8 + 1) * 8)
                    if lvl < 4:
                        S.op("act", "copy", out=Mp[nxt][:, gsl, :].rearrange("p a b -> p (a b)"), in_=ps[bM][0:64, :], r=[pk[bM]], w=[("Mp%d" % nxt, g8)])
                    S.op("dve", "tensor_copy", out=Np[nxt][:, gsl, :].rearrange("p a b -> p (a b)"), in_=ps[bN][0:64, :], r=[pk[bN]], w=[("Np%d" % nxt, g8)])
                for g8 in range(NBLK // 8):
                    bM, bN, bQ = ibank[g8]
                    for bi in range(8):
                        blk = g8 * 8 + bi
                        S.op("pe", "matmul", ps[bQ][0:64, bi * 64:(bi + 1) * 64], Np[nxt][:, blk, :], Qb[:, blk, :],
                             start=True, stop=True, r=[("Np%d" % nxt, g8), ("Qb", g8)], w=[pk[bQ]])
                for g8 in range(NBLK // 8):
                    bM, bN, bQ = ibank[g8]
                    gsl = slice(g8 * 8, (g8 + 1) * 8)
                    S.op("dve", "tensor_tensor", Q[:, gsl, :].rearrange("p a b -> p (a b)"), Q[:, gsl, :].rearrange("p a b -> p (a b)"),
                         ps[bQ][0:64, :], ALU.add, r=[("Q", g8, p_), pk[bQ]], w=[("Q", g8, p_)])
                    if lvl < 4:
                        S.op("pool", "tensor_copy", out=Qb[:, gsl, :], in_=Q[:, gsl, :], r=[("Q", g8, p_)], w=[("Qb", g8)])

        def chunk_gen(si):
            b, s = segs[si]
            t0 = b * ntok_b + s * SEG
            p_ = si % 2
            rT, kT, vT, gT, P, AR, Vtm, KBtm, AK, AB, Q = (rTs[p_], kTs[p_], vTs[p_], gTs[p_], Ps[p_], ARs[p_], Vtms[p_], KBtms[p_],
                                                          AKs[p_], ABs[p_], Qs[p_])
            c3 = lambda t: t[:].rearrange("p (c j) -> p c j", j=64)
            if s == 0:
                S.op("dve", "memset", H[:], 0.0, w=["H"])
            for c in range(NCH):
                yield
                S.op("pe", "matmul", ps[6][0:64, 0:128], AR[:, c, 0, :], H[:], start=True, stop=False, r=["AR", "H"], w=[pk[6]])
                for hh in range(2):
                    hb = hh * 64
                    blk = 2 * c + hh
                    S.op("pe", "matmul", ps[6][0:64, hb:hb + 64], AK[:, blk, 0:64], Vtm[:, c, hb:hb + 64], start=False, stop=(hh == 1),
                         r=["AK", "Vtm"], w=[pk[6]])
                S.op("act", "copy", out=Xs[:], in_=ps[6][0:64, 0:128], r=[pk[6]], w=["Xs"])
                for hh in range(2):
                    hb = hh * 64
                    blk = 2 * c + hh
                    S.op("pe", "matmul", ps[6][0:64, 128 + hb:128 + hb + 64], Q[:, blk, :], Xs[:, hb:hb + 64], start=True, stop=True,
                         r=[("Q", blk // 8, p_), "Xs"], w=[pk[6]])
                S.op("act", "copy", out=Us[:], in_=ps[6][0:64, 128:256], r=[pk[6]], w=["Us"])
                S.op("pe", "matmul", ps[6][0:64, 256:384], AR[:, c, 1, :], H[:], start=True, stop=False, r=["AR", "H"], w=[pk[6]])
                for hh in range(2):
                    hb = hh * 64
                    blk = 2 * c + hh
                    S.op("pe", "matmul", ps[6][0:64, 256 + hb:256 + hb + 64], AB[:, blk, 64:128], Us[:, hb:hb + 64], start=False, stop=False,
                         r=["AB", "Us"], w=[pk[6]])
                    S.op("pe", "matmul", ps[6][0:64, 256 + hb:256 + hb + 64], AK[:, blk, 64:128], Vtm[:, c, hb:hb + 64], start=False, stop=(hh == 1),
                         r=["AK", "Vtm"], w=[pk[6]])
                S.op("act", "copy", out=Ytm[:, c, :], in_=ps[6][0:64, 256:384], r=[pk[6]], w=["Ytm"])
                S.op("pe", "matmul", ps[7][:, 0:128], KBtm[:, c, 0, :], Vtm[:, c, :], start=True, stop=False, r=["KBtm", "Vtm"], w=[pk[7]])
                S.op("pe", "matmul", ps[7][:, 0:128], KBtm[:, c, 1, :], Us[:], start=False, stop=True, r=["KBtm", "Us"], w=[pk[7]])
                for hh in range(2):
                    hb = hh * 64
                    S.op("dve", "scalar_tensor_tensor", out=H[hb:hb + 64, hb:hb + 64], in0=H[hb:hb + 64, hb:hb + 64],
                         scalar=c3(P)[hb:hb + 64, c, 63:64], in1=ps[7][hb:hb + 64, hb:hb + 64],
                         op0=ALU.mult, op1=ALU.add, r=["H", "P", pk[7]], w=["H"])
            yield
            for c in range(NCH):
                S.op("pe", "transpose", ps[6][:, c * 64:(c + 1) * 64], Ytm[:, c, :], id64, r=["Ytm", "cst"], w=[pk[6]])
            S.op("act", "copy", out=yT[:], in_=ps[6][:], r=[pk[6]], w=["yT"])
            S.op("pe", "matmul", ps[7][:], bones, yT[:], start=True, stop=True, r=["cst", "yT"], w=[pk[7]])
            S.op("dve", "scalar_tensor_tensor", out=yT[:], in0=ps[7][:], scalar=-1.0 / 64, in1=yT[:], op0=ALU.mult, op1=ALU.add,
                 r=[pk[7], "yT"], w=["yT"])
            S.op("act", "activation", out=u1[:], in_=yT[:], func=AF.Square, r=["yT"], w=["d0"])
            S.op("pe", "matmul", ps[7][:], bones, u1[:], start=True, stop=True, r=["cst", "d0"], w=[pk[7]])
            S.op("act", "activation", out=u2[:], in_=ps[7][:], func=AF.Sqrt, bias=GN_EPS, scale=1.0 / 64, r=[pk[7]], w=["d1"])
            S.op("dve", "reciprocal", u2[:], u2[:], r=["d1"], w=["d1"])
            S.op("dve", "tensor_tensor", yT[:], yT[:], u2[:], ALU.mult, r=["yT", "d1"], w=["yT"])
            S.op("act", "activation", out=yT[:], in_=yT[:], func=AF.Identity, scale=col(6), bias=col(7), r=["yT", "par"], w=["yT"])
            S.op("dve", "scalar_tensor_tensor", out=u1[:], in0=rT[:], scalar=col(5), in1=kT[:], op0=ALU.mult, op1=ALU.mult,
                 r=["rT", "kT", "par"], w=["d0"])
            S.op("pe", "matmul", ps[7][:], bones, u1[:], start=True, stop=True, r=["cst", "d0"], w=[pk[7]])
            S.op("dve", "tensor_tensor", u2[:], ps[7][:], vT[:], ALU.mult, r=[pk[7], "vT"], w=["d1"])
            S.op("dve", "tensor_tensor", yT[:], yT[:], u2[:], ALU.add, r=["yT", "d1"], w=["yT"])
            S.op("dve", "tensor_tensor", ob[:], yT[:], gT[:], ALU.mult, r=["yT", "gT"], w=["ob"])
            S.op("sp", "dma_start", out=mix_o[:, t0:t0 + SEG], in_=ob[:], r=["ob"], dma=True)

        def step(gen, al):
            S.alias = al
            try:
                next(gen)
                return True
            except StopIteration:
                return False

        aliases = [{n: (n, q) for n in ['rT', 'kT', 'vT', 'gT', 'P', 'AR', 'Vtm', 'KBtm', 'AK', 'AB']} for q in range(2)]
        g = prep_gen(0)
        while step(g, aliases[0]):
            pass
        for si in range(len(segs)):
            nxt = prep_gen(si + 1) if si + 1 < len(segs) else None
            cg = chunk_gen(si)
            alive = nxt is not None
            while step(cg, aliases[si % 2]):
                for _ in range(2):
                    if alive:
                        alive = step(nxt, aliases[(si + 1) % 2])
            while alive:
                alive = step(nxt, aliases[(si + 1) % 2])
        S.alias = {}
        S.emit()
    return nc


def rwkv_host_inputs(inp, li, core):
    cs = slice(core * 128, (core + 1) * 128)
    f = np.float32
    wproj = np.zeros((D, NPROJ), f)
    mu = np.zeros((D, NPROJ), f)
    M = inp["rwkv_mu"][li]
    wproj[:, 0:128] = inp["rwkv_w_rkv"][li, 0][:, cs]; mu[:, 0:128] = M[0][:, None]
    wproj[:, 128:256] = inp["rwkv_w_rkv"][li, 1][:, cs]; mu[:, 128:256] = M[1][:, None]
    wproj[:, 256:384] = inp["rwkv_w_rkv"][li, 2][:, cs]; mu[:, 256:384] = M[2][:, None]
    wproj[:, 384:448] = inp["rwkv_decay_w1"][li]; mu[:, 384:448] = M[3][:, None]
    wproj[:, 448:512] = inp["rwkv_iclr_a1"][li]; mu[:, 448:512] = M[4][:, None]
    wproj[:, 512:672] = inp["rwkv_gate_g1"][li]; mu[:, 512:672] = M[5][:, None]
    if li > 0:
        wproj[:, 672:704] = inp["rwkv_vres_v1"][li - 1]
    mu[:, 672:704] = M[2][:, None]
    w2 = np.zeros((128, 3, 128), f)
    w2[0:64, 0] = inp["rwkv_decay_w2"][li][:, cs]
    w2[64:128, 0] = inp["rwkv_iclr_a2"][li][:, cs]
    w2[:, 1] = inp["rwkv_gate_g2"][li][0:128, cs]
    w2[0:32, 2] = inp["rwkv_gate_g2"][li][128:160, cs]
    if li > 0:
        w2[32:64, 2] = inp["rwkv_vres_v2"][li - 1][:, cs]
    par = np.zeros((128, 8), f)
    par[:, 0] = inp["rwkv_decay_w0"][li][cs]
    par[:, 1] = inp["rwkv_iclr_a0"][li][cs]
    if li > 0:
        par[:, 2] = inp["rwkv_vres_v0"][li - 1][cs]
    par[:, 3] = inp["rwkv_k_k"][li][cs]
    par[:, 4] = inp["rwkv_k_a"][li][cs]
    par[:, 5] = inp["rwkv_r_k"][li].reshape(-1)[cs]
    par[:, 6] = inp["rwkv_gn_g"][li][cs]
    par[:, 7] = inp["rwkv_gn_b"][li][cs]
    mu_l = np.ascontiguousarray(mu.reshape(KC, 128, NPROJ).transpose(1, 0, 2))
    return {"wproj": wproj, "mu": mu_l, "w2": w2, "par": par, "cst": rwkv_consts()}


NEG = -1.0e30
BLK = 256


def moba_consts():
    c = {}
    ident = np.eye(128, dtype=np.float32)
    c["identf"] = ident
    key = np.arange(128)[:, None]
    q = np.arange(128)[None, :]
    tri = np.where(key <= q, 0.0, NEG).astype(np.float32)
    oh = np.zeros((32, 32, 128), np.float32)
    for n in range(32):
        oh[n, n, :] = 1.0e30
    cb = np.zeros((128, 128 + 128 + 32 * 128), np.float32)
    cb[:, 0:128] = ident
    cb[:, 128:256] = tri
    cb[0:32, 256:] = oh.reshape(32, 32 * 128)
    return {"cstf": ident, "cstb": cb.astype(ml_dtypes.bfloat16)}


def build_H_moba(separate_kv, ntok_b=T, nb=B, dbg=()):
    import contextlib
    nseg = ntok_b // 512
    ntile = ntok_b // 128
    nblkb = ntok_b // BLK
    ntot = nb * ntok_b
    nc = bass.Bass("TRN2", target_bir_lowering=False)
    with contextlib.ExitStack() as es:
        cx = Ctx(nc, es)
        S = cx.S
        xT = cx.dram("xT", [D, ntot], F32, "ExternalInput")
        if separate_kv:
            xkvT = cx.dram("xkvT", [D, ntot], F32, "ExternalInput")
        else:
            xkvT = xT
        wqkv_d = cx.dram("wqkv", [D, 3, 128], F32, "ExternalInput")
        cstf_d = cx.dram("cstf", [128, 128], F32, "ExternalInput")
        cstb_d = cx.dram("cstb", [128, 256 + 32 * 128], BF16, "ExternalInput")
        ind_d = cx.dram("ind", [32, ntok_b], BF16, "ExternalInput")
        mix_o = cx.dram("mix", [128, ntot], BF16, "ExternalOutput")

        wqkv = cx.sb("wqkv_sb", [128, KC, 3, 128], BF16)
        identf = cx.sb("identf", [128, 128], F32)
        cstb = cx.sb("cstb_sb", [128, 256 + 32 * 128], BF16)
        identb = cstb[:, 0:128]
        trib = cstb[:, 128:256]
        xq = [cx.sb("xq%d" % i, [128, KC, 512], BF16) for i in range(2)]
        xkv = [cx.sb("xkv%d" % i, [128, KC, 512], BF16) for i in range(2)] if separate_kv else xq
        KTa = [cx.sb("KTa%d" % i, [128, ntok_b], BF16) for i in range(2)]
        Va = cx.sb("Va", [128, ntile, 2, 65], BF16)
        qf = cx.sb("qf", [128, ntok_b], F32)
        qz = [cx.sb("qz%d" % i, [128, ntok_b], BF16) for i in range(2)]
        km = cx.sb("km", [128, nblkb], F32)
        kmblk = cx.sb("kmblk", [128, 2, nblkb], F32)
        gsb = cx.sb("gsb", [128, 2, 32], F32)
        top8 = cx.sb("top8", [128, 2, 8], F32)
        mm1p = [cx.sb("mm1p%d" % i, [128, 128], F32) for i in range(2)]
        mT = [cx.sb("mT%d" % i, [32, 2, 128], BF16) for i in range(2)]
        PT = [cx.sb("PT%d" % i, [128, 1024], BF16) for i in range(3)]
        osb = cx.sb("osb", [128, 128], F32)
        rs = cx.sb("rs", [128, 1], F32)
        ob = cx.sb("ob", [128, 512], BF16)
        psS = [cx.ps("psS%d" % i, [128, 1024]) for i in range(2)]
        pSkeys = [("psS", i) for i in range(2)]
        ps = [psS[0][:, 0:512], psS[0][:, 512:1024], psS[1][:, 0:512], cx.ps("ps3", [128, 512]), cx.ps("ps4", [128, 512]), None,
              cx.ps("ps6", [128, 512]), cx.ps("ps7", [128, 512])]
        pk = [("psS", 0), ("psS", 0), ("psS", 1), ("ps", 3), ("ps", 4), ("ps", 5), ("ps", 6), ("ps", 7)]

        S.op("sp", "dma_start", out=identf[:], in_=cstf_d, w=["identf"], dma=True)
        S.op("sp", "dma_start", out=cstb[:], in_=cstb_d, w=["cstb"], dma=True)
        S.op("pool", "dma_start", out=wqkv[:], in_=wqkv_d.rearrange("(c p) a e -> p c a e", p=128), w=["wqkv"], dma=True)
        S.op("dve", "memset", Va[:, :, :, 64:65], 1.0, w=["Va"])
        S.op("dve", "memset", qz[0][64:128, :], 0.0, w=[("qa", 0, t_) for t_ in range(ntile)])
        S.op("dve", "memset", qz[1][0:64, :], 0.0, w=[("qa", 1, t_) for t_ in range(ntile)])
        S.op("dve", "memset", kmblk[:], 0.0, w=["kmblk"])
        S.op("dve", "memset", KTa[0][64:128, :], 0.0, w=["KTa0"])
        S.op("dve", "memset", KTa[1][0:64, :], 0.0, w=["KTa1"])
        S.op("sp", "dma_start", out=KTa[0][64:96, :], in_=ind_d, w=["KTa0"], dma=True)
        S.op("sp", "dma_start", out=KTa[1][0:32, :], in_=ind_d, w=["KTa1"], dma=True)
        for hh in range(2):
            S.op("dve", "memset", mm1p[hh][:], 0.0, w=["mm1p%d" % hh])

        fm = lambda ap: ap.rearrange("(c p) t -> p c t", p=128)

        def load_seg(b, s, slot):
            t0 = b * ntok_b + s * 512
            S.op("pool", "dma_start", out=xq[slot][:], in_=fm(xT[:, t0:t0 + 512]), w=[("xq", slot)], dma=True)
            if separate_kv:
                S.op("pool", "dma_start", out=xkv[slot][:], in_=fm(xkvT[:, t0:t0 + 512]), w=[("xkv", slot)], dma=True)

        segs = [(b, s) for b in range(nb) for s in range(nseg)]
        kvk = (lambda slot: ("xkv", slot)) if separate_kv else (lambda slot: ("xq", slot))
        load_seg(0, 0, 0)
        scount = 0
        pcount = 0
        for b in range(nb):
            S.op("dve", "memset", gsb[:], NEG, w=["gsb"])
            S.op("dve", "memset", km[:], 0.0, w=["km"])
            S.op("dve", "memset", qz[0][64:96, 0:256], 0.0, w=[("qa", 0, 0), ("qa", 0, 1)])
            S.op("dve", "memset", qz[1][0:32, 0:256], 0.0, w=[("qa", 1, 0), ("qa", 1, 1)])
            for s in range(nseg if "cut0" not in dbg else 0):
                si = b * nseg + s
                slot = si % 2
                if si + 1 < len(segs):
                    load_seg(segs[si + 1][0], segs[si + 1][1], 1 - slot)
                ss = slice(s * 512, (s + 1) * 512)
                for k in range(KC):
                    S.op("pe", "matmul", ps[0][:], wqkv[:, k, 1, :], xkv[slot][:, k, :], start=(k == 0), stop=(k == KC - 1),
                         r=["wqkv", kvk(slot)], w=[pk[0]])
                for hb2 in range(2):
                    for hh in range(2):
                        hb = hh * 64
                        S.op("act", "activation", out=KTa[hh][hb:hb + 64, s * 512 + hb2 * BLK:s * 512 + (hb2 + 1) * BLK],
                             in_=ps[0][hb:hb + 64, hb2 * BLK:(hb2 + 1) * BLK], func=AF.Copy,
                             accum_out=km[hb:hb + 64, 2 * s + hb2:2 * s + hb2 + 1], r=[pk[0]], w=["KTa%d" % hh, "km"])
                if "cut1" in dbg:
                    continue
                for tt in range(4):
                    for k in range(KC):
                        S.op("pe", "matmul", ps[1][:, tt * 128:(tt + 1) * 128], xkv[slot][:, k, tt * 128:(tt + 1) * 128], wqkv[:, k, 2, :],
                             start=(k == 0), stop=(k == KC - 1), r=["wqkv", kvk(slot)], w=[pk[1]])
                S.op("act", "copy", out=Va[:, 4 * s:4 * s + 4, :, 0:64], in_=ps[1][:].rearrange("p (t h d) -> p t h d", h=2, d=64),
                     r=[pk[1]], w=["Va"])
                if "cut2" in dbg:
                    continue
                for k in range(KC):
                    S.op("pe", "matmul", ps[2][:], wqkv[:, k, 0, :], xq[slot][:, k, :], start=(k == 0), stop=(k == KC - 1),
                         r=["wqkv", ("xq", slot)], w=[pk[2]])
                S.op("act", "copy", out=qf[:, ss], in_=ps[2][:], r=[pk[2]], w=["qf"])
                S.op("dve", "tensor_copy", out=qz[0][0:64, ss], in_=qf[0:64, ss], r=["qf"], w=[("qd", 0)])
                S.op("dve", "tensor_copy", out=qz[1][64:128, ss], in_=qf[64:128, ss], r=["qf"], w=[("qd", 1)])
            for hh in range(2):
                hb = hh * 64
                S.op("dve", "tensor_scalar", kmblk[hb:hb + 64, hh, :], km[hb:hb + 64, :], 1.0 / BLK, None, ALU.mult, r=["km"], w=["kmblk"])
            def rec_gate1(qt):
                own = qt // 2
                qs = slice(qt * 128, (qt + 1) * 128)
                S.op("pe", "matmul", ps[3][:, 0:2 * nblkb], qf[:, qs], kmblk[:].rearrange("p a b -> p (a b)"), start=True, stop=True,
                     r=["qf", "kmblk"], w=[pk[3]])
                S.op("dve", "tensor_copy", out=gsb[:, :, 0:own], in_=ps[3][:, 0:2 * nblkb].rearrange("p (a b) -> p a b", b=nblkb)[:, :, 0:own],
                     r=[pk[3]], w=["gsb"])
                for hh in range(2):
                    base = 64 if hh == 0 else 0
                    S.op("dve", "max", out=top8[:, hh, :], in_=gsb[:, hh, :], r=["gsb"], w=["top8"])
                    S.op("dve", "tensor_scalar", mm1p[hh][:, base:base + 32], gsb[:, hh, :], top8[:, hh, 2:3], -1.0, ALU.is_ge, ALU.add,
                         r=["gsb", "top8"], w=["mm1p%d" % hh])
                    S.op("dve", "memset", mm1p[hh][:, base + own:base + own + 1], 0.0, w=["mm1p%d" % hh])

            def rec_gate2(qt):
                qs = slice(qt * 128, (qt + 1) * 128)
                for hh in range(2):
                    S.op("pe", "matmul", ps[3][:, 256 + hh * 128:256 + (hh + 1) * 128], mm1p[hh][:], identf[:], start=True, stop=True,
                         r=["mm1p%d" % hh, "identf"], w=[pk[3]])
                S.op("act", "copy", out=qz[0][64:96, qs], in_=ps[3][64:96, 256:384], r=[pk[3]], w=[("qa", 0, qt)])
                S.op("act", "copy", out=qz[1][0:32, qs], in_=ps[3][0:32, 384:512], r=[pk[3]], w=[("qa", 1, qt)])

            items = []
            for qt in range(ntile if "proj_only" not in dbg else 0):
                for hh in range(2):
                    kts = list(range(0, qt + 1))
                    ngrp = (len(kts) + 7) // 8
                    pi_ = pcount % 2
                    pcount += 1
                    for gi in range(ngrp):
                        items.append(dict(qt=qt, hh=hh, gi=gi, ngrp=ngrp, grp=kts[gi * 8:(gi + 1) * 8], po=ps[6 + pi_], pok=pk[6 + pi_]))

            def rec_S(it):
                qt, hh, grp = it["qt"], it["hh"], it["grp"]
                own = qt // 2
                qs = slice(qt * 128, (qt + 1) * 128)
                pS, pSk = psS[it["sb"] % 2], pSkeys[it["sb"] % 2]
                pt, ptk = PT[it["sb"] % 3], ("PT", it["sb"] % 3)
                for j, kt in enumerate(grp):
                    S.op("pe", "matmul", pS[:, j * 128:(j + 1) * 128], KTa[hh][:, kt * 128:(kt + 1) * 128], qz[hh][:, qs], start=True, stop=(kt != qt),
                         r=["KTa%d" % hh, ("qd", hh), ("qa", hh, qt)], w=[pSk])
                    if kt == qt:
                        S.op("pe", "matmul", pS[:, j * 128:(j + 1) * 128], identb, trib, start=False, stop=True, r=["cstb"], w=[pSk])
                w_ = len(grp) * 128
                S.op("act", "activation", out=pt[:, 0:w_], in_=pS[:, 0:w_], func=AF.Exp, scale=0.125, r=[pSk], w=[ptk])

            def rec_PV(it):
                qt, hh, gi, ngrp, grp, po, pok = it["qt"], it["hh"], it["gi"], it["ngrp"], it["grp"], it["po"], it["pok"]
                hb = hh * 64
                pt, ptk = PT[it["sb"] % 3], ("PT", it["sb"] % 3)
                for j, kt in enumerate(grp):
                    first = (gi == 0 and j == 0)
                    last = (gi == ngrp - 1 and j == len(grp) - 1)
                    S.op("pe", "matmul", po[:, 0:65], pt[:, j * 128:(j + 1) * 128], Va[:, kt, hh, :], start=first, stop=last,
                         r=[ptk, "Va"], w=[pok])
                if gi == ngrp - 1:
                    S.op("dve", "reciprocal", rs[:], po[:, 64:65], r=[pok], w=["rs"])
                    S.op("dve", "tensor_scalar", osb[:, hb:hb + 64], po[:, 0:64], rs[:, 0:1], None, ALU.mult, r=[pok, "rs"], w=["osb"])
                    if hh == 1:
                        S.op("pe", "transpose", ps[4][:, 0:128], osb[:], identf[:], r=["osb", "identf"], w=[pk[4]])
                        S.op("act", "copy", out=ob[:, (qt % 4) * 128:(qt % 4 + 1) * 128], in_=ps[4][:, 0:128], r=[pk[4]], w=["ob"])
                        if qt % 4 == 3:
                            t0 = b * ntok_b + (qt - 3) * 128
                            S.op("sp", "dma_start", out=mix_o[:, t0:t0 + 512], in_=ob[:], r=["ob"], dma=True)

            pending = []
            for it in items:
                it["sb"] = scount
                scount += 1
                rec_S(it)
                pending.append(it)
                if len(pending) > 1:
                    rec_PV(pending.pop(0))
                nq = it["qt"] + 1
                if "nogate" not in dbg and it["gi"] == 0 and nq < ntile and nq // 2 > 0:
                    if it["hh"] == 0:
                        rec_gate1(nq)
                    else:
                        rec_gate2(nq)
            for it in pending:
                rec_PV(it)
        S.emit()
    return nc


def moba_host_inputs(inp, j, core, ntok_b=T):
    cs = slice(core * 128, (core + 1) * 128)
    w = np.stack([inp["moba_w_q"][j][:, cs], inp["moba_w_k"][:, cs], inp["moba_w_v"][:, cs]], axis=1)
    m = {"wqkv": np.ascontiguousarray(w.astype(np.float32))}
    m.update(moba_consts())
    ind = np.zeros((32, ntok_b), np.float32)
    for n in range(32):
        ind[n, n * BLK:(n + 1) * BLK] = 1.0e30
    m["ind"] = ind[:, :ntok_b].astype(ml_dtypes.bfloat16)
    return m


_PROGS = {}
_DUMP = None
_STRICT = True
_NL = 4


def _prog(name, fn):
    if name not in _PROGS:
        _PROGS[name] = fn()
    return _PROGS[name]


def _run(nc, maps):
    res = run_bass_kernel_spmd(nc, maps, core_ids=list(range(NCORES)))
    return res.results


def _t_phase(inp, layer, mixT, hT):
    moe = (layer % 2 == 1)
    e = layer // 2
    nc = _prog("T_moe" if moe else "T_dense", lambda: build_T(moe))
    if layer < 2:
        w_o = inp["rwkv_w_out"][layer]
    else:
        w_o = inp["moba_w_o"][layer - 2]
    base = {"w_o": np.ascontiguousarray(w_o, dtype=np.float32), "lnp": lnp_layout(inp["ln_g"], inp["ln_b"], layer),
            "consts": make_consts()}
    if moe:
        base["router"] = np.ascontiguousarray(inp["moe_router"][e])
        base["w_gate"] = np.ascontiguousarray(inp["moe_w_gate"][e])
        base["w_up"] = np.ascontiguousarray(inp["moe_w_up"][e])
        base["w_down"] = np.ascontiguousarray(inp["moe_w_down"][e])
    else:
        base["w_gate"] = np.ascontiguousarray(inp["ffn_w_gate"][e][None])
        base["w_up"] = np.ascontiguousarray(inp["ffn_w_up"][e][None])
        base["w_down"] = np.ascontiguousarray(inp["ffn_w_down"][e][None])
    maps = []
    for c in range(NCORES):
        m = dict(base)
        m["mixT"] = np.ascontiguousarray(mixT[:, c * NT:(c + 1) * NT])
        m["hT"] = np.ascontiguousarray(hT[:, c * NT:(c + 1) * NT])
        maps.append(m)
    res = _run(nc, maps)
    return np.concatenate([r["outT"] for r in res], axis=1)


def kernel(**inp):
    inp = {k: np.asarray(v) for k, v in inp.items()}
    x = inp["x"].astype(np.float32, copy=False)
    hT = np.ascontiguousarray(x.reshape(NTOK, D).T)
    vfirst = None
    h_kv = None
    for layer in range(_NL):
        if layer < 2:
            nc = _prog("H_rwkv%d" % layer, lambda: build_H_rwkv2(layer == 1))
            maps = []
            for c in range(NCORES):
                m = rwkv_host_inputs(inp, layer, c)
                m["xT"] = hT
                if layer == 1:
                    m["vfirst"] = np.ascontiguousarray(vfirst[c * 128:(c + 1) * 128])
                maps.append(m)
            res = _run(nc, maps)
            if layer == 0:
                vfirst = np.concatenate([r["vfirst_out"] for r in res], axis=0)
        else:
            j = layer - 2
            nc = _prog("H_moba%d" % j, lambda: build_H_moba(j == 1))
            maps = []
            for c in range(NCORES):
                m = moba_host_inputs(inp, j, c)
                m["xT"] = hT
                if j == 1:
                    m["xkvT"] = h_kv
                maps.append(m)
            res = _run(nc, maps)
        mixT = np.concatenate([r["mix"] for r in res], axis=0)
        if _DUMP is not None:
            _DUMP["mix%d" % layer] = mixT
        hT = _t_phase(inp, layer, mixT, hT)
        if _DUMP is not None:
            _DUMP["h%d" % layer] = hT
        if layer == 1:
            h_kv = hT
    return np.ascontiguousarray(hT.T).reshape(B, T, D).astype(np.float32)
```
